# Optimizing a Trainium2 kernel written in Bass

```python
import math
import jax
import jax.numpy as jnp
from jax import lax
import numpy as np

D_MODEL = 1024
BATCH = 2
SEQ = 8192
DEPTH = 1

CTX_LEN = 256
GRID_W = 64

S5_GROUP_CH = 16
S5_STATE = 64
S5_WIDTH = D_MODEL // 2
S5_GROUPS = S5_WIDTH // S5_GROUP_CH
S5_DT_MIN = 1e-3
S5_DT_MAX = 1e-1

HEAD_DIM = 128
N_HEADS = D_MODEL // HEAD_DIM
N_KV_HEADS = 2
GQA_GROUP = N_HEADS // N_KV_HEADS
ATTN_WIDTH = N_HEADS * HEAD_DIM
KV_WIDTH = N_KV_HEADS * HEAD_DIM
Q_BLOCK = 128
ROPE_THETA = 10000.0
ROPE_AXIS_DIM = HEAD_DIM // 2
ROPE_AXIS_FREQS = ROPE_AXIS_DIM // 2

N_BRANCHES = 2
IN_SPLITS = (S5_WIDTH, S5_WIDTH + ATTN_WIDTH, S5_WIDTH + ATTN_WIDTH + KV_WIDTH,
             S5_WIDTH + ATTN_WIDTH + 2 * KV_WIDTH)
IN_WIDTH = S5_WIDTH + ATTN_WIDTH + 2 * KV_WIDTH + N_BRANCHES * D_MODEL

N_EXPERT_GROUPS = 4
EXPERTS_PER_GROUP = 8
N_EXPERTS = N_EXPERT_GROUPS * EXPERTS_PER_GROUP
EXPERT_TOP_K = 2
D_EXPERT = D_MODEL // 2

N_MOD = 6
NORM_EPS = 1e-6

kernel_name = "hybrid_s5_gqa_hmoe_flow_block"


def _layer_norm(x):
    xf = x.astype(jnp.float32)
    mu = jnp.mean(xf, axis=-1, keepdims=True)
    var = jnp.mean(jnp.square(xf - mu), axis=-1, keepdims=True)
    return ((xf - mu) * lax.rsqrt(var + NORM_EPS)).astype(x.dtype)


def _post_norm(x, g, b):
    return _layer_norm(x) * g + b


def _modulate(x, shift, scale):
    return _layer_norm(x) * (1.0 + scale) + shift


def _adaln(cond, w, b):
    return jax.nn.silu(cond) @ w + b


def _rms_norm(x, g):
    xf = x.astype(jnp.float32)
    return (xf * lax.rsqrt(jnp.mean(xf * xf, axis=-1, keepdims=True) + NORM_EPS)).astype(x.dtype) * g


def _axial_rope_tables(rows, dtype):
    row = jnp.repeat(jnp.arange(rows, dtype=jnp.float32), GRID_W)
    col = jnp.tile(jnp.arange(GRID_W, dtype=jnp.float32), rows)
    inv = ROPE_THETA ** (-jnp.arange(0, ROPE_AXIS_DIM, 2, dtype=jnp.float32) / ROPE_AXIS_DIM)
    ang = jnp.stack([row[:, None] * inv, col[:, None] * inv], axis=1)
    return jnp.cos(ang)[:, None].astype(dtype), jnp.sin(ang)[:, None].astype(dtype)


def _rope(x, cos, sin):
    xs = x.reshape(x.shape[:-1] + (2, 2, ROPE_AXIS_FREQS))
    x1, x2 = xs[..., 0, :], xs[..., 1, :]
    out = jnp.stack([x1 * cos - x2 * sin, x1 * sin + x2 * cos], axis=-2)
    return out.reshape(x.shape)


def _block_attention(q, k, v):
    bsz, n_q = q.shape[:2]
    n_blk = n_q // Q_BLOCK
    scale = HEAD_DIM ** -0.5
    qb = q.reshape(bsz, n_blk, Q_BLOCK, N_KV_HEADS, GQA_GROUP, HEAD_DIM).transpose(1, 0, 2, 3, 4, 5)

    def attend(q_blk):
        s = jnp.einsum('bqhgd,bkhd->bhgqk', q_blk, k).astype(jnp.float32) * scale
        p = jax.nn.softmax(s, axis=-1).astype(v.dtype)
        return jnp.einsum('bhgqk,bkhd->bqhgd', p, v)

    o = lax.map(attend, qb)
    return o.transpose(1, 0, 2, 3, 4, 5).reshape(bsz, n_q, N_HEADS * HEAD_DIM)


def _s5_discretise(a_re, a_im, log_dt, b_re, b_im):
    dt = jnp.exp(log_dt)[:, None]
    ea = jnp.exp(a_re * dt)
    ab_re, ab_im = ea * jnp.cos(a_im * dt), ea * jnp.sin(a_im * dt)
    den = a_re * a_re + a_im * a_im
    nr, ni = ab_re - 1.0, ab_im
    rr = (nr * a_re + ni * a_im) / den
    ri = (ni * a_re - nr * a_im) / den
    bb_re = rr[..., None] * b_re - ri[..., None] * b_im
    bb_im = rr[..., None] * b_im + ri[..., None] * b_re
    return ab_re, ab_im, bb_re, bb_im


def _complex_affine_combine(e1, e2):
    a1r, a1i, b1r, b1i = e1
    a2r, a2i, b2r, b2i = e2
    return (a2r * a1r - a2i * a1i, a2r * a1i + a2i * a1r,
            a2r * b1r - a2i * b1i + b2r, a2r * b1i + a2i * b1r + b2i)


def _s5_scan(u, ab_re, ab_im, bb_re, bb_im, reverse, init=None):
    bsz, n = u.shape[:2]
    ug = u.reshape(bsz, n, S5_GROUPS, S5_GROUP_CH)
    bu_re = jnp.einsum('gpc,blgc->blgp', bb_re, ug)
    bu_im = jnp.einsum('gpc,blgc->blgp', bb_im, ug)
    a_re = jnp.broadcast_to(ab_re, (1, n) + ab_re.shape)
    a_im = jnp.broadcast_to(ab_im, (1, n) + ab_im.shape)
    acum_re, acum_im, h_re, h_im = lax.associative_scan(
        _complex_affine_combine, (a_re, a_im, bu_re, bu_im), reverse=reverse, axis=1)
    if init is not None:
        s_re, s_im = init
        h_re = h_re + acum_re * s_re - acum_im * s_im
        h_im = h_im + acum_re * s_im + acum_im * s_re
    return h_re, h_im


def _s5_readout(h_re, h_im, c_re, c_im):
    bsz, n = h_re.shape[:2]
    y = jnp.einsum('gcp,blgp->blgc', c_re, h_re) - jnp.einsum('gcp,blgp->blgc', c_im, h_im)
    return y.reshape(bsz, n, S5_WIDTH)


def _merge(y_ssm, o, gate_logits, w_glu_a, w_glu_b, w_attn_o, w_out):
    g = jax.nn.gelu(y_ssm)
    ssm_branch = (g @ w_glu_a) * jax.nn.sigmoid(g @ w_glu_b)
    attn_branch = o @ w_attn_o
    gate_ssm, gate_attn = jnp.split(jax.nn.sigmoid(gate_logits), N_BRANCHES, axis=-1)
    return (gate_ssm * ssm_branch + gate_attn * attn_branch) @ w_out


def _hier_moe(h, w_rg, b_rg, w_re, b_re, w_gate, w_up, w_down):
    shape = h.shape
    hf = h.reshape(-1, shape[-1])
    n_tok = hf.shape[0]
    g_logits = (hf @ w_rg + b_rg).astype(jnp.float32)
    g_idx = jnp.argmax(g_logits, axis=-1)
    g_w = jnp.take_along_axis(jax.nn.softmax(g_logits, axis=-1), g_idx[:, None], axis=-1)
    e_logits = (hf @ w_re + b_re).astype(jnp.float32).reshape(n_tok, N_EXPERT_GROUPS, EXPERTS_PER_GROUP)
    e_in = jnp.take_along_axis(e_logits, g_idx[:, None, None], axis=1)[:, 0]
    top_v, top_i = lax.top_k(e_in, EXPERT_TOP_K)
    top_w = jax.nn.softmax(top_v, axis=-1) * g_w
    expert_id = g_idx[:, None] * EXPERTS_PER_GROUP + top_i
    combine = jnp.sum(jax.nn.one_hot(expert_id, N_EXPERTS, dtype=jnp.float32) * top_w[..., None],
                      axis=1).astype(h.dtype)
    y = jnp.zeros_like(hf)
    for gi in range(N_EXPERT_GROUPS):
        sl = slice(gi * EXPERTS_PER_GROUP, (gi + 1) * EXPERTS_PER_GROUP)
        a = jnp.einsum('td,edf->tef', hf, w_gate[sl])
        b = jnp.einsum('td,edf->tef', hf, w_up[sl])
        y = y + jnp.einsum('tef,efd->td', jax.nn.silu(a) * b * combine[:, sl, None], w_down[sl])
    return y.reshape(shape)


def setup_inputs(seed: int = 0) -> dict:
    key = jax.random.key(seed)
    ks = list(jax.random.split(key, 40))
    f32 = jnp.float32

    def nrm(shape):
        return jax.random.normal(ks.pop(), shape, f32)

    beta = (8.0 * DEPTH) ** -0.25
    L = DEPTH
    n_idx = jnp.arange(S5_STATE, dtype=f32)
    log_dt = jax.random.uniform(ks.pop(), (L, 2, S5_GROUPS), f32, math.log(S5_DT_MIN), math.log(S5_DT_MAX))
    return {
        "x": nrm((BATCH, SEQ, D_MODEL)),
        "c": nrm((BATCH, D_MODEL)),
        "ctx": nrm((BATCH, CTX_LEN, D_MODEL)),
        "c_ctx": nrm((D_MODEL,)),
        "w_mod": nrm((L, D_MODEL, N_MOD * D_MODEL)) * (0.5 * D_MODEL ** -0.5),
        "b_mod": 0.02 * nrm((L, N_MOD * D_MODEL)),
        "w_in": nrm((L, D_MODEL, IN_WIDTH)) * D_MODEL ** -0.5,
        "s5_a_re": -0.5 + 0.01 * nrm((L, 2, S5_GROUPS, S5_STATE)),
        "s5_a_im": math.pi * n_idx + 0.01 * nrm((L, 2, S5_GROUPS, S5_STATE)),
        "s5_log_dt": log_dt,
        "s5_b_re": nrm((L, 2, S5_GROUPS, S5_STATE, S5_GROUP_CH)) * (2.0 * S5_GROUP_CH) ** -0.5,
        "s5_b_im": nrm((L, 2, S5_GROUPS, S5_STATE, S5_GROUP_CH)) * (2.0 * S5_GROUP_CH) ** -0.5,
        "s5_c_re": nrm((L, 2, S5_GROUPS, S5_GROUP_CH, S5_STATE)) * S5_STATE ** -0.5,
        "s5_c_im": nrm((L, 2, S5_GROUPS, S5_GROUP_CH, S5_STATE)) * S5_STATE ** -0.5,
        "s5_d": nrm((L, S5_WIDTH)),
        "w_glu_a": nrm((L, S5_WIDTH, D_MODEL)) * S5_WIDTH ** -0.5,
        "w_glu_b": nrm((L, S5_WIDTH, D_MODEL)) * S5_WIDTH ** -0.5,
        "q_gain": 1.0 + 0.02 * nrm((L, HEAD_DIM)),
        "k_gain": 1.0 + 0.02 * nrm((L, HEAD_DIM)),
        "w_attn_o": nrm((L, ATTN_WIDTH, D_MODEL)) * ATTN_WIDTH ** -0.5,
        "w_out": nrm((L, D_MODEL, D_MODEL)) * (beta * D_MODEL ** -0.5),
        "ln1_g": 1.0 + 0.02 * nrm((L, D_MODEL)),
        "ln1_b": 0.02 * nrm((L, D_MODEL)),
        "w_router_group": nrm((L, D_MODEL, N_EXPERT_GROUPS)) * D_MODEL ** -0.5,
        "b_router_group": 0.01 * nrm((L, N_EXPERT_GROUPS)),
        "w_router_expert": nrm((L, D_MODEL, N_EXPERTS)) * D_MODEL ** -0.5,
        "b_router_expert": 0.01 * nrm((L, N_EXPERTS)),
        "w_exp_gate": nrm((L, N_EXPERTS, D_MODEL, D_EXPERT)) * D_MODEL ** -0.5,
        "w_exp_up": nrm((L, N_EXPERTS, D_MODEL, D_EXPERT)) * D_MODEL ** -0.5,
        "w_exp_down": nrm((L, N_EXPERTS, D_EXPERT, D_MODEL)) * (beta * D_EXPERT ** -0.5),
        "ln2_g": 1.0 + 0.02 * nrm((L, D_MODEL)),
        "ln2_b": 0.02 * nrm((L, D_MODEL)),
    }


def reference(x, c, ctx, c_ctx, w_mod, b_mod, w_in, s5_a_re, s5_a_im, s5_log_dt, s5_b_re, s5_b_im,
              s5_c_re, s5_c_im, s5_d, w_glu_a, w_glu_b, q_gain, k_gain, w_attn_o, w_out, ln1_g, ln1_b,
              w_router_group, b_router_group, w_router_expert, b_router_expert, w_exp_gate, w_exp_up,
              w_exp_down, ln2_g, ln2_b):
    bsz, n_lat, _ = x.shape
    n_ctx = ctx.shape[1]
    rows = n_lat // GRID_W
    cos, sin = _axial_rope_tables(rows, x.dtype)
    alpha = (2.0 * DEPTH) ** 0.25
    for i in range(DEPTH):
        is_last = i == DEPTH - 1
        sh1, sc1, g1, sh2, sc2, g2 = jnp.split(_adaln(c, w_mod[i], b_mod[i])[:, None, :], N_MOD, axis=-1)
        sh1c, sc1c, g1c, sh2c, sc2c, g2c = jnp.split(_adaln(c_ctx, w_mod[i], b_mod[i]), N_MOD, axis=-1)

        h = _modulate(x, sh1, sc1)
        hc = _modulate(ctx, sh1c, sc1c)
        u, q, k, v, gate_logits = jnp.split(h @ w_in[i], IN_SPLITS, axis=-1)
        uc, qc, kc, vc, gate_logits_c = jnp.split(hc @ w_in[i], IN_SPLITS, axis=-1)

        y_ssm = u * s5_d[i]
        ctx_states = []
        for d, reverse in enumerate((False, True)):
            disc = _s5_discretise(s5_a_re[i, d], s5_a_im[i, d], s5_log_dt[i, d], s5_b_re[i, d], s5_b_im[i, d])
            hc_re, hc_im = _s5_scan(uc, *disc, reverse=reverse)
            edge = slice(0, 1) if reverse else slice(n_ctx - 1, n_ctx)
            hl_re, hl_im = _s5_scan(u, *disc, reverse=reverse, init=(hc_re[:, edge], hc_im[:, edge]))
            y_ssm = y_ssm + _s5_readout(hl_re, hl_im, s5_c_re[i, d], s5_c_im[i, d])
            if not is_last:
                ctx_states.append((hc_re, hc_im))

        q = _rope(_rms_norm(q.reshape(bsz, n_lat, N_HEADS, HEAD_DIM), q_gain[i]), cos, sin)
        k = _rope(_rms_norm(k.reshape(bsz, n_lat, N_KV_HEADS, HEAD_DIM), k_gain[i]), cos, sin)
        v = v.reshape(bsz, n_lat, N_KV_HEADS, HEAD_DIM)
        kc = _rms_norm(kc.reshape(bsz, n_ctx, N_KV_HEADS, HEAD_DIM), k_gain[i])
        vc = vc.reshape(bsz, n_ctx, N_KV_HEADS, HEAD_DIM)
        o = _block_attention(q, jnp.concatenate([k, kc], axis=1), jnp.concatenate([v, vc], axis=1))

        mix = _merge(y_ssm, o, gate_logits, w_glu_a[i], w_glu_b[i], w_attn_o[i], w_out[i])
        x = _post_norm(alpha * x + g1 * mix, ln1_g[i], ln1_b[i])

        if not is_last:
            y_ssm_c = uc * s5_d[i]
            for d, (hc_re, hc_im) in enumerate(ctx_states):
                y_ssm_c = y_ssm_c + _s5_readout(hc_re, hc_im, s5_c_re[i, d], s5_c_im[i, d])
            qc = _rms_norm(qc.reshape(bsz, n_ctx, N_HEADS, HEAD_DIM), q_gain[i])
            oc = _block_attention(qc, kc, vc)
            mix_c = _merge(y_ssm_c, oc, gate_logits_c, w_glu_a[i], w_glu_b[i], w_attn_o[i], w_out[i])
            ctx = _post_norm(alpha * ctx + g1c * mix_c, ln1_g[i], ln1_b[i])

        moe_w = (w_router_group[i], b_router_group[i], w_router_expert[i], b_router_expert[i],
                 w_exp_gate[i], w_exp_up[i], w_exp_down[i])
        x = _post_norm(alpha * x + g2 * _hier_moe(_modulate(x, sh2, sc2), *moe_w), ln2_g[i], ln2_b[i])
        if not is_last:
            ctx = _post_norm(alpha * ctx + g2c * _hier_moe(_modulate(ctx, sh2c, sc2c), *moe_w),
                             ln2_g[i], ln2_b[i])
    return x
```

```python
import os
import math
import numpy as np
from contextlib import ExitStack
import concourse.bass as bass
import concourse.mybir as mybir
from concourse.bass_utils import run_bass_kernel_spmd

F32 = mybir.dt.float32
BF16 = mybir.dt.bfloat16
ACT = mybir.ActivationFunctionType
ALU = mybir.AluOpType
AX = mybir.AxisListType

D = 1024
NLAT = 8192
NCTX = 256
NFULL = NLAT + NCTX
NOWN = 2048
NTF = NFULL // 128
NTO = NOWN // 128
NJ = NFULL // 64
NJF = NFULL // 8
EPS = 1e-6
ALPHA = 2.0 ** 0.25
STOP = int(os.environ.get("K_STOP", "99"))
DEBUG = os.environ.get("K_DEBUG", "") != ""


class Prog:
    ENGS = ["pe", "act", "dve", "pool", "sp"]

    def __init__(self, nc):
        self.nc = nc
        self.ops = []

    def add(self, eng, fn, r=(), w=(), dma=None, ndma=1):
        self.ops.append(dict(eng=eng, fn=fn, r=tuple(r), w=tuple(w), dma=dma, ndma=ndma, barrier=False))

    def pe(self, fn, r=(), w=()):
        self.add("pe", fn, r, w)

    def act(self, fn, r=(), w=()):
        self.add("act", fn, r, w)

    def dve(self, fn, r=(), w=()):
        self.add("dve", fn, r, w)

    def pool(self, fn, r=(), w=()):
        self.add("pool", fn, r, w)

    def dma(self, fn, key, r=(), w=(), eng="sp", n=1):
        self.add(eng, fn, r, w, dma=key, ndma=n)

    def barrier(self):
        for e in self.ENGS:
            self.ops.append(dict(eng=e, fn=None, r=(), w=(), dma=None, ndma=0, barrier=True))

    def emit(self):
        nc = self.nc
        ops = self.ops
        n = len(ops)
        last_w, readers = {}, {}
        deps = [None] * n
        last_eng, last_dma = {}, {}
        for i, op in enumerate(ops):
            d = set()
            if op["barrier"]:
                for e, j in last_eng.items():
                    if e != op["eng"]:
                        d.add(j)
                for k, j in last_dma.items():
                    d.add(j)
            for b in op["r"]:
                if b in last_w:
                    d.add(last_w[b])
            for b in op["w"]:
                if b in last_w:
                    d.add(last_w[b])
                for j in readers.get(b, ()):
                    d.add(j)
            for b in op["r"]:
                readers.setdefault(b, []).append(i)
            for b in op["w"]:
                readers[b] = []
                last_w[b] = i
            d.discard(i)
            deps[i] = d
            if op["dma"] is not None:
                last_dma[op["dma"]] = i
            elif not op["barrier"]:
                last_eng[op["eng"]] = i
        signal = [False] * n
        for i, op in enumerate(ops):
            for j in deps[i]:
                pj = ops[j]
                if pj["dma"] is not None:
                    continue
                if pj["eng"] == "pe" and op["eng"] == "pe" and op["dma"] is None:
                    continue
                signal[j] = True
        tick = [0] * n
        cnt = {e: 0 for e in self.ENGS}
        dcnt = {}
        for i, op in enumerate(ops):
            if op["dma"] is not None:
                dcnt[op["dma"]] = dcnt.get(op["dma"], 0) + op["ndma"]
                tick[i] = dcnt[op["dma"]] * 16
            elif signal[i]:
                cnt[op["eng"]] += 1
                tick[i] = cnt[op["eng"]]
        es = ExitStack()
        esem = {e: es.enter_context(nc.semaphore("s_" + e)) for e in self.ENGS}
        dsem = {}
        for k in dcnt:
            dsem[k] = es.enter_context(nc.semaphore("d_%d" % len(dsem)))
        waits = [None] * n
        seen = {e: {} for e in self.ENGS}
        for i, op in enumerate(ops):
            wl = {}
            for j in deps[i]:
                pj = ops[j]
                if pj["dma"] is not None:
                    key = ("d", pj["dma"])
                    sem = dsem[pj["dma"]]
                else:
                    if pj["eng"] == "pe" and op["eng"] == "pe" and op["dma"] is None:
                        continue
                    key = ("e", pj["eng"])
                    sem = esem[pj["eng"]]
                v = tick[j]
                if seen[op["eng"]].get(key, 0) >= v:
                    continue
                if key not in wl or wl[key][1] < v:
                    wl[key] = (sem, v)
            for key, (sem, v) in wl.items():
                seen[op["eng"]][key] = v
            waits[i] = list(wl.values())
        self.stats = dict(n=n, sig=dict(cnt), dkeys=len(dcnt), nwaits=sum(len(w) for w in waits))
        block = es.enter_context(nc.Block())

        def run(engname, eng):
            for i, op in enumerate(ops):
                if op["eng"] != engname:
                    continue
                for sem, v in waits[i]:
                    eng.wait_ge(sem, v)
                if op["fn"] is None:
                    continue
                res = op["fn"](eng)
                if op["dma"] is not None:
                    if not isinstance(res, (list, tuple)):
                        res = [res]
                    assert len(res) == op["ndma"], (len(res), op["ndma"])
                    for ins in res:
                        ins.then_inc(dsem[op["dma"]], 16)
                elif signal[i]:
                    if isinstance(res, (list, tuple)):
                        res = res[-1]
                    res.then_inc(esem[engname], 1)

        @block.tensor
        def _(e):
            run("pe", e)

        @block.scalar
        def _(e):
            run("act", e)

        @block.vector
        def _(e):
            run("dve", e)

        @block.gpsimd
        def _(e):
            run("pool", e)

        @block.sync
        def _(e):
            run("sp", e)

        es.close()


def AP_(t, offset, dims):
    return bass.AP(t, offset, [list(d) for d in dims])


def pstride(t):
    return t[:].ap[0][0]


def build(dbg_names=()):
    nc = bass.Bass("TRN2", target_bir_lowering=False)
    dram_in = lambda name, shape, dt=F32: nc.dram_tensor(name, list(shape), dt, kind="ExternalInput").ap()
    xf = dram_in("xf", [NFULL, D])
    xo = dram_in("xo", [NOWN, D])
    ccT = dram_in("ccT", [128, 8, 2])
    w_mod = dram_in("w_mod", [D, 6 * D])
    b_mod = dram_in("b_mod", [1, 6 * D])
    w_in = dram_in("w_in", [D, 4096])
    rope_f = dram_in("rope_f", [NFULL, 128])
    rope_o = dram_in("rope_o", [NOWN, 128])
    q_gain = dram_in("q_gain", [1, 128])
    k_gain = dram_in("k_gain", [1, 128])
    cst_mask8 = dram_in("cst_mask8", [128, 8])
    cst_mask8c = dram_in("cst_mask8c", [128, 8])
    cst_sel16 = dram_in("cst_sel16", [128, 16])
    s5_a = dram_in("s5_a", [128, 2, 2, 16])
    s5_ldt = dram_in("s5_ldt", [128, 2, 16])
    s5_b = dram_in("s5_b", [128, 2, 2, 16, 16])
    s5_c = dram_in("s5_c", [128, 2, 2, 16, 16])
    s5_dcol = dram_in("s5_dcol", [128, 32])
    cst_eZ = dram_in("cst_eZ", [128, 2, 64])
    cst_eR = dram_in("cst_eR", [128, 2, 72])
    cst_mf = dram_in("cst_mf", [128, 128])
    cst_mb = dram_in("cst_mb", [128, 128])
    cst_selg = dram_in("cst_selg", [128, 8, 128])
    cmask = dram_in("cmask", [128, 4])
    w_glu_a = dram_in("w_glu_a", [512, D])
    w_glu_b = dram_in("w_glu_b", [512, D])
    w_attn_o = dram_in("w_attn_o", [D, D])
    w_out = dram_in("w_out", [D, D])
    ln1_g = dram_in("ln1_g", [1, D])
    ln1_b = dram_in("ln1_b", [1, D])
    ln2_g = dram_in("ln2_g", [1, D])
    ln2_b = dram_in("ln2_b", [1, D])
    w_rt = dram_in("w_rt", [D, 36])
    b_rt = dram_in("b_rt", [1, 36])
    w_eg = dram_in("w_eg", [32, D, 512])
    w_eu = dram_in("w_eu", [32, D, 512])
    w_ed = dram_in("w_ed", [32, 512, D])
    cst_sele = dram_in("cst_sele", [32, 32, 128])
    out = nc.dram_tensor("out", [NOWN, D], F32, kind="ExternalOutput").ap()
    scr = lambda name, shape, dt: nc.dram_tensor(name, list(shape), dt, kind="Internal").ap()
    mod_d = scr("mod_d", [2, 6 * D], F32)
    kT_d = scr("kT_d", [2, 128, NFULL], BF16)
    v_d = scr("v_d", [NFULL, 256], BF16)
    x1_d = scr("x1_d", [NOWN, D], F32)
    dbg_out = {}

    P = Prog(nc)
    ges = ExitStack()

    free_list = [[16640, 229376]]
    peak = [0]

    def _alloc(nbytes):
        nbytes = (nbytes + 63) // 64 * 64
        for iv in free_list:
            if iv[1] - iv[0] >= nbytes:
                off = iv[0]
                iv[0] += nbytes
                peak[0] = max(peak[0], off + nbytes)
                return off, nbytes
        raise RuntimeError("SBUF manual allocator out of space for %d bytes; free=%s" % (nbytes, free_list))

    def _free(off, nbytes):
        free_list.append([off, off + nbytes])
        free_list.sort()
        merged = []
        for iv in free_list:
            if iv[1] == iv[0]:
                continue
            if merged and merged[-1][1] == iv[0]:
                merged[-1][1] = iv[1]
            else:
                merged.append(iv)
        free_list[:] = merged

    def SB(es, name, shape, dt):
        esz = 4 if dt == F32 else 2
        nb = esz
        for s_ in shape[1:]:
            nb *= s_
        off, nbytes = _alloc(nb)
        t = nc.alloc_sbuf_tensor_at(name, list(shape), dt, offset=off)
        es.callback(_free, off, nbytes)
        return t

    def PS(es, name, shape, dt):
        return es.enter_context(nc.psum_tensor(name, list(shape), dt))

    def dump(name, t_ap, shape, dt=F32, r=()):
        if name not in dbg_names:
            return
        o = nc.dram_tensor("dbg_" + name, list(shape), dt, kind="ExternalOutput").ap()
        dbg_out[name] = o
        P.dma(lambda e: e.dma_start(out=o, in_=t_ap), key="dbg_" + name, r=r, w=["dbg_" + name])

    ident_f = SB(ges, "ident_f", [128, 128], F32)
    ident_b = SB(ges, "ident_b", [128, 128], BF16)
    ones_b = SB(ges, "ones_b", [1, 128], BF16)
    eps_t = SB(ges, "eps_t", [128, 1], F32)
    modT = SB(ges, "modT", [128, 48, 2], F32)
    op1p = SB(ges, "op1p", [128, 8, 2], F32)
    sh1T = SB(ges, "sh1T", [128, 8, 2], F32)
    gk_bc = SB(ges, "gk_bc", [128, 128], F32)
    gq_bc = SB(ges, "gq_bc", [128, 128], F32)
    negC = SB(ges, "negC", [128, 1], F32)
    mask8 = SB(ges, "mask8", [128, 8], BF16)
    sel16 = SB(ges, "sel16", [128, 16], BF16)

    P.pool(lambda e: e.memset(ident_f[:], 1.0), w=["ident_f"])
    P.pool(lambda e: e.affine_select(out=ident_f[:], in_=ident_f[:], pattern=[[-1, 128]], compare_op=ALU.is_equal,
                                     fill=0.0, base=0, channel_multiplier=1), r=["ident_f"], w=["ident_f"])
    P.dve(lambda e: e.tensor_copy(out=ident_b[:], in_=ident_f[:]), r=["ident_f"], w=["ident_b"])
    P.dve(lambda e: e.memset(ones_b[:], 1.0), w=["ones_b"])
    P.dve(lambda e: e.memset(eps_t[:], EPS), w=["eps_t"])
    P.dma(lambda e: e.dma_start(out=gk_bc[:], in_=k_gain.partition_broadcast(128)), key="gk_bc", w=["gk_bc"])
    P.dma(lambda e: e.dma_start(out=gq_bc[:], in_=q_gain.partition_broadcast(128)), key="gq_bc", w=["gq_bc"])

    with ExitStack() as es:
        scT = SB(es, "scT", [128, 8, 2], F32)
        ccs = SB(es, "ccs", [128, 8, 2], F32)
        wm = [SB(es, "wm%d" % i, [128, 8, 512], F32) for i in range(2)]
        bm = SB(es, "bm", [2, 6 * D], F32)
        mod_sb = SB(es, "mod_sb", [2, 6 * D], F32)
        m8f = SB(es, "m8f", [128, 8], F32)
        s16f = SB(es, "s16f", [128, 16], F32)
        tmpg = SB(es, "tmpg", [128, 2], F32)
        pM = [PS(es, "pM%d" % i, [2, 512], F32) for i in range(2)]
        pMT = PS(es, "pMT", [128, 48, 2], F32)
        P.dma(lambda e: e.dma_start(out=ccs[:], in_=ccT), key="ccs", w=["ccs"])
        P.dma(lambda e: e.dma_start(out=bm[:], in_=b_mod.partition_broadcast(2)), key="bm", w=["bm"])
        P.dma(lambda e: e.dma_start(out=m8f[:], in_=cst_mask8), key="m8f", w=["m8f"])
        P.dma(lambda e: e.dma_start(out=s16f[:], in_=cst_sel16), key="s16f", w=["s16f"])
        P.dve(lambda e: e.tensor_copy(out=mask8[:], in_=m8f[:]), r=["m8f"], w=["mask8"])
        P.dve(lambda e: e.tensor_copy(out=sel16[:], in_=s16f[:]), r=["s16f"], w=["sel16"])
        P.act(lambda e: e.activation(out=scT[:], in_=ccs[:], func=ACT.Silu), r=["ccs"], w=["scT"])
        P.dve(lambda e: e.tensor_reduce(out=tmpg[:, 0:1], in_=gq_bc[:], axis=AX.X, op=ALU.max, apply_absolute_value=True),
              r=["gq_bc"], w=["tmpg0"])
        P.dve(lambda e: e.tensor_reduce(out=tmpg[:, 1:2], in_=gk_bc[:], axis=AX.X, op=ALU.max, apply_absolute_value=True),
              r=["gk_bc"], w=["tmpg1"])
        P.dve(lambda e: e.scalar_tensor_tensor(out=negC[:], in0=tmpg[:, 0:1], scalar=-math.sqrt(128.0), in1=tmpg[:, 1:2],
                                               op0=ALU.mult, op1=ALU.mult), r=["tmpg0", "tmpg1"], w=["negC"])
        for nb in range(12):
            s = nb % 2
            P.dma(lambda e, nb=nb, s=s: e.dma_start(out=wm[s][:], in_=w_mod[:, nb * 512:(nb + 1) * 512].rearrange("(kc p) n -> p kc n", p=128)),
                  key=("wm", s), w=[("wm", s)])
            for kc in range(8):
                P.pe(lambda e, kc=kc, s=s: e.matmul(out=pM[s][:], lhsT=scT[:, kc, :], rhs=wm[s][:, kc, :], start=(kc == 0), stop=(kc == 7)),
                     r=["scT", ("wm", s)], w=[("pM", s)])
            P.dve(lambda e, nb=nb, s=s: e.tensor_tensor(out=mod_sb[:, nb * 512:(nb + 1) * 512], in0=pM[s][:], in1=bm[:, nb * 512:(nb + 1) * 512], op=ALU.add),
                  r=[("pM", s), "bm"], w=["mod_sb"])
        P.dma(lambda e: e.dma_start(out=mod_d, in_=mod_sb[:]), key="mod_d", r=["mod_sb"], w=["mod_d"])
        for j in range(48):
            P.pe(lambda e, j=j: e.transpose(out=pMT[:, j, :], in_=mod_sb[:, j * 128:(j + 1) * 128], identity=ident_f[0:2, 0:2]),
                 r=["mod_sb", "ident_f"], w=["pMT"])
        P.dve(lambda e: e.tensor_copy(out=modT[:], in_=pMT[:]), r=["pMT"], w=["modT"])
        P.dve(lambda e: e.tensor_copy(out=sh1T[:], in_=modT[:, 0:8, :]), r=["modT"], w=["sh1T"])
        P.dve(lambda e: e.tensor_scalar(out=op1p[:], in0=modT[:, 8:16, :], scalar1=1.0, scalar2=None, op0=ALU.add), r=["modT"], w=["op1p"])
        dump("mod", mod_sb[:], [2, 6 * D], r=["mod_sb"])
        P.barrier()
    if STOP <= 0:
        return finish(nc, P, ges, out, dbg_out)

    def fv(ap, off, dims):
        return bass.AP(ap.tensor, ap.offset + off, [list(ap.ap[0])] + [list(d) for d in dims])

    bias_sb = SB(ges, "bias_sb", [1, 5120], BF16)

    def prep_wblock(stage, pB, s, dst, col0, r, bcol, tag):
        P.dma(lambda e: e.dma_start(out=stage[s][:], in_=w_in[:, col0:col0 + 512].rearrange("(kc p) n -> p kc n", p=128)),
              key=("stg", s), w=[("stg", s)])
        for kc in range(8):
            P.pool(lambda e, kc=kc: e.tensor_scalar(out=dst[:, kc, :], in0=stage[s][:, kc, :], scalar1=op1p[:, kc, r:r + 1], scalar2=None, op0=ALU.mult),
                   r=[("stg", s), "op1p"], w=[tag])
        for kc in range(8):
            P.pe(lambda e, kc=kc: e.matmul(out=pB[:], lhsT=sh1T[:, kc, r:r + 1], rhs=stage[s][:, kc, :], start=(kc == 0), stop=(kc == 7)),
                 r=[("stg", s), "sh1T"], w=["pB"])
        P.act(lambda e: e.activation(out=bias_sb[:, bcol:bcol + 512], in_=pB[:], func=ACT.Copy), r=["pB"], w=["bias_sb"])

    def ln_tile(xt_ap, xkey, st, mv, rstd, nb, hb_ap, hkey, sfx):
        for i in range(2):
            P.dve(lambda e, i=i: e.bn_stats(out=st[:, i, :], in_=xt_ap[:, i * 512:(i + 1) * 512]), r=[xkey], w=[("st", sfx, i)])
        P.dve(lambda e: e.bn_aggr(out=mv[:], in_=st[:].rearrange("p a b -> p (a b)")), r=[("st", sfx, 0), ("st", sfx, 1)], w=[("mv", sfx)])
        P.act(lambda e: e.activation(out=rstd[:], in_=mv[:, 1:2], func=ACT.Sqrt, bias=eps_t[:], scale=1.0), r=[("mv", sfx), "eps_t"], w=[("rstd", sfx)])
        P.dve(lambda e: e.reciprocal(out=rstd[:], in_=rstd[:]), r=[("rstd", sfx)], w=[("rstd", sfx)])
        P.dve(lambda e: e.scalar_tensor_tensor(out=nb[:], in0=mv[:, 0:1], scalar=-1.0, in1=rstd[:], op0=ALU.mult, op1=ALU.mult),
              r=[("mv", sfx), ("rstd", sfx)], w=[("nb", sfx)])
        P.act(lambda e: e.activation(out=hb_ap, in_=xt_ap, func=ACT.Identity, bias=nb[:], scale=rstd[:]), r=[xkey, ("nb", sfx), ("rstd", sfx)], w=[hkey])

    def rms_rope(eng_add, src, nh, gain_bc, rt, dst, tmp, keys_r, key_w, sfx):
        sq, ss, rk, ta, tb = tmp
        n = nh * 128
        P.dve(lambda e: e.tensor_tensor(out=sq[:, :n], in0=src[:, :n], in1=src[:, :n], op=ALU.mult), r=keys_r, w=[("sq", sfx)])
        P.dve(lambda e: e.tensor_reduce(out=ss[:, :nh], in_=sq[:, :n].rearrange("p (h d) -> p h d", d=128), axis=AX.X, op=ALU.add), r=[("sq", sfx)], w=[("ss", sfx)])
        P.act(lambda e: e.activation(out=rk[:, :nh], in_=ss[:, :nh], func=ACT.Sqrt, bias=eps_t[:], scale=1.0 / 128.0), r=[("ss", sfx), "eps_t"], w=[("rk", sfx)])
        P.dve(lambda e: e.reciprocal(out=rk[:, :nh], in_=rk[:, :nh]), r=[("rk", sfx)], w=[("rk", sfx)])
        P.dve(lambda e: e.tensor_tensor(out=src[:, :n].rearrange("p (h d) -> p h d", d=128), in0=src[:, :n].rearrange("p (h d) -> p h d", d=128),
                                        in1=fv(rk[:], 0, [[1, nh], [0, 128]]), op=ALU.mult), r=keys_r + [("rk", sfx)], w=keys_r)
        eng_add(lambda e: e.tensor_tensor(out=src[:, :n].rearrange("p (h d) -> p h d", d=128), in0=src[:, :n].rearrange("p (h d) -> p h d", d=128),
                                          in1=fv(gain_bc[:], 0, [[0, nh], [1, 128]]), op=ALU.mult), r=keys_r, w=keys_r)
        x1 = fv(src[:], 0, [[128, nh], [64, 2], [1, 32]])
        x2 = fv(src[:], 32, [[128, nh], [64, 2], [1, 32]])
        cs = fv(rt[:], 0, [[0, nh], [32, 2], [1, 32]])
        sn = fv(rt[:], 64, [[0, nh], [32, 2], [1, 32]])
        o1 = fv(dst[:], 0, [[128, nh], [64, 2], [1, 32]])
        o2 = fv(dst[:], 32, [[128, nh], [64, 2], [1, 32]])
        tav = fv(ta[:], 0, [[64, nh], [32, 2], [1, 32]])
        tbv = fv(tb[:], 0, [[64, nh], [32, 2], [1, 32]])
        rtk = keys_r[-1:] if False else []
        eng_add(lambda e: e.tensor_tensor(out=tav, in0=x1, in1=cs, op=ALU.mult), r=keys_r + [("rt", sfx)], w=[("ta", sfx)])
        eng_add(lambda e: e.tensor_tensor(out=tbv, in0=x2, in1=sn, op=ALU.mult), r=keys_r + [("rt", sfx)], w=[("tb", sfx)])
        eng_add(lambda e: e.tensor_tensor(out=o1, in0=tav, in1=tbv, op=ALU.subtract), r=[("ta", sfx), ("tb", sfx)], w=[key_w])
        eng_add(lambda e: e.tensor_tensor(out=tav, in0=x1, in1=sn, op=ALU.mult), r=keys_r + [("rt", sfx)], w=[("ta", sfx)])
        eng_add(lambda e: e.tensor_tensor(out=tbv, in0=x2, in1=cs, op=ALU.mult), r=keys_r + [("rt", sfx)], w=[("tb", sfx)])
        eng_add(lambda e: e.tensor_tensor(out=o2, in0=tav, in1=tbv, op=ALU.add), r=[("ta", sfx), ("tb", sfx)], w=[key_w])

    esA = ExitStack()
    u8 = SB(esA, "u8", [128, 32, NJF], BF16)
    with ExitStack() as es:
        stage = [SB(es, "stage%d" % i, [128, 8, 512], F32) for i in range(2)]
        Wkv = [SB(es, "Wkv%d" % r, [128, 8, 512], BF16) for r in range(2)]
        Wu = [SB(es, "Wu%d" % r, [128, 8, 512], BF16) for r in range(2)]
        xt = [SB(es, "xt%d" % i, [128, D], F32) for i in range(3)]
        rt = [SB(es, "rt%d" % i, [128, 128], F32) for i in range(2)]
        hb = [SB(es, "hb%d" % i, [128, D], BF16) for i in range(2)]
        hT = [SB(es, "hT%d" % i, [128, 8, 128], BF16) for i in range(2)]
        st = SB(es, "st", [128, 2, 6], F32)
        mv = SB(es, "mv", [128, 2], F32)
        rstd = SB(es, "rstd", [128, 1], F32)
        nbt = SB(es, "nbt", [128, 1], F32)
        k_sb = SB(es, "k_sb", [128, 256], F32)
        v_bf = [SB(es, "v_bf%d" % i, [128, 256], BF16) for i in range(2)]
        kr = SB(es, "kr", [128, 256], BF16)
        kT_sb = [SB(es, "kT_sb%d" % i, [128, 2, 128], BF16) for i in range(2)]
        tmp = (SB(es, "sq", [128, 256], F32), SB(es, "ss", [128, 2], F32), SB(es, "rk", [128, 2], F32),
               SB(es, "ta", [128, 128], F32), SB(es, "tb", [128, 128], F32))
        u_bf = SB(es, "u_bf", [128, 512], BF16)
        Um = [SB(es, "Um%d" % i, [128, 32, 128], BF16) for i in range(2)]
        pB = PS(es, "pB", [1, 512], F32)
        pT = [PS(es, "pT%d" % i, [128, 8, 128], BF16) for i in range(2)]
        pKV = PS(es, "pKV", [128, 512], F32)
        pU = PS(es, "pU", [128, 512], F32)
        pKT = PS(es, "pKT", [128, 2, 128], BF16)
        pU8 = PS(es, "pU8", [128, 32, 16], F32)
        prep_wblock(stage, pB, 0, Wkv[1], 1536, 1, 4096, ("Wkv", 1))
        prep_wblock(stage, pB, 1, Wu[1], 0, 1, 4608, ("Wu", 1))
        prep_wblock(stage, pB, 0, Wkv[0], 1536, 0, 1536, ("Wkv", 0))
        prep_wblock(stage, pB, 1, Wu[0], 0, 0, 0, ("Wu", 0))
        for tt in range(int(os.environ.get('K_NT', NTF))):
            r = 1 if tt < 2 else 0
            s3, s2 = tt % 3, tt % 2
            t0 = tt * 128
            bkv, bu = (4096, 4608) if r == 1 else (1536, 0)
            P.dma(lambda e, s3=s3, t0=t0: e.dma_start(out=xt[s3][:], in_=xf[t0:t0 + 128, :]), key=("xt", s3), w=[("xt", s3)])
            P.dma(lambda e, s2=s2, t0=t0: e.dma_start(out=rt[s2][:], in_=rope_f[t0:t0 + 128, :]), key=("rtd", s2), w=[("rt", "A")] if False else [("rtA", s2)])
            ln_tile(xt[s3][:], ("xt", s3), st, mv, rstd, nbt, hb[s2][:], ("hb", s2), "A")
            for kc in range(8):
                P.pe(lambda e, kc=kc, s2=s2: e.transpose(out=pT[s2][:, kc, :], in_=hb[s2][:, kc * 128:(kc + 1) * 128], identity=ident_b[:]),
                     r=[("hb", s2), "ident_b"], w=[("pT", s2)])
            P.dve(lambda e, s2=s2: e.tensor_copy(out=hT[s2][:], in_=pT[s2][:]), r=[("pT", s2)], w=[("hT", s2)])
            for kc in range(8):
                P.pe(lambda e, kc=kc, s2=s2, r=r: e.matmul(out=pKV[:], lhsT=hT[s2][:, kc, :], rhs=Wkv[r][:, kc, :], start=(kc == 0), stop=False),
                     r=[("hT", s2), ("Wkv", r)], w=["pKV"])
            P.pe(lambda e, bkv=bkv: e.matmul(out=pKV[:], lhsT=ones_b[0:1, :], rhs=bias_sb[0:1, bkv:bkv + 512], start=False, stop=True),
                 r=["ones_b", "bias_sb"], w=["pKV"])
            for kc in range(8):
                P.pe(lambda e, kc=kc, s2=s2, r=r: e.matmul(out=pU[:], lhsT=hT[s2][:, kc, :], rhs=Wu[r][:, kc, :], start=(kc == 0), stop=False),
                     r=[("hT", s2), ("Wu", r)], w=["pU"])
            P.pe(lambda e, bu=bu: e.matmul(out=pU[:], lhsT=ones_b[0:1, :], rhs=bias_sb[0:1, bu:bu + 512], start=False, stop=True),
                 r=["ones_b", "bias_sb"], w=["pU"])
            KP = int(os.environ.get('K_PATH', '15'))
            if KP & 1:
                P.act(lambda e: e.activation(out=k_sb[:], in_=pKV[:, 0:256], func=ACT.Copy), r=["pKV"], w=["k_sb"])
                if KP & 8:
                    rms_rope(P.pool, k_sb, 2, gk_bc, rt[s2], kr, tmp, ["k_sb", ("rtA", s2), "gk_bc"], "kr", "A")
                else:
                    P.dve(lambda e: e.tensor_copy(out=kr[:], in_=k_sb[:]), r=["k_sb"], w=["kr"])
                for h in range(2):
                    P.pe(lambda e, h=h: e.transpose(out=pKT[:, h, :], in_=kr[:, h * 128:(h + 1) * 128], identity=ident_b[:]), r=["kr", "ident_b"], w=["pKT"])
                P.act(lambda e, s2=s2: e.activation(out=kT_sb[s2][:], in_=pKT[:], func=ACT.Copy), r=["pKT"], w=[("kT_sb", s2)])
                P.dma(lambda e, s2=s2, t0=t0: e.dma_start(out=kT_d[:, :, t0:t0 + 128].rearrange("h p t -> p h t"), in_=kT_sb[s2][:]),
                      key=("kTd", s2), r=[("kT_sb", s2)], w=["kT_d"])
            if KP & 2:
                P.dve(lambda e, s2=s2: e.tensor_copy(out=v_bf[s2][:], in_=pKV[:, 256:512]), r=["pKV"], w=[("v_bf", s2)])
                P.dma(lambda e, s2=s2, t0=t0: e.dma_start(out=v_d[t0:t0 + 128, :], in_=v_bf[s2][:]), key=("vd", s2), r=[("v_bf", s2)], w=["v_d"])
            if KP & 4:
                P.act(lambda e: e.activation(out=u_bf[:], in_=pU[:], func=ACT.Copy), r=["pU"], w=["u_bf"])
                P.dve(lambda e, s2=s2: e.tensor_tensor(out=Um[s2][:].rearrange("p g (s c) -> p g s c", c=16), in0=fv(u_bf[:], 0, [[16, 32], [0, 8], [1, 16]]),
                                                       in1=fv(mask8[:], 0, [[0, 32], [1, 8], [0, 16]]), op=ALU.mult), r=["u_bf", "mask8"], w=[("Um", s2)])
                for g in range(32):
                    P.pe(lambda e, g=g, s2=s2: e.matmul(out=pU8[:, g, :], lhsT=Um[s2][:, g, :], rhs=sel16[:], start=True, stop=True),
                         r=[("Um", s2), "sel16"], w=["pU8"])
                P.act(lambda e, tt=tt: e.activation(out=u8[:, :, tt * 16:(tt + 1) * 16], in_=pU8[:], func=ACT.Copy), r=["pU8"], w=["u8"])
        dump("u8", u8[:], [128, 32, NJF], BF16, r=["u8"])
        if "kT" in dbg_names:
            o = nc.dram_tensor("dbg_kT", [2, 128, NFULL], BF16, kind="ExternalOutput").ap()
            dbg_out["kT"] = o
            P.dma(lambda e: e.dma_start(out=o, in_=kT_d), key="dbg_kT", r=["kT_d"], w=["dbg_kT"])
        if "v" in dbg_names:
            o2 = nc.dram_tensor("dbg_v", [NFULL, 256], BF16, kind="ExternalOutput").ap()
            dbg_out["v"] = o2
            P.dma(lambda e: e.dma_start(out=o2, in_=v_d), key="dbg_v", r=["v_d"], w=["dbg_v"])
        P.barrier()
    if STOP <= 1:
        esA.close()
        return finish(nc, P, ges, out, dbg_out)

    TWO_PI = 2.0 * math.pi
    MAGIC = 12582912.0
    esS = ExitStack()
    esH = ExitStack()
    esZ = ExitStack()
    S_re = SB(esH, "S_re", [128, 2, 16, NJ], F32)
    S_im = SB(esH, "S_im", [128, 2, 16, NJ], F32)
    erZ = SB(esZ, "erZ", [128, 2, 16, 64], F32)
    eiZ = SB(esZ, "eiZ", [128, 2, 16, 64], F32)
    erZ8 = SB(esS, "erZ8", [128, 2, 16, 8], F32)
    eiZ8 = SB(esS, "eiZ8", [128, 2, 16, 8], F32)
    erR = SB(esS, "erR", [128, 2, 16, 72], F32)
    eiR = SB(esS, "eiR", [128, 2, 16, 72], F32)
    bbre = SB(esS, "bbre", [128, 2, 16, 16], F32)
    bbim = SB(esS, "bbim", [128, 2, 16, 16], F32)
    ccre = SB(esS, "ccre", [128, 2, 16, 16], F32)
    ccim = SB(esS, "ccim", [128, 2, 16, 16], F32)
    A64r = SB(esS, "A64r", [128, 2, 16, 1], F32)
    A64i = SB(esS, "A64i", [128, 2, 16, 1], F32)
    ardt = SB(esS, "ardt", [128, 2, 16], F32)
    aidt = SB(esS, "aidt", [128, 2, 16], F32)
    ptmp = []
    piT = SB(esS, "piT", [128, 1], F32)

    def powtab(exps_ap, n, er_t, ei_t, tag):
        def v4(t):
            return t[:, :, :, 0:n]
        a_b = lambda t: fv(t[:], 0, [[16, 2], [1, 16], [0, n]])
        e_b = fv(exps_ap, 0, [[exps_ap.ap[1][0], 2], [0, 16], [1, n]])
        t0, t1, t2 = ptmp
        k = lambda i: ("ptmp", i)
        P.dve(lambda e: e.tensor_tensor(out=v4(t0), in0=a_b(ardt), in1=e_b, op=ALU.mult), r=["ardt", tag + "_e"], w=[k(0)])
        P.act(lambda e: e.activation(out=v4(t0), in_=v4(t0), func=ACT.Exp), r=[k(0)], w=[k(0)])
        P.dve(lambda e: e.tensor_tensor(out=v4(t1), in0=a_b(aidt), in1=e_b, op=ALU.mult), r=["aidt", tag + "_e"], w=[k(1)])
        P.dve(lambda e: e.tensor_scalar(out=v4(t2), in0=v4(t1), scalar1=MAGIC, scalar2=None, op0=ALU.add), r=[k(1)], w=[k(2)])
        P.dve(lambda e: e.tensor_scalar(out=v4(t2), in0=v4(t2), scalar1=MAGIC, scalar2=None, op0=ALU.subtract), r=[k(2)], w=[k(2)])
        P.dve(lambda e: e.tensor_tensor(out=v4(t2), in0=v4(t1), in1=v4(t2), op=ALU.subtract), r=[k(1), k(2)], w=[k(2)])
        P.act(lambda e: e.activation(out=v4(t2), in_=v4(t2), func=ACT.Sin, scale=TWO_PI), r=[k(2)], w=[k(2)])
        P.dve(lambda e: e.tensor_tensor(out=v4(ei_t), in0=v4(t0), in1=v4(t2), op=ALU.mult), r=[k(0), k(2)], w=[tag + "_ei"])
        P.dve(lambda e: e.tensor_scalar(out=v4(t1), in0=v4(t1), scalar1=0.25, scalar2=None, op0=ALU.add), r=[k(1)], w=[k(1)])
        P.dve(lambda e: e.tensor_scalar(out=v4(t2), in0=v4(t1), scalar1=MAGIC, scalar2=None, op0=ALU.add), r=[k(1)], w=[k(2)])
        P.dve(lambda e: e.tensor_scalar(out=v4(t2), in0=v4(t2), scalar1=MAGIC, scalar2=None, op0=ALU.subtract), r=[k(2)], w=[k(2)])
        P.dve(lambda e: e.tensor_tensor(out=v4(t2), in0=v4(t1), in1=v4(t2), op=ALU.subtract), r=[k(1), k(2)], w=[k(2)])
        P.act(lambda e: e.activation(out=v4(t2), in_=v4(t2), func=ACT.Sin, scale=TWO_PI), r=[k(2)], w=[k(2)])
        P.dve(lambda e: e.tensor_tensor(out=v4(er_t), in0=v4(t0), in1=v4(t2), op=ALU.mult), r=[k(0), k(2)], w=[tag + "_er"])

    with ExitStack() as es:
        ptmp.extend([SB(es, "ptmp%d" % i, [128, 2, 16, 72], F32) for i in range(3)])
        a_sb = SB(es, "a_sb", [128, 2, 2, 16], F32)
        ldt_sb = SB(es, "ldt_sb", [128, 2, 16], F32)
        b_sb = SB(es, "b_sb", [128, 2, 2, 16, 16], F32)
        c_sb = SB(es, "c_sb", [128, 2, 2, 16, 16], F32)
        eZ_sb = SB(es, "eZ_sb", [128, 2, 64], F32)
        eR_sb = SB(es, "eR_sb", [128, 2, 72], F32)
        e1_sb = SB(es, "e1_sb", [128, 2, 1], F32)
        e64_sb = SB(es, "e64_sb", [128, 2, 1], F32)
        abr = SB(es, "abr", [128, 2, 16, 1], F32)
        abi = SB(es, "abi", [128, 2, 16, 1], F32)
        dsc = [SB(es, "dsc%d" % i, [128, 2, 16], F32) for i in range(5)]
        bt = [SB(es, "bt%d" % i, [128, 2, 16, 16], F32) for i in range(2)]
        P.dma(lambda e: e.dma_start(out=a_sb[:], in_=s5_a), key="a_sb", w=["a_sb"])
        P.dma(lambda e: e.dma_start(out=ldt_sb[:], in_=s5_ldt), key="ldt_sb", w=["ldt_sb"])
        P.dma(lambda e: e.dma_start(out=b_sb[:], in_=s5_b), key="b_sb", w=["b_sb"])
        P.dma(lambda e: e.dma_start(out=c_sb[:], in_=s5_c), key="c_sb", w=["c_sb"])
        P.dma(lambda e: e.dma_start(out=eZ_sb[:], in_=cst_eZ), key="eZ_sb", w=["Z_e"])
        P.dma(lambda e: e.dma_start(out=eR_sb[:], in_=cst_eR), key="eR_sb", w=["R_e"])
        P.dve(lambda e: e.memset(e1_sb[:], 1.0), w=["ab_e"])
        P.dve(lambda e: e.memset(e64_sb[:], 64.0), w=["A64_e"])
        P.act(lambda e: e.activation(out=ldt_sb[:], in_=ldt_sb[:], func=ACT.Exp), r=["ldt_sb"], w=["ldt_sb"])
        P.dve(lambda e: e.tensor_tensor(out=ardt[:], in0=a_sb[:, 0], in1=ldt_sb[:], op=ALU.mult), r=["a_sb", "ldt_sb"], w=["ardt"])
        P.dve(lambda e: e.scalar_tensor_tensor(out=aidt[:], in0=a_sb[:, 1], scalar=1.0 / TWO_PI, in1=ldt_sb[:], op0=ALU.mult, op1=ALU.mult),
              r=["a_sb", "ldt_sb"], w=["aidt"])
        powtab(e1_sb[:], 1, abr, abi, "ab")
        powtab(e64_sb[:], 1, A64r, A64i, "A64")
        powtab(eZ_sb[:], 64, erZ, eiZ, "Z")
        powtab(eR_sb[:], 72, erR, eiR, "R")
        for (src_t, dst_t, kk) in ((erZ, erZ8, "Z_er"), (eiZ, eiZ8, "Z_ei")):
            P.dve(lambda e, src_t=src_t, dst_t=dst_t: e.tensor_copy(out=dst_t[:, 0], in_=src_t[:, 0, :, 56:64]), r=[kk], w=[kk + "8"])
            P.dve(lambda e, src_t=src_t, dst_t=dst_t: e.tensor_copy(out=dst_t[:, 1], in_=src_t[:, 1, :, 0:8]), r=[kk], w=[kk + "8"])
        are, aim = a_sb[:, 0], a_sb[:, 1]
        d0, d1, d2, d3, d4 = [t[:] for t in dsc]
        ab_r = abr[:].rearrange("p d q o -> p d (q o)")
        ab_i = abi[:].rearrange("p d q o -> p d (q o)")
        kd = lambda i: ("dsc", i)
        P.dve(lambda e: e.tensor_tensor(out=d0, in0=are, in1=are, op=ALU.mult), r=["a_sb"], w=[kd(0)])
        P.dve(lambda e: e.tensor_tensor(out=d1, in0=aim, in1=aim, op=ALU.mult), r=["a_sb"], w=[kd(1)])
        P.dve(lambda e: e.tensor_tensor(out=d0, in0=d0, in1=d1, op=ALU.add), r=[kd(0), kd(1)], w=[kd(0)])
        P.dve(lambda e: e.reciprocal(out=d0, in_=d0), r=[kd(0)], w=[kd(0)])
        P.dve(lambda e: e.tensor_scalar(out=d1, in0=ab_r, scalar1=-1.0, scalar2=None, op0=ALU.add), r=["ab_er"], w=[kd(1)])
        P.dve(lambda e: e.tensor_tensor(out=d2, in0=d1, in1=are, op=ALU.mult), r=[kd(1), "a_sb"], w=[kd(2)])
        P.dve(lambda e: e.tensor_tensor(out=d3, in0=ab_i, in1=aim, op=ALU.mult), r=["ab_ei", "a_sb"], w=[kd(3)])
        P.dve(lambda e: e.tensor_tensor(out=d2, in0=d2, in1=d3, op=ALU.add), r=[kd(2), kd(3)], w=[kd(2)])
        P.dve(lambda e: e.tensor_tensor(out=d2, in0=d2, in1=d0, op=ALU.mult), r=[kd(2), kd(0)], w=[kd(2)])
        P.dve(lambda e: e.tensor_tensor(out=d3, in0=ab_i, in1=are, op=ALU.mult), r=["ab_ei", "a_sb"], w=[kd(3)])
        P.dve(lambda e: e.tensor_tensor(out=d4, in0=d1, in1=aim, op=ALU.mult), r=[kd(1), "a_sb"], w=[kd(4)])
        P.dve(lambda e: e.tensor_tensor(out=d3, in0=d3, in1=d4, op=ALU.subtract), r=[kd(3), kd(4)], w=[kd(3)])
        P.dve(lambda e: e.tensor_tensor(out=d3, in0=d3, in1=d0, op=ALU.mult), r=[kd(3), kd(0)], w=[kd(3)])
        rr_b = fv(dsc[2][:], 0, [[16, 2], [1, 16], [0, 16]])
        ri_b = fv(dsc[3][:], 0, [[16, 2], [1, 16], [0, 16]])
        bre, bim = b_sb[:, 0], b_sb[:, 1]
        P.dve(lambda e: e.tensor_tensor(out=bt[0][:], in0=bre, in1=rr_b, op=ALU.mult), r=["b_sb", kd(2)], w=["bt0"])
        P.dve(lambda e: e.tensor_tensor(out=bt[1][:], in0=bim, in1=ri_b, op=ALU.mult), r=["b_sb", kd(3)], w=["bt1"])
        P.dve(lambda e: e.tensor_tensor(out=bbre[:], in0=bt[0][:], in1=bt[1][:], op=ALU.subtract), r=["bt0", "bt1"], w=["bbre"])
        P.dve(lambda e: e.tensor_tensor(out=bt[0][:], in0=bim, in1=rr_b, op=ALU.mult), r=["b_sb", kd(2)], w=["bt0"])
        P.dve(lambda e: e.tensor_tensor(out=bt[1][:], in0=bre, in1=ri_b, op=ALU.mult), r=["b_sb", kd(3)], w=["bt1"])
        P.dve(lambda e: e.tensor_tensor(out=bbim[:], in0=bt[0][:], in1=bt[1][:], op=ALU.add), r=["bt0", "bt1"], w=["bbim"])
        P.dve(lambda e: e.tensor_copy(out=ccre[:], in_=c_sb[:, 0]), r=["c_sb"], w=["ccre"])
        P.dve(lambda e: e.tensor_copy(out=ccim[:], in_=c_sb[:, 1]), r=["c_sb"], w=["ccim"])
        P.barrier()

    def ztab(eng_add, pair, tsl, nt, bufs, tag, erZ=erZ, eiZ=eiZ):
        zre, zim, ztm = bufs
        def ev(t, d):
            return fv(t[:, d, pair, tsl[d]:tsl[d] + nt], 0, [[1, nt], [0, 16]])
        def bv(t, d):
            return fv(t[:, d, pair, :], 0, [[0, nt], [1, 16]])
        for d in range(2):
            eng_add(lambda e, d=d: e.tensor_tensor(out=zre[:, d], in0=bv(bbre, d), in1=ev(erZ, d), op=ALU.mult), r=["Z_er", "bbre"], w=[tag + "zre"])
            eng_add(lambda e, d=d: e.tensor_tensor(out=ztm[:, d], in0=bv(bbim, d), in1=ev(eiZ, d), op=ALU.mult), r=["Z_ei", "bbim"], w=[tag + "ztm"])
            eng_add(lambda e, d=d: e.tensor_tensor(out=zre[:, d], in0=zre[:, d], in1=ztm[:, d], op=ALU.subtract), r=[tag + "zre", tag + "ztm"], w=[tag + "zre"])
            eng_add(lambda e, d=d: e.tensor_tensor(out=zim[:, d], in0=bv(bbim, d), in1=ev(erZ, d), op=ALU.mult), r=["Z_er", "bbim"], w=[tag + "zim"])
            eng_add(lambda e, d=d: e.tensor_tensor(out=ztm[:, d], in0=bv(bbre, d), in1=ev(eiZ, d), op=ALU.mult), r=["Z_ei", "bbre"], w=[tag + "ztm"])
            eng_add(lambda e, d=d: e.tensor_tensor(out=zim[:, d], in0=zim[:, d], in1=ztm[:, d], op=ALU.add), r=[tag + "zim", tag + "ztm"], w=[tag + "zim"])

    with ExitStack() as es:
        Wsum = [SB(es, "Wsum%d" % i, [128, 2, 8, 2, 128], BF16) for i in range(2)]
        zb = [[SB(es, "zb%d_%d" % (i, j), [128, 2, 64, 16], F32) for j in range(3)] for i in range(1)]
        pW = [PS(es, "pW%d" % i, [128, 4, 128], F32) for i in range(2)]
        pSr = PS(es, "pSr", [128, 2, NJ], F32)
        pSi = PS(es, "pSi", [128, 2, NJ], F32)
        ci = 0
        for pair in range(16):
            sl = int(os.environ.get('K_SL', pair % 2))
            ea = P.pool
            ztab(ea, pair, (0, 0), 64, zb[0], "z0")
            zre, zim, _ = zb[0]
            for d in range(2):
                for reim in range(2):
                    zt = zre if reim == 0 else zim
                    for mq in range(2):
                        pw = pW[ci % 2]
                        for j in range(4):
                            m_ = mq * 4 + j
                            P.pe(lambda e, zt=zt, d=d, m_=m_, pw=pw, j=j: e.transpose(out=pw[:, j, :], in_=zt[:, d, m_ * 8:(m_ + 1) * 8, :].rearrange("p s c -> p (s c)"), identity=ident_f[:]),
                                 r=["z0z%s" % ("re" if reim == 0 else "im"), "ident_f"], w=[("pW", ci % 2)])
                        dst = Wsum[sl][:, d, mq * 4:(mq + 1) * 4, reim, :]
                        if ci % 2 == 0:
                            P.act(lambda e, dst=dst, pw=pw: e.activation(out=dst, in_=pw[:], func=ACT.Copy), r=[("pW", ci % 2)], w=[("Wsum", sl)])
                        else:
                            P.dve(lambda e, dst=dst, pw=pw: e.tensor_copy(out=dst, in_=pw[:]), r=[("pW", ci % 2)], w=[("Wsum", sl)])
                        ci += 1
            for d in range(2):
                for gh in range(2):
                    g = 2 * pair + gh
                    for reim, pS_ in ((0, pSr), (1, pSi)):
                        for m_ in range(8):
                            P.pe(lambda e, d=d, gh=gh, g=g, reim=reim, pS_=pS_, m_=m_, sl=sl: e.matmul(
                                out=pS_[64 * gh:64 * gh + 64, d, :], lhsT=Wsum[sl][:, d, m_, reim, 64 * gh:64 * gh + 64],
                                rhs=fv(u8[:, g, :], m_, [[8, NJ]]), start=(m_ == 0), stop=(m_ == 7)),
                                r=[("Wsum", sl), "u8"], w=["pSr" if reim == 0 else "pSi"])
            P.act(lambda e, pair=pair: e.activation(out=S_re[:, :, pair, :], in_=pSr[:], func=ACT.Copy), r=["pSr"], w=["S_re"])
            P.dve(lambda e, pair=pair: e.tensor_copy(out=S_im[:, :, pair, :], in_=pSi[:]), r=["pSi"], w=["S_im"])
        P.barrier()
    esA.close()
    esZ.close()

    with ExitStack() as es:
        sct = [[SB(es, "sct%d_%d" % (d, i), [128, 16], F32) for i in range(4)] for d in range(2)]
        orders = [[(J, J - 1 if J > 0 else None) for J in range(NJ)],
                  [(3, None), (2, 3), (1, 2), (0, 1), (131, 0)] + [(J, J + 1) for J in range(130, 3, -1)]]
        for d in range(2):
            ea = P.dve if d == 0 else P.pool
            Ar = A64r[:, d, :, 0]
            Ai = A64i[:, d, :, 0]
            t = [x[:] for x in sct[d]]
            kt = lambda i: ("sct", d, i)
            kr_, ki_ = ("Hre", d), ("Him", d)
            for (J, Jp) in orders[d]:
                if Jp is None:
                    continue
                hr_p, hi_p = S_re[:, d, :, Jp], S_im[:, d, :, Jp]
                hr, hi = S_re[:, d, :, J], S_im[:, d, :, J]
                ea(lambda e, t=t, hr_p=hr_p, Ar=Ar: e.tensor_tensor(out=t[0], in0=Ar, in1=hr_p, op=ALU.mult), r=["A64_er", kr_, "S_re"], w=[kt(0)])
                ea(lambda e, t=t, hi_p=hi_p, Ai=Ai: e.tensor_tensor(out=t[1], in0=Ai, in1=hi_p, op=ALU.mult), r=["A64_ei", ki_, "S_im"], w=[kt(1)])
                ea(lambda e, t=t: e.tensor_tensor(out=t[0], in0=t[0], in1=t[1], op=ALU.subtract), r=[kt(0), kt(1)], w=[kt(0)])
                ea(lambda e, t=t, hi_p=hi_p, Ar=Ar: e.tensor_tensor(out=t[2], in0=Ar, in1=hi_p, op=ALU.mult), r=["A64_er", ki_, "S_im"], w=[kt(2)])
                ea(lambda e, t=t, hr_p=hr_p, Ai=Ai: e.tensor_tensor(out=t[3], in0=Ai, in1=hr_p, op=ALU.mult), r=["A64_ei", kr_, "S_re"], w=[kt(3)])
                ea(lambda e, t=t: e.tensor_tensor(out=t[2], in0=t[2], in1=t[3], op=ALU.add), r=[kt(2), kt(3)], w=[kt(2)])
                ea(lambda e, t=t, hr=hr: e.tensor_tensor(out=hr, in0=hr, in1=t[0], op=ALU.add), r=[kt(0), kr_], w=[kr_])
                ea(lambda e, t=t, hi=hi: e.tensor_tensor(out=hi, in0=hi, in1=t[2], op=ALU.add), r=[kt(2), ki_], w=[ki_])
        P.barrier()
    dump("H_re", S_re[:], [128, 2, 16, NJ])
    dump("H_im", S_im[:], [128, 2, 16, NJ])
    HO = [[SB(esS, "HO%d_%d" % (d, x), [128, 16, 32], BF16) for x in range(2)] for d in range(2)]
    with ExitStack() as es:
        cm = SB(es, "cm", [128, 4], F32)
        hacc = SB(es, "hacc", [128, 16, 32], F32)
        P.dma(lambda e: e.dma_start(out=cm[:], in_=cmask), key="cm", w=["cm"])
        for d in range(2):
            for x, Sx in enumerate((S_re, S_im)):
                for r_ in range(4):
                    lo = (3 + 32 * r_) if d == 0 else (5 + 32 * r_)
                    segs = [(lo, 0, 32)] if not (d == 1 and r_ == 3) else [(lo, 0, 31), (0, 31, 1)]
                    for (a0, o0, n_) in segs:
                        src_ = Sx[:, d, :, a0:a0 + n_]
                        dst_ = hacc[:, :, o0:o0 + n_]
                        if r_ == 0:
                            P.dve(lambda e, src_=src_, dst_=dst_: e.tensor_scalar(out=dst_, in0=src_, scalar1=cm[:, 0:1], scalar2=None, op0=ALU.mult),
                                  r=["cm"], w=["hacc"])
                        else:
                            P.dve(lambda e, src_=src_, dst_=dst_, r_=r_: e.scalar_tensor_tensor(out=dst_, in0=src_, scalar=cm[:, r_:r_ + 1], in1=dst_, op0=ALU.mult, op1=ALU.add),
                                  r=["cm", "hacc"], w=["hacc"])
                P.dve(lambda e, d=d, x=x: e.tensor_copy(out=HO[d][x][:], in_=hacc[:]), r=["hacc"], w=[("HO", d, x)])
        P.barrier()
    esH.close()
    if STOP <= 2:
        esS.close()
        return finish(nc, P, ges, out, dbg_out)

    esP = ExitStack()
    hTo = SB(esP, "hTo", [128, 8, NOWN], BF16)
    qT = SB(esP, "qT", [128, 8, NOWN], BF16)
    esU = ExitStack()
    u8o = SB(esU, "u8o", [128, 32, 256], BF16)

    def load_bc(dst, src_row, key, add_one=False):
        P.dma(lambda e: e.dma_start(out=dst[:], in_=src_row.partition_broadcast(128)), key=key, w=[key])
        if add_one:
            P.dve(lambda e: e.tensor_scalar(out=dst[:], in0=dst[:], scalar1=1.0, scalar2=None, op0=ALU.add), r=[key], w=[key])

    with ExitStack() as es:
        Wq = SB(es, "Wq", [128, 8, 1024], BF16)
        Wuo = SB(es, "Wuo", [128, 8, 512], BF16)
        sc1p_bc = SB(es, "sc1p_bc", [128, D], F32)
        sh1_bc = SB(es, "sh1_bc", [128, D], F32)
        xt_B = [SB(es, "xtB%d" % i, [128, D], F32) for i in range(2)]
        rt_B = [SB(es, "rtB%d" % i, [128, 128], F32) for i in range(2)]
        hn = SB(es, "hn", [128, D], F32)
        hb_B = [SB(es, "hbB%d" % i, [128, D], BF16) for i in range(2)]
        st_B = SB(es, "stB", [128, 2, 6], F32)
        mv_B = SB(es, "mvB", [128, 2], F32)
        rstd_B = SB(es, "rstdB", [128, 1], F32)
        nbt_B = SB(es, "nbtB", [128, 1], F32)
        q_sb = SB(es, "q_sb", [128, 1024], F32)
        qr = SB(es, "qr", [128, 1024], BF16)
        tmp_B = (SB(es, "sqB", [128, 1024], F32), SB(es, "ssB", [128, 8], F32), SB(es, "rkB", [128, 8], F32),
               SB(es, "taB", [128, 512], F32), SB(es, "tbB", [128, 512], F32))
        u_bf_B = SB(es, "u_bfB", [128, 512], BF16)
        Um_B = [SB(es, "UmB0", [128, 32, 128], BF16)] * 2
        pT_B = [PS(es, "pTB%d" % i, [128, 8, 128], BF16) for i in range(2)]
        pQ = [PS(es, "pQ%d" % i, [128, 512], F32) for i in range(2)]
        pQT = PS(es, "pQT", [128, 8, 128], BF16)
        pU_B = PS(es, "pUB", [128, 512], F32)
        pU8_B = PS(es, "pU8B", [128, 32, 16], F32)
        P.dma(lambda e: e.dma_start(out=Wq[:], in_=w_in[:, 512:1536].rearrange("(kc p) n -> p kc n", p=128)), key="Wq", w=["Wq"], eng="pool")
        P.dma(lambda e: e.dma_start(out=Wuo[:], in_=w_in[:, 0:512].rearrange("(kc p) n -> p kc n", p=128)), key="Wuo", w=["Wuo"], eng="pool")
        load_bc(sc1p_bc, mod_d[0:1, 1024:2048], "sc1p_bc", True)
        load_bc(sh1_bc, mod_d[0:1, 0:1024], "sh1_bc")
        for tt in range(NTO):
            s2 = tt % 2
            t0 = tt * 128
            P.dma(lambda e, s2=s2, t0=t0: e.dma_start(out=xt_B[s2][:], in_=xo[t0:t0 + 128, :]), key=("xtB", s2), w=[("xtB", s2)])
            P.dma(lambda e, s2=s2, t0=t0: e.dma_start(out=rt_B[s2][:], in_=rope_o[t0:t0 + 128, :]), key=("rtB", s2), w=[("rtB", s2)])
            ln_tile(xt_B[s2][:], ("xtB", s2), st_B, mv_B, rstd_B, nbt_B, hn[:], "hn", "B")
            P.dve(lambda e: e.tensor_tensor(out=hn[:], in0=hn[:], in1=sc1p_bc[:], op=ALU.mult), r=["hn", "sc1p_bc"], w=["hn"])
            P.dve(lambda e, s2=s2: e.tensor_tensor(out=hb_B[s2][:], in0=hn[:], in1=sh1_bc[:], op=ALU.add), r=["hn", "sh1_bc"], w=[("hbB", s2)])
            for kc in range(8):
                P.pe(lambda e, kc=kc, s2=s2: e.transpose(out=pT_B[s2][:, kc, :], in_=hb_B[s2][:, kc * 128:(kc + 1) * 128], identity=ident_b[:]),
                     r=[("hbB", s2), "ident_b"], w=[("pTB", s2)])
            P.act(lambda e, s2=s2, t0=t0: e.activation(out=hTo[:, :, t0:t0 + 128], in_=pT_B[s2][:], func=ACT.Copy), r=[("pTB", s2)], w=["hTo"])
            for half in range(2):
                for kc in range(8):
                    P.pe(lambda e, kc=kc, half=half, t0=t0: e.matmul(out=pQ[half][:], lhsT=hTo[:, kc, t0:t0 + 128], rhs=Wq[:, kc, half * 512:(half + 1) * 512],
                                                                     start=(kc == 0), stop=(kc == 7)), r=["hTo", "Wq"], w=[("pQ", half)])
                P.act(lambda e, half=half: e.activation(out=q_sb[:, half * 512:(half + 1) * 512], in_=pQ[half][:], func=ACT.Copy), r=[("pQ", half)], w=["q_sb"])
            for kc in range(8):
                P.pe(lambda e, kc=kc, t0=t0: e.matmul(out=pU_B[:], lhsT=hTo[:, kc, t0:t0 + 128], rhs=Wuo[:, kc, :], start=(kc == 0), stop=(kc == 7)),
                     r=["hTo", "Wuo"], w=["pUB"])
            rms_rope(P.pool, q_sb, 8, gq_bc, rt_B[s2], qr, tmp_B, ["q_sb", ("rtB", s2), "gq_bc"], "qr", "B")
            for h in range(8):
                P.pe(lambda e, h=h: e.transpose(out=pQT[:, h, :], in_=qr[:, h * 128:(h + 1) * 128], identity=ident_b[:]), r=["qr", "ident_b"], w=["pQT"])
            P.act(lambda e, t0=t0: e.activation(out=qT[:, :, t0:t0 + 128], in_=pQT[:], func=ACT.Copy), r=["pQT"], w=["qT"])
            P.act(lambda e: e.activation(out=u_bf_B[:], in_=pU_B[:], func=ACT.Copy), r=["pUB"], w=["u_bfB"])
            P.dve(lambda e, s2=s2: e.tensor_tensor(out=Um_B[s2][:].rearrange("p g (s c) -> p g s c", c=16), in0=fv(u_bf_B[:], 0, [[16, 32], [0, 8], [1, 16]]),
                                                   in1=fv(mask8[:], 0, [[0, 32], [1, 8], [0, 16]]), op=ALU.mult), r=["u_bfB", "mask8"], w=["UmB"])
            for g in range(32):
                P.pe(lambda e, g=g, s2=s2: e.matmul(out=pU8_B[:, g, :], lhsT=Um_B[s2][:, g, :], rhs=sel16[:], start=True, stop=True),
                     r=["UmB", "sel16"], w=["pU8B"])
            P.act(lambda e, tt=tt: e.activation(out=u8o[:, :, tt * 16:(tt + 1) * 16], in_=pU8_B[:], func=ACT.Copy), r=["pU8B"], w=["u8o"])
        P.barrier()
    if "qT" in dbg_names:
        dump("qT", qT[:], [128, 8, NOWN], BF16)
    if STOP <= 3:
        esU.close(); esS.close(); esP.close()
        return finish(nc, P, ges, out, dbg_out)

    gT = SB(esP, "gT", [128, 4, NOWN], BF16)
    with ExitStack() as es:
        mf_sb = SB(es, "mf_sb", [128, 128], F32)
        mb_sb = SB(es, "mb_sb", [128, 128], F32)
        dcol_sb = SB(es, "dcol_sb", [128, 32], F32)
        selg_b = SB(es, "selg_b", [128, 8, 128], BF16)
        m8c_b = SB(es, "m8c_b", [128, 8], BF16)
        z8 = [SB(es, "z8_%d" % j, [128, 2, 8, 16], F32) for j in range(3)]
        Rre_p = SB(es, "Rre_p", [128, 2, 72, 16], F32)
        Rim_p = SB(es, "Rim_p", [128, 2, 72, 16], F32)
        Rt0 = SB(es, "Rt0", [128, 1, 72, 16], F32)
        Rt1 = SB(es, "Rt1", [128, 1, 72, 16], F32)
        Rre_b = SB(es, "Rre_b", [128, 2, 1152], BF16)
        Rim_b = SB(es, "Rim_b", [128, 2, 1152], BF16)
        Tw = [SB(es, "Tw0", [128, 2, 15, 128], BF16)] * 2
        tt0 = SB(es, "tt0", [128, 128], F32)
        tt1 = SB(es, "tt1", [128, 128], F32)
        Ye = [SB(es, "Ye0", [128, 2048], BF16)] * 2
        ysb = SB(es, "ysb", [128, 1024], F32)
        yx2 = SB(es, "yx2", [128, 1024], F32)
        pTb = [PS(es, "pTb%d" % i, [128, 128], F32) for i in range(2)]
        pY8 = [PS(es, "pY8_%d" % i, [128, 8, 32], F32) for i in range(2)]
        pYT = PS(es, "pYT", [128, 2048], F32)
        P.dma(lambda e: e.dma_start(out=mf_sb[:], in_=cst_mf), key="mf_sb", w=["mf_sb"])
        P.dma(lambda e: e.dma_start(out=mb_sb[:], in_=cst_mb), key="mb_sb", w=["mb_sb"])
        P.dma(lambda e: e.dma_start(out=dcol_sb[:], in_=s5_dcol), key="dcol_sb", w=["dcol_sb"])
        P.dma(lambda e: e.dma_start(out=selg_b[:], in_=cst_selg), key="selg_b", w=["selg_b"], eng="pool")
        P.dma(lambda e: e.dma_start(out=m8c_b[:], in_=cst_mask8c), key="m8c_b", w=["m8c_b"], eng="pool")
        ecnt = 0
        for pair in range(16):
            sl = 0
            ztab(P.dve, pair, (0, 0), 8, z8, "z8", erZ=erZ8, eiZ=eiZ8)
            for d in range(2):
                eb = lambda t, d=d, pair=pair: fv(t[:, d, pair, :], 0, [[1, 72], [0, 16]])
                cb = lambda t, d=d, pair=pair: fv(t[:, d, pair, :], 0, [[0, 72], [1, 16]])
                P.pool(lambda e, d=d, eb=eb, cb=cb: e.tensor_tensor(out=Rre_p[:, d], in0=cb(ccre), in1=eb(erR), op=ALU.mult), r=["R_er", "ccre"], w=["Rre_p"])
                P.pool(lambda e, d=d, eb=eb, cb=cb: e.tensor_tensor(out=Rt0[:, 0], in0=cb(ccim), in1=eb(eiR), op=ALU.mult), r=["R_ei", "ccim"], w=["Rt0"])
                P.pool(lambda e, d=d: e.tensor_tensor(out=Rre_p[:, d], in0=Rre_p[:, d], in1=Rt0[:, 0], op=ALU.subtract), r=["Rre_p", "Rt0"], w=["Rre_p"])
                P.dve(lambda e, d=d, eb=eb, cb=cb: e.tensor_tensor(out=Rim_p[:, d], in0=cb(ccim), in1=eb(erR), op=ALU.mult), r=["R_er", "ccim"], w=["Rim_p"])
                P.dve(lambda e, d=d, eb=eb, cb=cb: e.tensor_tensor(out=Rt1[:, 0], in0=cb(ccre), in1=eb(eiR), op=ALU.mult), r=["R_ei", "ccre"], w=["Rt1"])
                P.dve(lambda e, d=d: e.scalar_tensor_tensor(out=Rim_p[:, d], in0=Rt1[:, 0], scalar=-1.0, in1=Rim_p[:, d], op0=ALU.mult, op1=ALU.subtract),
                      r=["Rim_p", "Rt1"], w=["Rim_p"])
            P.act(lambda e: e.activation(out=Rre_b[:], in_=Rre_p[:].rearrange("p d i c -> p d (i c)"), func=ACT.Copy), r=["Rre_p"], w=["Rre_b"])
            P.act(lambda e: e.activation(out=Rim_b[:], in_=Rim_p[:].rearrange("p d i c -> p d (i c)"), func=ACT.Copy), r=["Rim_p"], w=["Rim_b"])
            for gh in range(2):
                g = 2 * pair + gh
                rows = slice(64 * gh, 64 * gh + 64)
                blocks = [(0, 0), (1, 0)] + [(0, k) for k in range(1, 8)] + [(1, k) for k in range(1, 8)]
                for (d, k) in blocks:
                    pt = pTb[ecnt % 2]
                    kk = ("pTb", ecnt % 2)
                    P.pe(lambda e, pt=pt, d=d, k=k, rows=rows: e.matmul(out=pt[:], lhsT=z8[0][rows, d].rearrange("p s c -> p (s c)"),
                                                                        rhs=Rre_p[rows, d, 8 * k:8 * k + 8, :].rearrange("p s c -> p (s c)"), start=True, stop=False),
                         r=["z8zre", "Rre_p"], w=[kk])
                    P.pe(lambda e, pt=pt, d=d, k=k, rows=rows: e.matmul(out=pt[:], lhsT=z8[1][rows, d].rearrange("p s c -> p (s c)"),
                                                                        rhs=Rim_p[rows, d, 8 * k:8 * k + 8, :].rearrange("p s c -> p (s c)"), start=False, stop=True),
                         r=["z8zim", "Rim_p"], w=[kk])
                    if k == 0 and d == 0:
                        P.dve(lambda e, pt=pt: e.tensor_tensor(out=tt0[:], in0=pt[:], in1=mf_sb[:], op=ALU.mult), r=[kk, "mf_sb"], w=["tt0"])
                    elif k == 0 and d == 1:
                        P.dve(lambda e, pt=pt: e.tensor_tensor(out=tt1[:], in0=pt[:], in1=mb_sb[:], op=ALU.mult), r=[kk, "mb_sb"], w=["tt1"])
                        P.dve(lambda e: e.tensor_tensor(out=tt0[:], in0=tt0[:], in1=tt1[:], op=ALU.add), r=["tt0", "tt1"], w=["tt0"])
                        P.dve(lambda e, g=g, gh=gh, sl=sl: e.scalar_tensor_tensor(out=Tw[sl][:, gh, 7, :], in0=ident_f[:], scalar=dcol_sb[:, g:g + 1], in1=tt0[:],
                                                                                 op0=ALU.mult, op1=ALU.add), r=["tt0", "dcol_sb", "ident_f"], w=[("Tw", sl)])
                    else:
                        idx = 7 + k if d == 0 else 7 - k
                        if ecnt % 2 == 0:
                            P.act(lambda e, pt=pt, gh=gh, sl=sl, idx=idx: e.activation(out=Tw[sl][:, gh, idx, :], in_=pt[:], func=ACT.Copy), r=[kk], w=[("Tw", sl)])
                        else:
                            P.dve(lambda e, pt=pt, gh=gh, sl=sl, idx=idx: e.tensor_copy(out=Tw[sl][:, gh, idx, :], in_=pt[:]), r=[kk], w=[("Tw", sl)])
                    ecnt += 1
            for gh in range(2):
                g = 2 * pair + gh
                rows = slice(64 * gh, 64 * gh + 64)
                py = pY8[g % 2]
                ky = ("pY8", g % 2)
                for m in range(8):
                    for m_ in range(8):
                        P.pe(lambda e, py=py, m=m, m_=m_, gh=gh, g=g, sl=sl: e.matmul(out=py[:, m, :], lhsT=Tw[sl][:, gh, 7 + m - m_, :],
                                                                                     rhs=fv(u8o[:, g, :], m_, [[8, 32]]), start=(m_ == 0), stop=False),
                             r=[("Tw", sl), "u8o"], w=[ky])
                    fo = 8 * (m + 1) * 16
                    bo = 8 * (8 - m) * 16
                    P.pe(lambda e, py=py, m=m, rows=rows, fo=fo, pair=pair: e.matmul(out=py[:, m, :], lhsT=Rre_b[rows, 0, fo:fo + 128], rhs=HO[0][0][rows, pair, :], start=False, stop=False),
                         r=["Rre_b", ("HO", 0, 0)], w=[ky])
                    P.pe(lambda e, py=py, m=m, rows=rows, fo=fo, pair=pair: e.matmul(out=py[:, m, :], lhsT=Rim_b[rows, 0, fo:fo + 128], rhs=HO[0][1][rows, pair, :], start=False, stop=False),
                         r=["Rim_b", ("HO", 0, 1)], w=[ky])
                    P.pe(lambda e, py=py, m=m, rows=rows, bo=bo, pair=pair: e.matmul(out=py[:, m, :], lhsT=Rre_b[rows, 1, bo:bo + 128], rhs=HO[1][0][rows, pair, :], start=False, stop=False),
                         r=["Rre_b", ("HO", 1, 0)], w=[ky])
                    P.pe(lambda e, py=py, m=m, rows=rows, bo=bo, pair=pair: e.matmul(out=py[:, m, :], lhsT=Rim_b[rows, 1, bo:bo + 128], rhs=HO[1][1][rows, pair, :], start=False, stop=True),
                         r=["Rim_b", ("HO", 1, 1)], w=[ky])
                ye = Ye[g % 2]
                P.dve(lambda e, py=py, ye=ye: e.tensor_tensor(out=ye[:].rearrange("p (j m s) -> p j m s", m=8, s=8), in0=fv(py[:], 0, [[1, 32], [32, 8], [0, 8]]),
                                                              in1=fv(m8c_b[:], 0, [[0, 32], [0, 8], [1, 8]]), op=ALU.mult), r=[ky, "m8c_b"], w=["Ye"])
                for c4 in range(4):
                    P.pe(lambda e, ye=ye, g=g, c4=c4: e.matmul(out=pYT[:, c4 * 512:(c4 + 1) * 512], lhsT=selg_b[:, g % 8, :], rhs=ye[:, c4 * 512:(c4 + 1) * 512],
                                                              start=(g % 8 == 0), stop=(g % 8 == 7)), r=["Ye", "selg_b"], w=["pYT"])
            if pair % 4 == 3:
                tile_ = pair // 4
                for hf in range(2):
                    cs = slice(hf * 1024, (hf + 1) * 1024)
                    P.act(lambda e, cs=cs: e.activation(out=ysb[:], in_=pYT[:, cs], func=ACT.Copy), r=["pYT"], w=["ysb"])
                    P.dve(lambda e: e.tensor_tensor(out=yx2[:], in0=ysb[:], in1=ysb[:], op=ALU.mult), r=["ysb"], w=["yx2"])
                    P.dve(lambda e: e.tensor_scalar(out=yx2[:], in0=yx2[:], scalar1=0.044715, scalar2=1.0, op0=ALU.mult, op1=ALU.add), r=["yx2"], w=["yx2"])
                    P.dve(lambda e: e.tensor_tensor(out=yx2[:], in0=yx2[:], in1=ysb[:], op=ALU.mult), r=["yx2", "ysb"], w=["yx2"])
                    P.act(lambda e: e.activation(out=yx2[:], in_=yx2[:], func=ACT.Sigmoid, scale=1.5957691216057308), r=["yx2"], w=["yx2"])
                    P.dve(lambda e, tile_=tile_, cs=cs: e.tensor_tensor(out=gT[:, tile_, cs], in0=ysb[:], in1=yx2[:], op=ALU.mult), r=["ysb", "yx2"], w=["gT"])
        P.barrier()
    esU.close()
    esS.close()
    if "gT" in dbg_names:
        dump("gT", gT[:], [128, 4, NOWN], BF16)
    if STOP <= 4:
        esP.close()
        return finish(nc, P, ges, out, dbg_out)

    oT = qT
    NKC = NFULL // 128
    SCALE = 128.0 ** -0.5
    with ExitStack() as es:
        kT_all = SB(es, "kT_all", [128, 2, NFULL], BF16)
        V_aug = SB(es, "V_aug", [128, NKC, 2, 132], BF16)
        pTt = [SB(es, "pTt%d" % i, [128, 512], BF16) for i in range(3)]
        rden = SB(es, "rden", [128, 4], F32)
        on_b = SB(es, "on_b", [128, 4, 128], BF16)
        pS = [PS(es, "pS%d" % i, [128, 512], F32) for i in range(2)]
        pO = [PS(es, "pO%d" % i, [128, 512], F32) for i in range(4)]
        pOT = PS(es, "pOT", [128, 4, 128], BF16)
        P.dve(lambda e: e.memset(V_aug[:], 1.0), w=["V_aug"])
        for h2 in range(2):
            P.dma(lambda e, h2=h2: e.dma_start(out=kT_all[:, h2, :], in_=kT_d[h2]), key=("kT_all", h2), r=["kT_d"], w=["kT_all"])
            P.dma(lambda e, h2=h2: e.dma_start(out=V_aug[:, :, h2, 0:128], in_=v_d[:, h2 * 128:(h2 + 1) * 128].rearrange("(c p) d -> p c d", p=128)),
                  key=("V_aug", h2), r=["v_d", "V_aug"], w=["V_aug"])
        it = 0
        for h in range(8):
            kvh = h // 4
            for qc in range(4):
                qs = slice(qc * 512, (qc + 1) * 512)
                for kc in range(NKC):
                    s2, s3 = it % 2, it % 3
                    P.pe(lambda e, s2=s2, kvh=kvh, kc=kc, h=h, qs=qs: e.matmul(out=pS[s2][:], lhsT=kT_all[:, kvh, kc * 128:(kc + 1) * 128], rhs=qT[:, h, qs], start=True, stop=True),
                         r=["kT_all", ("qTc", h, qc)], w=[("pS", s2)])
                    P.act(lambda e, s2=s2, s3=s3: e.activation(out=pTt[s3][:], in_=pS[s2][:], func=ACT.Exp, bias=negC[:], scale=SCALE),
                          r=[("pS", s2), "negC"], w=[("pTt", s3)])
                    for qi in range(4):
                        P.pe(lambda e, s3=s3, qi=qi, kc=kc, kvh=kvh: e.matmul(out=pO[qi][:, 0:129], lhsT=pTt[s3][:, qi * 128:(qi + 1) * 128], rhs=V_aug[:, kc, kvh, 0:129],
                                                                             start=(kc == 0), stop=(kc == NKC - 1)), r=[("pTt", s3), "V_aug"], w=[("pO", qi)])
                    it += 1
                for qi in range(4):
                    P.dve(lambda e, qi=qi: e.reciprocal(out=rden[:, qi:qi + 1], in_=pO[qi][:, 128:129]), r=[("pO", qi)], w=[("rden", qi)])
                    P.dve(lambda e, qi=qi: e.tensor_scalar(out=on_b[:, qi, :], in0=pO[qi][:, 0:128], scalar1=rden[:, qi:qi + 1], scalar2=None, op0=ALU.mult),
                          r=[("pO", qi), ("rden", qi)], w=[("on_b", qi)])
                    P.pe(lambda e, qi=qi: e.transpose(out=pOT[:, qi, :], in_=on_b[:, qi, :], identity=ident_b[:]), r=[("on_b", qi), "ident_b"], w=["pOT"])
                P.dve(lambda e, h=h, qs=qs: e.tensor_copy(out=oT[:, h, qs], in_=pOT[:].rearrange("p a b -> p (a b)")), r=["pOT"], w=[("qTc", h, qc)])
        P.barrier()
    if "oT" in dbg_names:
        dump("oT", oT[:], [128, 8, NOWN], BF16)
    if STOP <= 5:
        esP.close()
        return finish(nc, P, ges, out, dbg_out)

    esM = ExitStack()
    mT_all = SB(esM, "mT_all", [128, 8, NOWN], BF16)
    with ExitStack() as es:
        Wg = SB(es, "Wg", [128, 8, 2048], BF16)
        Wa = SB(es, "Wa", [128, 4, 1024], BF16)
        Wb = SB(es, "Wb", [128, 4, 1024], BF16)
        Wo = SB(es, "Wo", [128, 8, 1024], BF16)
        sg1 = SB(es, "sg1", [128, 512], F32)
        sg2 = SB(es, "sg2", [128, 512], F32)
        sgb = SB(es, "sgb", [128, 512], F32)
        mt1 = SB(es, "mt1", [128, 512], F32)
        mt2 = SB(es, "mt2", [128, 512], F32)
        pG1 = PS(es, "pG1", [128, 512], F32)
        pG2 = PS(es, "pG2", [128, 512], F32)
        pA_D = PS(es, "pA_D", [128, 512], F32)
        pB_D = PS(es, "pB_D", [128, 512], F32)
        pC_D = PS(es, "pC_D", [128, 512], F32)
        for c2 in range(2):
            P.dma(lambda e, c2=c2: e.dma_start(out=Wg[:, :, c2 * 1024:(c2 + 1) * 1024], in_=w_in[:, 2048 + c2 * 1024:2048 + (c2 + 1) * 1024].rearrange("(kc p) n -> p kc n", p=128)),
                  key=("Wg", c2), w=["Wg"], eng="pool")
        P.dma(lambda e: e.dma_start(out=Wa[:], in_=w_glu_a.rearrange("(kc p) n -> p kc n", p=128)), key="Wa", w=["Wa"], eng="pool")
        P.dma(lambda e: e.dma_start(out=Wb[:], in_=w_glu_b.rearrange("(kc p) n -> p kc n", p=128)), key="Wb", w=["Wb"], eng="pool")
        P.dma(lambda e: e.dma_start(out=Wo[:], in_=w_attn_o.rearrange("(kc p) n -> p kc n", p=128)), key="Wo", w=["Wo"], eng="pool")
        for st_ in range(4):
            ts = slice(st_ * 512, (st_ + 1) * 512)
            for dt_ in range(8):
                ds = slice(dt_ * 128, (dt_ + 1) * 128)
                ds2 = slice(1024 + dt_ * 128, 1024 + (dt_ + 1) * 128)
                for kc in range(8):
                    P.pe(lambda e, kc=kc, ds=ds, ts=ts: e.matmul(out=pG1[:], lhsT=Wg[:, kc, ds], rhs=hTo[:, kc, ts], start=(kc == 0), stop=(kc == 7)), r=["Wg", "hTo"], w=["pG1"])
                for kc in range(8):
                    P.pe(lambda e, kc=kc, ds2=ds2, ts=ts: e.matmul(out=pG2[:], lhsT=Wg[:, kc, ds2], rhs=hTo[:, kc, ts], start=(kc == 0), stop=(kc == 7)), r=["Wg", "hTo"], w=["pG2"])
                for c in range(4):
                    P.pe(lambda e, c=c, ds=ds, ts=ts: e.matmul(out=pA_D[:], lhsT=Wa[:, c, ds], rhs=gT[:, c, ts], start=(c == 0), stop=(c == 3)), r=["Wa", "gT"], w=["pA_D"])
                for c in range(4):
                    P.pe(lambda e, c=c, ds=ds, ts=ts: e.matmul(out=pB_D[:], lhsT=Wb[:, c, ds], rhs=gT[:, c, ts], start=(c == 0), stop=(c == 3)), r=["Wb", "gT"], w=["pB_D"])
                for hh in range(8):
                    P.pe(lambda e, hh=hh, ds=ds, ts=ts: e.matmul(out=pC_D[:], lhsT=Wo[:, hh, ds], rhs=oT[:, hh, ts], start=(hh == 0), stop=(hh == 7)), r=["Wo", "oT"], w=["pC_D"])
                P.act(lambda e: e.activation(out=sg1[:], in_=pG1[:], func=ACT.Sigmoid), r=["pG1"], w=["sg1"])
                P.act(lambda e: e.activation(out=sg2[:], in_=pG2[:], func=ACT.Sigmoid), r=["pG2"], w=["sg2"])
                P.act(lambda e: e.activation(out=sgb[:], in_=pB_D[:], func=ACT.Sigmoid), r=["pB_D"], w=["sgb"])
                P.dve(lambda e: e.tensor_tensor(out=mt1[:], in0=pA_D[:], in1=sgb[:], op=ALU.mult), r=["pA_D", "sgb"], w=["mt1"])
                P.dve(lambda e: e.tensor_tensor(out=mt1[:], in0=mt1[:], in1=sg1[:], op=ALU.mult), r=["mt1", "sg1"], w=["mt1"])
                P.dve(lambda e: e.tensor_tensor(out=mt2[:], in0=pC_D[:], in1=sg2[:], op=ALU.mult), r=["pC_D", "sg2"], w=["mt2"])
                P.dve(lambda e, dt_=dt_, ts=ts: e.tensor_tensor(out=mT_all[:, dt_, ts], in0=mt1[:], in1=mt2[:], op=ALU.add), r=["mt1", "mt2"], w=["mT_all"])
        P.barrier()
    esP.close()
    if "mT" in dbg_names:
        dump("mT", mT_all[:], [128, 8, NOWN], BF16)
    if STOP <= 6:
        esM.close()
        return finish(nc, P, ges, out, dbg_out)

    esE = ExitStack()
    h2T = SB(esE, "h2T", [128, 8, NOWN], BF16)
    combT = SB(esE, "combT", [32, NOWN], BF16)
    with ExitStack() as es:
        Wout = SB(es, "Wout", [128, 8, 1024], BF16)
        wrt_sb = SB(es, "wrt_sb", [128, 8, 36], F32)
        brt_bc = SB(es, "brt_bc", [128, 36], F32)
        g1_bc = SB(es, "g1_bc", [128, D], F32)
        l1g_bc = SB(es, "l1g_bc", [128, D], F32)
        l1b_bc = SB(es, "l1b_bc", [128, D], F32)
        sc2p_bc = SB(es, "sc2p_bc", [128, D], F32)
        sh2_bc = SB(es, "sh2_bc", [128, D], F32)
        xt_D = [SB(es, "xt_D%d" % i, [128, D], F32) for i in range(2)]
        zt_D = SB(es, "zt_D", [128, D], F32)
        zn_D = SB(es, "zn_D", [128, D], F32)
        x1_D = [SB(es, "x1_D%d" % i, [128, D], F32) for i in range(2)]
        h2_D = SB(es, "h2_D", [128, D], F32)
        h2b_D = SB(es, "h2b_D", [128, D], BF16)
        h2Tf = SB(es, "h2Tf", [128, 8, 128], F32)
        st_D = SB(es, "st_D", [128, 2, 6], F32)
        mv_D = SB(es, "mv_D", [128, 2], F32)
        rstd_D = SB(es, "rstd_D", [128, 1], F32)
        nb_D = SB(es, "nb_D", [128, 1], F32)
        L_D = SB(es, "L_D", [128, 36], F32)
        rs = SB(es, "rs", [128, 16], F32)
        ohg = SB(es, "ohg", [128, 4], F32)
        gex = SB(es, "gex", [128, 4], F32)
        msk = SB(es, "msk", [128, 32], F32)
        ein = SB(es, "ein", [128, 8], F32)
        e2_ = SB(es, "e2_", [128, 8], F32)
        oh1 = SB(es, "oh1", [128, 8], F32)
        oh2 = SB(es, "oh2", [128, 8], F32)
        cg = SB(es, "cg", [128, 8], F32)
        comb = SB(es, "comb", [128, 32], F32)
        pMix = [PS(es, "pMix%d" % i, [128, 512], F32) for i in range(2)]
        pT_D = PS(es, "pT_D", [128, 8, 128], BF16)
        pTf = PS(es, "pTf", [128, 4, 128], F32)
        pR = PS(es, "pR", [128, 36], F32)
        pCT = PS(es, "pCT", [32, 128], F32)
        P.dma(lambda e: e.dma_start(out=Wout[:], in_=w_out.rearrange("(kc p) n -> p kc n", p=128)), key="Wout", w=["Wout"], eng="pool")
        P.dma(lambda e: e.dma_start(out=wrt_sb[:], in_=w_rt.rearrange("(kc p) n -> p kc n", p=128)), key="wrt_sb", w=["wrt_sb"])
        load_bc(brt_bc, b_rt, "brt_bc")
        load_bc(g1_bc, mod_d[0:1, 2048:3072], "g1_bc")
        load_bc(l1g_bc, ln1_g, "l1g_bc")
        load_bc(l1b_bc, ln1_b, "l1b_bc")
        load_bc(sc2p_bc, mod_d[0:1, 4096:5120], "sc2p_bc", True)
        load_bc(sh2_bc, mod_d[0:1, 3072:4096], "sh2_bc")
        for tt in range(NTO):
            s2 = tt % 2
            t0 = tt * 128
            tsl_ = slice(t0, t0 + 128)
            P.dma(lambda e, s2=s2, t0=t0: e.dma_start(out=xt_D[s2][:], in_=xo[t0:t0 + 128, :]), key=("xt_D", s2), w=[("xt_D", s2)])
            for half in range(2):
                hs = slice(half * 512, (half + 1) * 512)
                for kc in range(8):
                    P.pe(lambda e, kc=kc, half=half, hs=hs, tsl_=tsl_: e.matmul(out=pMix[half][:], lhsT=mT_all[:, kc, tsl_], rhs=Wout[:, kc, hs], start=(kc == 0), stop=(kc == 7)),
                         r=["mT_all", "Wout"], w=[("pMix", half)])
                P.dve(lambda e, half=half, hs=hs: e.tensor_tensor(out=zt_D[:, hs], in0=pMix[half][:], in1=g1_bc[:, hs], op=ALU.mult), r=[("pMix", half), "g1_bc"], w=["zt_D"])
            P.dve(lambda e, s2=s2: e.scalar_tensor_tensor(out=zt_D[:], in0=xt_D[s2][:], scalar=ALPHA, in1=zt_D[:], op0=ALU.mult, op1=ALU.add), r=["zt_D", ("xt_D", s2)], w=["zt_D"])
            ln_tile(zt_D[:], "zt_D", st_D, mv_D, rstd_D, nb_D, zn_D[:], "zn_D", "D1")
            P.dve(lambda e: e.tensor_tensor(out=zn_D[:], in0=zn_D[:], in1=l1g_bc[:], op=ALU.mult), r=["zn_D", "l1g_bc"], w=["zn_D"])
            P.dve(lambda e, s2=s2: e.tensor_tensor(out=x1_D[s2][:], in0=zn_D[:], in1=l1b_bc[:], op=ALU.add), r=["zn_D", "l1b_bc"], w=[("x1_D", s2)])
            P.dma(lambda e, s2=s2, t0=t0: e.dma_start(out=x1_d[t0:t0 + 128, :], in_=x1_D[s2][:]), key=("x1d", s2), r=[("x1_D", s2)], w=["x1_d"])
            ln_tile(x1_D[s2][:], ("x1_D", s2), st_D, mv_D, rstd_D, nb_D, h2_D[:], "h2_D", "D2")
            P.dve(lambda e: e.tensor_tensor(out=h2_D[:], in0=h2_D[:], in1=sc2p_bc[:], op=ALU.mult), r=["h2_D", "sc2p_bc"], w=["h2_D"])
            P.dve(lambda e: e.tensor_tensor(out=h2_D[:], in0=h2_D[:], in1=sh2_bc[:], op=ALU.add), r=["h2_D", "sh2_bc"], w=["h2_D"])
            P.act(lambda e: e.activation(out=h2b_D[:], in_=h2_D[:], func=ACT.Copy), r=["h2_D"], w=["h2b_D"])
            for kc in range(8):
                P.pe(lambda e, kc=kc: e.transpose(out=pT_D[:, kc, :], in_=h2b_D[:, kc * 128:(kc + 1) * 128], identity=ident_b[:]), r=["h2b_D", "ident_b"], w=["pT_D"])
            P.act(lambda e, tsl_=tsl_: e.activation(out=h2T[:, :, tsl_], in_=pT_D[:], func=ACT.Copy), r=["pT_D"], w=["h2T"])
            for q4 in range(2):
                for j in range(4):
                    kc = q4 * 4 + j
                    P.pe(lambda e, kc=kc, j=j: e.transpose(out=pTf[:, j, :], in_=h2_D[:, kc * 128:(kc + 1) * 128], identity=ident_f[:]), r=["h2_D", "ident_f"], w=["pTf"])
                P.dve(lambda e, q4=q4: e.tensor_copy(out=h2Tf[:, q4 * 4:(q4 + 1) * 4, :], in_=pTf[:]), r=["pTf"], w=["h2Tf"])
            for kc in range(8):
                P.pe(lambda e, kc=kc: e.matmul(out=pR[:], lhsT=h2Tf[:, kc, :], rhs=wrt_sb[:, kc, :], start=(kc == 0), stop=(kc == 7)), r=["h2Tf", "wrt_sb"], w=["pR"])
            P.dve(lambda e: e.tensor_tensor(out=L_D[:], in0=pR[:], in1=brt_bc[:], op=ALU.add), r=["pR", "brt_bc"], w=["L_D"])
            R_ = lambda i: rs[:, i:i + 1]
            kR = lambda i: ("rs", i)
            P.dve(lambda e: e.tensor_reduce(out=R_(0), in_=L_D[:, 0:4], axis=AX.X, op=ALU.max), r=["L_D"], w=[kR(0)])
            P.dve(lambda e: e.tensor_scalar(out=ohg[:], in0=L_D[:, 0:4], scalar1=R_(0), scalar2=None, op0=ALU.is_equal), r=["L_D", kR(0)], w=["ohg"])
            P.dve(lambda e: e.tensor_scalar(out=R_(1), in0=R_(0), scalar1=-1.0, scalar2=None, op0=ALU.mult), r=[kR(0)], w=[kR(1)])
            P.act(lambda e: e.activation(out=gex[:], in_=L_D[:, 0:4], func=ACT.Exp, bias=R_(1), scale=1.0), r=["L_D", kR(1)], w=["gex"])
            P.dve(lambda e: e.tensor_reduce(out=R_(2), in_=gex[:], axis=AX.X, op=ALU.add), r=["gex"], w=[kR(2)])
            P.dve(lambda e: e.reciprocal(out=R_(2), in_=R_(2)), r=[kR(2)], w=[kR(2)])
            P.dve(lambda e: e.tensor_tensor(out=msk[:].rearrange("p (g x) -> p g x", x=8), in0=L_D[:, 4:36].rearrange("p (g x) -> p g x", x=8),
                                            in1=fv(ohg[:], 0, [[1, 4], [0, 8]]), op=ALU.mult), r=["L_D", "ohg"], w=["msk"])
            P.dve(lambda e: e.tensor_reduce(out=ein[:], in_=fv(msk[:], 0, [[1, 8], [8, 4]]), axis=AX.X, op=ALU.add), r=["msk"], w=["ein"])
            P.dve(lambda e: e.tensor_reduce(out=R_(3), in_=ein[:], axis=AX.X, op=ALU.max), r=["ein"], w=[kR(3)])
            P.dve(lambda e: e.tensor_scalar(out=oh1[:], in0=ein[:], scalar1=R_(3), scalar2=None, op0=ALU.is_equal), r=["ein", kR(3)], w=["oh1"])
            P.dve(lambda e: e.scalar_tensor_tensor(out=e2_[:], in0=oh1[:], scalar=-1e30, in1=ein[:], op0=ALU.mult, op1=ALU.add), r=["oh1", "ein"], w=["e2_"])
            P.dve(lambda e: e.tensor_reduce(out=R_(4), in_=e2_[:], axis=AX.X, op=ALU.max), r=["e2_"], w=[kR(4)])
            P.dve(lambda e: e.tensor_scalar(out=oh2[:], in0=e2_[:], scalar1=R_(4), scalar2=None, op0=ALU.is_equal), r=["e2_", kR(4)], w=["oh2"])
            P.dve(lambda e: e.tensor_tensor(out=R_(5), in0=R_(4), in1=R_(3), op=ALU.subtract), r=[kR(3), kR(4)], w=[kR(5)])
            P.act(lambda e: e.activation(out=R_(6), in_=R_(5), func=ACT.Exp), r=[kR(5)], w=[kR(6)])
            P.dve(lambda e: e.tensor_scalar(out=R_(7), in0=R_(6), scalar1=1.0, scalar2=None, op0=ALU.add), r=[kR(6)], w=[kR(7)])
            P.dve(lambda e: e.reciprocal(out=R_(7), in_=R_(7)), r=[kR(7)], w=[kR(7)])
            P.dve(lambda e: e.tensor_tensor(out=R_(8), in0=R_(6), in1=R_(7), op=ALU.mult), r=[kR(6), kR(7)], w=[kR(8)])
            P.dve(lambda e: e.tensor_tensor(out=R_(7), in0=R_(7), in1=R_(2), op=ALU.mult), r=[kR(7), kR(2)], w=[kR(7)])
            P.dve(lambda e: e.tensor_tensor(out=R_(8), in0=R_(8), in1=R_(2), op=ALU.mult), r=[kR(8), kR(2)], w=[kR(8)])
            P.dve(lambda e: e.tensor_scalar(out=cg[:], in0=oh1[:], scalar1=R_(7), scalar2=None, op0=ALU.mult), r=["oh1", kR(7)], w=["cg"])
            P.dve(lambda e: e.scalar_tensor_tensor(out=cg[:], in0=oh2[:], scalar=R_(8), in1=cg[:], op0=ALU.mult, op1=ALU.add), r=["oh2", kR(8), "cg"], w=["cg"])
            P.dve(lambda e: e.tensor_tensor(out=comb[:].rearrange("p (g x) -> p g x", x=8), in0=fv(cg[:], 0, [[0, 4], [1, 8]]), in1=fv(ohg[:], 0, [[1, 4], [0, 8]]), op=ALU.mult),
                  r=["cg", "ohg"], w=["comb"])
            P.pe(lambda e: e.transpose(out=pCT[:], in_=comb[:], identity=ident_f[:]), r=["comb", "ident_f"], w=["pCT"])
            P.act(lambda e, tsl_=tsl_: e.activation(out=combT[:, tsl_], in_=pCT[:], func=ACT.Copy), r=["pCT"], w=["combT"])
        P.barrier()
    esM.close()
    if "x1" in dbg_names:
        o_ = nc.dram_tensor("dbg_x1", [NOWN, D], F32, kind="ExternalOutput").ap()
        P.dma(lambda e: e.dma_start(out=o_, in_=x1_d), key="dbg_x1", r=["x1_d"], w=["dbg_x1"])
    if "combT" in dbg_names:
        dump("combT", combT[:], [32, NOWN], BF16)
    if STOP <= 7:
        esE.close()
        return finish(nc, P, ges, out, dbg_out)

    yacc = SB(esE, "yacc", [128, NTO, D], F32)
    with ExitStack() as es:
        Weg = [SB(es, "Weg%d" % i, [128, 8, 512], BF16) for i in range(2)]
        Weu = [SB(es, "Weu%d" % i, [128, 8, 512], BF16) for i in range(2)]
        Wed = [SB(es, "Wed%d" % i, [128, 4, 1024], BF16) for i in range(2)]
        sele_b = SB(es, "sele_b", [32, 32, 128], BF16)
        bc_sb = SB(es, "bc_sb", [128, 512], F32)
        sa_E = [SB(es, "sa_E%d" % i, [128, 512], F32) for i in range(2)]
        actT = [SB(es, "actT%d" % i, [128, 4, 512], BF16) for i in range(2)]
        pBC = PS(es, "pBC", [128, 512], F32)
        pA_E = [PS(es, "pA_E%d" % i, [128, 512], F32) for i in range(2)]
        pB_E = [PS(es, "pB_E%d" % i, [128, 512], F32) for i in range(2)]
        pY_E = [PS(es, "pY_E%d" % i, [128, 512], F32) for i in range(2)]
        P.dma(lambda e: e.dma_start(out=sele_b[:], in_=cst_sele), key="sele_b", w=["sele_b"], eng="pool")
        NEXP = int(os.environ.get("K_NEXP", "32"))
        fci = 0
        yi = 0
        for ex in range(NEXP):
            se = ex % 2
            P.dma(lambda e, ex=ex, se=se: e.dma_start(out=Weg[se][:], in_=w_eg[ex].rearrange("(kc p) f -> p kc f", p=128)), key=("Weg", se), w=[("Weg", se)], eng="pool")
            P.dma(lambda e, ex=ex, se=se: e.dma_start(out=Weu[se][:], in_=w_eu[ex].rearrange("(kc p) f -> p kc f", p=128)), key=("Weu", se), w=[("Weu", se)], eng="pool")
            P.dma(lambda e, ex=ex, se=se: e.dma_start(out=Wed[se][:], in_=w_ed[ex].rearrange("(fc p) n -> p fc n", p=128)), key=("Wed", se), w=[("Wed", se)], eng="pool")
            for st_ in range(4):
                ts = slice(st_ * 512, (st_ + 1) * 512)
                sa_ = (ex * 4 + st_) % 2
                P.pe(lambda e, ex=ex, ts=ts: e.matmul(out=pBC[:], lhsT=sele_b[:, ex, :], rhs=combT[:, ts], start=True, stop=True), r=["sele_b", "combT"], w=["pBC"])
                P.act(lambda e: e.activation(out=bc_sb[:], in_=pBC[:], func=ACT.Copy), r=["pBC"], w=["bc_sb"])
                for fc in range(4):
                    fs = slice(fc * 128, (fc + 1) * 128)
                    sp_ = fci % 2
                    for kc in range(8):
                        P.pe(lambda e, kc=kc, fs=fs, ts=ts, se=se, sp_=sp_: e.matmul(out=pA_E[sp_][:], lhsT=Weg[se][:, kc, fs], rhs=h2T[:, kc, ts], start=(kc == 0), stop=(kc == 7)),
                             r=[("Weg", se), "h2T"], w=[("pA_E", sp_)])
                    for kc in range(8):
                        P.pe(lambda e, kc=kc, fs=fs, ts=ts, se=se, sp_=sp_: e.matmul(out=pB_E[sp_][:], lhsT=Weu[se][:, kc, fs], rhs=h2T[:, kc, ts], start=(kc == 0), stop=(kc == 7)),
                             r=[("Weu", se), "h2T"], w=[("pB_E", sp_)])
                    P.act(lambda e, sp_=sp_: e.activation(out=sa_E[sp_][:], in_=pA_E[sp_][:], func=ACT.Silu), r=[("pA_E", sp_)], w=[("sa_E", sp_)])
                    P.dve(lambda e, sp_=sp_: e.tensor_tensor(out=sa_E[sp_][:], in0=sa_E[sp_][:], in1=pB_E[sp_][:], op=ALU.mult), r=[("sa_E", sp_), ("pB_E", sp_)], w=[("sa_E", sp_)])
                    P.dve(lambda e, sp_=sp_, sa_=sa_, fc=fc: e.tensor_tensor(out=actT[sa_][:, fc, :], in0=sa_E[sp_][:], in1=bc_sb[:], op=ALU.mult),
                          r=[("sa_E", sp_), "bc_sb"], w=[("actT", sa_)])
                    fci += 1
                for j in range(4):
                    tile_ = st_ * 4 + j
                    js = slice(j * 128, (j + 1) * 128)
                    for half in range(2):
                        hs = slice(half * 512, (half + 1) * 512)
                        sy = yi % 2
                        for fc in range(4):
                            P.pe(lambda e, fc=fc, js=js, hs=hs, sa_=sa_, se=se, sy=sy: e.matmul(out=pY_E[sy][:], lhsT=actT[sa_][:, fc, js], rhs=Wed[se][:, fc, hs], start=(fc == 0), stop=(fc == 3)),
                                 r=[("actT", sa_), ("Wed", se)], w=[("pY_E", sy)])
                        if ex == 0:
                            P.dve(lambda e, tile_=tile_, hs=hs, sy=sy: e.tensor_copy(out=yacc[:, tile_, hs], in_=pY_E[sy][:]), r=[("pY_E", sy)], w=[("yacc", tile_)])
                        else:
                            P.dve(lambda e, tile_=tile_, hs=hs, sy=sy: e.tensor_tensor(out=yacc[:, tile_, hs], in0=yacc[:, tile_, hs], in1=pY_E[sy][:], op=ALU.add),
                                  r=[("pY_E", sy), ("yacc", tile_)], w=[("yacc", tile_)])
                        yi += 1
        P.barrier()

    with ExitStack() as es:
        g2_bc = SB(es, "g2_bc", [128, D], F32)
        l2g_bc = SB(es, "l2g_bc", [128, D], F32)
        l2b_bc = SB(es, "l2b_bc", [128, D], F32)
        x1_F = [SB(es, "x1_F%d" % i, [128, D], F32) for i in range(2)]
        z_F = SB(es, "z_F", [128, D], F32)
        zn_F = SB(es, "zn_F", [128, D], F32)
        o_F = [SB(es, "o_F%d" % i, [128, D], F32) for i in range(2)]
        st_F = SB(es, "st_F", [128, 2, 6], F32)
        mv_F = SB(es, "mv_F", [128, 2], F32)
        rstd_F = SB(es, "rstd_F", [128, 1], F32)
        nb_F = SB(es, "nb_F", [128, 1], F32)
        load_bc(g2_bc, mod_d[0:1, 5120:6144], "g2_bc")
        load_bc(l2g_bc, ln2_g, "l2g_bc")
        load_bc(l2b_bc, ln2_b, "l2b_bc")
        for tt in range(NTO):
            s2 = tt % 2
            t0 = tt * 128
            P.dma(lambda e, s2=s2, t0=t0: e.dma_start(out=x1_F[s2][:], in_=x1_d[t0:t0 + 128, :]), key=("x1_F", s2), r=["x1_d"], w=[("x1_F", s2)])
            P.dve(lambda e, tt=tt: e.tensor_tensor(out=z_F[:], in0=yacc[:, tt, :], in1=g2_bc[:], op=ALU.mult), r=[("yacc", tt), "g2_bc"], w=["z_F"])
            P.dve(lambda e, s2=s2: e.scalar_tensor_tensor(out=z_F[:], in0=x1_F[s2][:], scalar=ALPHA, in1=z_F[:], op0=ALU.mult, op1=ALU.add), r=["z_F", ("x1_F", s2)], w=["z_F"])
            ln_tile(z_F[:], "z_F", st_F, mv_F, rstd_F, nb_F, zn_F[:], "zn_F", "F")
            P.dve(lambda e: e.tensor_tensor(out=zn_F[:], in0=zn_F[:], in1=l2g_bc[:], op=ALU.mult), r=["zn_F", "l2g_bc"], w=["zn_F"])
            P.dve(lambda e, s2=s2: e.tensor_tensor(out=o_F[s2][:], in0=zn_F[:], in1=l2b_bc[:], op=ALU.add), r=["zn_F", "l2b_bc"], w=[("o_F", s2)])
            P.dma(lambda e, s2=s2, t0=t0: e.dma_start(out=out[t0:t0 + 128, :], in_=o_F[s2][:]), key=("outd", s2), r=[("o_F", s2)], w=["out"])
        P.barrier()
    esE.close()
    return finish(nc, P, ges, out, dbg_out)


def finish(nc, P, ges, out, dbg_out):
    P.barrier()
    P.emit()
    ges.close()
    nc._dbg_out = dbg_out
    nc._stats = P.stats
    return nc


def rope_tables():
    rows = NLAT // 64
    row = np.repeat(np.arange(rows, dtype=np.float32), 64)
    col = np.tile(np.arange(64, dtype=np.float32), rows)
    inv = (np.float32(10000.0) ** (-np.arange(0, 64, 2, dtype=np.float32) / np.float32(64))).astype(np.float32)
    ang = np.stack([row[:, None] * inv, col[:, None] * inv], axis=1).astype(np.float32)
    tab = np.concatenate([np.cos(ang).reshape(NLAT, 64), np.sin(ang).reshape(NLAT, 64)], axis=1).astype(np.float32)
    return tab


def make_in_maps(inp):
    f32 = np.float32
    g = lambda k: np.asarray(inp[k], dtype=f32)
    x, c, ctx, c_ctx = g("x"), g("c"), g("ctx"), g("c_ctx")
    tab = rope_tables()
    tab_ctx = np.concatenate([np.ones((NCTX, 64), f32), np.zeros((NCTX, 64), f32)], axis=1)
    rope_full = np.concatenate([tab_ctx, tab], axis=0)
    tok = np.arange(128)
    mask8 = (tok[:, None] % 8 == np.arange(8)[None, :]).astype(f32)
    sel16 = (tok[:, None] // 8 == np.arange(16)[None, :]).astype(f32)
    mask8c = (tok[:, None] // 16 == np.arange(8)[None, :]).astype(f32)

    def pairlay(a):
        sh = a.shape
        a = a.reshape((2, 16, 2, 64) + sh[3:])
        perm = (2, 3, 0, 1) + tuple(range(4, a.ndim))
        a = a.transpose(perm)
        return np.ascontiguousarray(a.reshape((128, 2, 16) + sh[3:]))

    a_re, a_im = g("s5_a_re")[0], g("s5_a_im")[0]
    s5_a = np.stack([pairlay(a_re), pairlay(a_im)], axis=1)
    ldt = g("s5_log_dt")[0]
    s5_ldt = np.ascontiguousarray(np.broadcast_to(ldt.reshape(1, 2, 16, 2).transpose(0, 3, 1, 2), (64, 2, 2, 16)).transpose(1, 0, 2, 3).reshape(128, 2, 16))
    s5_b = np.stack([pairlay(g("s5_b_re")[0]), pairlay(g("s5_b_im")[0])], axis=1)
    cre = g("s5_c_re")[0].transpose(0, 1, 3, 2)
    cim = g("s5_c_im")[0].transpose(0, 1, 3, 2)
    s5_c = np.stack([pairlay(cre), pairlay(cim)], axis=1)
    dvec = g("s5_d")[0]
    s5_dcol = np.ascontiguousarray(np.broadcast_to(dvec.reshape(32, 16).T[None], (8, 16, 32)).reshape(128, 32))
    eZ = np.stack([63.0 - np.arange(64), np.arange(64)], axis=0).astype(f32)
    qs = np.arange(72)
    eRf = (qs - 7).astype(f32)
    eRb = (8 * (qs // 8 - 1) + 8 - (qs % 8)).astype(f32)
    eR = np.stack([eRf, eRb], axis=0)
    cst_eZ = np.ascontiguousarray(np.broadcast_to(eZ[None], (128, 2, 64)))
    cst_eR = np.ascontiguousarray(np.broadcast_to(eR[None], (128, 2, 72)))
    sidx = tok // 16
    cst_mf = (sidx[None, :] >= sidx[:, None]).astype(f32)
    cst_mb = (sidx[:, None] >= sidx[None, :]).astype(f32)
    selg = np.zeros((128, 8, 128), f32)
    for g8 in range(8):
        for co in range(16):
            selg[np.arange(8) * 16 + co, g8, g8 * 16 + co] = 1.0
    sele = np.zeros((32, 32, 128), f32)
    for e in range(32):
        sele[e, e, :] = 1.0
    w_rt = np.concatenate([g("w_router_group")[0], g("w_router_expert")[0]], axis=1)
    b_rt = np.concatenate([g("b_router_group")[0], g("b_router_expert")[0]], axis=0)[None]
    common = dict(
        w_mod=g("w_mod")[0], b_mod=g("b_mod"), w_in=g("w_in")[0], rope_f=rope_full,
        q_gain=g("q_gain"), k_gain=g("k_gain"), cst_mask8=mask8, cst_mask8c=mask8c, cst_sel16=sel16,
        s5_a=s5_a, s5_ldt=s5_ldt, s5_b=s5_b, s5_c=s5_c, s5_dcol=s5_dcol, cst_eZ=cst_eZ, cst_eR=cst_eR,
        cst_mf=cst_mf, cst_mb=cst_mb, cst_selg=selg,
        w_glu_a=g("w_glu_a")[0], w_glu_b=g("w_glu_b")[0], w_attn_o=g("w_attn_o")[0], w_out=g("w_out")[0],
        ln1_g=g("ln1_g"), ln1_b=g("ln1_b"), ln2_g=g("ln2_g"), ln2_b=g("ln2_b"),
        w_rt=w_rt, b_rt=b_rt, w_eg=g("w_exp_gate")[0], w_eu=g("w_exp_up")[0], w_ed=g("w_exp_down")[0],
        cst_sele=sele,
    )
    maps = []
    for core in range(8):
        b, r = core // 4, core % 4
        m = dict(common)
        m["xf"] = np.ascontiguousarray(np.concatenate([ctx[b], x[b]], axis=0))
        m["xo"] = np.ascontiguousarray(x[b, r * NOWN:(r + 1) * NOWN])
        cc = np.stack([c[b], c_ctx], axis=0)
        m["ccT"] = np.ascontiguousarray(cc.reshape(2, 8, 128).transpose(2, 1, 0))
        m["rope_o"] = np.ascontiguousarray(tab[r * NOWN:(r + 1) * NOWN])
        cm = np.zeros((128, 4), f32)
        cm[:, r] = 1.0
        m["cmask"] = cm
        maps.append(m)
    return maps


_NC_CACHE = {}


def kernel(**inputs):
    maps = make_in_maps(inputs)
    if "nc" not in _NC_CACHE:
        _NC_CACHE["nc"] = build()
    nc = _NC_CACHE["nc"]
    res = run_bass_kernel_spmd(nc, maps, core_ids=list(range(8)))
    outp = np.zeros((2, NLAT, D), np.float32)
    for core in range(8):
        b, r = core // 4, core % 4
        outp[b, r * NOWN:(r + 1) * NOWN] = res.results[core]["out"]
    return outp
```

```python
import os
import math
import numpy as np
from contextlib import ExitStack
import concourse.bass as bass
import concourse.mybir as mybir
from concourse.bass_utils import run_bass_kernel_spmd

F32 = mybir.dt.float32
BF16 = mybir.dt.bfloat16
ACT = mybir.ActivationFunctionType
ALU = mybir.AluOpType
AX = mybir.AxisListType

D = 1024
NLAT = 8192
NCTX = 256
NFULL = NLAT + NCTX
NOWN = 2048
NTF = NFULL // 128
NTO = NOWN // 128
NJ = NFULL // 64
NJF = NFULL // 8
EPS = 1e-6
ALPHA = 2.0 ** 0.25
STOP = int(os.environ.get("K_STOP", "99"))
DEBUG = os.environ.get("K_DEBUG", "") != ""


class Prog:
    ENGS = ["pe", "act", "dve", "pool", "sp"]

    def __init__(self, nc):
        self.nc = nc
        self.ops = []

    def add(self, eng, fn, r=(), w=(), dma=None, ndma=1):
        self.ops.append(dict(eng=eng, fn=fn, r=tuple(r), w=tuple(w), dma=dma, ndma=ndma, barrier=False))

    def pe(self, fn, r=(), w=()):
        self.add("pe", fn, r, w)

    def act(self, fn, r=(), w=()):
        self.add("act", fn, r, w)

    def dve(self, fn, r=(), w=()):
        self.add("dve", fn, r, w)

    def pool(self, fn, r=(), w=()):
        self.add("pool", fn, r, w)

    def dma(self, fn, key, r=(), w=(), eng="sp", n=1):
        self.add(eng, fn, r, w, dma=key, ndma=n)

    def capture(self):
        self._saved = self.ops
        self.ops = []

    def end_capture(self):
        lst = self.ops
        self.ops = self._saved
        return lst

    def barrier(self):
        for e in self.ENGS:
            self.ops.append(dict(eng=e, fn=None, r=(), w=(), dma=None, ndma=0, barrier=True))

    def emit(self):
        nc = self.nc
        ops = self.ops
        n = len(ops)
        last_w, readers = {}, {}
        deps = [None] * n
        last_eng, last_dma = {}, {}
        for i, op in enumerate(ops):
            d = set()
            if op["barrier"]:
                for e, j in last_eng.items():
                    if e != op["eng"]:
                        d.add(j)
                for k, j in last_dma.items():
                    d.add(j)
            for b in op["r"]:
                if b in last_w:
                    d.add(last_w[b])
            for b in op["w"]:
                if b in last_w:
                    d.add(last_w[b])
                for j in readers.get(b, ()):
                    d.add(j)
            for b in op["r"]:
                readers.setdefault(b, []).append(i)
            for b in op["w"]:
                readers[b] = []
                last_w[b] = i
            d.discard(i)
            deps[i] = d
            if op["dma"] is not None:
                last_dma[op["dma"]] = i
            elif not op["barrier"]:
                last_eng[op["eng"]] = i
        signal = [False] * n
        for i, op in enumerate(ops):
            for j in deps[i]:
                pj = ops[j]
                if pj["dma"] is not None:
                    continue
                if pj["eng"] == "pe" and op["eng"] == "pe" and op["dma"] is None:
                    continue
                signal[j] = True
        tick = [0] * n
        cnt = {e: 0 for e in self.ENGS}
        dcnt = {}
        for i, op in enumerate(ops):
            if op["dma"] is not None:
                dcnt[op["dma"]] = dcnt.get(op["dma"], 0) + op["ndma"]
                tick[i] = dcnt[op["dma"]] * 16
            elif signal[i]:
                cnt[op["eng"]] += 1
                tick[i] = cnt[op["eng"]]
        es = ExitStack()
        esem = {e: es.enter_context(nc.semaphore("s_" + e)) for e in self.ENGS}
        dsem = {}
        for k in dcnt:
            dsem[k] = es.enter_context(nc.semaphore("d_%d" % len(dsem)))
        waits = [None] * n
        seen = {e: {} for e in self.ENGS}
        for i, op in enumerate(ops):
            wl = {}
            for j in deps[i]:
                pj = ops[j]
                if pj["dma"] is not None:
                    key = ("d", pj["dma"])
                    sem = dsem[pj["dma"]]
                else:
                    if pj["eng"] == "pe" and op["eng"] == "pe" and op["dma"] is None:
                        continue
                    key = ("e", pj["eng"])
                    sem = esem[pj["eng"]]
                v = tick[j]
                if seen[op["eng"]].get(key, 0) >= v:
                    continue
                if key not in wl or wl[key][1] < v:
                    wl[key] = (sem, v)
            for key, (sem, v) in wl.items():
                seen[op["eng"]][key] = v
            waits[i] = list(wl.values())
        self.stats = dict(n=n, sig=dict(cnt), dkeys=len(dcnt), nwaits=sum(len(w) for w in waits))
        block = es.enter_context(nc.Block())

        def run(engname, eng):
            for i, op in enumerate(ops):
                if op["eng"] != engname:
                    continue
                for sem, v in waits[i]:
                    eng.wait_ge(sem, v)
                if op["fn"] is None:
                    continue
                res = op["fn"](eng)
                if op["dma"] is not None:
                    if not isinstance(res, (list, tuple)):
                        res = [res]
                    assert len(res) == op["ndma"], (len(res), op["ndma"])
                    for ins in res:
                        ins.then_inc(dsem[op["dma"]], 16)
                elif signal[i]:
                    if isinstance(res, (list, tuple)):
                        res = res[-1]
                    res.then_inc(esem[engname], 1)

        @block.tensor
        def _(e):
            run("pe", e)

        @block.scalar
        def _(e):
            run("act", e)

        @block.vector
        def _(e):
            run("dve", e)

        @block.gpsimd
        def _(e):
            run("pool", e)

        @block.sync
        def _(e):
            run("sp", e)

        es.close()


def AP_(t, offset, dims):
    return bass.AP(t, offset, [list(d) for d in dims])


def pstride(t):
    return t[:].ap[0][0]


def build(dbg_names=()):
    nc = bass.Bass("TRN2", target_bir_lowering=False)
    dram_in = lambda name, shape, dt=F32: nc.dram_tensor(name, list(shape), dt, kind="ExternalInput").ap()
    xf = dram_in("xf", [NFULL, D])
    xo = dram_in("xo", [NOWN, D])
    ccT = dram_in("ccT", [128, 8, 2])
    w_mod = dram_in("w_mod", [D, 6 * D])
    b_mod = dram_in("b_mod", [1, 6 * D])
    w_in = dram_in("w_in", [D, 4096])
    rope_f = dram_in("rope_f", [NFULL, 128])
    rope_o = dram_in("rope_o", [NOWN, 128])
    q_gain = dram_in("q_gain", [1, 128])
    k_gain = dram_in("k_gain", [1, 128])
    cst_mask8 = dram_in("cst_mask8", [128, 8])
    cst_mask8c = dram_in("cst_mask8c", [128, 8])
    cst_sel16 = dram_in("cst_sel16", [128, 16])
    s5_a = dram_in("s5_a", [128, 2, 2, 16])
    s5_ldt = dram_in("s5_ldt", [128, 2, 16])
    s5_b = dram_in("s5_b", [128, 2, 2, 16, 16])
    s5_c = dram_in("s5_c", [128, 2, 2, 16, 16])
    s5_dcol = dram_in("s5_dcol", [128, 32])
    cst_eZ = dram_in("cst_eZ", [128, 2, 64])
    cst_eR = dram_in("cst_eR", [128, 2, 72])
    cst_mf = dram_in("cst_mf", [128, 128])
    cst_mb = dram_in("cst_mb", [128, 128])
    cst_selg = dram_in("cst_selg", [128, 8, 128])
    cmask = dram_in("cmask", [128, 4])
    w_glu_a = dram_in("w_glu_a", [512, D])
    w_glu_b = dram_in("w_glu_b", [512, D])
    w_attn_o = dram_in("w_attn_o", [D, D])
    w_out = dram_in("w_out", [D, D])
    ln1_g = dram_in("ln1_g", [1, D])
    ln1_b = dram_in("ln1_b", [1, D])
    ln2_g = dram_in("ln2_g", [1, D])
    ln2_b = dram_in("ln2_b", [1, D])
    w_rt = dram_in("w_rt", [D, 36])
    b_rt = dram_in("b_rt", [1, 36])
    w_eg = dram_in("w_eg", [32, D, 512])
    w_eu = dram_in("w_eu", [32, D, 512])
    w_ed = dram_in("w_ed", [32, 512, D])
    cst_sele = dram_in("cst_sele", [32, 32, 128])
    out = nc.dram_tensor("out", [NOWN, D], F32, kind="ExternalOutput").ap()
    scr = lambda name, shape, dt: nc.dram_tensor(name, list(shape), dt, kind="Internal").ap()
    mod_d = scr("mod_d", [2, 6 * D], F32)
    kT_d = scr("kT_d", [2, 128, NFULL], BF16)
    v_d = scr("v_d", [NFULL, 256], BF16)
    x1_d = scr("x1_d", [NOWN, D], F32)
    dbg_out = {}

    P = Prog(nc)
    ges = ExitStack()

    free_list = [[16640, 229376]]
    peak = [0]

    def _alloc(nbytes):
        nbytes = (nbytes + 63) // 64 * 64
        for iv in free_list:
            if iv[1] - iv[0] >= nbytes:
                off = iv[0]
                iv[0] += nbytes
                peak[0] = max(peak[0], off + nbytes)
                return off, nbytes
        raise RuntimeError("SBUF manual allocator out of space for %d bytes; free=%s" % (nbytes, free_list))

    def _free(off, nbytes):
        free_list.append([off, off + nbytes])
        free_list.sort()
        merged = []
        for iv in free_list:
            if iv[1] == iv[0]:
                continue
            if merged and merged[-1][1] == iv[0]:
                merged[-1][1] = iv[1]
            else:
                merged.append(iv)
        free_list[:] = merged

    def SB(es, name, shape, dt):
        esz = 4 if dt == F32 else 2
        nb = esz
        for s_ in shape[1:]:
            nb *= s_
        off, nbytes = _alloc(nb)
        t = nc.alloc_sbuf_tensor_at(name, list(shape), dt, offset=off)
        es.callback(_free, off, nbytes)
        return t

    def PS(es, name, shape, dt):
        return es.enter_context(nc.psum_tensor(name, list(shape), dt))

    def dump(name, t_ap, shape, dt=F32, r=()):
        if name not in dbg_names:
            return
        o = nc.dram_tensor("dbg_" + name, list(shape), dt, kind="ExternalOutput").ap()
        dbg_out[name] = o
        P.dma(lambda e: e.dma_start(out=o, in_=t_ap), key="dbg_" + name, r=r, w=["dbg_" + name])

    ident_f = SB(ges, "ident_f", [128, 128], F32)
    ident_b = SB(ges, "ident_b", [128, 128], BF16)
    ones_b = SB(ges, "ones_b", [1, 128], BF16)
    eps_t = SB(ges, "eps_t", [128, 1], F32)
    modT = SB(ges, "modT", [128, 48, 2], F32)
    op1p = SB(ges, "op1p", [128, 8, 2], F32)
    sh1T = SB(ges, "sh1T", [128, 8, 2], F32)
    gk_bc = SB(ges, "gk_bc", [128, 128], F32)
    gq_bc = SB(ges, "gq_bc", [128, 128], F32)
    negC = SB(ges, "negC", [128, 1], F32)
    mask8 = SB(ges, "mask8", [128, 8], BF16)
    sel16 = SB(ges, "sel16", [128, 16], BF16)

    P.pool(lambda e: e.memset(ident_f[:], 1.0), w=["ident_f"])
    P.pool(lambda e: e.affine_select(out=ident_f[:], in_=ident_f[:], pattern=[[-1, 128]], compare_op=ALU.is_equal,
                                     fill=0.0, base=0, channel_multiplier=1), r=["ident_f"], w=["ident_f"])
    P.dve(lambda e: e.tensor_copy(out=ident_b[:], in_=ident_f[:]), r=["ident_f"], w=["ident_b"])
    P.dve(lambda e: e.memset(ones_b[:], 1.0), w=["ones_b"])
    P.dve(lambda e: e.memset(eps_t[:], EPS), w=["eps_t"])
    P.dma(lambda e: e.dma_start(out=gk_bc[:], in_=k_gain.partition_broadcast(128)), key="gk_bc", w=["gk_bc"])
    P.dma(lambda e: e.dma_start(out=gq_bc[:], in_=q_gain.partition_broadcast(128)), key="gq_bc", w=["gq_bc"])

    with ExitStack() as es:
        scT = SB(es, "scT", [128, 8, 2], F32)
        ccs = SB(es, "ccs", [128, 8, 2], F32)
        wm = [SB(es, "wm%d" % i, [128, 8, 512], F32) for i in range(2)]
        bm = SB(es, "bm", [2, 6 * D], F32)
        mod_sb = SB(es, "mod_sb", [2, 6 * D], F32)
        m8f = SB(es, "m8f", [128, 8], F32)
        s16f = SB(es, "s16f", [128, 16], F32)
        tmpg = SB(es, "tmpg", [128, 2], F32)
        pM = [PS(es, "pM%d" % i, [2, 512], F32) for i in range(2)]
        pMT = PS(es, "pMT", [128, 48, 2], F32)
        P.dma(lambda e: e.dma_start(out=ccs[:], in_=ccT), key="ccs", w=["ccs"])
        P.dma(lambda e: e.dma_start(out=bm[:], in_=b_mod.partition_broadcast(2)), key="bm", w=["bm"])
        P.dma(lambda e: e.dma_start(out=m8f[:], in_=cst_mask8), key="m8f", w=["m8f"])
        P.dma(lambda e: e.dma_start(out=s16f[:], in_=cst_sel16), key="s16f", w=["s16f"])
        P.dve(lambda e: e.tensor_copy(out=mask8[:], in_=m8f[:]), r=["m8f"], w=["mask8"])
        P.dve(lambda e: e.tensor_copy(out=sel16[:], in_=s16f[:]), r=["s16f"], w=["sel16"])
        P.act(lambda e: e.activation(out=scT[:], in_=ccs[:], func=ACT.Silu), r=["ccs"], w=["scT"])
        P.dve(lambda e: e.tensor_reduce(out=tmpg[:, 0:1], in_=gq_bc[:], axis=AX.X, op=ALU.max, apply_absolute_value=True),
              r=["gq_bc"], w=["tmpg0"])
        P.dve(lambda e: e.tensor_reduce(out=tmpg[:, 1:2], in_=gk_bc[:], axis=AX.X, op=ALU.max, apply_absolute_value=True),
              r=["gk_bc"], w=["tmpg1"])
        P.dve(lambda e: e.scalar_tensor_tensor(out=negC[:], in0=tmpg[:, 0:1], scalar=-math.sqrt(128.0), in1=tmpg[:, 1:2],
                                               op0=ALU.mult, op1=ALU.mult), r=["tmpg0", "tmpg1"], w=["negC"])
        for nb in range(12):
            s = nb % 2
            P.dma(lambda e, nb=nb, s=s: e.dma_start(out=wm[s][:], in_=w_mod[:, nb * 512:(nb + 1) * 512].rearrange("(kc p) n -> p kc n", p=128)),
                  key=("wm", s), w=[("wm", s)])
            for kc in range(8):
                P.pe(lambda e, kc=kc, s=s: e.matmul(out=pM[s][:], lhsT=scT[:, kc, :], rhs=wm[s][:, kc, :], start=(kc == 0), stop=(kc == 7)),
                     r=["scT", ("wm", s)], w=[("pM", s)])
            P.dve(lambda e, nb=nb, s=s: e.tensor_tensor(out=mod_sb[:, nb * 512:(nb + 1) * 512], in0=pM[s][:], in1=bm[:, nb * 512:(nb + 1) * 512], op=ALU.add),
                  r=[("pM", s), "bm"], w=["mod_sb"])
        P.dma(lambda e: e.dma_start(out=mod_d, in_=mod_sb[:]), key="mod_d", r=["mod_sb"], w=["mod_d"])
        for j in range(48):
            P.pe(lambda e, j=j: e.transpose(out=pMT[:, j, :], in_=mod_sb[:, j * 128:(j + 1) * 128], identity=ident_f[0:2, 0:2]),
                 r=["mod_sb", "ident_f"], w=["pMT"])
        P.dve(lambda e: e.tensor_copy(out=modT[:], in_=pMT[:]), r=["pMT"], w=["modT"])
        P.dve(lambda e: e.tensor_copy(out=sh1T[:], in_=modT[:, 0:8, :]), r=["modT"], w=["sh1T"])
        P.dve(lambda e: e.tensor_scalar(out=op1p[:], in0=modT[:, 8:16, :], scalar1=1.0, scalar2=None, op0=ALU.add), r=["modT"], w=["op1p"])
        dump("mod", mod_sb[:], [2, 6 * D], r=["mod_sb"])
        P.barrier()
    if STOP <= 0:
        return finish(nc, P, ges, out, dbg_out)

    def fv(ap, off, dims):
        return bass.AP(ap.tensor, ap.offset + off, [list(ap.ap[0])] + [list(d) for d in dims])

    bias_sb = SB(ges, "bias_sb", [1, 5120], BF16)

    def prep_wblock(stage, pB, s, dst, col0, r, bcol, tag):
        P.dma(lambda e: e.dma_start(out=stage[s][:], in_=w_in[:, col0:col0 + 512].rearrange("(kc p) n -> p kc n", p=128)),
              key=("stg", s), w=[("stg", s)])
        for kc in range(8):
            P.pool(lambda e, kc=kc: e.tensor_scalar(out=dst[:, kc, :], in0=stage[s][:, kc, :], scalar1=op1p[:, kc, r:r + 1], scalar2=None, op0=ALU.mult),
                   r=[("stg", s), "op1p"], w=[tag])
        for kc in range(8):
            P.pe(lambda e, kc=kc: e.matmul(out=pB[:], lhsT=sh1T[:, kc, r:r + 1], rhs=stage[s][:, kc, :], start=(kc == 0), stop=(kc == 7)),
                 r=[("stg", s), "sh1T"], w=["pB"])
        P.act(lambda e: e.activation(out=bias_sb[:, bcol:bcol + 512], in_=pB[:], func=ACT.Copy), r=["pB"], w=["bias_sb"])

    def ln_tile(xt_ap, xkey, st, mv, rstd, nb, hb_ap, hkey, sfx):
        for i in range(2):
            P.dve(lambda e, i=i: e.bn_stats(out=st[:, i, :], in_=xt_ap[:, i * 512:(i + 1) * 512]), r=[xkey], w=[("st", sfx, i)])
        P.dve(lambda e: e.bn_aggr(out=mv[:], in_=st[:].rearrange("p a b -> p (a b)")), r=[("st", sfx, 0), ("st", sfx, 1)], w=[("mv", sfx)])
        P.act(lambda e: e.activation(out=rstd[:], in_=mv[:, 1:2], func=ACT.Sqrt, bias=eps_t[:], scale=1.0), r=[("mv", sfx), "eps_t"], w=[("rstd", sfx)])
        P.dve(lambda e: e.reciprocal(out=rstd[:], in_=rstd[:]), r=[("rstd", sfx)], w=[("rstd", sfx)])
        P.dve(lambda e: e.scalar_tensor_tensor(out=nb[:], in0=mv[:, 0:1], scalar=-1.0, in1=rstd[:], op0=ALU.mult, op1=ALU.mult),
              r=[("mv", sfx), ("rstd", sfx)], w=[("nb", sfx)])
        P.act(lambda e: e.activation(out=hb_ap, in_=xt_ap, func=ACT.Identity, bias=nb[:], scale=rstd[:]), r=[xkey, ("nb", sfx), ("rstd", sfx)], w=[hkey])

    def rms_rope(eng_add, src, nh, gain_bc, rt, dst, tmp, keys_r, key_w, sfx):
        sq, ss, rk, ta, tb = tmp
        n = nh * 128
        P.dve(lambda e: e.tensor_tensor(out=sq[:, :n], in0=src[:, :n], in1=src[:, :n], op=ALU.mult), r=keys_r, w=[("sq", sfx)])
        P.dve(lambda e: e.tensor_reduce(out=ss[:, :nh], in_=sq[:, :n].rearrange("p (h d) -> p h d", d=128), axis=AX.X, op=ALU.add), r=[("sq", sfx)], w=[("ss", sfx)])
        P.act(lambda e: e.activation(out=rk[:, :nh], in_=ss[:, :nh], func=ACT.Sqrt, bias=eps_t[:], scale=1.0 / 128.0), r=[("ss", sfx), "eps_t"], w=[("rk", sfx)])
        P.dve(lambda e: e.reciprocal(out=rk[:, :nh], in_=rk[:, :nh]), r=[("rk", sfx)], w=[("rk", sfx)])
        P.dve(lambda e: e.tensor_tensor(out=src[:, :n].rearrange("p (h d) -> p h d", d=128), in0=src[:, :n].rearrange("p (h d) -> p h d", d=128),
                                        in1=fv(rk[:], 0, [[1, nh], [0, 128]]), op=ALU.mult), r=keys_r + [("rk", sfx)], w=keys_r)
        eng_add(lambda e: e.tensor_tensor(out=src[:, :n].rearrange("p (h d) -> p h d", d=128), in0=src[:, :n].rearrange("p (h d) -> p h d", d=128),
                                          in1=fv(gain_bc[:], 0, [[0, nh], [1, 128]]), op=ALU.mult), r=keys_r, w=keys_r)
        x1 = fv(src[:], 0, [[128, nh], [64, 2], [1, 32]])
        x2 = fv(src[:], 32, [[128, nh], [64, 2], [1, 32]])
        cs = fv(rt[:], 0, [[0, nh], [32, 2], [1, 32]])
        sn = fv(rt[:], 64, [[0, nh], [32, 2], [1, 32]])
        o1 = fv(dst[:], 0, [[128, nh], [64, 2], [1, 32]])
        o2 = fv(dst[:], 32, [[128, nh], [64, 2], [1, 32]])
        tav = fv(ta[:], 0, [[64, nh], [32, 2], [1, 32]])
        tbv = fv(tb[:], 0, [[64, nh], [32, 2], [1, 32]])
        rtk = keys_r[-1:] if False else []
        eng_add(lambda e: e.tensor_tensor(out=tav, in0=x1, in1=cs, op=ALU.mult), r=keys_r + [("rt", sfx)], w=[("ta", sfx)])
        eng_add(lambda e: e.tensor_tensor(out=tbv, in0=x2, in1=sn, op=ALU.mult), r=keys_r + [("rt", sfx)], w=[("tb", sfx)])
        eng_add(lambda e: e.tensor_tensor(out=o1, in0=tav, in1=tbv, op=ALU.subtract), r=[("ta", sfx), ("tb", sfx)], w=[key_w])
        eng_add(lambda e: e.tensor_tensor(out=tav, in0=x1, in1=sn, op=ALU.mult), r=keys_r + [("rt", sfx)], w=[("ta", sfx)])
        eng_add(lambda e: e.tensor_tensor(out=tbv, in0=x2, in1=cs, op=ALU.mult), r=keys_r + [("rt", sfx)], w=[("tb", sfx)])
        eng_add(lambda e: e.tensor_tensor(out=o2, in0=tav, in1=tbv, op=ALU.add), r=[("ta", sfx), ("tb", sfx)], w=[key_w])

    esA = ExitStack()
    u8 = SB(esA, "u8", [128, 32, NJF], BF16)
    with ExitStack() as es:
        stage = [SB(es, "stage%d" % i, [128, 8, 512], F32) for i in range(2)]
        Wkv = [SB(es, "Wkv%d" % r, [128, 8, 512], BF16) for r in range(2)]
        Wu = [SB(es, "Wu%d" % r, [128, 8, 512], BF16) for r in range(2)]
        xt = [SB(es, "xt%d" % i, [128, D], F32) for i in range(3)]
        rt = [SB(es, "rt%d" % i, [128, 128], F32) for i in range(2)]
        hb = [SB(es, "hb%d" % i, [128, D], BF16) for i in range(2)]
        hT = [SB(es, "hT%d" % i, [128, 8, 128], BF16) for i in range(2)]
        st = SB(es, "st", [128, 2, 6], F32)
        mv = SB(es, "mv", [128, 2], F32)
        rstd = SB(es, "rstd", [128, 1], F32)
        nbt = SB(es, "nbt", [128, 1], F32)
        k_sb = SB(es, "k_sb", [128, 256], F32)
        v_bf = [SB(es, "v_bf%d" % i, [128, 256], BF16) for i in range(2)]
        kr = SB(es, "kr", [128, 256], BF16)
        kT_sb = [SB(es, "kT_sb%d" % i, [128, 2, 128], BF16) for i in range(2)]
        tmp = (SB(es, "sq", [128, 256], F32), SB(es, "ss", [128, 2], F32), SB(es, "rk", [128, 2], F32),
               SB(es, "ta", [128, 128], F32), SB(es, "tb", [128, 128], F32))
        u_bf = SB(es, "u_bf", [128, 512], BF16)
        Um = [SB(es, "Um%d" % i, [128, 32, 128], BF16) for i in range(2)]
        pB = PS(es, "pB", [1, 512], F32)
        pT = [PS(es, "pT%d" % i, [128, 8, 128], BF16) for i in range(2)]
        pKV = PS(es, "pKV", [128, 512], F32)
        pU = PS(es, "pU", [128, 512], F32)
        pKT = PS(es, "pKT", [128, 2, 128], BF16)
        pU8 = PS(es, "pU8", [128, 32, 16], F32)
        prep_wblock(stage, pB, 0, Wkv[1], 1536, 1, 4096, ("Wkv", 1))
        prep_wblock(stage, pB, 1, Wu[1], 0, 1, 4608, ("Wu", 1))
        prep_wblock(stage, pB, 0, Wkv[0], 1536, 0, 1536, ("Wkv", 0))
        prep_wblock(stage, pB, 1, Wu[0], 0, 0, 0, ("Wu", 0))
        for tt in range(int(os.environ.get('K_NT', NTF))):
            r = 1 if tt < 2 else 0
            s3, s2 = tt % 3, tt % 2
            t0 = tt * 128
            bkv, bu = (4096, 4608) if r == 1 else (1536, 0)
            P.dma(lambda e, s3=s3, t0=t0: e.dma_start(out=xt[s3][:], in_=xf[t0:t0 + 128, :]), key=("xt", s3), w=[("xt", s3)])
            P.dma(lambda e, s2=s2, t0=t0: e.dma_start(out=rt[s2][:], in_=rope_f[t0:t0 + 128, :]), key=("rtd", s2), w=[("rt", "A")] if False else [("rtA", s2)])
            ln_tile(xt[s3][:], ("xt", s3), st, mv, rstd, nbt, hb[s2][:], ("hb", s2), "A")
            for kc in range(8):
                P.pe(lambda e, kc=kc, s2=s2: e.transpose(out=pT[s2][:, kc, :], in_=hb[s2][:, kc * 128:(kc + 1) * 128], identity=ident_b[:]),
                     r=[("hb", s2), "ident_b"], w=[("pT", s2)])
            P.dve(lambda e, s2=s2: e.tensor_copy(out=hT[s2][:], in_=pT[s2][:]), r=[("pT", s2)], w=[("hT", s2)])
            for kc in range(8):
                P.pe(lambda e, kc=kc, s2=s2, r=r: e.matmul(out=pKV[:], lhsT=hT[s2][:, kc, :], rhs=Wkv[r][:, kc, :], start=(kc == 0), stop=False),
                     r=[("hT", s2), ("Wkv", r)], w=["pKV"])
            P.pe(lambda e, bkv=bkv: e.matmul(out=pKV[:], lhsT=ones_b[0:1, :], rhs=bias_sb[0:1, bkv:bkv + 512], start=False, stop=True),
                 r=["ones_b", "bias_sb"], w=["pKV"])
            for kc in range(8):
                P.pe(lambda e, kc=kc, s2=s2, r=r: e.matmul(out=pU[:], lhsT=hT[s2][:, kc, :], rhs=Wu[r][:, kc, :], start=(kc == 0), stop=False),
                     r=[("hT", s2), ("Wu", r)], w=["pU"])
            P.pe(lambda e, bu=bu: e.matmul(out=pU[:], lhsT=ones_b[0:1, :], rhs=bias_sb[0:1, bu:bu + 512], start=False, stop=True),
                 r=["ones_b", "bias_sb"], w=["pU"])
            KP = int(os.environ.get('K_PATH', '15'))
            if KP & 1:
                P.act(lambda e: e.activation(out=k_sb[:], in_=pKV[:, 0:256], func=ACT.Copy), r=["pKV"], w=["k_sb"])
                if KP & 8:
                    rms_rope(P.pool, k_sb, 2, gk_bc, rt[s2], kr, tmp, ["k_sb", ("rtA", s2), "gk_bc"], "kr", "A")
                else:
                    P.dve(lambda e: e.tensor_copy(out=kr[:], in_=k_sb[:]), r=["k_sb"], w=["kr"])
                for h in range(2):
                    P.pe(lambda e, h=h: e.transpose(out=pKT[:, h, :], in_=kr[:, h * 128:(h + 1) * 128], identity=ident_b[:]), r=["kr", "ident_b"], w=["pKT"])
                P.act(lambda e, s2=s2: e.activation(out=kT_sb[s2][:], in_=pKT[:], func=ACT.Copy), r=["pKT"], w=[("kT_sb", s2)])
                P.dma(lambda e, s2=s2, t0=t0: e.dma_start(out=kT_d[:, :, t0:t0 + 128].rearrange("h p t -> p h t"), in_=kT_sb[s2][:]),
                      key=("kTd", s2), r=[("kT_sb", s2)], w=["kT_d"])
            if KP & 2:
                P.dve(lambda e, s2=s2: e.tensor_copy(out=v_bf[s2][:], in_=pKV[:, 256:512]), r=["pKV"], w=[("v_bf", s2)])
                P.dma(lambda e, s2=s2, t0=t0: e.dma_start(out=v_d[t0:t0 + 128, :], in_=v_bf[s2][:]), key=("vd", s2), r=[("v_bf", s2)], w=["v_d"])
            if KP & 4:
                P.act(lambda e: e.activation(out=u_bf[:], in_=pU[:], func=ACT.Copy), r=["pU"], w=["u_bf"])
                P.dve(lambda e, s2=s2: e.tensor_tensor(out=Um[s2][:].rearrange("p g (s c) -> p g s c", c=16), in0=fv(u_bf[:], 0, [[16, 32], [0, 8], [1, 16]]),
                                                       in1=fv(mask8[:], 0, [[0, 32], [1, 8], [0, 16]]), op=ALU.mult), r=["u_bf", "mask8"], w=[("Um", s2)])
                for g in range(32):
                    P.pe(lambda e, g=g, s2=s2: e.matmul(out=pU8[:, g, :], lhsT=Um[s2][:, g, :], rhs=sel16[:], start=True, stop=True),
                         r=[("Um", s2), "sel16"], w=["pU8"])
                P.act(lambda e, tt=tt: e.activation(out=u8[:, :, tt * 16:(tt + 1) * 16], in_=pU8[:], func=ACT.Copy), r=["pU8"], w=["u8"])
        dump("u8", u8[:], [128, 32, NJF], BF16, r=["u8"])
        if "kT" in dbg_names:
            o = nc.dram_tensor("dbg_kT", [2, 128, NFULL], BF16, kind="ExternalOutput").ap()
            dbg_out["kT"] = o
            P.dma(lambda e: e.dma_start(out=o, in_=kT_d), key="dbg_kT", r=["kT_d"], w=["dbg_kT"])
        if "v" in dbg_names:
            o2 = nc.dram_tensor("dbg_v", [NFULL, 256], BF16, kind="ExternalOutput").ap()
            dbg_out["v"] = o2
            P.dma(lambda e: e.dma_start(out=o2, in_=v_d), key="dbg_v", r=["v_d"], w=["dbg_v"])
        P.barrier()
    if STOP <= 1:
        esA.close()
        return finish(nc, P, ges, out, dbg_out)

    TWO_PI = 2.0 * math.pi
    MAGIC = 12582912.0
    esS = ExitStack()
    esH = ExitStack()
    esZ = ExitStack()
    S_re = SB(esH, "S_re", [128, 2, 16, NJ], F32)
    S_im = SB(esH, "S_im", [128, 2, 16, NJ], F32)
    erZ = SB(esZ, "erZ", [128, 2, 16, 64], F32)
    eiZ = SB(esZ, "eiZ", [128, 2, 16, 64], F32)
    erZ8 = SB(esS, "erZ8", [128, 2, 16, 8], F32)
    eiZ8 = SB(esS, "eiZ8", [128, 2, 16, 8], F32)
    erR = SB(esS, "erR", [128, 2, 16, 72], F32)
    eiR = SB(esS, "eiR", [128, 2, 16, 72], F32)
    bbre = SB(esS, "bbre", [128, 2, 16, 16], F32)
    bbim = SB(esS, "bbim", [128, 2, 16, 16], F32)
    ccre = SB(esS, "ccre", [128, 2, 16, 16], F32)
    ccim = SB(esS, "ccim", [128, 2, 16, 16], F32)
    A64r = SB(esS, "A64r", [128, 2, 16, 1], F32)
    A64i = SB(esS, "A64i", [128, 2, 16, 1], F32)
    ardt = SB(esS, "ardt", [128, 2, 16], F32)
    aidt = SB(esS, "aidt", [128, 2, 16], F32)
    ptmp = []
    piT = SB(esS, "piT", [128, 1], F32)

    def powtab(exps_ap, n, er_t, ei_t, tag):
        def v4(t):
            return t[:, :, :, 0:n]
        a_b = lambda t: fv(t[:], 0, [[16, 2], [1, 16], [0, n]])
        e_b = fv(exps_ap, 0, [[exps_ap.ap[1][0], 2], [0, 16], [1, n]])
        t0, t1, t2 = ptmp
        k = lambda i: ("ptmp", i)
        P.dve(lambda e: e.tensor_tensor(out=v4(t0), in0=a_b(ardt), in1=e_b, op=ALU.mult), r=["ardt", tag + "_e"], w=[k(0)])
        P.act(lambda e: e.activation(out=v4(t0), in_=v4(t0), func=ACT.Exp), r=[k(0)], w=[k(0)])
        P.dve(lambda e: e.tensor_tensor(out=v4(t1), in0=a_b(aidt), in1=e_b, op=ALU.mult), r=["aidt", tag + "_e"], w=[k(1)])
        P.dve(lambda e: e.tensor_scalar(out=v4(t2), in0=v4(t1), scalar1=MAGIC, scalar2=None, op0=ALU.add), r=[k(1)], w=[k(2)])
        P.dve(lambda e: e.tensor_scalar(out=v4(t2), in0=v4(t2), scalar1=MAGIC, scalar2=None, op0=ALU.subtract), r=[k(2)], w=[k(2)])
        P.dve(lambda e: e.tensor_tensor(out=v4(t2), in0=v4(t1), in1=v4(t2), op=ALU.subtract), r=[k(1), k(2)], w=[k(2)])
        P.act(lambda e: e.activation(out=v4(t2), in_=v4(t2), func=ACT.Sin, scale=TWO_PI), r=[k(2)], w=[k(2)])
        P.dve(lambda e: e.tensor_tensor(out=v4(ei_t), in0=v4(t0), in1=v4(t2), op=ALU.mult), r=[k(0), k(2)], w=[tag + "_ei"])
        P.dve(lambda e: e.tensor_scalar(out=v4(t1), in0=v4(t1), scalar1=0.25, scalar2=None, op0=ALU.add), r=[k(1)], w=[k(1)])
        P.dve(lambda e: e.tensor_scalar(out=v4(t2), in0=v4(t1), scalar1=MAGIC, scalar2=None, op0=ALU.add), r=[k(1)], w=[k(2)])
        P.dve(lambda e: e.tensor_scalar(out=v4(t2), in0=v4(t2), scalar1=MAGIC, scalar2=None, op0=ALU.subtract), r=[k(2)], w=[k(2)])
        P.dve(lambda e: e.tensor_tensor(out=v4(t2), in0=v4(t1), in1=v4(t2), op=ALU.subtract), r=[k(1), k(2)], w=[k(2)])
        P.act(lambda e: e.activation(out=v4(t2), in_=v4(t2), func=ACT.Sin, scale=TWO_PI), r=[k(2)], w=[k(2)])
        P.dve(lambda e: e.tensor_tensor(out=v4(er_t), in0=v4(t0), in1=v4(t2), op=ALU.mult), r=[k(0), k(2)], w=[tag + "_er"])

    with ExitStack() as es:
        ptmp.extend([SB(es, "ptmp%d" % i, [128, 2, 16, 72], F32) for i in range(3)])
        a_sb = SB(es, "a_sb", [128, 2, 2, 16], F32)
        ldt_sb = SB(es, "ldt_sb", [128, 2, 16], F32)
        b_sb = SB(es, "b_sb", [128, 2, 2, 16, 16], F32)
        c_sb = SB(es, "c_sb", [128, 2, 2, 16, 16], F32)
        eZ_sb = SB(es, "eZ_sb", [128, 2, 64], F32)
        eR_sb = SB(es, "eR_sb", [128, 2, 72], F32)
        e1_sb = SB(es, "e1_sb", [128, 2, 1], F32)
        e64_sb = SB(es, "e64_sb", [128, 2, 1], F32)
        abr = SB(es, "abr", [128, 2, 16, 1], F32)
        abi = SB(es, "abi", [128, 2, 16, 1], F32)
        dsc = [SB(es, "dsc%d" % i, [128, 2, 16], F32) for i in range(5)]
        bt = [SB(es, "bt%d" % i, [128, 2, 16, 16], F32) for i in range(2)]
        P.dma(lambda e: e.dma_start(out=a_sb[:], in_=s5_a), key="a_sb", w=["a_sb"])
        P.dma(lambda e: e.dma_start(out=ldt_sb[:], in_=s5_ldt), key="ldt_sb", w=["ldt_sb"])
        P.dma(lambda e: e.dma_start(out=b_sb[:], in_=s5_b), key="b_sb", w=["b_sb"])
        P.dma(lambda e: e.dma_start(out=c_sb[:], in_=s5_c), key="c_sb", w=["c_sb"])
        P.dma(lambda e: e.dma_start(out=eZ_sb[:], in_=cst_eZ), key="eZ_sb", w=["Z_e"])
        P.dma(lambda e: e.dma_start(out=eR_sb[:], in_=cst_eR), key="eR_sb", w=["R_e"])
        P.dve(lambda e: e.memset(e1_sb[:], 1.0), w=["ab_e"])
        P.dve(lambda e: e.memset(e64_sb[:], 64.0), w=["A64_e"])
        P.act(lambda e: e.activation(out=ldt_sb[:], in_=ldt_sb[:], func=ACT.Exp), r=["ldt_sb"], w=["ldt_sb"])
        P.dve(lambda e: e.tensor_tensor(out=ardt[:], in0=a_sb[:, 0], in1=ldt_sb[:], op=ALU.mult), r=["a_sb", "ldt_sb"], w=["ardt"])
        P.dve(lambda e: e.scalar_tensor_tensor(out=aidt[:], in0=a_sb[:, 1], scalar=1.0 / TWO_PI, in1=ldt_sb[:], op0=ALU.mult, op1=ALU.mult),
              r=["a_sb", "ldt_sb"], w=["aidt"])
        powtab(e1_sb[:], 1, abr, abi, "ab")
        powtab(e64_sb[:], 1, A64r, A64i, "A64")
        powtab(eZ_sb[:], 64, erZ, eiZ, "Z")
        powtab(eR_sb[:], 72, erR, eiR, "R")
        for (src_t, dst_t, kk) in ((erZ, erZ8, "Z_er"), (eiZ, eiZ8, "Z_ei")):
            P.dve(lambda e, src_t=src_t, dst_t=dst_t: e.tensor_copy(out=dst_t[:, 0], in_=src_t[:, 0, :, 56:64]), r=[kk], w=[kk + "8"])
            P.dve(lambda e, src_t=src_t, dst_t=dst_t: e.tensor_copy(out=dst_t[:, 1], in_=src_t[:, 1, :, 0:8]), r=[kk], w=[kk + "8"])
        are, aim = a_sb[:, 0], a_sb[:, 1]
        d0, d1, d2, d3, d4 = [t[:] for t in dsc]
        ab_r = abr[:].rearrange("p d q o -> p d (q o)")
        ab_i = abi[:].rearrange("p d q o -> p d (q o)")
        kd = lambda i: ("dsc", i)
        P.dve(lambda e: e.tensor_tensor(out=d0, in0=are, in1=are, op=ALU.mult), r=["a_sb"], w=[kd(0)])
        P.dve(lambda e: e.tensor_tensor(out=d1, in0=aim, in1=aim, op=ALU.mult), r=["a_sb"], w=[kd(1)])
        P.dve(lambda e: e.tensor_tensor(out=d0, in0=d0, in1=d1, op=ALU.add), r=[kd(0), kd(1)], w=[kd(0)])
        P.dve(lambda e: e.reciprocal(out=d0, in_=d0), r=[kd(0)], w=[kd(0)])
        P.dve(lambda e: e.tensor_scalar(out=d1, in0=ab_r, scalar1=-1.0, scalar2=None, op0=ALU.add), r=["ab_er"], w=[kd(1)])
        P.dve(lambda e: e.tensor_tensor(out=d2, in0=d1, in1=are, op=ALU.mult), r=[kd(1), "a_sb"], w=[kd(2)])
        P.dve(lambda e: e.tensor_tensor(out=d3, in0=ab_i, in1=aim, op=ALU.mult), r=["ab_ei", "a_sb"], w=[kd(3)])
        P.dve(lambda e: e.tensor_tensor(out=d2, in0=d2, in1=d3, op=ALU.add), r=[kd(2), kd(3)], w=[kd(2)])
        P.dve(lambda e: e.tensor_tensor(out=d2, in0=d2, in1=d0, op=ALU.mult), r=[kd(2), kd(0)], w=[kd(2)])
        P.dve(lambda e: e.tensor_tensor(out=d3, in0=ab_i, in1=are, op=ALU.mult), r=["ab_ei", "a_sb"], w=[kd(3)])
        P.dve(lambda e: e.tensor_tensor(out=d4, in0=d1, in1=aim, op=ALU.mult), r=[kd(1), "a_sb"], w=[kd(4)])
        P.dve(lambda e: e.tensor_tensor(out=d3, in0=d3, in1=d4, op=ALU.subtract), r=[kd(3), kd(4)], w=[kd(3)])
        P.dve(lambda e: e.tensor_tensor(out=d3, in0=d3, in1=d0, op=ALU.mult), r=[kd(3), kd(0)], w=[kd(3)])
        rr_b = fv(dsc[2][:], 0, [[16, 2], [1, 16], [0, 16]])
        ri_b = fv(dsc[3][:], 0, [[16, 2], [1, 16], [0, 16]])
        bre, bim = b_sb[:, 0], b_sb[:, 1]
        P.dve(lambda e: e.tensor_tensor(out=bt[0][:], in0=bre, in1=rr_b, op=ALU.mult), r=["b_sb", kd(2)], w=["bt0"])
        P.dve(lambda e: e.tensor_tensor(out=bt[1][:], in0=bim, in1=ri_b, op=ALU.mult), r=["b_sb", kd(3)], w=["bt1"])
        P.dve(lambda e: e.tensor_tensor(out=bbre[:], in0=bt[0][:], in1=bt[1][:], op=ALU.subtract), r=["bt0", "bt1"], w=["bbre"])
        P.dve(lambda e: e.tensor_tensor(out=bt[0][:], in0=bim, in1=rr_b, op=ALU.mult), r=["b_sb", kd(2)], w=["bt0"])
        P.dve(lambda e: e.tensor_tensor(out=bt[1][:], in0=bre, in1=ri_b, op=ALU.mult), r=["b_sb", kd(3)], w=["bt1"])
        P.dve(lambda e: e.tensor_tensor(out=bbim[:], in0=bt[0][:], in1=bt[1][:], op=ALU.add), r=["bt0", "bt1"], w=["bbim"])
        P.dve(lambda e: e.tensor_copy(out=ccre[:], in_=c_sb[:, 0]), r=["c_sb"], w=["ccre"])
        P.dve(lambda e: e.tensor_copy(out=ccim[:], in_=c_sb[:, 1]), r=["c_sb"], w=["ccim"])
        P.barrier()

    def ztab(eng_add, pair, tsl, nt, bufs, tag, erZ=erZ, eiZ=eiZ):
        zre, zim, ztm = bufs
        def ev(t, d):
            return fv(t[:, d, pair, tsl[d]:tsl[d] + nt], 0, [[1, nt], [0, 16]])
        def bv(t, d):
            return fv(t[:, d, pair, :], 0, [[0, nt], [1, 16]])
        for d in range(2):
            eng_add(lambda e, d=d: e.tensor_tensor(out=zre[:, d], in0=bv(bbre, d), in1=ev(erZ, d), op=ALU.mult), r=["Z_er", "bbre"], w=[tag + "zre"])
            eng_add(lambda e, d=d: e.tensor_tensor(out=ztm[:, d], in0=bv(bbim, d), in1=ev(eiZ, d), op=ALU.mult), r=["Z_ei", "bbim"], w=[tag + "ztm"])
            eng_add(lambda e, d=d: e.tensor_tensor(out=zre[:, d], in0=zre[:, d], in1=ztm[:, d], op=ALU.subtract), r=[tag + "zre", tag + "ztm"], w=[tag + "zre"])
            eng_add(lambda e, d=d: e.tensor_tensor(out=zim[:, d], in0=bv(bbim, d), in1=ev(erZ, d), op=ALU.mult), r=["Z_er", "bbim"], w=[tag + "zim"])
            eng_add(lambda e, d=d: e.tensor_tensor(out=ztm[:, d], in0=bv(bbre, d), in1=ev(eiZ, d), op=ALU.mult), r=["Z_ei", "bbre"], w=[tag + "ztm"])
            eng_add(lambda e, d=d: e.tensor_tensor(out=zim[:, d], in0=zim[:, d], in1=ztm[:, d], op=ALU.add), r=[tag + "zim", tag + "ztm"], w=[tag + "zim"])

    with ExitStack() as es:
        Wsum = [SB(es, "Wsum%d" % i, [128, 2, 8, 2, 128], BF16) for i in range(2)]
        zb = [[SB(es, "zb%d_%d" % (i, j), [128, 2, 64, 16], F32) for j in range(3)] for i in range(1)]
        pW = [PS(es, "pW%d" % i, [128, 4, 128], F32) for i in range(2)]
        pSr = PS(es, "pSr", [128, 2, NJ], F32)
        pSi = PS(es, "pSi", [128, 2, NJ], F32)
        ci = 0
        for pair in range(16):
            sl = int(os.environ.get('K_SL', pair % 2))
            ea = P.pool
            ztab(ea, pair, (0, 0), 64, zb[0], "z0")
            zre, zim, _ = zb[0]
            for d in range(2):
                for reim in range(2):
                    zt = zre if reim == 0 else zim
                    for mq in range(2):
                        pw = pW[ci % 2]
                        for j in range(4):
                            m_ = mq * 4 + j
                            P.pe(lambda e, zt=zt, d=d, m_=m_, pw=pw, j=j: e.transpose(out=pw[:, j, :], in_=zt[:, d, m_ * 8:(m_ + 1) * 8, :].rearrange("p s c -> p (s c)"), identity=ident_f[:]),
                                 r=["z0z%s" % ("re" if reim == 0 else "im"), "ident_f"], w=[("pW", ci % 2)])
                        dst = Wsum[sl][:, d, mq * 4:(mq + 1) * 4, reim, :]
                        if ci % 2 == 0:
                            P.act(lambda e, dst=dst, pw=pw: e.activation(out=dst, in_=pw[:], func=ACT.Copy), r=[("pW", ci % 2)], w=[("Wsum", sl)])
                        else:
                            P.dve(lambda e, dst=dst, pw=pw: e.tensor_copy(out=dst, in_=pw[:]), r=[("pW", ci % 2)], w=[("Wsum", sl)])
                        ci += 1
            for d in range(2):
                for gh in range(2):
                    g = 2 * pair + gh
                    for reim, pS_ in ((0, pSr), (1, pSi)):
                        for m_ in range(8):
                            P.pe(lambda e, d=d, gh=gh, g=g, reim=reim, pS_=pS_, m_=m_, sl=sl: e.matmul(
                                out=pS_[64 * gh:64 * gh + 64, d, :], lhsT=Wsum[sl][:, d, m_, reim, 64 * gh:64 * gh + 64],
                                rhs=fv(u8[:, g, :], m_, [[8, NJ]]), start=(m_ == 0), stop=(m_ == 7)),
                                r=[("Wsum", sl), "u8"], w=["pSr" if reim == 0 else "pSi"])
            P.act(lambda e, pair=pair: e.activation(out=S_re[:, :, pair, :], in_=pSr[:], func=ACT.Copy), r=["pSr"], w=["S_re"])
            P.dve(lambda e, pair=pair: e.tensor_copy(out=S_im[:, :, pair, :], in_=pSi[:]), r=["pSi"], w=["S_im"])
        P.barrier()
    esA.close()
    esZ.close()

    with ExitStack() as es:
        sct = [[SB(es, "sct%d_%d" % (d, i), [128, 16], F32) for i in range(4)] for d in range(2)]
        orders = [[(J, J - 1 if J > 0 else None) for J in range(NJ)],
                  [(3, None), (2, 3), (1, 2), (0, 1), (131, 0)] + [(J, J + 1) for J in range(130, 3, -1)]]
        for d in range(2):
            ea = P.dve if d == 0 else P.pool
            Ar = A64r[:, d, :, 0]
            Ai = A64i[:, d, :, 0]
            t = [x[:] for x in sct[d]]
            kt = lambda i: ("sct", d, i)
            kr_, ki_ = ("Hre", d), ("Him", d)
            for (J, Jp) in orders[d]:
                if Jp is None:
                    continue
                hr_p, hi_p = S_re[:, d, :, Jp], S_im[:, d, :, Jp]
                hr, hi = S_re[:, d, :, J], S_im[:, d, :, J]
                ea(lambda e, t=t, hr_p=hr_p, Ar=Ar: e.tensor_tensor(out=t[0], in0=Ar, in1=hr_p, op=ALU.mult), r=["A64_er", kr_, "S_re"], w=[kt(0)])
                ea(lambda e, t=t, hi_p=hi_p, Ai=Ai: e.tensor_tensor(out=t[1], in0=Ai, in1=hi_p, op=ALU.mult), r=["A64_ei", ki_, "S_im"], w=[kt(1)])
                ea(lambda e, t=t: e.tensor_tensor(out=t[0], in0=t[0], in1=t[1], op=ALU.subtract), r=[kt(0), kt(1)], w=[kt(0)])
                ea(lambda e, t=t, hi_p=hi_p, Ar=Ar: e.tensor_tensor(out=t[2], in0=Ar, in1=hi_p, op=ALU.mult), r=["A64_er", ki_, "S_im"], w=[kt(2)])
                ea(lambda e, t=t, hr_p=hr_p, Ai=Ai: e.tensor_tensor(out=t[3], in0=Ai, in1=hr_p, op=ALU.mult), r=["A64_ei", kr_, "S_re"], w=[kt(3)])
                ea(lambda e, t=t: e.tensor_tensor(out=t[2], in0=t[2], in1=t[3], op=ALU.add), r=[kt(2), kt(3)], w=[kt(2)])
                ea(lambda e, t=t, hr=hr: e.tensor_tensor(out=hr, in0=hr, in1=t[0], op=ALU.add), r=[kt(0), kr_], w=[kr_])
                ea(lambda e, t=t, hi=hi: e.tensor_tensor(out=hi, in0=hi, in1=t[2], op=ALU.add), r=[kt(2), ki_], w=[ki_])
        P.barrier()
    dump("H_re", S_re[:], [128, 2, 16, NJ])
    dump("H_im", S_im[:], [128, 2, 16, NJ])
    HO = [[SB(esS, "HO%d_%d" % (d, x), [128, 16, 32], BF16) for x in range(2)] for d in range(2)]
    with ExitStack() as es:
        cm = SB(es, "cm", [128, 4], F32)
        hacc = SB(es, "hacc", [128, 16, 32], F32)
        P.dma(lambda e: e.dma_start(out=cm[:], in_=cmask), key="cm", w=["cm"])
        for d in range(2):
            for x, Sx in enumerate((S_re, S_im)):
                for r_ in range(4):
                    lo = (3 + 32 * r_) if d == 0 else (5 + 32 * r_)
                    segs = [(lo, 0, 32)] if not (d == 1 and r_ == 3) else [(lo, 0, 31), (0, 31, 1)]
                    for (a0, o0, n_) in segs:
                        src_ = Sx[:, d, :, a0:a0 + n_]
                        dst_ = hacc[:, :, o0:o0 + n_]
                        if r_ == 0:
                            P.dve(lambda e, src_=src_, dst_=dst_: e.tensor_scalar(out=dst_, in0=src_, scalar1=cm[:, 0:1], scalar2=None, op0=ALU.mult),
                                  r=["cm"], w=["hacc"])
                        else:
                            P.dve(lambda e, src_=src_, dst_=dst_, r_=r_: e.scalar_tensor_tensor(out=dst_, in0=src_, scalar=cm[:, r_:r_ + 1], in1=dst_, op0=ALU.mult, op1=ALU.add),
                                  r=["cm", "hacc"], w=["hacc"])
                P.dve(lambda e, d=d, x=x: e.tensor_copy(out=HO[d][x][:], in_=hacc[:]), r=["hacc"], w=[("HO", d, x)])
        P.barrier()
    esH.close()
    if STOP <= 2:
        esS.close()
        return finish(nc, P, ges, out, dbg_out)

    esP = ExitStack()
    hTo = SB(esP, "hTo", [128, 8, NOWN], BF16)
    qT = SB(esP, "qT", [128, 8, NOWN], BF16)
    esU = ExitStack()
    u8o = SB(esU, "u8o", [128, 32, 256], BF16)

    def load_bc(dst, src_row, key, add_one=False):
        P.dma(lambda e: e.dma_start(out=dst[:], in_=src_row.partition_broadcast(128)), key=key, w=[key])
        if add_one:
            P.dve(lambda e: e.tensor_scalar(out=dst[:], in0=dst[:], scalar1=1.0, scalar2=None, op0=ALU.add), r=[key], w=[key])

    with ExitStack() as es:
        Wq = SB(es, "Wq", [128, 8, 1024], BF16)
        Wuo = SB(es, "Wuo", [128, 8, 512], BF16)
        sc1p_bc = SB(es, "sc1p_bc", [128, D], F32)
        sh1_bc = SB(es, "sh1_bc", [128, D], F32)
        xt_B = [SB(es, "xtB%d" % i, [128, D], F32) for i in range(2)]
        rt_B = [SB(es, "rtB%d" % i, [128, 128], F32) for i in range(2)]
        hn = SB(es, "hn", [128, D], F32)
        hb_B = [SB(es, "hbB%d" % i, [128, D], BF16) for i in range(2)]
        st_B = SB(es, "stB", [128, 2, 6], F32)
        mv_B = SB(es, "mvB", [128, 2], F32)
        rstd_B = SB(es, "rstdB", [128, 1], F32)
        nbt_B = SB(es, "nbtB", [128, 1], F32)
        q_sb = SB(es, "q_sb", [128, 1024], F32)
        qr = SB(es, "qr", [128, 1024], BF16)
        tmp_B = (SB(es, "sqB", [128, 1024], F32), SB(es, "ssB", [128, 8], F32), SB(es, "rkB", [128, 8], F32),
               SB(es, "taB", [128, 512], F32), SB(es, "tbB", [128, 512], F32))
        u_bf_B = SB(es, "u_bfB", [128, 512], BF16)
        Um_B = [SB(es, "UmB0", [128, 32, 128], BF16)] * 2
        pT_B = [PS(es, "pTB%d" % i, [128, 8, 128], BF16) for i in range(2)]
        pQ = [PS(es, "pQ%d" % i, [128, 512], F32) for i in range(2)]
        pQT = PS(es, "pQT", [128, 8, 128], BF16)
        pU_B = PS(es, "pUB", [128, 512], F32)
        pU8_B = PS(es, "pU8B", [128, 32, 16], F32)
        P.dma(lambda e: e.dma_start(out=Wq[:], in_=w_in[:, 512:1536].rearrange("(kc p) n -> p kc n", p=128)), key="Wq", w=["Wq"], eng="pool")
        P.dma(lambda e: e.dma_start(out=Wuo[:], in_=w_in[:, 0:512].rearrange("(kc p) n -> p kc n", p=128)), key="Wuo", w=["Wuo"], eng="pool")
        load_bc(sc1p_bc, mod_d[0:1, 1024:2048], "sc1p_bc", True)
        load_bc(sh1_bc, mod_d[0:1, 0:1024], "sh1_bc")
        for tt in range(NTO):
            s2 = tt % 2
            t0 = tt * 128
            P.dma(lambda e, s2=s2, t0=t0: e.dma_start(out=xt_B[s2][:], in_=xo[t0:t0 + 128, :]), key=("xtB", s2), w=[("xtB", s2)])
            P.dma(lambda e, s2=s2, t0=t0: e.dma_start(out=rt_B[s2][:], in_=rope_o[t0:t0 + 128, :]), key=("rtB", s2), w=[("rtB", s2)])
            ln_tile(xt_B[s2][:], ("xtB", s2), st_B, mv_B, rstd_B, nbt_B, hn[:], "hn", "B")
            P.dve(lambda e: e.tensor_tensor(out=hn[:], in0=hn[:], in1=sc1p_bc[:], op=ALU.mult), r=["hn", "sc1p_bc"], w=["hn"])
            P.dve(lambda e, s2=s2: e.tensor_tensor(out=hb_B[s2][:], in0=hn[:], in1=sh1_bc[:], op=ALU.add), r=["hn", "sh1_bc"], w=[("hbB", s2)])
            for kc in range(8):
                P.pe(lambda e, kc=kc, s2=s2: e.transpose(out=pT_B[s2][:, kc, :], in_=hb_B[s2][:, kc * 128:(kc + 1) * 128], identity=ident_b[:]),
                     r=[("hbB", s2), "ident_b"], w=[("pTB", s2)])
            P.act(lambda e, s2=s2, t0=t0: e.activation(out=hTo[:, :, t0:t0 + 128], in_=pT_B[s2][:], func=ACT.Copy), r=[("pTB", s2)], w=["hTo"])
            for half in range(2):
                for kc in range(8):
                    P.pe(lambda e, kc=kc, half=half, t0=t0: e.matmul(out=pQ[half][:], lhsT=hTo[:, kc, t0:t0 + 128], rhs=Wq[:, kc, half * 512:(half + 1) * 512],
                                                                     start=(kc == 0), stop=(kc == 7)), r=["hTo", "Wq"], w=[("pQ", half)])
                P.act(lambda e, half=half: e.activation(out=q_sb[:, half * 512:(half + 1) * 512], in_=pQ[half][:], func=ACT.Copy), r=[("pQ", half)], w=["q_sb"])
            for kc in range(8):
                P.pe(lambda e, kc=kc, t0=t0: e.matmul(out=pU_B[:], lhsT=hTo[:, kc, t0:t0 + 128], rhs=Wuo[:, kc, :], start=(kc == 0), stop=(kc == 7)),
                     r=["hTo", "Wuo"], w=["pUB"])
            rms_rope(P.pool, q_sb, 8, gq_bc, rt_B[s2], qr, tmp_B, ["q_sb", ("rtB", s2), "gq_bc"], "qr", "B")
            for h in range(8):
                P.pe(lambda e, h=h: e.transpose(out=pQT[:, h, :], in_=qr[:, h * 128:(h + 1) * 128], identity=ident_b[:]), r=["qr", "ident_b"], w=["pQT"])
            P.act(lambda e, t0=t0: e.activation(out=qT[:, :, t0:t0 + 128], in_=pQT[:], func=ACT.Copy), r=["pQT"], w=["qT"])
            P.act(lambda e: e.activation(out=u_bf_B[:], in_=pU_B[:], func=ACT.Copy), r=["pUB"], w=["u_bfB"])
            P.dve(lambda e, s2=s2: e.tensor_tensor(out=Um_B[s2][:].rearrange("p g (s c) -> p g s c", c=16), in0=fv(u_bf_B[:], 0, [[16, 32], [0, 8], [1, 16]]),
                                                   in1=fv(mask8[:], 0, [[0, 32], [1, 8], [0, 16]]), op=ALU.mult), r=["u_bfB", "mask8"], w=["UmB"])
            for g in range(32):
                P.pe(lambda e, g=g, s2=s2: e.matmul(out=pU8_B[:, g, :], lhsT=Um_B[s2][:, g, :], rhs=sel16[:], start=True, stop=True),
                     r=["UmB", "sel16"], w=["pU8B"])
            P.act(lambda e, tt=tt: e.activation(out=u8o[:, :, tt * 16:(tt + 1) * 16], in_=pU8_B[:], func=ACT.Copy), r=["pU8B"], w=["u8o"])
        P.barrier()
    if "qT" in dbg_names:
        dump("qT", qT[:], [128, 8, NOWN], BF16)
    if STOP <= 3:
        esU.close(); esS.close(); esP.close()
        return finish(nc, P, ges, out, dbg_out)

    gT = SB(esP, "gT", [128, 4, NOWN], BF16)
    with ExitStack() as es:
        mf_sb = SB(es, "mf_sb", [128, 128], F32)
        mb_sb = SB(es, "mb_sb", [128, 128], F32)
        dcol_sb = SB(es, "dcol_sb", [128, 32], F32)
        selg_b = SB(es, "selg_b", [128, 8, 128], BF16)
        m8c_b = SB(es, "m8c_b", [128, 8], BF16)
        z8 = [SB(es, "z8_%d" % j, [128, 2, 8, 16], F32) for j in range(3)]
        Rre_p = SB(es, "Rre_p", [128, 2, 72, 16], F32)
        Rim_p = SB(es, "Rim_p", [128, 2, 72, 16], F32)
        Rt0 = SB(es, "Rt0", [128, 1, 72, 16], F32)
        Rt1 = SB(es, "Rt1", [128, 1, 72, 16], F32)
        Rre_b = SB(es, "Rre_b", [128, 2, 1152], BF16)
        Rim_b = SB(es, "Rim_b", [128, 2, 1152], BF16)
        Tw = [SB(es, "Tw0", [128, 2, 15, 128], BF16)] * 2
        tt0 = SB(es, "tt0", [128, 128], F32)
        tt1 = SB(es, "tt1", [128, 128], F32)
        Ye = [SB(es, "Ye0", [128, 2048], BF16)] * 2
        ysb = SB(es, "ysb", [128, 1024], F32)
        yx2 = SB(es, "yx2", [128, 1024], F32)
        pTb = [PS(es, "pTb%d" % i, [128, 128], F32) for i in range(2)]
        pY8 = [PS(es, "pY8_%d" % i, [128, 8, 32], F32) for i in range(2)]
        pYT = PS(es, "pYT", [128, 2048], F32)
        P.dma(lambda e: e.dma_start(out=mf_sb[:], in_=cst_mf), key="mf_sb", w=["mf_sb"])
        P.dma(lambda e: e.dma_start(out=mb_sb[:], in_=cst_mb), key="mb_sb", w=["mb_sb"])
        P.dma(lambda e: e.dma_start(out=dcol_sb[:], in_=s5_dcol), key="dcol_sb", w=["dcol_sb"])
        P.dma(lambda e: e.dma_start(out=selg_b[:], in_=cst_selg), key="selg_b", w=["selg_b"], eng="pool")
        P.dma(lambda e: e.dma_start(out=m8c_b[:], in_=cst_mask8c), key="m8c_b", w=["m8c_b"], eng="pool")
        ecnt = 0
        for pair in range(16):
            sl = 0
            ztab(P.dve, pair, (0, 0), 8, z8, "z8", erZ=erZ8, eiZ=eiZ8)
            for d in range(2):
                eb = lambda t, d=d, pair=pair: fv(t[:, d, pair, :], 0, [[1, 72], [0, 16]])
                cb = lambda t, d=d, pair=pair: fv(t[:, d, pair, :], 0, [[0, 72], [1, 16]])
                P.pool(lambda e, d=d, eb=eb, cb=cb: e.tensor_tensor(out=Rre_p[:, d], in0=cb(ccre), in1=eb(erR), op=ALU.mult), r=["R_er", "ccre"], w=["Rre_p"])
                P.pool(lambda e, d=d, eb=eb, cb=cb: e.tensor_tensor(out=Rt0[:, 0], in0=cb(ccim), in1=eb(eiR), op=ALU.mult), r=["R_ei", "ccim"], w=["Rt0"])
                P.pool(lambda e, d=d: e.tensor_tensor(out=Rre_p[:, d], in0=Rre_p[:, d], in1=Rt0[:, 0], op=ALU.subtract), r=["Rre_p", "Rt0"], w=["Rre_p"])
                P.dve(lambda e, d=d, eb=eb, cb=cb: e.tensor_tensor(out=Rim_p[:, d], in0=cb(ccim), in1=eb(erR), op=ALU.mult), r=["R_er", "ccim"], w=["Rim_p"])
                P.dve(lambda e, d=d, eb=eb, cb=cb: e.tensor_tensor(out=Rt1[:, 0], in0=cb(ccre), in1=eb(eiR), op=ALU.mult), r=["R_ei", "ccre"], w=["Rt1"])
                P.dve(lambda e, d=d: e.scalar_tensor_tensor(out=Rim_p[:, d], in0=Rt1[:, 0], scalar=-1.0, in1=Rim_p[:, d], op0=ALU.mult, op1=ALU.subtract),
                      r=["Rim_p", "Rt1"], w=["Rim_p"])
            P.act(lambda e: e.activation(out=Rre_b[:], in_=Rre_p[:].rearrange("p d i c -> p d (i c)"), func=ACT.Copy), r=["Rre_p"], w=["Rre_b"])
            P.act(lambda e: e.activation(out=Rim_b[:], in_=Rim_p[:].rearrange("p d i c -> p d (i c)"), func=ACT.Copy), r=["Rim_p"], w=["Rim_b"])
            for gh in range(2):
                g = 2 * pair + gh
                rows = slice(64 * gh, 64 * gh + 64)
                blocks = [(0, 0), (1, 0)] + [(0, k) for k in range(1, 8)] + [(1, k) for k in range(1, 8)]
                for (d, k) in blocks:
                    pt = pTb[ecnt % 2]
                    kk = ("pTb", ecnt % 2)
                    P.pe(lambda e, pt=pt, d=d, k=k, rows=rows: e.matmul(out=pt[:], lhsT=z8[0][rows, d].rearrange("p s c -> p (s c)"),
                                                                        rhs=Rre_p[rows, d, 8 * k:8 * k + 8, :].rearrange("p s c -> p (s c)"), start=True, stop=False),
                         r=["z8zre", "Rre_p"], w=[kk])
                    P.pe(lambda e, pt=pt, d=d, k=k, rows=rows: e.matmul(out=pt[:], lhsT=z8[1][rows, d].rearrange("p s c -> p (s c)"),
                                                                        rhs=Rim_p[rows, d, 8 * k:8 * k + 8, :].rearrange("p s c -> p (s c)"), start=False, stop=True),
                         r=["z8zim", "Rim_p"], w=[kk])
                    if k == 0 and d == 0:
                        P.dve(lambda e, pt=pt: e.tensor_tensor(out=tt0[:], in0=pt[:], in1=mf_sb[:], op=ALU.mult), r=[kk, "mf_sb"], w=["tt0"])
                    elif k == 0 and d == 1:
                        P.dve(lambda e, pt=pt: e.tensor_tensor(out=tt1[:], in0=pt[:], in1=mb_sb[:], op=ALU.mult), r=[kk, "mb_sb"], w=["tt1"])
                        P.dve(lambda e: e.tensor_tensor(out=tt0[:], in0=tt0[:], in1=tt1[:], op=ALU.add), r=["tt0", "tt1"], w=["tt0"])
                        P.dve(lambda e, g=g, gh=gh, sl=sl: e.scalar_tensor_tensor(out=Tw[sl][:, gh, 7, :], in0=ident_f[:], scalar=dcol_sb[:, g:g + 1], in1=tt0[:],
                                                                                 op0=ALU.mult, op1=ALU.add), r=["tt0", "dcol_sb", "ident_f"], w=[("Tw", sl)])
                    else:
                        idx = 7 + k if d == 0 else 7 - k
                        if ecnt % 2 == 0:
                            P.act(lambda e, pt=pt, gh=gh, sl=sl, idx=idx: e.activation(out=Tw[sl][:, gh, idx, :], in_=pt[:], func=ACT.Copy), r=[kk], w=[("Tw", sl)])
                        else:
                            P.dve(lambda e, pt=pt, gh=gh, sl=sl, idx=idx: e.tensor_copy(out=Tw[sl][:, gh, idx, :], in_=pt[:]), r=[kk], w=[("Tw", sl)])
                    ecnt += 1
            for gh in range(2):
                g = 2 * pair + gh
                rows = slice(64 * gh, 64 * gh + 64)
                py = pY8[g % 2]
                ky = ("pY8", g % 2)
                for m in range(8):
                    for m_ in range(8):
                        P.pe(lambda e, py=py, m=m, m_=m_, gh=gh, g=g, sl=sl: e.matmul(out=py[:, m, :], lhsT=Tw[sl][:, gh, 7 + m - m_, :],
                                                                                     rhs=fv(u8o[:, g, :], m_, [[8, 32]]), start=(m_ == 0), stop=False),
                             r=[("Tw", sl), "u8o"], w=[ky])
                    fo = 8 * (m + 1) * 16
                    bo = 8 * (8 - m) * 16
                    P.pe(lambda e, py=py, m=m, rows=rows, fo=fo, pair=pair: e.matmul(out=py[:, m, :], lhsT=Rre_b[rows, 0, fo:fo + 128], rhs=HO[0][0][rows, pair, :], start=False, stop=False),
                         r=["Rre_b", ("HO", 0, 0)], w=[ky])
                    P.pe(lambda e, py=py, m=m, rows=rows, fo=fo, pair=pair: e.matmul(out=py[:, m, :], lhsT=Rim_b[rows, 0, fo:fo + 128], rhs=HO[0][1][rows, pair, :], start=False, stop=False),
                         r=["Rim_b", ("HO", 0, 1)], w=[ky])
                    P.pe(lambda e, py=py, m=m, rows=rows, bo=bo, pair=pair: e.matmul(out=py[:, m, :], lhsT=Rre_b[rows, 1, bo:bo + 128], rhs=HO[1][0][rows, pair, :], start=False, stop=False),
                         r=["Rre_b", ("HO", 1, 0)], w=[ky])
                    P.pe(lambda e, py=py, m=m, rows=rows, bo=bo, pair=pair: e.matmul(out=py[:, m, :], lhsT=Rim_b[rows, 1, bo:bo + 128], rhs=HO[1][1][rows, pair, :], start=False, stop=True),
                         r=["Rim_b", ("HO", 1, 1)], w=[ky])
                ye = Ye[g % 2]
                P.dve(lambda e, py=py, ye=ye: e.tensor_tensor(out=ye[:].rearrange("p (j m s) -> p j m s", m=8, s=8), in0=fv(py[:], 0, [[1, 32], [32, 8], [0, 8]]),
                                                              in1=fv(m8c_b[:], 0, [[0, 32], [0, 8], [1, 8]]), op=ALU.mult), r=[ky, "m8c_b"], w=["Ye"])
                for c4 in range(4):
                    P.pe(lambda e, ye=ye, g=g, c4=c4: e.matmul(out=pYT[:, c4 * 512:(c4 + 1) * 512], lhsT=selg_b[:, g % 8, :], rhs=ye[:, c4 * 512:(c4 + 1) * 512],
                                                              start=(g % 8 == 0), stop=(g % 8 == 7)), r=["Ye", "selg_b"], w=["pYT"])
            if pair % 4 == 3:
                tile_ = pair // 4
                for hf in range(2):
                    cs = slice(hf * 1024, (hf + 1) * 1024)
                    P.act(lambda e, cs=cs: e.activation(out=ysb[:], in_=pYT[:, cs], func=ACT.Copy), r=["pYT"], w=["ysb"])
                    P.dve(lambda e: e.tensor_tensor(out=yx2[:], in0=ysb[:], in1=ysb[:], op=ALU.mult), r=["ysb"], w=["yx2"])
                    P.dve(lambda e: e.tensor_scalar(out=yx2[:], in0=yx2[:], scalar1=0.044715, scalar2=1.0, op0=ALU.mult, op1=ALU.add), r=["yx2"], w=["yx2"])
                    P.dve(lambda e: e.tensor_tensor(out=yx2[:], in0=yx2[:], in1=ysb[:], op=ALU.mult), r=["yx2", "ysb"], w=["yx2"])
                    P.act(lambda e: e.activation(out=yx2[:], in_=yx2[:], func=ACT.Sigmoid, scale=1.5957691216057308), r=["yx2"], w=["yx2"])
                    P.dve(lambda e, tile_=tile_, cs=cs: e.tensor_tensor(out=gT[:, tile_, cs], in0=ysb[:], in1=yx2[:], op=ALU.mult), r=["ysb", "yx2"], w=["gT"])
        P.barrier()
    esU.close()
    esS.close()
    if "gT" in dbg_names:
        dump("gT", gT[:], [128, 4, NOWN], BF16)
    if STOP <= 4:
        esP.close()
        return finish(nc, P, ges, out, dbg_out)

    oT = qT
    NKC = NFULL // 128
    SCALE = 128.0 ** -0.5
    with ExitStack() as es:
        kT_all = SB(es, "kT_all", [128, 2, NFULL], BF16)
        V_aug = SB(es, "V_aug", [128, NKC, 2, 132], BF16)
        pTt = [SB(es, "pTt%d" % i, [128, 512], BF16) for i in range(3)]
        rden = SB(es, "rden", [128, 4], F32)
        on_b = SB(es, "on_b", [128, 4, 128], BF16)
        pS = [PS(es, "pS%d" % i, [128, 512], F32) for i in range(2)]
        pO = [PS(es, "pO%d" % i, [128, 512], F32) for i in range(4)]
        pOT = PS(es, "pOT", [128, 4, 128], BF16)
        P.dve(lambda e: e.memset(V_aug[:], 1.0), w=["V_aug"])
        for h2 in range(2):
            P.dma(lambda e, h2=h2: e.dma_start(out=kT_all[:, h2, :], in_=kT_d[h2]), key=("kT_all", h2), r=["kT_d"], w=["kT_all"])
            P.dma(lambda e, h2=h2: e.dma_start(out=V_aug[:, :, h2, 0:128], in_=v_d[:, h2 * 128:(h2 + 1) * 128].rearrange("(c p) d -> p c d", p=128)),
                  key=("V_aug", h2), r=["v_d", "V_aug"], w=["V_aug"])
        iters = [(h, qc, kc) for h in range(8) for qc in range(4) for kc in range(NKC)]

        def emit_S(i):
            h, qc, kc = iters[i]
            kvh = h // 4
            s2 = i % 2
            qs = slice(qc * 512, (qc + 1) * 512)
            P.pe(lambda e, s2=s2, kvh=kvh, kc=kc, h=h, qs=qs: e.matmul(out=pS[s2][:], lhsT=kT_all[:, kvh, kc * 128:(kc + 1) * 128], rhs=qT[:, h, qs], start=True, stop=True),
                 r=["kT_all", ("qTc", h, qc)], w=[("pS", s2)])

        emit_S(0)
        for i, (h, qc, kc) in enumerate(iters):
            kvh = h // 4
            s2, s3 = i % 2, i % 3
            qs = slice(qc * 512, (qc + 1) * 512)
            if i + 1 < len(iters):
                emit_S(i + 1)
            P.act(lambda e, s2=s2, s3=s3: e.activation(out=pTt[s3][:], in_=pS[s2][:], func=ACT.Exp, bias=negC[:], scale=SCALE),
                  r=[("pS", s2), "negC"], w=[("pTt", s3)])
            for qi in range(4):
                P.pe(lambda e, s3=s3, qi=qi, kc=kc, kvh=kvh: e.matmul(out=pO[qi][:, 0:129], lhsT=pTt[s3][:, qi * 128:(qi + 1) * 128], rhs=V_aug[:, kc, kvh, 0:129],
                                                                     start=(kc == 0), stop=(kc == NKC - 1)), r=[("pTt", s3), "V_aug"], w=[("pO", qi)])
            if kc == NKC - 1:
                for qi in range(4):
                    P.dve(lambda e, qi=qi: e.reciprocal(out=rden[:, qi:qi + 1], in_=pO[qi][:, 128:129]), r=[("pO", qi)], w=[("rden", qi)])
                    P.dve(lambda e, qi=qi: e.tensor_scalar(out=on_b[:, qi, :], in0=pO[qi][:, 0:128], scalar1=rden[:, qi:qi + 1], scalar2=None, op0=ALU.mult),
                          r=[("pO", qi), ("rden", qi)], w=[("on_b", qi)])
                    P.pe(lambda e, qi=qi: e.transpose(out=pOT[:, qi, :], in_=on_b[:, qi, :], identity=ident_b[:]), r=[("on_b", qi), "ident_b"], w=["pOT"])
                P.dve(lambda e, h=h, qs=qs: e.tensor_copy(out=oT[:, h, qs], in_=pOT[:].rearrange("p a b -> p (a b)")), r=["pOT"], w=[("qTc", h, qc)])
        P.barrier()
    if "oT" in dbg_names:
        dump("oT", oT[:], [128, 8, NOWN], BF16)
    if STOP <= 5:
        esP.close()
        return finish(nc, P, ges, out, dbg_out)

    esM = ExitStack()
    mT_all = SB(esM, "mT_all", [128, 8, NOWN], BF16)
    with ExitStack() as es:
        Wg = SB(es, "Wg", [128, 8, 2048], BF16)
        Wa = SB(es, "Wa", [128, 4, 1024], BF16)
        Wb = SB(es, "Wb", [128, 4, 1024], BF16)
        Wo = SB(es, "Wo", [128, 8, 1024], BF16)
        sg1 = SB(es, "sg1", [128, 512], F32)
        sg2 = SB(es, "sg2", [128, 512], F32)
        sgb = SB(es, "sgb", [128, 512], F32)
        mt1 = SB(es, "mt1", [128, 512], F32)
        mt2 = SB(es, "mt2", [128, 512], F32)
        pG1 = PS(es, "pG1", [128, 512], F32)
        pG2 = PS(es, "pG2", [128, 512], F32)
        pA_D = PS(es, "pA_D", [128, 512], F32)
        pB_D = PS(es, "pB_D", [128, 512], F32)
        pC_D = PS(es, "pC_D", [128, 512], F32)
        for c2 in range(2):
            P.dma(lambda e, c2=c2: e.dma_start(out=Wg[:, :, c2 * 1024:(c2 + 1) * 1024], in_=w_in[:, 2048 + c2 * 1024:2048 + (c2 + 1) * 1024].rearrange("(kc p) n -> p kc n", p=128)),
                  key=("Wg", c2), w=["Wg"], eng="pool")
        P.dma(lambda e: e.dma_start(out=Wa[:], in_=w_glu_a.rearrange("(kc p) n -> p kc n", p=128)), key="Wa", w=["Wa"], eng="pool")
        P.dma(lambda e: e.dma_start(out=Wb[:], in_=w_glu_b.rearrange("(kc p) n -> p kc n", p=128)), key="Wb", w=["Wb"], eng="pool")
        P.dma(lambda e: e.dma_start(out=Wo[:], in_=w_attn_o.rearrange("(kc p) n -> p kc n", p=128)), key="Wo", w=["Wo"], eng="pool")
        for st_ in range(4):
            ts = slice(st_ * 512, (st_ + 1) * 512)
            for dt_ in range(8):
                ds = slice(dt_ * 128, (dt_ + 1) * 128)
                ds2 = slice(1024 + dt_ * 128, 1024 + (dt_ + 1) * 128)
                for kc in range(8):
                    P.pe(lambda e, kc=kc, ds=ds, ts=ts: e.matmul(out=pG1[:], lhsT=Wg[:, kc, ds], rhs=hTo[:, kc, ts], start=(kc == 0), stop=(kc == 7)), r=["Wg", "hTo"], w=["pG1"])
                for kc in range(8):
                    P.pe(lambda e, kc=kc, ds2=ds2, ts=ts: e.matmul(out=pG2[:], lhsT=Wg[:, kc, ds2], rhs=hTo[:, kc, ts], start=(kc == 0), stop=(kc == 7)), r=["Wg", "hTo"], w=["pG2"])
                for c in range(4):
                    P.pe(lambda e, c=c, ds=ds, ts=ts: e.matmul(out=pA_D[:], lhsT=Wa[:, c, ds], rhs=gT[:, c, ts], start=(c == 0), stop=(c == 3)), r=["Wa", "gT"], w=["pA_D"])
                for c in range(4):
                    P.pe(lambda e, c=c, ds=ds, ts=ts: e.matmul(out=pB_D[:], lhsT=Wb[:, c, ds], rhs=gT[:, c, ts], start=(c == 0), stop=(c == 3)), r=["Wb", "gT"], w=["pB_D"])
                for hh in range(8):
                    P.pe(lambda e, hh=hh, ds=ds, ts=ts: e.matmul(out=pC_D[:], lhsT=Wo[:, hh, ds], rhs=oT[:, hh, ts], start=(hh == 0), stop=(hh == 7)), r=["Wo", "oT"], w=["pC_D"])
                P.act(lambda e: e.activation(out=sg1[:], in_=pG1[:], func=ACT.Sigmoid), r=["pG1"], w=["sg1"])
                P.act(lambda e: e.activation(out=sg2[:], in_=pG2[:], func=ACT.Sigmoid), r=["pG2"], w=["sg2"])
                P.act(lambda e: e.activation(out=sgb[:], in_=pB_D[:], func=ACT.Sigmoid), r=["pB_D"], w=["sgb"])
                P.dve(lambda e: e.tensor_tensor(out=mt1[:], in0=pA_D[:], in1=sgb[:], op=ALU.mult), r=["pA_D", "sgb"], w=["mt1"])
                P.dve(lambda e: e.tensor_tensor(out=mt1[:], in0=mt1[:], in1=sg1[:], op=ALU.mult), r=["mt1", "sg1"], w=["mt1"])
                P.dve(lambda e: e.tensor_tensor(out=mt2[:], in0=pC_D[:], in1=sg2[:], op=ALU.mult), r=["pC_D", "sg2"], w=["mt2"])
                P.dve(lambda e, dt_=dt_, ts=ts: e.tensor_tensor(out=mT_all[:, dt_, ts], in0=mt1[:], in1=mt2[:], op=ALU.add), r=["mt1", "mt2"], w=["mT_all"])
        P.barrier()
    esP.close()
    if "mT" in dbg_names:
        dump("mT", mT_all[:], [128, 8, NOWN], BF16)
    if STOP <= 6:
        esM.close()
        return finish(nc, P, ges, out, dbg_out)

    esE = ExitStack()
    h2T = SB(esE, "h2T", [128, 8, NOWN], BF16)
    combT = SB(esE, "combT", [32, NOWN], BF16)
    with ExitStack() as es:
        Wout = SB(es, "Wout", [128, 8, 1024], BF16)
        wrt_sb = SB(es, "wrt_sb", [128, 8, 36], F32)
        brt_bc = SB(es, "brt_bc", [128, 36], F32)
        g1_bc = SB(es, "g1_bc", [128, D], F32)
        l1g_bc = SB(es, "l1g_bc", [128, D], F32)
        l1b_bc = SB(es, "l1b_bc", [128, D], F32)
        sc2p_bc = SB(es, "sc2p_bc", [128, D], F32)
        sh2_bc = SB(es, "sh2_bc", [128, D], F32)
        xt_D = [SB(es, "xt_D%d" % i, [128, D], F32) for i in range(2)]
        zt_D = SB(es, "zt_D", [128, D], F32)
        zn_D = SB(es, "zn_D", [128, D], F32)
        x1_D = [SB(es, "x1_D%d" % i, [128, D], F32) for i in range(2)]
        h2_D = SB(es, "h2_D", [128, D], F32)
        h2b_D = SB(es, "h2b_D", [128, D], BF16)
        h2Tf = SB(es, "h2Tf", [128, 8, 128], F32)
        st_D = SB(es, "st_D", [128, 2, 6], F32)
        mv_D = SB(es, "mv_D", [128, 2], F32)
        rstd_D = SB(es, "rstd_D", [128, 1], F32)
        nb_D = SB(es, "nb_D", [128, 1], F32)
        L_D = SB(es, "L_D", [128, 36], F32)
        rs = SB(es, "rs", [128, 16], F32)
        ohg = SB(es, "ohg", [128, 4], F32)
        gex = SB(es, "gex", [128, 4], F32)
        msk = SB(es, "msk", [128, 32], F32)
        ein = SB(es, "ein", [128, 8], F32)
        e2_ = SB(es, "e2_", [128, 8], F32)
        oh1 = SB(es, "oh1", [128, 8], F32)
        oh2 = SB(es, "oh2", [128, 8], F32)
        cg = SB(es, "cg", [128, 8], F32)
        comb = SB(es, "comb", [128, 32], F32)
        pMix = [PS(es, "pMix%d" % i, [128, 512], F32) for i in range(2)]
        pT_D = PS(es, "pT_D", [128, 8, 128], BF16)
        pTf = PS(es, "pTf", [128, 4, 128], F32)
        pR = PS(es, "pR", [128, 36], F32)
        pCT = PS(es, "pCT", [32, 128], F32)
        P.dma(lambda e: e.dma_start(out=Wout[:], in_=w_out.rearrange("(kc p) n -> p kc n", p=128)), key="Wout", w=["Wout"], eng="pool")
        P.dma(lambda e: e.dma_start(out=wrt_sb[:], in_=w_rt.rearrange("(kc p) n -> p kc n", p=128)), key="wrt_sb", w=["wrt_sb"])
        load_bc(brt_bc, b_rt, "brt_bc")
        load_bc(g1_bc, mod_d[0:1, 2048:3072], "g1_bc")
        load_bc(l1g_bc, ln1_g, "l1g_bc")
        load_bc(l1b_bc, ln1_b, "l1b_bc")
        load_bc(sc2p_bc, mod_d[0:1, 4096:5120], "sc2p_bc", True)
        load_bc(sh2_bc, mod_d[0:1, 3072:4096], "sh2_bc")
        for tt in range(NTO):
            s2 = tt % 2
            t0 = tt * 128
            tsl_ = slice(t0, t0 + 128)
            P.dma(lambda e, s2=s2, t0=t0: e.dma_start(out=xt_D[s2][:], in_=xo[t0:t0 + 128, :]), key=("xt_D", s2), w=[("xt_D", s2)])
            for half in range(2):
                hs = slice(half * 512, (half + 1) * 512)
                for kc in range(8):
                    P.pe(lambda e, kc=kc, half=half, hs=hs, tsl_=tsl_: e.matmul(out=pMix[half][:], lhsT=mT_all[:, kc, tsl_], rhs=Wout[:, kc, hs], start=(kc == 0), stop=(kc == 7)),
                         r=["mT_all", "Wout"], w=[("pMix", half)])
                P.dve(lambda e, half=half, hs=hs: e.tensor_tensor(out=zt_D[:, hs], in0=pMix[half][:], in1=g1_bc[:, hs], op=ALU.mult), r=[("pMix", half), "g1_bc"], w=["zt_D"])
            P.dve(lambda e, s2=s2: e.scalar_tensor_tensor(out=zt_D[:], in0=xt_D[s2][:], scalar=ALPHA, in1=zt_D[:], op0=ALU.mult, op1=ALU.add), r=["zt_D", ("xt_D", s2)], w=["zt_D"])
            ln_tile(zt_D[:], "zt_D", st_D, mv_D, rstd_D, nb_D, zn_D[:], "zn_D", "D1")
            P.dve(lambda e: e.tensor_tensor(out=zn_D[:], in0=zn_D[:], in1=l1g_bc[:], op=ALU.mult), r=["zn_D", "l1g_bc"], w=["zn_D"])
            P.dve(lambda e, s2=s2: e.tensor_tensor(out=x1_D[s2][:], in0=zn_D[:], in1=l1b_bc[:], op=ALU.add), r=["zn_D", "l1b_bc"], w=[("x1_D", s2)])
            P.dma(lambda e, s2=s2, t0=t0: e.dma_start(out=x1_d[t0:t0 + 128, :], in_=x1_D[s2][:]), key=("x1d", s2), r=[("x1_D", s2)], w=["x1_d"])
            ln_tile(x1_D[s2][:], ("x1_D", s2), st_D, mv_D, rstd_D, nb_D, h2_D[:], "h2_D", "D2")
            P.dve(lambda e: e.tensor_tensor(out=h2_D[:], in0=h2_D[:], in1=sc2p_bc[:], op=ALU.mult), r=["h2_D", "sc2p_bc"], w=["h2_D"])
            P.dve(lambda e: e.tensor_tensor(out=h2_D[:], in0=h2_D[:], in1=sh2_bc[:], op=ALU.add), r=["h2_D", "sh2_bc"], w=["h2_D"])
            P.act(lambda e: e.activation(out=h2b_D[:], in_=h2_D[:], func=ACT.Copy), r=["h2_D"], w=["h2b_D"])
            for kc in range(8):
                P.pe(lambda e, kc=kc: e.transpose(out=pT_D[:, kc, :], in_=h2b_D[:, kc * 128:(kc + 1) * 128], identity=ident_b[:]), r=["h2b_D", "ident_b"], w=["pT_D"])
            P.act(lambda e, tsl_=tsl_: e.activation(out=h2T[:, :, tsl_], in_=pT_D[:], func=ACT.Copy), r=["pT_D"], w=["h2T"])
            for q4 in range(2):
                for j in range(4):
                    kc = q4 * 4 + j
                    P.pe(lambda e, kc=kc, j=j: e.transpose(out=pTf[:, j, :], in_=h2_D[:, kc * 128:(kc + 1) * 128], identity=ident_f[:]), r=["h2_D", "ident_f"], w=["pTf"])
                P.dve(lambda e, q4=q4: e.tensor_copy(out=h2Tf[:, q4 * 4:(q4 + 1) * 4, :], in_=pTf[:]), r=["pTf"], w=["h2Tf"])
            for kc in range(8):
                P.pe(lambda e, kc=kc: e.matmul(out=pR[:], lhsT=h2Tf[:, kc, :], rhs=wrt_sb[:, kc, :], start=(kc == 0), stop=(kc == 7)), r=["h2Tf", "wrt_sb"], w=["pR"])
            P.dve(lambda e: e.tensor_tensor(out=L_D[:], in0=pR[:], in1=brt_bc[:], op=ALU.add), r=["pR", "brt_bc"], w=["L_D"])
            R_ = lambda i: rs[:, i:i + 1]
            kR = lambda i: ("rs", i)
            P.dve(lambda e: e.tensor_reduce(out=R_(0), in_=L_D[:, 0:4], axis=AX.X, op=ALU.max), r=["L_D"], w=[kR(0)])
            P.dve(lambda e: e.tensor_scalar(out=ohg[:], in0=L_D[:, 0:4], scalar1=R_(0), scalar2=None, op0=ALU.is_equal), r=["L_D", kR(0)], w=["ohg"])
            P.dve(lambda e: e.tensor_scalar(out=R_(1), in0=R_(0), scalar1=-1.0, scalar2=None, op0=ALU.mult), r=[kR(0)], w=[kR(1)])
            P.act(lambda e: e.activation(out=gex[:], in_=L_D[:, 0:4], func=ACT.Exp, bias=R_(1), scale=1.0), r=["L_D", kR(1)], w=["gex"])
            P.dve(lambda e: e.tensor_reduce(out=R_(2), in_=gex[:], axis=AX.X, op=ALU.add), r=["gex"], w=[kR(2)])
            P.dve(lambda e: e.reciprocal(out=R_(2), in_=R_(2)), r=[kR(2)], w=[kR(2)])
            P.dve(lambda e: e.tensor_tensor(out=msk[:].rearrange("p (g x) -> p g x", x=8), in0=L_D[:, 4:36].rearrange("p (g x) -> p g x", x=8),
                                            in1=fv(ohg[:], 0, [[1, 4], [0, 8]]), op=ALU.mult), r=["L_D", "ohg"], w=["msk"])
            P.dve(lambda e: e.tensor_reduce(out=ein[:], in_=fv(msk[:], 0, [[1, 8], [8, 4]]), axis=AX.X, op=ALU.add), r=["msk"], w=["ein"])
            P.dve(lambda e: e.tensor_reduce(out=R_(3), in_=ein[:], axis=AX.X, op=ALU.max), r=["ein"], w=[kR(3)])
            P.dve(lambda e: e.tensor_scalar(out=oh1[:], in0=ein[:], scalar1=R_(3), scalar2=None, op0=ALU.is_equal), r=["ein", kR(3)], w=["oh1"])
            P.dve(lambda e: e.scalar_tensor_tensor(out=e2_[:], in0=oh1[:], scalar=-1e30, in1=ein[:], op0=ALU.mult, op1=ALU.add), r=["oh1", "ein"], w=["e2_"])
            P.dve(lambda e: e.tensor_reduce(out=R_(4), in_=e2_[:], axis=AX.X, op=ALU.max), r=["e2_"], w=[kR(4)])
            P.dve(lambda e: e.tensor_scalar(out=oh2[:], in0=e2_[:], scalar1=R_(4), scalar2=None, op0=ALU.is_equal), r=["e2_", kR(4)], w=["oh2"])
            P.dve(lambda e: e.tensor_tensor(out=R_(5), in0=R_(4), in1=R_(3), op=ALU.subtract), r=[kR(3), kR(4)], w=[kR(5)])
            P.act(lambda e: e.activation(out=R_(6), in_=R_(5), func=ACT.Exp), r=[kR(5)], w=[kR(6)])
            P.dve(lambda e: e.tensor_scalar(out=R_(7), in0=R_(6), scalar1=1.0, scalar2=None, op0=ALU.add), r=[kR(6)], w=[kR(7)])
            P.dve(lambda e: e.reciprocal(out=R_(7), in_=R_(7)), r=[kR(7)], w=[kR(7)])
            P.dve(lambda e: e.tensor_tensor(out=R_(8), in0=R_(6), in1=R_(7), op=ALU.mult), r=[kR(6), kR(7)], w=[kR(8)])
            P.dve(lambda e: e.tensor_tensor(out=R_(7), in0=R_(7), in1=R_(2), op=ALU.mult), r=[kR(7), kR(2)], w=[kR(7)])
            P.dve(lambda e: e.tensor_tensor(out=R_(8), in0=R_(8), in1=R_(2), op=ALU.mult), r=[kR(8), kR(2)], w=[kR(8)])
            P.dve(lambda e: e.tensor_scalar(out=cg[:], in0=oh1[:], scalar1=R_(7), scalar2=None, op0=ALU.mult), r=["oh1", kR(7)], w=["cg"])
            P.dve(lambda e: e.scalar_tensor_tensor(out=cg[:], in0=oh2[:], scalar=R_(8), in1=cg[:], op0=ALU.mult, op1=ALU.add), r=["oh2", kR(8), "cg"], w=["cg"])
            P.dve(lambda e: e.tensor_tensor(out=comb[:].rearrange("p (g x) -> p g x", x=8), in0=fv(cg[:], 0, [[0, 4], [1, 8]]), in1=fv(ohg[:], 0, [[1, 4], [0, 8]]), op=ALU.mult),
                  r=["cg", "ohg"], w=["comb"])
            P.pe(lambda e: e.transpose(out=pCT[:], in_=comb[:], identity=ident_f[:]), r=["comb", "ident_f"], w=["pCT"])
            P.act(lambda e, tsl_=tsl_: e.activation(out=combT[:, tsl_], in_=pCT[:], func=ACT.Copy), r=["pCT"], w=["combT"])
        P.barrier()
    esM.close()
    if "x1" in dbg_names:
        o_ = nc.dram_tensor("dbg_x1", [NOWN, D], F32, kind="ExternalOutput").ap()
        P.dma(lambda e: e.dma_start(out=o_, in_=x1_d), key="dbg_x1", r=["x1_d"], w=["dbg_x1"])
    if "combT" in dbg_names:
        dump("combT", combT[:], [32, NOWN], BF16)
    if STOP <= 7:
        esE.close()
        return finish(nc, P, ges, out, dbg_out)

    yacc = SB(esE, "yacc", [128, NTO, D], F32)
    with ExitStack() as es:
        Weg = [SB(es, "Weg%d" % i, [128, 8, 512], BF16) for i in range(2)]
        Weu = [SB(es, "Weu%d" % i, [128, 8, 512], BF16) for i in range(2)]
        Wed = [SB(es, "Wed%d" % i, [128, 4, 1024], BF16) for i in range(2)]
        sele_b = SB(es, "sele_b", [32, 32, 128], BF16)
        bc_sb = SB(es, "bc_sb", [128, 512], F32)
        sa_E = [SB(es, "sa_E%d" % i, [128, 512], F32) for i in range(2)]
        actT = [SB(es, "actT%d" % i, [128, 4, 512], BF16) for i in range(2)]
        pBC = PS(es, "pBC", [128, 512], F32)
        pA_E = [PS(es, "pA_E%d" % i, [128, 512], F32) for i in range(2)]
        pB_E = [PS(es, "pB_E%d" % i, [128, 512], F32) for i in range(2)]
        pY_E = [PS(es, "pY_E%d" % i, [128, 512], F32) for i in range(2)]
        P.dma(lambda e: e.dma_start(out=sele_b[:], in_=cst_sele), key="sele_b", w=["sele_b"], eng="pool")
        NEXP = int(os.environ.get("K_NEXP", "32"))
        fci = 0
        yi = 0
        for ex in range(NEXP):
            se = ex % 2
            P.dma(lambda e, ex=ex, se=se: e.dma_start(out=Weg[se][:], in_=w_eg[ex].rearrange("(kc p) f -> p kc f", p=128)), key=("Weg", se), w=[("Weg", se)], eng="pool")
            P.dma(lambda e, ex=ex, se=se: e.dma_start(out=Weu[se][:], in_=w_eu[ex].rearrange("(kc p) f -> p kc f", p=128)), key=("Weu", se), w=[("Weu", se)], eng="pool")
            P.dma(lambda e, ex=ex, se=se: e.dma_start(out=Wed[se][:], in_=w_ed[ex].rearrange("(fc p) n -> p fc n", p=128)), key=("Wed", se), w=[("Wed", se)], eng="pool")
            for st_ in range(4):
                ts = slice(st_ * 512, (st_ + 1) * 512)
                sa_ = (ex * 4 + st_) % 2
                P.pe(lambda e, ex=ex, ts=ts: e.matmul(out=pBC[:], lhsT=sele_b[:, ex, :], rhs=combT[:, ts], start=True, stop=True), r=["sele_b", "combT"], w=["pBC"])
                P.act(lambda e: e.activation(out=bc_sb[:], in_=pBC[:], func=ACT.Copy), r=["pBC"], w=["bc_sb"])
                for fc in range(4):
                    fs = slice(fc * 128, (fc + 1) * 128)
                    sp_ = fci % 2
                    for kc in range(8):
                        P.pe(lambda e, kc=kc, fs=fs, ts=ts, se=se, sp_=sp_: e.matmul(out=pA_E[sp_][:], lhsT=Weg[se][:, kc, fs], rhs=h2T[:, kc, ts], start=(kc == 0), stop=(kc == 7)),
                             r=[("Weg", se), "h2T"], w=[("pA_E", sp_)])
                    for kc in range(8):
                        P.pe(lambda e, kc=kc, fs=fs, ts=ts, se=se, sp_=sp_: e.matmul(out=pB_E[sp_][:], lhsT=Weu[se][:, kc, fs], rhs=h2T[:, kc, ts], start=(kc == 0), stop=(kc == 7)),
                             r=[("Weu", se), "h2T"], w=[("pB_E", sp_)])
                    P.act(lambda e, sp_=sp_: e.activation(out=sa_E[sp_][:], in_=pA_E[sp_][:], func=ACT.Silu), r=[("pA_E", sp_)], w=[("sa_E", sp_)])
                    P.dve(lambda e, sp_=sp_: e.tensor_tensor(out=sa_E[sp_][:], in0=sa_E[sp_][:], in1=pB_E[sp_][:], op=ALU.mult), r=[("sa_E", sp_), ("pB_E", sp_)], w=[("sa_E", sp_)])
                    P.dve(lambda e, sp_=sp_, sa_=sa_, fc=fc: e.tensor_tensor(out=actT[sa_][:, fc, :], in0=sa_E[sp_][:], in1=bc_sb[:], op=ALU.mult),
                          r=[("sa_E", sp_), "bc_sb"], w=[("actT", sa_)])
                    fci += 1
                for j in range(4):
                    tile_ = st_ * 4 + j
                    js = slice(j * 128, (j + 1) * 128)
                    for half in range(2):
                        hs = slice(half * 512, (half + 1) * 512)
                        sy = yi % 2
                        for fc in range(4):
                            P.pe(lambda e, fc=fc, js=js, hs=hs, sa_=sa_, se=se, sy=sy: e.matmul(out=pY_E[sy][:], lhsT=actT[sa_][:, fc, js], rhs=Wed[se][:, fc, hs], start=(fc == 0), stop=(fc == 3)),
                                 r=[("actT", sa_), ("Wed", se)], w=[("pY_E", sy)])
                        if ex == 0:
                            P.dve(lambda e, tile_=tile_, hs=hs, sy=sy: e.tensor_copy(out=yacc[:, tile_, hs], in_=pY_E[sy][:]), r=[("pY_E", sy)], w=[("yacc", tile_)])
                        else:
                            P.dve(lambda e, tile_=tile_, hs=hs, sy=sy: e.tensor_tensor(out=yacc[:, tile_, hs], in0=yacc[:, tile_, hs], in1=pY_E[sy][:], op=ALU.add),
                                  r=[("pY_E", sy), ("yacc", tile_)], w=[("yacc", tile_)])
                        yi += 1
        P.barrier()

    with ExitStack() as es:
        g2_bc = SB(es, "g2_bc", [128, D], F32)
        l2g_bc = SB(es, "l2g_bc", [128, D], F32)
        l2b_bc = SB(es, "l2b_bc", [128, D], F32)
        x1_F = [SB(es, "x1_F%d" % i, [128, D], F32) for i in range(2)]
        z_F = SB(es, "z_F", [128, D], F32)
        zn_F = SB(es, "zn_F", [128, D], F32)
        o_F = [SB(es, "o_F%d" % i, [128, D], F32) for i in range(2)]
        st_F = SB(es, "st_F", [128, 2, 6], F32)
        mv_F = SB(es, "mv_F", [128, 2], F32)
        rstd_F = SB(es, "rstd_F", [128, 1], F32)
        nb_F = SB(es, "nb_F", [128, 1], F32)
        load_bc(g2_bc, mod_d[0:1, 5120:6144], "g2_bc")
        load_bc(l2g_bc, ln2_g, "l2g_bc")
        load_bc(l2b_bc, ln2_b, "l2b_bc")
        for tt in range(NTO):
            s2 = tt % 2
            t0 = tt * 128
            P.dma(lambda e, s2=s2, t0=t0: e.dma_start(out=x1_F[s2][:], in_=x1_d[t0:t0 + 128, :]), key=("x1_F", s2), r=["x1_d"], w=[("x1_F", s2)])
            P.dve(lambda e, tt=tt: e.tensor_tensor(out=z_F[:], in0=yacc[:, tt, :], in1=g2_bc[:], op=ALU.mult), r=[("yacc", tt), "g2_bc"], w=["z_F"])
            P.dve(lambda e, s2=s2: e.scalar_tensor_tensor(out=z_F[:], in0=x1_F[s2][:], scalar=ALPHA, in1=z_F[:], op0=ALU.mult, op1=ALU.add), r=["z_F", ("x1_F", s2)], w=["z_F"])
            ln_tile(z_F[:], "z_F", st_F, mv_F, rstd_F, nb_F, zn_F[:], "zn_F", "F")
            P.dve(lambda e: e.tensor_tensor(out=zn_F[:], in0=zn_F[:], in1=l2g_bc[:], op=ALU.mult), r=["zn_F", "l2g_bc"], w=["zn_F"])
            P.dve(lambda e, s2=s2: e.tensor_tensor(out=o_F[s2][:], in0=zn_F[:], in1=l2b_bc[:], op=ALU.add), r=["zn_F", "l2b_bc"], w=[("o_F", s2)])
            P.dma(lambda e, s2=s2, t0=t0: e.dma_start(out=out[t0:t0 + 128, :], in_=o_F[s2][:]), key=("outd", s2), r=[("o_F", s2)], w=["out"])
        P.barrier()
    esE.close()
    return finish(nc, P, ges, out, dbg_out)


def finish(nc, P, ges, out, dbg_out):
    P.barrier()
    P.emit()
    ges.close()
    nc._dbg_out = dbg_out
    nc._stats = P.stats
    return nc


def rope_tables():
    rows = NLAT // 64
    row = np.repeat(np.arange(rows, dtype=np.float32), 64)
    col = np.tile(np.arange(64, dtype=np.float32), rows)
    inv = (np.float32(10000.0) ** (-np.arange(0, 64, 2, dtype=np.float32) / np.float32(64))).astype(np.float32)
    ang = np.stack([row[:, None] * inv, col[:, None] * inv], axis=1).astype(np.float32)
    tab = np.concatenate([np.cos(ang).reshape(NLAT, 64), np.sin(ang).reshape(NLAT, 64)], axis=1).astype(np.float32)
    return tab


def make_in_maps(inp):
    f32 = np.float32
    g = lambda k: np.asarray(inp[k], dtype=f32)
    x, c, ctx, c_ctx = g("x"), g("c"), g("ctx"), g("c_ctx")
    tab = rope_tables()
    tab_ctx = np.concatenate([np.ones((NCTX, 64), f32), np.zeros((NCTX, 64), f32)], axis=1)
    rope_full = np.concatenate([tab_ctx, tab], axis=0)
    tok = np.arange(128)
    mask8 = (tok[:, None] % 8 == np.arange(8)[None, :]).astype(f32)
    sel16 = (tok[:, None] // 8 == np.arange(16)[None, :]).astype(f32)
    mask8c = (tok[:, None] // 16 == np.arange(8)[None, :]).astype(f32)

    def pairlay(a):
        sh = a.shape
        a = a.reshape((2, 16, 2, 64) + sh[3:])
        perm = (2, 3, 0, 1) + tuple(range(4, a.ndim))
        a = a.transpose(perm)
        return np.ascontiguousarray(a.reshape((128, 2, 16) + sh[3:]))

    a_re, a_im = g("s5_a_re")[0], g("s5_a_im")[0]
    s5_a = np.stack([pairlay(a_re), pairlay(a_im)], axis=1)
    ldt = g("s5_log_dt")[0]
    s5_ldt = np.ascontiguousarray(np.broadcast_to(ldt.reshape(1, 2, 16, 2).transpose(0, 3, 1, 2), (64, 2, 2, 16)).transpose(1, 0, 2, 3).reshape(128, 2, 16))
    s5_b = np.stack([pairlay(g("s5_b_re")[0]), pairlay(g("s5_b_im")[0])], axis=1)
    cre = g("s5_c_re")[0].transpose(0, 1, 3, 2)
    cim = g("s5_c_im")[0].transpose(0, 1, 3, 2)
    s5_c = np.stack([pairlay(cre), pairlay(cim)], axis=1)
    dvec = g("s5_d")[0]
    s5_dcol = np.ascontiguousarray(np.broadcast_to(dvec.reshape(32, 16).T[None], (8, 16, 32)).reshape(128, 32))
    eZ = np.stack([63.0 - np.arange(64), np.arange(64)], axis=0).astype(f32)
    qs = np.arange(72)
    eRf = (qs - 7).astype(f32)
    eRb = (8 * (qs // 8 - 1) + 8 - (qs % 8)).astype(f32)
    eR = np.stack([eRf, eRb], axis=0)
    cst_eZ = np.ascontiguousarray(np.broadcast_to(eZ[None], (128, 2, 64)))
    cst_eR = np.ascontiguousarray(np.broadcast_to(eR[None], (128, 2, 72)))
    sidx = tok // 16
    cst_mf = (sidx[None, :] >= sidx[:, None]).astype(f32)
    cst_mb = (sidx[:, None] >= sidx[None, :]).astype(f32)
    selg = np.zeros((128, 8, 128), f32)
    for g8 in range(8):
        for co in range(16):
            selg[np.arange(8) * 16 + co, g8, g8 * 16 + co] = 1.0
    sele = np.zeros((32, 32, 128), f32)
    for e in range(32):
        sele[e, e, :] = 1.0
    w_rt = np.concatenate([g("w_router_group")[0], g("w_router_expert")[0]], axis=1)
    b_rt = np.concatenate([g("b_router_group")[0], g("b_router_expert")[0]], axis=0)[None]
    common = dict(
        w_mod=g("w_mod")[0], b_mod=g("b_mod"), w_in=g("w_in")[0], rope_f=rope_full,
        q_gain=g("q_gain"), k_gain=g("k_gain"), cst_mask8=mask8, cst_mask8c=mask8c, cst_sel16=sel16,
        s5_a=s5_a, s5_ldt=s5_ldt, s5_b=s5_b, s5_c=s5_c, s5_dcol=s5_dcol, cst_eZ=cst_eZ, cst_eR=cst_eR,
        cst_mf=cst_mf, cst_mb=cst_mb, cst_selg=selg,
        w_glu_a=g("w_glu_a")[0], w_glu_b=g("w_glu_b")[0], w_attn_o=g("w_attn_o")[0], w_out=g("w_out")[0],
        ln1_g=g("ln1_g"), ln1_b=g("ln1_b"), ln2_g=g("ln2_g"), ln2_b=g("ln2_b"),
        w_rt=w_rt, b_rt=b_rt, w_eg=g("w_exp_gate")[0], w_eu=g("w_exp_up")[0], w_ed=g("w_exp_down")[0],
        cst_sele=sele,
    )
    maps = []
    for core in range(8):
        b, r = core // 4, core % 4
        m = dict(common)
        m["xf"] = np.ascontiguousarray(np.concatenate([ctx[b], x[b]], axis=0))
        m["xo"] = np.ascontiguousarray(x[b, r * NOWN:(r + 1) * NOWN])
        cc = np.stack([c[b], c_ctx], axis=0)
        m["ccT"] = np.ascontiguousarray(cc.reshape(2, 8, 128).transpose(2, 1, 0))
        m["rope_o"] = np.ascontiguousarray(tab[r * NOWN:(r + 1) * NOWN])
        cm = np.zeros((128, 4), f32)
        cm[:, r] = 1.0
        m["cmask"] = cm
        maps.append(m)
    return maps


_NC_CACHE = {}


def kernel(**inputs):
    maps = make_in_maps(inputs)
    if "nc" not in _NC_CACHE:
        _NC_CACHE["nc"] = build()
    nc = _NC_CACHE["nc"]
    res = run_bass_kernel_spmd(nc, maps, core_ids=list(range(8)))
    outp = np.zeros((2, NLAT, D), np.float32)
    for core in range(8):
        b, r = core // 4, core % 4
        outp[b, r * NOWN:(r + 1) * NOWN] = res.results[core]["out"]
    return outp
```

```python
import os
import math
import numpy as np
from contextlib import ExitStack
import concourse.bass as bass
import concourse.mybir as mybir
from concourse.bass_utils import run_bass_kernel_spmd

F32 = mybir.dt.float32
BF16 = mybir.dt.bfloat16
ACT = mybir.ActivationFunctionType
ALU = mybir.AluOpType
AX = mybir.AxisListType

D = 1024
NLAT = 8192
NCTX = 256
NFULL = NLAT + NCTX
NOWN = 2048
NTF = NFULL // 128
NTO = NOWN // 128
NJ = NFULL // 64
NJF = NFULL // 8
EPS = 1e-6
ALPHA = 2.0 ** 0.25
STOP = int(os.environ.get("K_STOP", "99"))
DEBUG = os.environ.get("K_DEBUG", "") != ""


class Prog:
    ENGS = ["pe", "act", "dve", "pool", "sp"]

    def __init__(self, nc):
        self.nc = nc
        self.ops = []

    def add(self, eng, fn, r=(), w=(), dma=None, ndma=1):
        self.ops.append(dict(eng=eng, fn=fn, r=tuple(r), w=tuple(w), dma=dma, ndma=ndma, barrier=False))

    def pe(self, fn, r=(), w=()):
        self.add("pe", fn, r, w)

    def act(self, fn, r=(), w=()):
        self.add("act", fn, r, w)

    def dve(self, fn, r=(), w=()):
        self.add("dve", fn, r, w)

    def pool(self, fn, r=(), w=()):
        self.add("pool", fn, r, w)

    def dma(self, fn, key, r=(), w=(), eng="sp", n=1):
        self.add(eng, fn, r, w, dma=key, ndma=n)

    def capture(self):
        self._saved = self.ops
        self.ops = []

    def end_capture(self):
        lst = self.ops
        self.ops = self._saved
        return lst

    def barrier(self):
        for e in self.ENGS:
            self.ops.append(dict(eng=e, fn=None, r=(), w=(), dma=None, ndma=0, barrier=True))

    def emit(self):
        nc = self.nc
        ops = self.ops
        n = len(ops)
        last_w, readers = {}, {}
        deps = [None] * n
        last_eng, last_dma = {}, {}
        for i, op in enumerate(ops):
            d = set()
            if op["barrier"]:
                for e, j in last_eng.items():
                    if e != op["eng"]:
                        d.add(j)
                for k, j in last_dma.items():
                    d.add(j)
            for b in op["r"]:
                if b in last_w:
                    d.add(last_w[b])
            for b in op["w"]:
                if b in last_w:
                    d.add(last_w[b])
                for j in readers.get(b, ()):
                    d.add(j)
            for b in op["r"]:
                readers.setdefault(b, []).append(i)
            for b in op["w"]:
                readers[b] = []
                last_w[b] = i
            d.discard(i)
            deps[i] = d
            if op["dma"] is not None:
                last_dma[op["dma"]] = i
            elif not op["barrier"]:
                last_eng[op["eng"]] = i
        signal = [False] * n
        for i, op in enumerate(ops):
            for j in deps[i]:
                pj = ops[j]
                if pj["dma"] is not None:
                    continue
                if pj["eng"] == "pe" and op["eng"] == "pe" and op["dma"] is None:
                    continue
                signal[j] = True
        tick = [0] * n
        cnt = {e: 0 for e in self.ENGS}
        dcnt = {}
        for i, op in enumerate(ops):
            if op["dma"] is not None:
                dcnt[op["dma"]] = dcnt.get(op["dma"], 0) + op["ndma"]
                tick[i] = dcnt[op["dma"]] * 16
            elif signal[i]:
                cnt[op["eng"]] += 1
                tick[i] = cnt[op["eng"]]
        es = ExitStack()
        esem = {e: es.enter_context(nc.semaphore("s_" + e)) for e in self.ENGS}
        dsem = {}
        for k in dcnt:
            dsem[k] = es.enter_context(nc.semaphore("d_%d" % len(dsem)))
        waits = [None] * n
        seen = {e: {} for e in self.ENGS}
        for i, op in enumerate(ops):
            wl = {}
            for j in deps[i]:
                pj = ops[j]
                if pj["dma"] is not None:
                    key = ("d", pj["dma"])
                    sem = dsem[pj["dma"]]
                else:
                    if pj["eng"] == "pe" and op["eng"] == "pe" and op["dma"] is None:
                        continue
                    key = ("e", pj["eng"])
                    sem = esem[pj["eng"]]
                v = tick[j]
                if seen[op["eng"]].get(key, 0) >= v:
                    continue
                if key not in wl or wl[key][1] < v:
                    wl[key] = (sem, v)
            for key, (sem, v) in wl.items():
                seen[op["eng"]][key] = v
            waits[i] = list(wl.values())
        self.stats = dict(n=n, sig=dict(cnt), dkeys=len(dcnt), nwaits=sum(len(w) for w in waits))
        block = es.enter_context(nc.Block())

        def run(engname, eng):
            for i, op in enumerate(ops):
                if op["eng"] != engname:
                    continue
                for sem, v in waits[i]:
                    eng.wait_ge(sem, v)
                if op["fn"] is None:
                    continue
                res = op["fn"](eng)
                if op["dma"] is not None:
                    if not isinstance(res, (list, tuple)):
                        res = [res]
                    assert len(res) == op["ndma"], (len(res), op["ndma"])
                    for ins in res:
                        ins.then_inc(dsem[op["dma"]], 16)
                elif signal[i]:
                    if isinstance(res, (list, tuple)):
                        res = res[-1]
                    res.then_inc(esem[engname], 1)

        @block.tensor
        def _(e):
            run("pe", e)

        @block.scalar
        def _(e):
            run("act", e)

        @block.vector
        def _(e):
            run("dve", e)

        @block.gpsimd
        def _(e):
            run("pool", e)

        @block.sync
        def _(e):
            run("sp", e)

        es.close()


def AP_(t, offset, dims):
    return bass.AP(t, offset, [list(d) for d in dims])


def pstride(t):
    return t[:].ap[0][0]


def build(dbg_names=()):
    nc = bass.Bass("TRN2", target_bir_lowering=False)
    dram_in = lambda name, shape, dt=F32: nc.dram_tensor(name, list(shape), dt, kind="ExternalInput").ap()
    xf = dram_in("xf", [NFULL, D])
    xo = dram_in("xo", [NOWN, D])
    ccT = dram_in("ccT", [128, 8, 2])
    w_mod = dram_in("w_mod", [D, 6 * D])
    b_mod = dram_in("b_mod", [1, 6 * D])
    w_in = dram_in("w_in", [D, 4096])
    rope_f = dram_in("rope_f", [NFULL, 128])
    rope_o = dram_in("rope_o", [NOWN, 128])
    q_gain = dram_in("q_gain", [1, 128])
    k_gain = dram_in("k_gain", [1, 128])
    cst_mask8 = dram_in("cst_mask8", [128, 8])
    cst_mask8c = dram_in("cst_mask8c", [128, 8])
    cst_sel16 = dram_in("cst_sel16", [128, 16])
    s5_a = dram_in("s5_a", [128, 2, 2, 16])
    s5_ldt = dram_in("s5_ldt", [128, 2, 16])
    s5_b = dram_in("s5_b", [128, 2, 2, 16, 16])
    s5_c = dram_in("s5_c", [128, 2, 2, 16, 16])
    s5_dcol = dram_in("s5_dcol", [128, 32])
    cst_eZ = dram_in("cst_eZ", [128, 2, 64])
    cst_eR = dram_in("cst_eR", [128, 2, 72])
    cst_mf = dram_in("cst_mf", [128, 128])
    cst_mb = dram_in("cst_mb", [128, 128])
    cst_selg = dram_in("cst_selg", [128, 8, 128])
    cmask = dram_in("cmask", [128, 4])
    w_glu_a = dram_in("w_glu_a", [512, D])
    w_glu_b = dram_in("w_glu_b", [512, D])
    w_attn_o = dram_in("w_attn_o", [D, D])
    w_out = dram_in("w_out", [D, D])
    ln1_g = dram_in("ln1_g", [1, D])
    ln1_b = dram_in("ln1_b", [1, D])
    ln2_g = dram_in("ln2_g", [1, D])
    ln2_b = dram_in("ln2_b", [1, D])
    w_rt = dram_in("w_rt", [D, 36])
    b_rt = dram_in("b_rt", [1, 36])
    w_eg = dram_in("w_eg", [32, D, 512])
    w_eu = dram_in("w_eu", [32, D, 512])
    w_ed = dram_in("w_ed", [32, 512, D])
    cst_sele = dram_in("cst_sele", [32, 32, 128])
    out = nc.dram_tensor("out", [NOWN, D], F32, kind="ExternalOutput").ap()
    scr = lambda name, shape, dt: nc.dram_tensor(name, list(shape), dt, kind="Internal").ap()
    mod_d = scr("mod_d", [2, 6 * D], F32)
    kT_d = scr("kT_d", [2, 128, NFULL], BF16)
    v_d = scr("v_d", [NFULL, 256], BF16)
    x1_d = scr("x1_d", [NOWN, D], F32)
    dbg_out = {}

    P = Prog(nc)
    ges = ExitStack()

    free_list = [[16640, 229376]]
    peak = [0]

    def _alloc(nbytes):
        nbytes = (nbytes + 63) // 64 * 64
        for iv in free_list:
            if iv[1] - iv[0] >= nbytes:
                off = iv[0]
                iv[0] += nbytes
                peak[0] = max(peak[0], off + nbytes)
                return off, nbytes
        raise RuntimeError("SBUF manual allocator out of space for %d bytes; free=%s" % (nbytes, free_list))

    def _free(off, nbytes):
        free_list.append([off, off + nbytes])
        free_list.sort()
        merged = []
        for iv in free_list:
            if iv[1] == iv[0]:
                continue
            if merged and merged[-1][1] == iv[0]:
                merged[-1][1] = iv[1]
            else:
                merged.append(iv)
        free_list[:] = merged

    def SB(es, name, shape, dt):
        esz = 4 if dt == F32 else 2
        nb = esz
        for s_ in shape[1:]:
            nb *= s_
        off, nbytes = _alloc(nb)
        t = nc.alloc_sbuf_tensor_at(name, list(shape), dt, offset=off)
        es.callback(_free, off, nbytes)
        return t

    def PS(es, name, shape, dt):
        return es.enter_context(nc.psum_tensor(name, list(shape), dt))

    def dump(name, t_ap, shape, dt=F32, r=()):
        if name not in dbg_names:
            return
        o = nc.dram_tensor("dbg_" + name, list(shape), dt, kind="ExternalOutput").ap()
        dbg_out[name] = o
        P.dma(lambda e: e.dma_start(out=o, in_=t_ap), key="dbg_" + name, r=r, w=["dbg_" + name])

    ident_f = SB(ges, "ident_f", [128, 128], F32)
    ident_b = SB(ges, "ident_b", [128, 128], BF16)
    ones_b = SB(ges, "ones_b", [1, 128], BF16)
    eps_t = SB(ges, "eps_t", [128, 1], F32)
    modT = SB(ges, "modT", [128, 48, 2], F32)
    op1p = SB(ges, "op1p", [128, 8, 2], F32)
    sh1T = SB(ges, "sh1T", [128, 8, 2], F32)
    gk_bc = SB(ges, "gk_bc", [128, 128], F32)
    gq_bc = SB(ges, "gq_bc", [128, 128], F32)
    negC = SB(ges, "negC", [128, 1], F32)
    mask8 = SB(ges, "mask8", [128, 8], BF16)
    mask8f = SB(ges, "mask8f", [128, 8], F32)
    sel16 = SB(ges, "sel16", [128, 16], BF16)

    P.pool(lambda e: e.memset(ident_f[:], 1.0), w=["ident_f"])
    P.pool(lambda e: e.affine_select(out=ident_f[:], in_=ident_f[:], pattern=[[-1, 128]], compare_op=ALU.is_equal,
                                     fill=0.0, base=0, channel_multiplier=1), r=["ident_f"], w=["ident_f"])
    P.dve(lambda e: e.tensor_copy(out=ident_b[:], in_=ident_f[:]), r=["ident_f"], w=["ident_b"])
    P.dve(lambda e: e.memset(ones_b[:], 1.0), w=["ones_b"])
    P.dve(lambda e: e.memset(eps_t[:], EPS), w=["eps_t"])
    P.dma(lambda e: e.dma_start(out=gk_bc[:], in_=k_gain.partition_broadcast(128)), key="gk_bc", w=["gk_bc"])
    P.dma(lambda e: e.dma_start(out=gq_bc[:], in_=q_gain.partition_broadcast(128)), key="gq_bc", w=["gq_bc"])

    with ExitStack() as es:
        scT = SB(es, "scT", [128, 8, 2], F32)
        ccs = SB(es, "ccs", [128, 8, 2], F32)
        wm = [SB(es, "wm%d" % i, [128, 8, 512], F32) for i in range(2)]
        bm = SB(es, "bm", [2, 6 * D], F32)
        mod_sb = SB(es, "mod_sb", [2, 6 * D], F32)
        m8f = SB(es, "m8f", [128, 8], F32)
        s16f = SB(es, "s16f", [128, 16], F32)
        tmpg = SB(es, "tmpg", [128, 2], F32)
        pM = [PS(es, "pM%d" % i, [2, 512], F32) for i in range(2)]
        pMT = PS(es, "pMT", [128, 48, 2], F32)
        P.dma(lambda e: e.dma_start(out=ccs[:], in_=ccT), key="ccs", w=["ccs"])
        P.dma(lambda e: e.dma_start(out=bm[:], in_=b_mod.partition_broadcast(2)), key="bm", w=["bm"])
        P.dma(lambda e: e.dma_start(out=m8f[:], in_=cst_mask8), key="m8f", w=["m8f"])
        P.dma(lambda e: e.dma_start(out=s16f[:], in_=cst_sel16), key="s16f", w=["s16f"])
        P.dve(lambda e: e.tensor_copy(out=mask8[:], in_=m8f[:]), r=["m8f"], w=["mask8"])
        P.dve(lambda e: e.tensor_copy(out=mask8f[:], in_=m8f[:]), r=["m8f"], w=["mask8f"])
        P.dve(lambda e: e.tensor_copy(out=sel16[:], in_=s16f[:]), r=["s16f"], w=["sel16"])
        P.act(lambda e: e.activation(out=scT[:], in_=ccs[:], func=ACT.Silu), r=["ccs"], w=["scT"])
        P.dve(lambda e: e.tensor_reduce(out=tmpg[:, 0:1], in_=gq_bc[:], axis=AX.X, op=ALU.max, apply_absolute_value=True),
              r=["gq_bc"], w=["tmpg0"])
        P.dve(lambda e: e.tensor_reduce(out=tmpg[:, 1:2], in_=gk_bc[:], axis=AX.X, op=ALU.max, apply_absolute_value=True),
              r=["gk_bc"], w=["tmpg1"])
        P.dve(lambda e: e.scalar_tensor_tensor(out=negC[:], in0=tmpg[:, 0:1], scalar=-math.sqrt(128.0), in1=tmpg[:, 1:2],
                                               op0=ALU.mult, op1=ALU.mult), r=["tmpg0", "tmpg1"], w=["negC"])
        for nb in range(12):
            s = nb % 2
            P.dma(lambda e, nb=nb, s=s: e.dma_start(out=wm[s][:], in_=w_mod[:, nb * 512:(nb + 1) * 512].rearrange("(kc p) n -> p kc n", p=128)),
                  key=("wm", s), w=[("wm", s)])
            for kc in range(8):
                P.pe(lambda e, kc=kc, s=s: e.matmul(out=pM[s][:], lhsT=scT[:, kc, :], rhs=wm[s][:, kc, :], start=(kc == 0), stop=(kc == 7)),
                     r=["scT", ("wm", s)], w=[("pM", s)])
            P.dve(lambda e, nb=nb, s=s: e.tensor_tensor(out=mod_sb[:, nb * 512:(nb + 1) * 512], in0=pM[s][:], in1=bm[:, nb * 512:(nb + 1) * 512], op=ALU.add),
                  r=[("pM", s), "bm"], w=["mod_sb"])
        P.dma(lambda e: e.dma_start(out=mod_d, in_=mod_sb[:]), key="mod_d", r=["mod_sb"], w=["mod_d"])
        for j in range(48):
            P.pe(lambda e, j=j: e.transpose(out=pMT[:, j, :], in_=mod_sb[:, j * 128:(j + 1) * 128], identity=ident_f[0:2, 0:2]),
                 r=["mod_sb", "ident_f"], w=["pMT"])
        P.dve(lambda e: e.tensor_copy(out=modT[:], in_=pMT[:]), r=["pMT"], w=["modT"])
        P.dve(lambda e: e.tensor_copy(out=sh1T[:], in_=modT[:, 0:8, :]), r=["modT"], w=["sh1T"])
        P.dve(lambda e: e.tensor_scalar(out=op1p[:], in0=modT[:, 8:16, :], scalar1=1.0, scalar2=None, op0=ALU.add), r=["modT"], w=["op1p"])
        dump("mod", mod_sb[:], [2, 6 * D], r=["mod_sb"])
        P.barrier()
    if STOP <= 0:
        return finish(nc, P, ges, out, dbg_out)

    def fv(ap, off, dims):
        return bass.AP(ap.tensor, ap.offset + off, [list(ap.ap[0])] + [list(d) for d in dims])

    bias_sb = SB(ges, "bias_sb", [1, 5120], BF16)

    def prep_wblock(stage, pB, s, dst, col0, r, bcol, tag):
        P.dma(lambda e: e.dma_start(out=stage[s][:], in_=w_in[:, col0:col0 + 512].rearrange("(kc p) n -> p kc n", p=128)),
              key=("stg", s), w=[("stg", s)])
        for kc in range(8):
            P.pool(lambda e, kc=kc: e.tensor_scalar(out=dst[:, kc, :], in0=stage[s][:, kc, :], scalar1=op1p[:, kc, r:r + 1], scalar2=None, op0=ALU.mult),
                   r=[("stg", s), "op1p"], w=[tag])
        for kc in range(8):
            P.pe(lambda e, kc=kc: e.matmul(out=pB[:], lhsT=sh1T[:, kc, r:r + 1], rhs=stage[s][:, kc, :], start=(kc == 0), stop=(kc == 7)),
                 r=[("stg", s), "sh1T"], w=["pB"])
        P.act(lambda e: e.activation(out=bias_sb[:, bcol:bcol + 512], in_=pB[:], func=ACT.Copy), r=["pB"], w=["bias_sb"])

    def ln_tile(xt_ap, xkey, st, mv, rstd, nb, hb_ap, hkey, sfx):
        for i in range(2):
            P.dve(lambda e, i=i: e.bn_stats(out=st[:, i, :], in_=xt_ap[:, i * 512:(i + 1) * 512]), r=[xkey], w=[("st", sfx, i)])
        P.dve(lambda e: e.bn_aggr(out=mv[:], in_=st[:].rearrange("p a b -> p (a b)")), r=[("st", sfx, 0), ("st", sfx, 1)], w=[("mv", sfx)])
        P.act(lambda e: e.activation(out=rstd[:], in_=mv[:, 1:2], func=ACT.Sqrt, bias=eps_t[:], scale=1.0), r=[("mv", sfx), "eps_t"], w=[("rstd", sfx)])
        P.dve(lambda e: e.reciprocal(out=rstd[:], in_=rstd[:]), r=[("rstd", sfx)], w=[("rstd", sfx)])
        P.dve(lambda e: e.scalar_tensor_tensor(out=nb[:], in0=mv[:, 0:1], scalar=-1.0, in1=rstd[:], op0=ALU.mult, op1=ALU.mult),
              r=[("mv", sfx), ("rstd", sfx)], w=[("nb", sfx)])
        P.act(lambda e: e.activation(out=hb_ap, in_=xt_ap, func=ACT.Identity, bias=nb[:], scale=rstd[:]), r=[xkey, ("nb", sfx), ("rstd", sfx)], w=[hkey])

    def rms_rope(eng_add, src, nh, gain_bc, rt, dst, tmp, keys_r, key_w, sfx, eng2=None, tmp2=None):
        sq, ss, rk, ta, tb = tmp
        eng2 = eng2 or eng_add
        tc, td = tmp2 if tmp2 is not None else (ta, tb)
        kc_, kd_ = (("tc", sfx), ("td", sfx)) if tmp2 is not None else (("ta", sfx), ("tb", sfx))
        n = nh * 128
        P.dve(lambda e: e.tensor_tensor(out=sq[:, :n], in0=src[:, :n], in1=src[:, :n], op=ALU.mult), r=keys_r, w=[("sq", sfx)])
        P.dve(lambda e: e.tensor_reduce(out=ss[:, :nh], in_=sq[:, :n].rearrange("p (h d) -> p h d", d=128), axis=AX.X, op=ALU.add), r=[("sq", sfx)], w=[("ss", sfx)])
        P.act(lambda e: e.activation(out=rk[:, :nh], in_=ss[:, :nh], func=ACT.Sqrt, bias=eps_t[:], scale=1.0 / 128.0), r=[("ss", sfx), "eps_t"], w=[("rk", sfx)])
        P.dve(lambda e: e.reciprocal(out=rk[:, :nh], in_=rk[:, :nh]), r=[("rk", sfx)], w=[("rk", sfx)])
        P.dve(lambda e: e.tensor_tensor(out=src[:, :n].rearrange("p (h d) -> p h d", d=128), in0=src[:, :n].rearrange("p (h d) -> p h d", d=128),
                                        in1=fv(rk[:], 0, [[1, nh], [0, 128]]), op=ALU.mult), r=keys_r + [("rk", sfx)], w=keys_r)
        eng_add(lambda e: e.tensor_tensor(out=src[:, :n].rearrange("p (h d) -> p h d", d=128), in0=src[:, :n].rearrange("p (h d) -> p h d", d=128),
                                          in1=fv(gain_bc[:], 0, [[0, nh], [1, 128]]), op=ALU.mult), r=keys_r, w=keys_r)
        x1 = fv(src[:], 0, [[128, nh], [64, 2], [1, 32]])
        x2 = fv(src[:], 32, [[128, nh], [64, 2], [1, 32]])
        cs = fv(rt[:], 0, [[0, nh], [32, 2], [1, 32]])
        sn = fv(rt[:], 64, [[0, nh], [32, 2], [1, 32]])
        o1 = fv(dst[:], 0, [[128, nh], [64, 2], [1, 32]])
        o2 = fv(dst[:], 32, [[128, nh], [64, 2], [1, 32]])
        tav = fv(ta[:], 0, [[64, nh], [32, 2], [1, 32]])
        tbv = fv(tb[:], 0, [[64, nh], [32, 2], [1, 32]])
        tcv = fv(tc[:], 0, [[64, nh], [32, 2], [1, 32]])
        tdv = fv(td[:], 0, [[64, nh], [32, 2], [1, 32]])
        eng_add(lambda e: e.tensor_tensor(out=tav, in0=x1, in1=cs, op=ALU.mult), r=keys_r + [("rt", sfx)], w=[("ta", sfx)])
        eng2(lambda e: e.tensor_tensor(out=tbv, in0=x2, in1=sn, op=ALU.mult), r=keys_r + [("rt", sfx)], w=[("tb", sfx)])
        eng2(lambda e: e.tensor_tensor(out=o1, in0=tav, in1=tbv, op=ALU.subtract), r=[("ta", sfx), ("tb", sfx)], w=[key_w])
        eng_add(lambda e: e.tensor_tensor(out=tcv, in0=x1, in1=sn, op=ALU.mult), r=keys_r + [("rt", sfx)], w=[kc_])
        eng2(lambda e: e.tensor_tensor(out=tdv, in0=x2, in1=cs, op=ALU.mult), r=keys_r + [("rt", sfx)], w=[kd_])
        eng2(lambda e: e.tensor_tensor(out=o2, in0=tcv, in1=tdv, op=ALU.add), r=[kc_, kd_], w=[key_w])

    esA = ExitStack()
    u8 = SB(esA, "u8", [128, 32, NJF], BF16)
    with ExitStack() as es:
        stage = [SB(es, "stage%d" % i, [128, 8, 512], F32) for i in range(2)]
        Wkv = [SB(es, "Wkv%d" % r, [128, 8, 512], BF16) for r in range(2)]
        Wu = [SB(es, "Wu%d" % r, [128, 8, 512], BF16) for r in range(2)]
        xt = [SB(es, "xt%d" % i, [128, D], F32) for i in range(3)]
        rt = [SB(es, "rt%d" % i, [128, 128], F32) for i in range(2)]
        hb = [SB(es, "hb%d" % i, [128, D], BF16) for i in range(2)]
        hT = [SB(es, "hT%d" % i, [128, 8, 128], BF16) for i in range(2)]
        st = SB(es, "st", [128, 2, 6], F32)
        mv = SB(es, "mv", [128, 2], F32)
        rstd = SB(es, "rstd", [128, 1], F32)
        nbt = SB(es, "nbt", [128, 1], F32)
        k_sb = SB(es, "k_sb", [128, 256], F32)
        v_bf = [SB(es, "v_bf%d" % i, [128, 256], BF16) for i in range(2)]
        kr = SB(es, "kr", [128, 256], BF16)
        kT_sb = [SB(es, "kT_sb%d" % i, [128, 2, 128], BF16) for i in range(2)]
        tmp = (SB(es, "sq", [128, 256], F32), SB(es, "ss", [128, 2], F32), SB(es, "rk", [128, 2], F32),
               SB(es, "ta", [128, 128], F32), SB(es, "tb", [128, 128], F32))
        tmp2_A = (SB(es, "tcA", [128, 128], F32), SB(es, "tdA", [128, 128], F32))
        u_bf = SB(es, "u_bf", [128, 512], BF16)
        Um = [SB(es, "Um%d" % i, [128, 32, 128], BF16) for i in range(2)]
        pB = PS(es, "pB", [1, 512], F32)
        pT = [PS(es, "pT%d" % i, [128, 8, 128], BF16) for i in range(2)]
        pKV = PS(es, "pKV", [128, 512], F32)
        pU = PS(es, "pU", [128, 512], F32)
        pKT = PS(es, "pKT", [128, 2, 128], BF16)
        pU8 = PS(es, "pU8", [128, 32, 16], F32)
        prep_wblock(stage, pB, 0, Wkv[1], 1536, 1, 4096, ("Wkv", 1))
        prep_wblock(stage, pB, 1, Wu[1], 0, 1, 4608, ("Wu", 1))
        prep_wblock(stage, pB, 0, Wkv[0], 1536, 0, 1536, ("Wkv", 0))
        prep_wblock(stage, pB, 1, Wu[0], 0, 0, 0, ("Wu", 0))
        krA = [kr, SB(es, "kr2", [128, 256], BF16)]
        NTA = int(os.environ.get('K_NT', NTF))
        PIPE = int(os.environ.get('K_PIPEA', '3'))
        NSA = int(os.environ.get('K_NSA', '4'))
        stg = [[None] * NTA for _ in range(3)]
        stg1a = [None] * NTA
        for tt in range(NTA):
            r = 1 if tt < 2 else 0
            s3, s2 = tt % 3, tt % 2
            t0 = tt * 128
            bkv, bu = (4096, 4608) if r == 1 else (1536, 0)
            P.capture()
            P.dma(lambda e, s3=s3, t0=t0: e.dma_start(out=xt[s3][:], in_=xf[t0:t0 + 128, :]), key=("xt", s3), w=[("xt", s3)])
            P.dma(lambda e, s2=s2, t0=t0: e.dma_start(out=rt[s2][:], in_=rope_f[t0:t0 + 128, :]), key=("rtd", s2), w=[("rtA", s2)])
            ln_tile(xt[s3][:], ("xt", s3), st, mv, rstd, nbt, hb[s2][:], ("hb", s2), "A")
            stg[0][tt] = P.end_capture()
            P.capture()
            for kc in range(8):
                P.pe(lambda e, kc=kc, s2=s2: e.transpose(out=pT[s2][:, kc, :], in_=hb[s2][:, kc * 128:(kc + 1) * 128], identity=ident_b[:]),
                     r=[("hb", s2), "ident_b"], w=[("pT", s2)])
            P.dve(lambda e, s2=s2: e.tensor_copy(out=hT[s2][:], in_=pT[s2][:]), r=[("pT", s2)], w=[("hT", s2)])
            stg1a[tt] = P.end_capture()
            P.capture()
            for kc in range(8):
                P.pe(lambda e, kc=kc, s2=s2, r=r: e.matmul(out=pKV[:], lhsT=hT[s2][:, kc, :], rhs=Wkv[r][:, kc, :], start=(kc == 0), stop=False),
                     r=[("hT", s2), ("Wkv", r)], w=["pKV"])
            P.pe(lambda e, bkv=bkv: e.matmul(out=pKV[:], lhsT=ones_b[0:1, :], rhs=bias_sb[0:1, bkv:bkv + 512], start=False, stop=True),
                 r=["ones_b", "bias_sb"], w=["pKV"])
            for kc in range(8):
                P.pe(lambda e, kc=kc, s2=s2, r=r: e.matmul(out=pU[:], lhsT=hT[s2][:, kc, :], rhs=Wu[r][:, kc, :], start=(kc == 0), stop=False),
                     r=[("hT", s2), ("Wu", r)], w=["pU"])
            P.pe(lambda e, bu=bu: e.matmul(out=pU[:], lhsT=ones_b[0:1, :], rhs=bias_sb[0:1, bu:bu + 512], start=False, stop=True),
                 r=["ones_b", "bias_sb"], w=["pU"])
            P.act(lambda e: e.activation(out=k_sb[:], in_=pKV[:, 0:256], func=ACT.Copy), r=["pKV"], w=["k_sb"])
            P.act(lambda e, s2=s2: e.activation(out=v_bf[s2][:], in_=pKV[:, 256:512], func=ACT.Copy), r=["pKV"], w=[("v_bf", s2)])
            P.dma(lambda e, s2=s2, t0=t0: e.dma_start(out=v_d[t0:t0 + 128, :], in_=v_bf[s2][:]), key=("vd", s2), r=[("v_bf", s2)], w=["v_d"])
            P.act(lambda e: e.activation(out=u_bf[:], in_=pU[:], func=ACT.Copy), r=["pU"], w=["u_bf"])
            for s_ in range(NSA):
                P.act(lambda e, s2=s2, s_=s_: e.activation(out=Um[s2][:, :, s_ * 16:(s_ + 1) * 16], in_=u_bf[:].rearrange("p (g c) -> p g c", c=16),
                                                         func=ACT.Copy, scale=mask8f[:, s_:s_ + 1]), r=["u_bf", "mask8f"], w=[("Um", s2, s_)])
            P.dve(lambda e, s2=s2: e.tensor_tensor(out=Um[s2][:, :, NSA * 16:128].rearrange("p g (s c) -> p g s c", c=16), in0=fv(u_bf[:], 0, [[16, 32], [0, 8 - NSA], [1, 16]]),
                                                   in1=fv(mask8[:], NSA, [[0, 32], [1, 8 - NSA], [0, 16]]), op=ALU.mult), r=["u_bf", "mask8"], w=[("Um", s2, "v")])
            rms_rope(P.pool, k_sb, 2, gk_bc, rt[s2], krA[s2], tmp, ["k_sb", ("rtA", s2), "gk_bc"], ("kr", s2), "A", eng2=P.dve, tmp2=tmp2_A)
            stg[1][tt] = P.end_capture()
            P.capture()
            for h in range(2):
                P.pe(lambda e, h=h, s2=s2: e.transpose(out=pKT[:, h, :], in_=krA[s2][:, h * 128:(h + 1) * 128], identity=ident_b[:]), r=[("kr", s2), "ident_b"], w=["pKT"])
            P.act(lambda e, s2=s2: e.activation(out=kT_sb[s2][:], in_=pKT[:], func=ACT.Copy), r=["pKT"], w=[("kT_sb", s2)])
            P.dma(lambda e, s2=s2, t0=t0: e.dma_start(out=kT_d[:, :, t0:t0 + 128].rearrange("h p t -> p h t"), in_=kT_sb[s2][:]),
                  key=("kTd", s2), r=[("kT_sb", s2)], w=["kT_d"])
            for g in range(32):
                P.pe(lambda e, g=g, s2=s2: e.matmul(out=pU8[:, g, :], lhsT=Um[s2][:, g, :], rhs=sel16[:], start=True, stop=True),
                     r=[("Um", s2, x_) for x_ in list(range(NSA)) + ["v"]] + ["sel16"], w=["pU8"])
            P.act(lambda e, tt=tt: e.activation(out=u8[:, :, tt * 16:(tt + 1) * 16], in_=pU8[:], func=ACT.Copy), r=["pU8"], w=["u8"])
            stg[2][tt] = P.end_capture()
        if PIPE == 0:
            for tt in range(NTA):
                P.ops.extend(stg[0][tt]); P.ops.extend(stg1a[tt]); P.ops.extend(stg[1][tt]); P.ops.extend(stg[2][tt])
        else:
            for step in range(NTA + 2):
                for lst, off in ((stg1a, 1), (stg[0], 0), (stg[2], 2), (stg[1], 1)):
                    j = step - off
                    if 0 <= j < NTA:
                        P.ops.extend(lst[j])
        dump("u8", u8[:], [128, 32, NJF], BF16, r=["u8"])
        if "kT" in dbg_names:
            o = nc.dram_tensor("dbg_kT", [2, 128, NFULL], BF16, kind="ExternalOutput").ap()
            dbg_out["kT"] = o
            P.dma(lambda e: e.dma_start(out=o, in_=kT_d), key="dbg_kT", r=["kT_d"], w=["dbg_kT"])
        if "v" in dbg_names:
            o2 = nc.dram_tensor("dbg_v", [NFULL, 256], BF16, kind="ExternalOutput").ap()
            dbg_out["v"] = o2
            P.dma(lambda e: e.dma_start(out=o2, in_=v_d), key="dbg_v", r=["v_d"], w=["dbg_v"])
        P.barrier()
    if STOP <= 1:
        esA.close()
        return finish(nc, P, ges, out, dbg_out)

    TWO_PI = 2.0 * math.pi
    MAGIC = 12582912.0
    esS = ExitStack()
    esH = ExitStack()
    esZ = ExitStack()
    S_re = SB(esH, "S_re", [128, 2, 16, NJ], F32)
    S_im = SB(esH, "S_im", [128, 2, 16, NJ], F32)
    erZ = SB(esZ, "erZ", [128, 2, 16, 64], F32)
    eiZ = SB(esZ, "eiZ", [128, 2, 16, 64], F32)
    erZ8 = SB(esS, "erZ8", [128, 2, 16, 8], F32)
    eiZ8 = SB(esS, "eiZ8", [128, 2, 16, 8], F32)
    erR = SB(esS, "erR", [128, 2, 16, 72], F32)
    eiR = SB(esS, "eiR", [128, 2, 16, 72], F32)
    bbre = SB(esS, "bbre", [128, 2, 16, 16], F32)
    bbim = SB(esS, "bbim", [128, 2, 16, 16], F32)
    ccre = SB(esS, "ccre", [128, 2, 16, 16], F32)
    ccim = SB(esS, "ccim", [128, 2, 16, 16], F32)
    A64r = SB(esS, "A64r", [128, 2, 16, 1], F32)
    A64i = SB(esS, "A64i", [128, 2, 16, 1], F32)
    ardt = SB(esS, "ardt", [128, 2, 16], F32)
    aidt = SB(esS, "aidt", [128, 2, 16], F32)
    ptmp = []
    piT = SB(esS, "piT", [128, 1], F32)

    def powtab(exps_ap, n, er_t, ei_t, tag):
        def v4(t):
            return t[:, :, :, 0:n]
        a_b = lambda t: fv(t[:], 0, [[16, 2], [1, 16], [0, n]])
        e_b = fv(exps_ap, 0, [[exps_ap.ap[1][0], 2], [0, 16], [1, n]])
        t0, t1, t2 = ptmp
        k = lambda i: ("ptmp", i)
        P.dve(lambda e: e.tensor_tensor(out=v4(t0), in0=a_b(ardt), in1=e_b, op=ALU.mult), r=["ardt", tag + "_e"], w=[k(0)])
        P.act(lambda e: e.activation(out=v4(t0), in_=v4(t0), func=ACT.Exp), r=[k(0)], w=[k(0)])
        P.dve(lambda e: e.tensor_tensor(out=v4(t1), in0=a_b(aidt), in1=e_b, op=ALU.mult), r=["aidt", tag + "_e"], w=[k(1)])
        P.dve(lambda e: e.tensor_scalar(out=v4(t2), in0=v4(t1), scalar1=MAGIC, scalar2=None, op0=ALU.add), r=[k(1)], w=[k(2)])
        P.dve(lambda e: e.tensor_scalar(out=v4(t2), in0=v4(t2), scalar1=MAGIC, scalar2=None, op0=ALU.subtract), r=[k(2)], w=[k(2)])
        P.dve(lambda e: e.tensor_tensor(out=v4(t2), in0=v4(t1), in1=v4(t2), op=ALU.subtract), r=[k(1), k(2)], w=[k(2)])
        P.act(lambda e: e.activation(out=v4(t2), in_=v4(t2), func=ACT.Sin, scale=TWO_PI), r=[k(2)], w=[k(2)])
        P.dve(lambda e: e.tensor_tensor(out=v4(ei_t), in0=v4(t0), in1=v4(t2), op=ALU.mult), r=[k(0), k(2)], w=[tag + "_ei"])
        P.dve(lambda e: e.tensor_scalar(out=v4(t1), in0=v4(t1), scalar1=0.25, scalar2=None, op0=ALU.add), r=[k(1)], w=[k(1)])
        P.dve(lambda e: e.tensor_scalar(out=v4(t2), in0=v4(t1), scalar1=MAGIC, scalar2=None, op0=ALU.add), r=[k(1)], w=[k(2)])
        P.dve(lambda e: e.tensor_scalar(out=v4(t2), in0=v4(t2), scalar1=MAGIC, scalar2=None, op0=ALU.subtract), r=[k(2)], w=[k(2)])
        P.dve(lambda e: e.tensor_tensor(out=v4(t2), in0=v4(t1), in1=v4(t2), op=ALU.subtract), r=[k(1), k(2)], w=[k(2)])
        P.act(lambda e: e.activation(out=v4(t2), in_=v4(t2), func=ACT.Sin, scale=TWO_PI), r=[k(2)], w=[k(2)])
        P.dve(lambda e: e.tensor_tensor(out=v4(er_t), in0=v4(t0), in1=v4(t2), op=ALU.mult), r=[k(0), k(2)], w=[tag + "_er"])

    with ExitStack() as es:
        ptmp.extend([SB(es, "ptmp%d" % i, [128, 2, 16, 72], F32) for i in range(3)])
        a_sb = SB(es, "a_sb", [128, 2, 2, 16], F32)
        ldt_sb = SB(es, "ldt_sb", [128, 2, 16], F32)
        b_sb = SB(es, "b_sb", [128, 2, 2, 16, 16], F32)
        c_sb = SB(es, "c_sb", [128, 2, 2, 16, 16], F32)
        eZ_sb = SB(es, "eZ_sb", [128, 2, 64], F32)
        eR_sb = SB(es, "eR_sb", [128, 2, 72], F32)
        e1_sb = SB(es, "e1_sb", [128, 2, 1], F32)
        e64_sb = SB(es, "e64_sb", [128, 2, 1], F32)
        abr = SB(es, "abr", [128, 2, 16, 1], F32)
        abi = SB(es, "abi", [128, 2, 16, 1], F32)
        dsc = [SB(es, "dsc%d" % i, [128, 2, 16], F32) for i in range(5)]
        bt = [SB(es, "bt%d" % i, [128, 2, 16, 16], F32) for i in range(2)]
        P.dma(lambda e: e.dma_start(out=a_sb[:], in_=s5_a), key="a_sb", w=["a_sb"])
        P.dma(lambda e: e.dma_start(out=ldt_sb[:], in_=s5_ldt), key="ldt_sb", w=["ldt_sb"])
        P.dma(lambda e: e.dma_start(out=b_sb[:], in_=s5_b), key="b_sb", w=["b_sb"])
        P.dma(lambda e: e.dma_start(out=c_sb[:], in_=s5_c), key="c_sb", w=["c_sb"])
        P.dma(lambda e: e.dma_start(out=eZ_sb[:], in_=cst_eZ), key="eZ_sb", w=["Z_e"])
        P.dma(lambda e: e.dma_start(out=eR_sb[:], in_=cst_eR), key="eR_sb", w=["R_e"])
        P.dve(lambda e: e.memset(e1_sb[:], 1.0), w=["ab_e"])
        P.dve(lambda e: e.memset(e64_sb[:], 64.0), w=["A64_e"])
        P.act(lambda e: e.activation(out=ldt_sb[:], in_=ldt_sb[:], func=ACT.Exp), r=["ldt_sb"], w=["ldt_sb"])
        P.dve(lambda e: e.tensor_tensor(out=ardt[:], in0=a_sb[:, 0], in1=ldt_sb[:], op=ALU.mult), r=["a_sb", "ldt_sb"], w=["ardt"])
        P.dve(lambda e: e.scalar_tensor_tensor(out=aidt[:], in0=a_sb[:, 1], scalar=1.0 / TWO_PI, in1=ldt_sb[:], op0=ALU.mult, op1=ALU.mult),
              r=["a_sb", "ldt_sb"], w=["aidt"])
        powtab(e1_sb[:], 1, abr, abi, "ab")
        powtab(e64_sb[:], 1, A64r, A64i, "A64")
        powtab(eZ_sb[:], 64, erZ, eiZ, "Z")
        powtab(eR_sb[:], 72, erR, eiR, "R")
        for (src_t, dst_t, kk) in ((erZ, erZ8, "Z_er"), (eiZ, eiZ8, "Z_ei")):
            P.dve(lambda e, src_t=src_t, dst_t=dst_t: e.tensor_copy(out=dst_t[:, 0], in_=src_t[:, 0, :, 56:64]), r=[kk], w=[kk + "8"])
            P.dve(lambda e, src_t=src_t, dst_t=dst_t: e.tensor_copy(out=dst_t[:, 1], in_=src_t[:, 1, :, 0:8]), r=[kk], w=[kk + "8"])
        are, aim = a_sb[:, 0], a_sb[:, 1]
        d0, d1, d2, d3, d4 = [t[:] for t in dsc]
        ab_r = abr[:].rearrange("p d q o -> p d (q o)")
        ab_i = abi[:].rearrange("p d q o -> p d (q o)")
        kd = lambda i: ("dsc", i)
        P.dve(lambda e: e.tensor_tensor(out=d0, in0=are, in1=are, op=ALU.mult), r=["a_sb"], w=[kd(0)])
        P.dve(lambda e: e.tensor_tensor(out=d1, in0=aim, in1=aim, op=ALU.mult), r=["a_sb"], w=[kd(1)])
        P.dve(lambda e: e.tensor_tensor(out=d0, in0=d0, in1=d1, op=ALU.add), r=[kd(0), kd(1)], w=[kd(0)])
        P.dve(lambda e: e.reciprocal(out=d0, in_=d0), r=[kd(0)], w=[kd(0)])
        P.dve(lambda e: e.tensor_scalar(out=d1, in0=ab_r, scalar1=-1.0, scalar2=None, op0=ALU.add), r=["ab_er"], w=[kd(1)])
        P.dve(lambda e: e.tensor_tensor(out=d2, in0=d1, in1=are, op=ALU.mult), r=[kd(1), "a_sb"], w=[kd(2)])
        P.dve(lambda e: e.tensor_tensor(out=d3, in0=ab_i, in1=aim, op=ALU.mult), r=["ab_ei", "a_sb"], w=[kd(3)])
        P.dve(lambda e: e.tensor_tensor(out=d2, in0=d2, in1=d3, op=ALU.add), r=[kd(2), kd(3)], w=[kd(2)])
        P.dve(lambda e: e.tensor_tensor(out=d2, in0=d2, in1=d0, op=ALU.mult), r=[kd(2), kd(0)], w=[kd(2)])
        P.dve(lambda e: e.tensor_tensor(out=d3, in0=ab_i, in1=are, op=ALU.mult), r=["ab_ei", "a_sb"], w=[kd(3)])
        P.dve(lambda e: e.tensor_tensor(out=d4, in0=d1, in1=aim, op=ALU.mult), r=[kd(1), "a_sb"], w=[kd(4)])
        P.dve(lambda e: e.tensor_tensor(out=d3, in0=d3, in1=d4, op=ALU.subtract), r=[kd(3), kd(4)], w=[kd(3)])
        P.dve(lambda e: e.tensor_tensor(out=d3, in0=d3, in1=d0, op=ALU.mult), r=[kd(3), kd(0)], w=[kd(3)])
        rr_b = fv(dsc[2][:], 0, [[16, 2], [1, 16], [0, 16]])
        ri_b = fv(dsc[3][:], 0, [[16, 2], [1, 16], [0, 16]])
        bre, bim = b_sb[:, 0], b_sb[:, 1]
        P.dve(lambda e: e.tensor_tensor(out=bt[0][:], in0=bre, in1=rr_b, op=ALU.mult), r=["b_sb", kd(2)], w=["bt0"])
        P.dve(lambda e: e.tensor_tensor(out=bt[1][:], in0=bim, in1=ri_b, op=ALU.mult), r=["b_sb", kd(3)], w=["bt1"])
        P.dve(lambda e: e.tensor_tensor(out=bbre[:], in0=bt[0][:], in1=bt[1][:], op=ALU.subtract), r=["bt0", "bt1"], w=["bbre"])
        P.dve(lambda e: e.tensor_tensor(out=bt[0][:], in0=bim, in1=rr_b, op=ALU.mult), r=["b_sb", kd(2)], w=["bt0"])
        P.dve(lambda e: e.tensor_tensor(out=bt[1][:], in0=bre, in1=ri_b, op=ALU.mult), r=["b_sb", kd(3)], w=["bt1"])
        P.dve(lambda e: e.tensor_tensor(out=bbim[:], in0=bt[0][:], in1=bt[1][:], op=ALU.add), r=["bt0", "bt1"], w=["bbim"])
        P.dve(lambda e: e.tensor_copy(out=ccre[:], in_=c_sb[:, 0]), r=["c_sb"], w=["ccre"])
        P.dve(lambda e: e.tensor_copy(out=ccim[:], in_=c_sb[:, 1]), r=["c_sb"], w=["ccim"])
        P.barrier()

    def ztab(eng_add, pair, tsl, nt, bufs, tag, erZ=erZ, eiZ=eiZ, tmkey=None):
        zre, zim, ztm = bufs
        tmkey = tmkey or (tag + "ztm")
        def ev(t, d):
            return fv(t[:, d, pair, tsl[d]:tsl[d] + nt], 0, [[1, nt], [0, 16]])
        def bv(t, d):
            return fv(t[:, d, pair, :], 0, [[0, nt], [1, 16]])
        for d in range(2):
            eng_add(lambda e, d=d: e.tensor_tensor(out=zre[:, d], in0=bv(bbre, d), in1=ev(erZ, d), op=ALU.mult), r=["Z_er", "bbre"], w=[(tag + "zre", d)])
            eng_add(lambda e, d=d: e.tensor_tensor(out=ztm[:, d], in0=bv(bbim, d), in1=ev(eiZ, d), op=ALU.mult), r=["Z_ei", "bbim"], w=[(tmkey, d)])
            eng_add(lambda e, d=d: e.tensor_tensor(out=zre[:, d], in0=zre[:, d], in1=ztm[:, d], op=ALU.subtract), r=[(tag + "zre", d), (tmkey, d)], w=[(tag + "zre", d)])
            eng_add(lambda e, d=d: e.tensor_tensor(out=zim[:, d], in0=bv(bbim, d), in1=ev(erZ, d), op=ALU.mult), r=["Z_er", "bbim"], w=[(tag + "zim", d)])
            eng_add(lambda e, d=d: e.tensor_tensor(out=ztm[:, d], in0=bv(bbre, d), in1=ev(eiZ, d), op=ALU.mult), r=["Z_ei", "bbre"], w=[(tmkey, d)])
            eng_add(lambda e, d=d: e.tensor_tensor(out=zim[:, d], in0=zim[:, d], in1=ztm[:, d], op=ALU.add), r=[(tag + "zim", d), (tmkey, d)], w=[(tag + "zim", d)])

    with ExitStack() as es:
        Wsum = [SB(es, "Wsum%d" % i, [128, 2, 8, 2, 128], BF16) for i in range(2)]
        zb = [[SB(es, "zb%d_%d" % (i, j), [128, 2, 64, 16], F32) for j in range(3)] for i in range(1)]
        pW = [PS(es, "pW%d" % i, [128, 4, 128], F32) for i in range(2)]
        pSr = PS(es, "pSr", [128, 2, NJ], F32)
        pSi = PS(es, "pSi", [128, 2, NJ], F32)
        ci = 0
        for pair in range(16):
            sl = int(os.environ.get('K_SL', pair % 2))
            ea = P.dve
            zt_ = "z0"
            ztab(ea, pair, (0, 0), 64, zb[0], zt_)
            zre, zim, _ = zb[0]
            for d in range(2):
                for reim in range(2):
                    zt = zre if reim == 0 else zim
                    for mq in range(2):
                        pw = pW[ci % 2]
                        for j in range(4):
                            m_ = mq * 4 + j
                            P.pe(lambda e, zt=zt, d=d, m_=m_, pw=pw, j=j: e.transpose(out=pw[:, j, :], in_=zt[:, d, m_ * 8:(m_ + 1) * 8, :].rearrange("p s c -> p (s c)"), identity=ident_f[:]),
                                 r=[(zt_ + "z%s" % ("re" if reim == 0 else "im"), d), "ident_f"], w=[("pW", ci % 2)])
                        dst = Wsum[sl][:, d, mq * 4:(mq + 1) * 4, reim, :]
                        P.act(lambda e, dst=dst, pw=pw: e.activation(out=dst, in_=pw[:], func=ACT.Copy), r=[("pW", ci % 2)], w=[("Wsum", sl)])
                        ci += 1
            for d in range(2):
                for gh in range(2):
                    g = 2 * pair + gh
                    for reim, pS_ in ((0, pSr), (1, pSi)):
                        for m_ in range(8):
                            P.pe(lambda e, d=d, gh=gh, g=g, reim=reim, pS_=pS_, m_=m_, sl=sl: e.matmul(
                                out=pS_[64 * gh:64 * gh + 64, d, :], lhsT=Wsum[sl][:, d, m_, reim, 64 * gh:64 * gh + 64],
                                rhs=fv(u8[:, g, :], m_, [[8, NJ]]), start=(m_ == 0), stop=(m_ == 7)),
                                r=[("Wsum", sl), "u8"], w=["pSr" if reim == 0 else "pSi"])
            P.act(lambda e, pair=pair: e.activation(out=S_re[:, :, pair, :], in_=pSr[:], func=ACT.Copy), r=["pSr"], w=["S_re"])
            P.act(lambda e, pair=pair: e.activation(out=S_im[:, :, pair, :], in_=pSi[:], func=ACT.Copy), r=["pSi"], w=["S_im"])
        P.barrier()
    esA.close()
    esZ.close()

    with ExitStack() as es:
        sct = [[SB(es, "sct%d_%d" % (d, i), [128, 16], F32) for i in range(4)] for d in range(2)]
        orders = [[(J, J - 1 if J > 0 else None) for J in range(NJ)],
                  [(3, None), (2, 3), (1, 2), (0, 1), (131, 0)] + [(J, J + 1) for J in range(130, 3, -1)]]
        for d in range(2):
            ea = P.dve if d == 0 else P.pool
            Ar = A64r[:, d, :, 0]
            Ai = A64i[:, d, :, 0]
            t = [x[:] for x in sct[d]]
            kt = lambda i: ("sct", d, i)
            kr_, ki_ = ("Hre", d), ("Him", d)
            for (J, Jp) in orders[d]:
                if Jp is None:
                    continue
                hr_p, hi_p = S_re[:, d, :, Jp], S_im[:, d, :, Jp]
                hr, hi = S_re[:, d, :, J], S_im[:, d, :, J]
                ea(lambda e, t=t, hr_p=hr_p, Ar=Ar: e.tensor_tensor(out=t[0], in0=Ar, in1=hr_p, op=ALU.mult), r=["A64_er", kr_, "S_re"], w=[kt(0)])
                ea(lambda e, t=t, hi_p=hi_p, Ai=Ai: e.tensor_tensor(out=t[1], in0=Ai, in1=hi_p, op=ALU.mult), r=["A64_ei", ki_, "S_im"], w=[kt(1)])
                ea(lambda e, t=t: e.tensor_tensor(out=t[0], in0=t[0], in1=t[1], op=ALU.subtract), r=[kt(0), kt(1)], w=[kt(0)])
                ea(lambda e, t=t, hi_p=hi_p, Ar=Ar: e.tensor_tensor(out=t[2], in0=Ar, in1=hi_p, op=ALU.mult), r=["A64_er", ki_, "S_im"], w=[kt(2)])
                ea(lambda e, t=t, hr_p=hr_p, Ai=Ai: e.tensor_tensor(out=t[3], in0=Ai, in1=hr_p, op=ALU.mult), r=["A64_ei", kr_, "S_re"], w=[kt(3)])
                ea(lambda e, t=t: e.tensor_tensor(out=t[2], in0=t[2], in1=t[3], op=ALU.add), r=[kt(2), kt(3)], w=[kt(2)])
                ea(lambda e, t=t, hr=hr: e.tensor_tensor(out=hr, in0=hr, in1=t[0], op=ALU.add), r=[kt(0), kr_], w=[kr_])
                ea(lambda e, t=t, hi=hi: e.tensor_tensor(out=hi, in0=hi, in1=t[2], op=ALU.add), r=[kt(2), ki_], w=[ki_])
        P.barrier()
    dump("H_re", S_re[:], [128, 2, 16, NJ])
    dump("H_im", S_im[:], [128, 2, 16, NJ])
    HO = [[SB(esS, "HO%d_%d" % (d, x), [128, 16, 32], BF16) for x in range(2)] for d in range(2)]
    with ExitStack() as es:
        cm = SB(es, "cm", [128, 4], F32)
        hacc = SB(es, "hacc", [128, 16, 32], F32)
        P.dma(lambda e: e.dma_start(out=cm[:], in_=cmask), key="cm", w=["cm"])
        for d in range(2):
            for x, Sx in enumerate((S_re, S_im)):
                for r_ in range(4):
                    lo = (3 + 32 * r_) if d == 0 else (5 + 32 * r_)
                    segs = [(lo, 0, 32)] if not (d == 1 and r_ == 3) else [(lo, 0, 31), (0, 31, 1)]
                    for (a0, o0, n_) in segs:
                        src_ = Sx[:, d, :, a0:a0 + n_]
                        dst_ = hacc[:, :, o0:o0 + n_]
                        if r_ == 0:
                            P.dve(lambda e, src_=src_, dst_=dst_: e.tensor_scalar(out=dst_, in0=src_, scalar1=cm[:, 0:1], scalar2=None, op0=ALU.mult),
                                  r=["cm"], w=["hacc"])
                        else:
                            P.dve(lambda e, src_=src_, dst_=dst_, r_=r_: e.scalar_tensor_tensor(out=dst_, in0=src_, scalar=cm[:, r_:r_ + 1], in1=dst_, op0=ALU.mult, op1=ALU.add),
                                  r=["cm", "hacc"], w=["hacc"])
                P.dve(lambda e, d=d, x=x: e.tensor_copy(out=HO[d][x][:], in_=hacc[:]), r=["hacc"], w=[("HO", d, x)])
        P.barrier()
    esH.close()
    if STOP <= 2:
        esS.close()
        return finish(nc, P, ges, out, dbg_out)

    esP = ExitStack()
    hTo = SB(esP, "hTo", [128, 8, NOWN], BF16)
    qT = SB(esP, "qT", [128, 8, NOWN], BF16)
    esU = ExitStack()
    u8o = SB(esU, "u8o", [128, 32, 256], BF16)

    def load_bc(dst, src_row, key, add_one=False):
        P.dma(lambda e: e.dma_start(out=dst[:], in_=src_row.partition_broadcast(128)), key=key, w=[key])
        if add_one:
            P.dve(lambda e: e.tensor_scalar(out=dst[:], in0=dst[:], scalar1=1.0, scalar2=None, op0=ALU.add), r=[key], w=[key])

    with ExitStack() as es:
        Wq = SB(es, "Wq", [128, 8, 1024], BF16)
        Wuo = SB(es, "Wuo", [128, 8, 512], BF16)
        sc1p_bc = SB(es, "sc1p_bc", [128, D], F32)
        sh1_bc = SB(es, "sh1_bc", [128, D], F32)
        xt_B = [SB(es, "xtB%d" % i, [128, D], F32) for i in range(2)]
        rt_B = [SB(es, "rtB%d" % i, [128, 128], F32) for i in range(2)]
        hn = SB(es, "hn", [128, D], F32)
        hb_B = [SB(es, "hbB%d" % i, [128, D], BF16) for i in range(2)]
        st_B = SB(es, "stB", [128, 2, 6], F32)
        mv_B = SB(es, "mvB", [128, 2], F32)
        rstd_B = SB(es, "rstdB", [128, 1], F32)
        nbt_B = SB(es, "nbtB", [128, 1], F32)
        q_sb = SB(es, "q_sb", [128, 1024], F32)
        qr = SB(es, "qr", [128, 1024], BF16)
        tmp_B = (SB(es, "sqB", [128, 1024], F32), SB(es, "ssB", [128, 8], F32), SB(es, "rkB", [128, 8], F32),
               SB(es, "taB", [128, 512], F32), SB(es, "tbB", [128, 512], F32))
        u_bf_B = SB(es, "u_bfB", [128, 512], BF16)
        Um_B = [SB(es, "UmB0", [128, 32, 128], BF16)] * 2
        pT_B = [PS(es, "pTB%d" % i, [128, 8, 128], BF16) for i in range(2)]
        pQ = [PS(es, "pQ%d" % i, [128, 512], F32) for i in range(2)]
        pQT = PS(es, "pQT", [128, 8, 128], BF16)
        pU_B = PS(es, "pUB", [128, 512], F32)
        pU8_B = PS(es, "pU8B", [128, 32, 16], F32)
        P.dma(lambda e: e.dma_start(out=Wq[:], in_=w_in[:, 512:1536].rearrange("(kc p) n -> p kc n", p=128)), key="Wq", w=["Wq"], eng="pool")
        P.dma(lambda e: e.dma_start(out=Wuo[:], in_=w_in[:, 0:512].rearrange("(kc p) n -> p kc n", p=128)), key="Wuo", w=["Wuo"], eng="pool")
        load_bc(sc1p_bc, mod_d[0:1, 1024:2048], "sc1p_bc", True)
        load_bc(sh1_bc, mod_d[0:1, 0:1024], "sh1_bc")
        for tt in range(NTO):
            s2 = tt % 2
            t0 = tt * 128
            P.dma(lambda e, s2=s2, t0=t0: e.dma_start(out=xt_B[s2][:], in_=xo[t0:t0 + 128, :]), key=("xtB", s2), w=[("xtB", s2)])
            P.dma(lambda e, s2=s2, t0=t0: e.dma_start(out=rt_B[s2][:], in_=rope_o[t0:t0 + 128, :]), key=("rtB", s2), w=[("rtB", s2)])
            ln_tile(xt_B[s2][:], ("xtB", s2), st_B, mv_B, rstd_B, nbt_B, hn[:], "hn", "B")
            P.dve(lambda e: e.tensor_tensor(out=hn[:], in0=hn[:], in1=sc1p_bc[:], op=ALU.mult), r=["hn", "sc1p_bc"], w=["hn"])
            P.dve(lambda e, s2=s2: e.tensor_tensor(out=hb_B[s2][:], in0=hn[:], in1=sh1_bc[:], op=ALU.add), r=["hn", "sh1_bc"], w=[("hbB", s2)])
            for kc in range(8):
                P.pe(lambda e, kc=kc, s2=s2: e.transpose(out=pT_B[s2][:, kc, :], in_=hb_B[s2][:, kc * 128:(kc + 1) * 128], identity=ident_b[:]),
                     r=[("hbB", s2), "ident_b"], w=[("pTB", s2)])
            P.act(lambda e, s2=s2, t0=t0: e.activation(out=hTo[:, :, t0:t0 + 128], in_=pT_B[s2][:], func=ACT.Copy), r=[("pTB", s2)], w=["hTo"])
            for half in range(2):
                for kc in range(8):
                    P.pe(lambda e, kc=kc, half=half, t0=t0: e.matmul(out=pQ[half][:], lhsT=hTo[:, kc, t0:t0 + 128], rhs=Wq[:, kc, half * 512:(half + 1) * 512],
                                                                     start=(kc == 0), stop=(kc == 7)), r=["hTo", "Wq"], w=[("pQ", half)])
                P.act(lambda e, half=half: e.activation(out=q_sb[:, half * 512:(half + 1) * 512], in_=pQ[half][:], func=ACT.Copy), r=[("pQ", half)], w=["q_sb"])
            for kc in range(8):
                P.pe(lambda e, kc=kc, t0=t0: e.matmul(out=pU_B[:], lhsT=hTo[:, kc, t0:t0 + 128], rhs=Wuo[:, kc, :], start=(kc == 0), stop=(kc == 7)),
                     r=["hTo", "Wuo"], w=["pUB"])
            rms_rope(P.pool, q_sb, 8, gq_bc, rt_B[s2], qr, tmp_B, ["q_sb", ("rtB", s2), "gq_bc"], "qr", "B")
            for h in range(8):
                P.pe(lambda e, h=h: e.transpose(out=pQT[:, h, :], in_=qr[:, h * 128:(h + 1) * 128], identity=ident_b[:]), r=["qr", "ident_b"], w=["pQT"])
            P.act(lambda e, t0=t0: e.activation(out=qT[:, :, t0:t0 + 128], in_=pQT[:], func=ACT.Copy), r=["pQT"], w=["qT"])
            P.act(lambda e: e.activation(out=u_bf_B[:], in_=pU_B[:], func=ACT.Copy), r=["pUB"], w=["u_bfB"])
            P.dve(lambda e, s2=s2: e.tensor_tensor(out=Um_B[s2][:].rearrange("p g (s c) -> p g s c", c=16), in0=fv(u_bf_B[:], 0, [[16, 32], [0, 8], [1, 16]]),
                                                   in1=fv(mask8[:], 0, [[0, 32], [1, 8], [0, 16]]), op=ALU.mult), r=["u_bfB", "mask8"], w=["UmB"])
            for g in range(32):
                P.pe(lambda e, g=g, s2=s2: e.matmul(out=pU8_B[:, g, :], lhsT=Um_B[s2][:, g, :], rhs=sel16[:], start=True, stop=True),
                     r=["UmB", "sel16"], w=["pU8B"])
            P.act(lambda e, tt=tt: e.activation(out=u8o[:, :, tt * 16:(tt + 1) * 16], in_=pU8_B[:], func=ACT.Copy), r=["pU8B"], w=["u8o"])
        P.barrier()
    if "qT" in dbg_names:
        dump("qT", qT[:], [128, 8, NOWN], BF16)
    if STOP <= 3:
        esU.close(); esS.close(); esP.close()
        return finish(nc, P, ges, out, dbg_out)

    gT = SB(esP, "gT", [128, 4, NOWN], BF16)
    with ExitStack() as es:
        mf_sb = SB(es, "mf_sb", [128, 128], F32)
        mb_sb = SB(es, "mb_sb", [128, 128], F32)
        dcol_sb = SB(es, "dcol_sb", [128, 32], F32)
        selg_b = SB(es, "selg_b", [128, 8, 128], BF16)
        m8c_b = SB(es, "m8c_b", [128, 8], BF16)
        z8 = [SB(es, "z8_%d" % j, [128, 2, 8, 16], F32) for j in range(3)]
        Rre_p = SB(es, "Rre_p", [128, 2, 72, 16], F32)
        Rim_p = SB(es, "Rim_p", [128, 2, 72, 16], F32)
        Rt0 = SB(es, "Rt0", [128, 1, 72, 16], F32)
        Rt1 = SB(es, "Rt1", [128, 1, 72, 16], F32)
        Rre_b = SB(es, "Rre_b", [128, 2, 1152], BF16)
        Rim_b = SB(es, "Rim_b", [128, 2, 1152], BF16)
        Tw = [SB(es, "Tw0", [128, 2, 15, 128], BF16)] * 2
        tt0 = SB(es, "tt0", [128, 128], F32)
        tt1 = SB(es, "tt1", [128, 128], F32)
        Ye = [SB(es, "Ye0", [128, 2048], BF16)] * 2
        ysb = SB(es, "ysb", [128, 1024], F32)
        yx2 = SB(es, "yx2", [128, 1024], F32)
        pTb = [PS(es, "pTb%d" % i, [128, 128], F32) for i in range(2)]
        pY8 = [PS(es, "pY8_%d" % i, [128, 8, 32], F32) for i in range(2)]
        pYT = PS(es, "pYT", [128, 2048], F32)
        P.dma(lambda e: e.dma_start(out=mf_sb[:], in_=cst_mf), key="mf_sb", w=["mf_sb"])
        P.dma(lambda e: e.dma_start(out=mb_sb[:], in_=cst_mb), key="mb_sb", w=["mb_sb"])
        P.dma(lambda e: e.dma_start(out=dcol_sb[:], in_=s5_dcol), key="dcol_sb", w=["dcol_sb"])
        P.dma(lambda e: e.dma_start(out=selg_b[:], in_=cst_selg), key="selg_b", w=["selg_b"], eng="pool")
        P.dma(lambda e: e.dma_start(out=m8c_b[:], in_=cst_mask8c), key="m8c_b", w=["m8c_b"], eng="pool")
        ecnt = 0
        for pair in range(16):
            sl = 0
            ztab(P.dve, pair, (0, 0), 8, z8, "z8", erZ=erZ8, eiZ=eiZ8)
            for d in range(2):
                eb = lambda t, d=d, pair=pair: fv(t[:, d, pair, :], 0, [[1, 72], [0, 16]])
                cb = lambda t, d=d, pair=pair: fv(t[:, d, pair, :], 0, [[0, 72], [1, 16]])
                P.pool(lambda e, d=d, eb=eb, cb=cb: e.tensor_tensor(out=Rre_p[:, d], in0=cb(ccre), in1=eb(erR), op=ALU.mult), r=["R_er", "ccre"], w=["Rre_p"])
                P.pool(lambda e, d=d, eb=eb, cb=cb: e.tensor_tensor(out=Rt0[:, 0], in0=cb(ccim), in1=eb(eiR), op=ALU.mult), r=["R_ei", "ccim"], w=["Rt0"])
                P.pool(lambda e, d=d: e.tensor_tensor(out=Rre_p[:, d], in0=Rre_p[:, d], in1=Rt0[:, 0], op=ALU.subtract), r=["Rre_p", "Rt0"], w=["Rre_p"])
                P.dve(lambda e, d=d, eb=eb, cb=cb: e.tensor_tensor(out=Rim_p[:, d], in0=cb(ccim), in1=eb(erR), op=ALU.mult), r=["R_er", "ccim"], w=["Rim_p"])
                P.dve(lambda e, d=d, eb=eb, cb=cb: e.tensor_tensor(out=Rt1[:, 0], in0=cb(ccre), in1=eb(eiR), op=ALU.mult), r=["R_ei", "ccre"], w=["Rt1"])
                P.dve(lambda e, d=d: e.scalar_tensor_tensor(out=Rim_p[:, d], in0=Rt1[:, 0], scalar=-1.0, in1=Rim_p[:, d], op0=ALU.mult, op1=ALU.subtract),
                      r=["Rim_p", "Rt1"], w=["Rim_p"])
            P.act(lambda e: e.activation(out=Rre_b[:], in_=Rre_p[:].rearrange("p d i c -> p d (i c)"), func=ACT.Copy), r=["Rre_p"], w=["Rre_b"])
            P.act(lambda e: e.activation(out=Rim_b[:], in_=Rim_p[:].rearrange("p d i c -> p d (i c)"), func=ACT.Copy), r=["Rim_p"], w=["Rim_b"])
            for gh in range(2):
                g = 2 * pair + gh
                rows = slice(64 * gh, 64 * gh + 64)
                blocks = [(0, 0), (1, 0)] + [(0, k) for k in range(1, 8)] + [(1, k) for k in range(1, 8)]
                for (d, k) in blocks:
                    pt = pTb[ecnt % 2]
                    kk = ("pTb", ecnt % 2)
                    P.pe(lambda e, pt=pt, d=d, k=k, rows=rows: e.matmul(out=pt[:], lhsT=z8[0][rows, d].rearrange("p s c -> p (s c)"),
                                                                        rhs=Rre_p[rows, d, 8 * k:8 * k + 8, :].rearrange("p s c -> p (s c)"), start=True, stop=False),
                         r=[("z8zre", d), "Rre_p"], w=[kk])
                    P.pe(lambda e, pt=pt, d=d, k=k, rows=rows: e.matmul(out=pt[:], lhsT=z8[1][rows, d].rearrange("p s c -> p (s c)"),
                                                                        rhs=Rim_p[rows, d, 8 * k:8 * k + 8, :].rearrange("p s c -> p (s c)"), start=False, stop=True),
                         r=[("z8zim", d), "Rim_p"], w=[kk])
                    if k == 0 and d == 0:
                        P.dve(lambda e, pt=pt: e.tensor_tensor(out=tt0[:], in0=pt[:], in1=mf_sb[:], op=ALU.mult), r=[kk, "mf_sb"], w=["tt0"])
                    elif k == 0 and d == 1:
                        P.dve(lambda e, pt=pt: e.tensor_tensor(out=tt1[:], in0=pt[:], in1=mb_sb[:], op=ALU.mult), r=[kk, "mb_sb"], w=["tt1"])
                        P.dve(lambda e: e.tensor_tensor(out=tt0[:], in0=tt0[:], in1=tt1[:], op=ALU.add), r=["tt0", "tt1"], w=["tt0"])
                        P.dve(lambda e, g=g, gh=gh, sl=sl: e.scalar_tensor_tensor(out=Tw[sl][:, gh, 7, :], in0=ident_f[:], scalar=dcol_sb[:, g:g + 1], in1=tt0[:],
                                                                                 op0=ALU.mult, op1=ALU.add), r=["tt0", "dcol_sb", "ident_f"], w=[("Tw", sl)])
                    else:
                        idx = 7 + k if d == 0 else 7 - k
                        if ecnt % 2 == 0:
                            P.act(lambda e, pt=pt, gh=gh, sl=sl, idx=idx: e.activation(out=Tw[sl][:, gh, idx, :], in_=pt[:], func=ACT.Copy), r=[kk], w=[("Tw", sl)])
                        else:
                            P.dve(lambda e, pt=pt, gh=gh, sl=sl, idx=idx: e.tensor_copy(out=Tw[sl][:, gh, idx, :], in_=pt[:]), r=[kk], w=[("Tw", sl)])
                    ecnt += 1
            for gh in range(2):
                g = 2 * pair + gh
                rows = slice(64 * gh, 64 * gh + 64)
                py = pY8[g % 2]
                ky = ("pY8", g % 2)
                for m in range(8):
                    for m_ in range(8):
                        P.pe(lambda e, py=py, m=m, m_=m_, gh=gh, g=g, sl=sl: e.matmul(out=py[:, m, :], lhsT=Tw[sl][:, gh, 7 + m - m_, :],
                                                                                     rhs=fv(u8o[:, g, :], m_, [[8, 32]]), start=(m_ == 0), stop=False),
                             r=[("Tw", sl), "u8o"], w=[ky])
                    fo = 8 * (m + 1) * 16
                    bo = 8 * (8 - m) * 16
                    P.pe(lambda e, py=py, m=m, rows=rows, fo=fo, pair=pair: e.matmul(out=py[:, m, :], lhsT=Rre_b[rows, 0, fo:fo + 128], rhs=HO[0][0][rows, pair, :], start=False, stop=False),
                         r=["Rre_b", ("HO", 0, 0)], w=[ky])
                    P.pe(lambda e, py=py, m=m, rows=rows, fo=fo, pair=pair: e.matmul(out=py[:, m, :], lhsT=Rim_b[rows, 0, fo:fo + 128], rhs=HO[0][1][rows, pair, :], start=False, stop=False),
                         r=["Rim_b", ("HO", 0, 1)], w=[ky])
                    P.pe(lambda e, py=py, m=m, rows=rows, bo=bo, pair=pair: e.matmul(out=py[:, m, :], lhsT=Rre_b[rows, 1, bo:bo + 128], rhs=HO[1][0][rows, pair, :], start=False, stop=False),
                         r=["Rre_b", ("HO", 1, 0)], w=[ky])
                    P.pe(lambda e, py=py, m=m, rows=rows, bo=bo, pair=pair: e.matmul(out=py[:, m, :], lhsT=Rim_b[rows, 1, bo:bo + 128], rhs=HO[1][1][rows, pair, :], start=False, stop=True),
                         r=["Rim_b", ("HO", 1, 1)], w=[ky])
                ye = Ye[g % 2]
                P.dve(lambda e, py=py, ye=ye: e.tensor_tensor(out=ye[:].rearrange("p (j m s) -> p j m s", m=8, s=8), in0=fv(py[:], 0, [[1, 32], [32, 8], [0, 8]]),
                                                              in1=fv(m8c_b[:], 0, [[0, 32], [0, 8], [1, 8]]), op=ALU.mult), r=[ky, "m8c_b"], w=["Ye"])
                for c4 in range(4):
                    P.pe(lambda e, ye=ye, g=g, c4=c4: e.matmul(out=pYT[:, c4 * 512:(c4 + 1) * 512], lhsT=selg_b[:, g % 8, :], rhs=ye[:, c4 * 512:(c4 + 1) * 512],
                                                              start=(g % 8 == 0), stop=(g % 8 == 7)), r=["Ye", "selg_b"], w=["pYT"])
            if pair % 4 == 3:
                tile_ = pair // 4
                for hf in range(2):
                    cs = slice(hf * 1024, (hf + 1) * 1024)
                    P.act(lambda e, cs=cs: e.activation(out=ysb[:], in_=pYT[:, cs], func=ACT.Copy), r=["pYT"], w=["ysb"])
                    P.dve(lambda e: e.tensor_tensor(out=yx2[:], in0=ysb[:], in1=ysb[:], op=ALU.mult), r=["ysb"], w=["yx2"])
                    P.dve(lambda e: e.tensor_scalar(out=yx2[:], in0=yx2[:], scalar1=0.044715, scalar2=1.0, op0=ALU.mult, op1=ALU.add), r=["yx2"], w=["yx2"])
                    P.dve(lambda e: e.tensor_tensor(out=yx2[:], in0=yx2[:], in1=ysb[:], op=ALU.mult), r=["yx2", "ysb"], w=["yx2"])
                    P.act(lambda e: e.activation(out=yx2[:], in_=yx2[:], func=ACT.Sigmoid, scale=1.5957691216057308), r=["yx2"], w=["yx2"])
                    P.dve(lambda e, tile_=tile_, cs=cs: e.tensor_tensor(out=gT[:, tile_, cs], in0=ysb[:], in1=yx2[:], op=ALU.mult), r=["ysb", "yx2"], w=["gT"])
        P.barrier()
    esU.close()
    esS.close()
    if "gT" in dbg_names:
        dump("gT", gT[:], [128, 4, NOWN], BF16)
    if STOP <= 4:
        esP.close()
        return finish(nc, P, ges, out, dbg_out)

    oT = qT
    NKC = NFULL // 128
    SCALE = 128.0 ** -0.5
    with ExitStack() as es:
        kT_all = SB(es, "kT_all", [128, 2, NFULL], BF16)
        V_aug = SB(es, "V_aug", [128, NKC, 2, 132], BF16)
        pTt = [SB(es, "pTt%d" % i, [128, 512], BF16) for i in range(3)]
        rden = SB(es, "rden", [128, 4], F32)
        on_b = SB(es, "on_b", [128, 4, 128], BF16)
        pS = [PS(es, "pS%d" % i, [128, 512], F32) for i in range(2)]
        pO = [PS(es, "pO%d" % i, [128, 512], F32) for i in range(4)]
        pOT = PS(es, "pOT", [128, 4, 128], BF16)
        P.dve(lambda e: e.memset(V_aug[:], 1.0), w=["V_aug"])
        for h2 in range(2):
            P.dma(lambda e, h2=h2: e.dma_start(out=kT_all[:, h2, :], in_=kT_d[h2]), key=("kT_all", h2), r=["kT_d"], w=["kT_all"])
            P.dma(lambda e, h2=h2: e.dma_start(out=V_aug[:, :, h2, 0:128], in_=v_d[:, h2 * 128:(h2 + 1) * 128].rearrange("(c p) d -> p c d", p=128)),
                  key=("V_aug", h2), r=["v_d", "V_aug"], w=["V_aug"])
        iters = [(h, qc, kc) for h in range(8) for qc in range(4) for kc in range(NKC)]

        def emit_S(i):
            h, qc, kc = iters[i]
            kvh = h // 4
            s2 = i % 2
            qs = slice(qc * 512, (qc + 1) * 512)
            P.pe(lambda e, s2=s2, kvh=kvh, kc=kc, h=h, qs=qs: e.matmul(out=pS[s2][:], lhsT=kT_all[:, kvh, kc * 128:(kc + 1) * 128], rhs=qT[:, h, qs], start=True, stop=True),
                 r=["kT_all", ("qTc", h, qc)], w=[("pS", s2)])

        emit_S(0)
        for i, (h, qc, kc) in enumerate(iters):
            kvh = h // 4
            s2, s3 = i % 2, i % 3
            qs = slice(qc * 512, (qc + 1) * 512)
            if i + 1 < len(iters):
                emit_S(i + 1)
            P.act(lambda e, s2=s2, s3=s3: e.activation(out=pTt[s3][:], in_=pS[s2][:], func=ACT.Exp, bias=negC[:], scale=SCALE),
                  r=[("pS", s2), "negC"], w=[("pTt", s3)])
            for qi in range(4):
                P.pe(lambda e, s3=s3, qi=qi, kc=kc, kvh=kvh: e.matmul(out=pO[qi][:, 0:129], lhsT=pTt[s3][:, qi * 128:(qi + 1) * 128], rhs=V_aug[:, kc, kvh, 0:129],
                                                                     start=(kc == 0), stop=(kc == NKC - 1)), r=[("pTt", s3), "V_aug"], w=[("pO", qi)])
            if kc == NKC - 1:
                for qi in range(4):
                    P.dve(lambda e, qi=qi: e.reciprocal(out=rden[:, qi:qi + 1], in_=pO[qi][:, 128:129]), r=[("pO", qi)], w=[("rden", qi)])
                    P.dve(lambda e, qi=qi: e.tensor_scalar(out=on_b[:, qi, :], in0=pO[qi][:, 0:128], scalar1=rden[:, qi:qi + 1], scalar2=None, op0=ALU.mult),
                          r=[("pO", qi), ("rden", qi)], w=[("on_b", qi)])
                    P.pe(lambda e, qi=qi: e.transpose(out=pOT[:, qi, :], in_=on_b[:, qi, :], identity=ident_b[:]), r=[("on_b", qi), "ident_b"], w=["pOT"])
                P.dve(lambda e, h=h, qs=qs: e.tensor_copy(out=oT[:, h, qs], in_=pOT[:].rearrange("p a b -> p (a b)")), r=["pOT"], w=[("qTc", h, qc)])
        P.barrier()
    if "oT" in dbg_names:
        dump("oT", oT[:], [128, 8, NOWN], BF16)
    if STOP <= 5:
        esP.close()
        return finish(nc, P, ges, out, dbg_out)

    esM = ExitStack()
    mT_all = SB(esM, "mT_all", [128, 8, NOWN], BF16)
    with ExitStack() as es:
        Wg = SB(es, "Wg", [128, 8, 2048], BF16)
        Wa = SB(es, "Wa", [128, 4, 1024], BF16)
        Wb = SB(es, "Wb", [128, 4, 1024], BF16)
        Wo = SB(es, "Wo", [128, 8, 1024], BF16)
        sg1 = SB(es, "sg1", [128, 512], F32)
        sg2 = SB(es, "sg2", [128, 512], F32)
        sgb = SB(es, "sgb", [128, 512], F32)
        mt1 = SB(es, "mt1", [128, 512], F32)
        mt2 = SB(es, "mt2", [128, 512], F32)
        pG1 = PS(es, "pG1", [128, 512], F32)
        pG2 = PS(es, "pG2", [128, 512], F32)
        pA_D = PS(es, "pA_D", [128, 512], F32)
        pB_D = PS(es, "pB_D", [128, 512], F32)
        pC_D = PS(es, "pC_D", [128, 512], F32)
        for c2 in range(2):
            P.dma(lambda e, c2=c2: e.dma_start(out=Wg[:, :, c2 * 1024:(c2 + 1) * 1024], in_=w_in[:, 2048 + c2 * 1024:2048 + (c2 + 1) * 1024].rearrange("(kc p) n -> p kc n", p=128)),
                  key=("Wg", c2), w=["Wg"], eng="pool")
        P.dma(lambda e: e.dma_start(out=Wa[:], in_=w_glu_a.rearrange("(kc p) n -> p kc n", p=128)), key="Wa", w=["Wa"], eng="pool")
        P.dma(lambda e: e.dma_start(out=Wb[:], in_=w_glu_b.rearrange("(kc p) n -> p kc n", p=128)), key="Wb", w=["Wb"], eng="pool")
        P.dma(lambda e: e.dma_start(out=Wo[:], in_=w_attn_o.rearrange("(kc p) n -> p kc n", p=128)), key="Wo", w=["Wo"], eng="pool")
        for st_ in range(4):
            ts = slice(st_ * 512, (st_ + 1) * 512)
            for dt_ in range(8):
                ds = slice(dt_ * 128, (dt_ + 1) * 128)
                ds2 = slice(1024 + dt_ * 128, 1024 + (dt_ + 1) * 128)
                for kc in range(8):
                    P.pe(lambda e, kc=kc, ds=ds, ts=ts: e.matmul(out=pG1[:], lhsT=Wg[:, kc, ds], rhs=hTo[:, kc, ts], start=(kc == 0), stop=(kc == 7)), r=["Wg", "hTo"], w=["pG1"])
                for kc in range(8):
                    P.pe(lambda e, kc=kc, ds2=ds2, ts=ts: e.matmul(out=pG2[:], lhsT=Wg[:, kc, ds2], rhs=hTo[:, kc, ts], start=(kc == 0), stop=(kc == 7)), r=["Wg", "hTo"], w=["pG2"])
                for c in range(4):
                    P.pe(lambda e, c=c, ds=ds, ts=ts: e.matmul(out=pA_D[:], lhsT=Wa[:, c, ds], rhs=gT[:, c, ts], start=(c == 0), stop=(c == 3)), r=["Wa", "gT"], w=["pA_D"])
                for c in range(4):
                    P.pe(lambda e, c=c, ds=ds, ts=ts: e.matmul(out=pB_D[:], lhsT=Wb[:, c, ds], rhs=gT[:, c, ts], start=(c == 0), stop=(c == 3)), r=["Wb", "gT"], w=["pB_D"])
                for hh in range(8):
                    P.pe(lambda e, hh=hh, ds=ds, ts=ts: e.matmul(out=pC_D[:], lhsT=Wo[:, hh, ds], rhs=oT[:, hh, ts], start=(hh == 0), stop=(hh == 7)), r=["Wo", "oT"], w=["pC_D"])
                P.act(lambda e: e.activation(out=sg1[:], in_=pG1[:], func=ACT.Sigmoid), r=["pG1"], w=["sg1"])
                P.act(lambda e: e.activation(out=sg2[:], in_=pG2[:], func=ACT.Sigmoid), r=["pG2"], w=["sg2"])
                P.act(lambda e: e.activation(out=sgb[:], in_=pB_D[:], func=ACT.Sigmoid), r=["pB_D"], w=["sgb"])
                P.dve(lambda e: e.tensor_tensor(out=mt1[:], in0=pA_D[:], in1=sgb[:], op=ALU.mult), r=["pA_D", "sgb"], w=["mt1"])
                P.dve(lambda e: e.tensor_tensor(out=mt1[:], in0=mt1[:], in1=sg1[:], op=ALU.mult), r=["mt1", "sg1"], w=["mt1"])
                P.dve(lambda e: e.tensor_tensor(out=mt2[:], in0=pC_D[:], in1=sg2[:], op=ALU.mult), r=["pC_D", "sg2"], w=["mt2"])
                P.dve(lambda e, dt_=dt_, ts=ts: e.tensor_tensor(out=mT_all[:, dt_, ts], in0=mt1[:], in1=mt2[:], op=ALU.add), r=["mt1", "mt2"], w=["mT_all"])
        P.barrier()
    esP.close()
    if "mT" in dbg_names:
        dump("mT", mT_all[:], [128, 8, NOWN], BF16)
    if STOP <= 6:
        esM.close()
        return finish(nc, P, ges, out, dbg_out)

    esE = ExitStack()
    h2T = SB(esE, "h2T", [128, 8, NOWN], BF16)
    combT = SB(esE, "combT", [32, NOWN], BF16)
    with ExitStack() as es:
        Wout = SB(es, "Wout", [128, 8, 1024], BF16)
        wrt_sb = SB(es, "wrt_sb", [128, 8, 36], F32)
        brt_bc = SB(es, "brt_bc", [128, 36], F32)
        g1_bc = SB(es, "g1_bc", [128, D], F32)
        l1g_bc = SB(es, "l1g_bc", [128, D], F32)
        l1b_bc = SB(es, "l1b_bc", [128, D], F32)
        sc2p_bc = SB(es, "sc2p_bc", [128, D], F32)
        sh2_bc = SB(es, "sh2_bc", [128, D], F32)
        xt_D = [SB(es, "xt_D%d" % i, [128, D], F32) for i in range(2)]
        zt_D = SB(es, "zt_D", [128, D], F32)
        zn_D = SB(es, "zn_D", [128, D], F32)
        x1_D = [SB(es, "x1_D%d" % i, [128, D], F32) for i in range(2)]
        h2_D = SB(es, "h2_D", [128, D], F32)
        h2b_D = SB(es, "h2b_D", [128, D], BF16)
        h2Tf = SB(es, "h2Tf", [128, 8, 128], F32)
        st_D = SB(es, "st_D", [128, 2, 6], F32)
        mv_D = SB(es, "mv_D", [128, 2], F32)
        rstd_D = SB(es, "rstd_D", [128, 1], F32)
        nb_D = SB(es, "nb_D", [128, 1], F32)
        L_D = SB(es, "L_D", [128, 36], F32)
        rs = SB(es, "rs", [128, 16], F32)
        ohg = SB(es, "ohg", [128, 4], F32)
        gex = SB(es, "gex", [128, 4], F32)
        msk = SB(es, "msk", [128, 32], F32)
        ein = SB(es, "ein", [128, 8], F32)
        e2_ = SB(es, "e2_", [128, 8], F32)
        oh1 = SB(es, "oh1", [128, 8], F32)
        oh2 = SB(es, "oh2", [128, 8], F32)
        cg = SB(es, "cg", [128, 8], F32)
        comb = SB(es, "comb", [128, 32], F32)
        pMix = [PS(es, "pMix%d" % i, [128, 512], F32) for i in range(2)]
        pT_D = PS(es, "pT_D", [128, 8, 128], BF16)
        pTf = PS(es, "pTf", [128, 4, 128], F32)
        pR = PS(es, "pR", [128, 36], F32)
        pCT = PS(es, "pCT", [32, 128], F32)
        P.dma(lambda e: e.dma_start(out=Wout[:], in_=w_out.rearrange("(kc p) n -> p kc n", p=128)), key="Wout", w=["Wout"], eng="pool")
        P.dma(lambda e: e.dma_start(out=wrt_sb[:], in_=w_rt.rearrange("(kc p) n -> p kc n", p=128)), key="wrt_sb", w=["wrt_sb"])
        load_bc(brt_bc, b_rt, "brt_bc")
        load_bc(g1_bc, mod_d[0:1, 2048:3072], "g1_bc")
        load_bc(l1g_bc, ln1_g, "l1g_bc")
        load_bc(l1b_bc, ln1_b, "l1b_bc")
        load_bc(sc2p_bc, mod_d[0:1, 4096:5120], "sc2p_bc", True)
        load_bc(sh2_bc, mod_d[0:1, 3072:4096], "sh2_bc")
        for tt in range(NTO):
            s2 = tt % 2
            t0 = tt * 128
            tsl_ = slice(t0, t0 + 128)
            P.dma(lambda e, s2=s2, t0=t0: e.dma_start(out=xt_D[s2][:], in_=xo[t0:t0 + 128, :]), key=("xt_D", s2), w=[("xt_D", s2)])
            for half in range(2):
                hs = slice(half * 512, (half + 1) * 512)
                for kc in range(8):
                    P.pe(lambda e, kc=kc, half=half, hs=hs, tsl_=tsl_: e.matmul(out=pMix[half][:], lhsT=mT_all[:, kc, tsl_], rhs=Wout[:, kc, hs], start=(kc == 0), stop=(kc == 7)),
                         r=["mT_all", "Wout"], w=[("pMix", half)])
                P.dve(lambda e, half=half, hs=hs: e.tensor_tensor(out=zt_D[:, hs], in0=pMix[half][:], in1=g1_bc[:, hs], op=ALU.mult), r=[("pMix", half), "g1_bc"], w=["zt_D"])
            P.dve(lambda e, s2=s2: e.scalar_tensor_tensor(out=zt_D[:], in0=xt_D[s2][:], scalar=ALPHA, in1=zt_D[:], op0=ALU.mult, op1=ALU.add), r=["zt_D", ("xt_D", s2)], w=["zt_D"])
            ln_tile(zt_D[:], "zt_D", st_D, mv_D, rstd_D, nb_D, zn_D[:], "zn_D", "D1")
            P.dve(lambda e: e.tensor_tensor(out=zn_D[:], in0=zn_D[:], in1=l1g_bc[:], op=ALU.mult), r=["zn_D", "l1g_bc"], w=["zn_D"])
            P.dve(lambda e, s2=s2: e.tensor_tensor(out=x1_D[s2][:], in0=zn_D[:], in1=l1b_bc[:], op=ALU.add), r=["zn_D", "l1b_bc"], w=[("x1_D", s2)])
            P.dma(lambda e, s2=s2, t0=t0: e.dma_start(out=x1_d[t0:t0 + 128, :], in_=x1_D[s2][:]), key=("x1d", s2), r=[("x1_D", s2)], w=["x1_d"])
            ln_tile(x1_D[s2][:], ("x1_D", s2), st_D, mv_D, rstd_D, nb_D, h2_D[:], "h2_D", "D2")
            P.dve(lambda e: e.tensor_tensor(out=h2_D[:], in0=h2_D[:], in1=sc2p_bc[:], op=ALU.mult), r=["h2_D", "sc2p_bc"], w=["h2_D"])
            P.dve(lambda e: e.tensor_tensor(out=h2_D[:], in0=h2_D[:], in1=sh2_bc[:], op=ALU.add), r=["h2_D", "sh2_bc"], w=["h2_D"])
            P.act(lambda e: e.activation(out=h2b_D[:], in_=h2_D[:], func=ACT.Copy), r=["h2_D"], w=["h2b_D"])
            for kc in range(8):
                P.pe(lambda e, kc=kc: e.transpose(out=pT_D[:, kc, :], in_=h2b_D[:, kc * 128:(kc + 1) * 128], identity=ident_b[:]), r=["h2b_D", "ident_b"], w=["pT_D"])
            P.act(lambda e, tsl_=tsl_: e.activation(out=h2T[:, :, tsl_], in_=pT_D[:], func=ACT.Copy), r=["pT_D"], w=["h2T"])
            for q4 in range(2):
                for j in range(4):
                    kc = q4 * 4 + j
                    P.pe(lambda e, kc=kc, j=j: e.transpose(out=pTf[:, j, :], in_=h2_D[:, kc * 128:(kc + 1) * 128], identity=ident_f[:]), r=["h2_D", "ident_f"], w=["pTf"])
                P.dve(lambda e, q4=q4: e.tensor_copy(out=h2Tf[:, q4 * 4:(q4 + 1) * 4, :], in_=pTf[:]), r=["pTf"], w=["h2Tf"])
            for kc in range(8):
                P.pe(lambda e, kc=kc: e.matmul(out=pR[:], lhsT=h2Tf[:, kc, :], rhs=wrt_sb[:, kc, :], start=(kc == 0), stop=(kc == 7)), r=["h2Tf", "wrt_sb"], w=["pR"])
            P.dve(lambda e: e.tensor_tensor(out=L_D[:], in0=pR[:], in1=brt_bc[:], op=ALU.add), r=["pR", "brt_bc"], w=["L_D"])
            R_ = lambda i: rs[:, i:i + 1]
            kR = lambda i: ("rs", i)
            P.dve(lambda e: e.tensor_reduce(out=R_(0), in_=L_D[:, 0:4], axis=AX.X, op=ALU.max), r=["L_D"], w=[kR(0)])
            P.dve(lambda e: e.tensor_scalar(out=ohg[:], in0=L_D[:, 0:4], scalar1=R_(0), scalar2=None, op0=ALU.is_equal), r=["L_D", kR(0)], w=["ohg"])
            P.dve(lambda e: e.tensor_scalar(out=R_(1), in0=R_(0), scalar1=-1.0, scalar2=None, op0=ALU.mult), r=[kR(0)], w=[kR(1)])
            P.act(lambda e: e.activation(out=gex[:], in_=L_D[:, 0:4], func=ACT.Exp, bias=R_(1), scale=1.0), r=["L_D", kR(1)], w=["gex"])
            P.dve(lambda e: e.tensor_reduce(out=R_(2), in_=gex[:], axis=AX.X, op=ALU.add), r=["gex"], w=[kR(2)])
            P.dve(lambda e: e.reciprocal(out=R_(2), in_=R_(2)), r=[kR(2)], w=[kR(2)])
            P.dve(lambda e: e.tensor_tensor(out=msk[:].rearrange("p (g x) -> p g x", x=8), in0=L_D[:, 4:36].rearrange("p (g x) -> p g x", x=8),
                                            in1=fv(ohg[:], 0, [[1, 4], [0, 8]]), op=ALU.mult), r=["L_D", "ohg"], w=["msk"])
            P.dve(lambda e: e.tensor_reduce(out=ein[:], in_=fv(msk[:], 0, [[1, 8], [8, 4]]), axis=AX.X, op=ALU.add), r=["msk"], w=["ein"])
            P.dve(lambda e: e.tensor_reduce(out=R_(3), in_=ein[:], axis=AX.X, op=ALU.max), r=["ein"], w=[kR(3)])
            P.dve(lambda e: e.tensor_scalar(out=oh1[:], in0=ein[:], scalar1=R_(3), scalar2=None, op0=ALU.is_equal), r=["ein", kR(3)], w=["oh1"])
            P.dve(lambda e: e.scalar_tensor_tensor(out=e2_[:], in0=oh1[:], scalar=-1e30, in1=ein[:], op0=ALU.mult, op1=ALU.add), r=["oh1", "ein"], w=["e2_"])
            P.dve(lambda e: e.tensor_reduce(out=R_(4), in_=e2_[:], axis=AX.X, op=ALU.max), r=["e2_"], w=[kR(4)])
            P.dve(lambda e: e.tensor_scalar(out=oh2[:], in0=e2_[:], scalar1=R_(4), scalar2=None, op0=ALU.is_equal), r=["e2_", kR(4)], w=["oh2"])
            P.dve(lambda e: e.tensor_tensor(out=R_(5), in0=R_(4), in1=R_(3), op=ALU.subtract), r=[kR(3), kR(4)], w=[kR(5)])
            P.act(lambda e: e.activation(out=R_(6), in_=R_(5), func=ACT.Exp), r=[kR(5)], w=[kR(6)])
            P.dve(lambda e: e.tensor_scalar(out=R_(7), in0=R_(6), scalar1=1.0, scalar2=None, op0=ALU.add), r=[kR(6)], w=[kR(7)])
            P.dve(lambda e: e.reciprocal(out=R_(7), in_=R_(7)), r=[kR(7)], w=[kR(7)])
            P.dve(lambda e: e.tensor_tensor(out=R_(8), in0=R_(6), in1=R_(7), op=ALU.mult), r=[kR(6), kR(7)], w=[kR(8)])
            P.dve(lambda e: e.tensor_tensor(out=R_(7), in0=R_(7), in1=R_(2), op=ALU.mult), r=[kR(7), kR(2)], w=[kR(7)])
            P.dve(lambda e: e.tensor_tensor(out=R_(8), in0=R_(8), in1=R_(2), op=ALU.mult), r=[kR(8), kR(2)], w=[kR(8)])
            P.dve(lambda e: e.tensor_scalar(out=cg[:], in0=oh1[:], scalar1=R_(7), scalar2=None, op0=ALU.mult), r=["oh1", kR(7)], w=["cg"])
            P.dve(lambda e: e.scalar_tensor_tensor(out=cg[:], in0=oh2[:], scalar=R_(8), in1=cg[:], op0=ALU.mult, op1=ALU.add), r=["oh2", kR(8), "cg"], w=["cg"])
            P.dve(lambda e: e.tensor_tensor(out=comb[:].rearrange("p (g x) -> p g x", x=8), in0=fv(cg[:], 0, [[0, 4], [1, 8]]), in1=fv(ohg[:], 0, [[1, 4], [0, 8]]), op=ALU.mult),
                  r=["cg", "ohg"], w=["comb"])
            P.pe(lambda e: e.transpose(out=pCT[:], in_=comb[:], identity=ident_f[:]), r=["comb", "ident_f"], w=["pCT"])
            P.act(lambda e, tsl_=tsl_: e.activation(out=combT[:, tsl_], in_=pCT[:], func=ACT.Copy), r=["pCT"], w=["combT"])
        P.barrier()
    esM.close()
    if "x1" in dbg_names:
        o_ = nc.dram_tensor("dbg_x1", [NOWN, D], F32, kind="ExternalOutput").ap()
        P.dma(lambda e: e.dma_start(out=o_, in_=x1_d), key="dbg_x1", r=["x1_d"], w=["dbg_x1"])
    if "combT" in dbg_names:
        dump("combT", combT[:], [32, NOWN], BF16)
    if STOP <= 7:
        esE.close()
        return finish(nc, P, ges, out, dbg_out)

    yacc = SB(esE, "yacc", [128, NTO, D], F32)
    with ExitStack() as es:
        Weg = [SB(es, "Weg%d" % i, [128, 8, 512], BF16) for i in range(2)]
        Weu = [SB(es, "Weu%d" % i, [128, 8, 512], BF16) for i in range(2)]
        Wed = [SB(es, "Wed%d" % i, [128, 4, 1024], BF16) for i in range(2)]
        sele_b = SB(es, "sele_b", [32, 32, 128], BF16)
        bc_sb = SB(es, "bc_sb", [128, 512], F32)
        sa_E = [SB(es, "sa_E%d" % i, [128, 512], F32) for i in range(2)]
        actT = [SB(es, "actT%d" % i, [128, 4, 512], BF16) for i in range(2)]
        pBC = PS(es, "pBC", [128, 512], F32)
        pA_E = [PS(es, "pA_E%d" % i, [128, 512], F32) for i in range(2)]
        pB_E = [PS(es, "pB_E%d" % i, [128, 512], F32) for i in range(2)]
        pY_E = [PS(es, "pY_E%d" % i, [128, 512], F32) for i in range(2)]
        P.dma(lambda e: e.dma_start(out=sele_b[:], in_=cst_sele), key="sele_b", w=["sele_b"], eng="pool")
        NEXP = int(os.environ.get("K_NEXP", "32"))
        fci = 0
        yi = 0
        for ex in range(NEXP):
            se = ex % 2
            P.dma(lambda e, ex=ex, se=se: e.dma_start(out=Weg[se][:], in_=w_eg[ex].rearrange("(kc p) f -> p kc f", p=128)), key=("Weg", se), w=[("Weg", se)], eng="pool")
            P.dma(lambda e, ex=ex, se=se: e.dma_start(out=Weu[se][:], in_=w_eu[ex].rearrange("(kc p) f -> p kc f", p=128)), key=("Weu", se), w=[("Weu", se)], eng="pool")
            P.dma(lambda e, ex=ex, se=se: e.dma_start(out=Wed[se][:], in_=w_ed[ex].rearrange("(fc p) n -> p fc n", p=128)), key=("Wed", se), w=[("Wed", se)], eng="pool")
            for st_ in range(4):
                ts = slice(st_ * 512, (st_ + 1) * 512)
                sa_ = (ex * 4 + st_) % 2
                P.pe(lambda e, ex=ex, ts=ts: e.matmul(out=pBC[:], lhsT=sele_b[:, ex, :], rhs=combT[:, ts], start=True, stop=True), r=["sele_b", "combT"], w=["pBC"])
                P.act(lambda e: e.activation(out=bc_sb[:], in_=pBC[:], func=ACT.Copy), r=["pBC"], w=["bc_sb"])
                for fc in range(4):
                    fs = slice(fc * 128, (fc + 1) * 128)
                    sp_ = fci % 2
                    for kc in range(8):
                        P.pe(lambda e, kc=kc, fs=fs, ts=ts, se=se, sp_=sp_: e.matmul(out=pA_E[sp_][:], lhsT=Weg[se][:, kc, fs], rhs=h2T[:, kc, ts], start=(kc == 0), stop=(kc == 7)),
                             r=[("Weg", se), "h2T"], w=[("pA_E", sp_)])
                    for kc in range(8):
                        P.pe(lambda e, kc=kc, fs=fs, ts=ts, se=se, sp_=sp_: e.matmul(out=pB_E[sp_][:], lhsT=Weu[se][:, kc, fs], rhs=h2T[:, kc, ts], start=(kc == 0), stop=(kc == 7)),
                             r=[("Weu", se), "h2T"], w=[("pB_E", sp_)])
                    P.act(lambda e, sp_=sp_: e.activation(out=sa_E[sp_][:], in_=pA_E[sp_][:], func=ACT.Silu), r=[("pA_E", sp_)], w=[("sa_E", sp_)])
                    P.dve(lambda e, sp_=sp_: e.tensor_tensor(out=sa_E[sp_][:], in0=sa_E[sp_][:], in1=pB_E[sp_][:], op=ALU.mult), r=[("sa_E", sp_), ("pB_E", sp_)], w=[("sa_E", sp_)])
                    P.dve(lambda e, sp_=sp_, sa_=sa_, fc=fc: e.tensor_tensor(out=actT[sa_][:, fc, :], in0=sa_E[sp_][:], in1=bc_sb[:], op=ALU.mult),
                          r=[("sa_E", sp_), "bc_sb"], w=[("actT", sa_)])
                    fci += 1
                for j in range(4):
                    tile_ = st_ * 4 + j
                    js = slice(j * 128, (j + 1) * 128)
                    for half in range(2):
                        hs = slice(half * 512, (half + 1) * 512)
                        sy = yi % 2
                        for fc in range(4):
                            P.pe(lambda e, fc=fc, js=js, hs=hs, sa_=sa_, se=se, sy=sy: e.matmul(out=pY_E[sy][:], lhsT=actT[sa_][:, fc, js], rhs=Wed[se][:, fc, hs], start=(fc == 0), stop=(fc == 3)),
                                 r=[("actT", sa_), ("Wed", se)], w=[("pY_E", sy)])
                        if ex == 0:
                            P.dve(lambda e, tile_=tile_, hs=hs, sy=sy: e.tensor_copy(out=yacc[:, tile_, hs], in_=pY_E[sy][:]), r=[("pY_E", sy)], w=[("yacc", tile_)])
                        else:
                            P.dve(lambda e, tile_=tile_, hs=hs, sy=sy: e.tensor_tensor(out=yacc[:, tile_, hs], in0=yacc[:, tile_, hs], in1=pY_E[sy][:], op=ALU.add),
                                  r=[("pY_E", sy), ("yacc", tile_)], w=[("yacc", tile_)])
                        yi += 1
        P.barrier()

    with ExitStack() as es:
        g2_bc = SB(es, "g2_bc", [128, D], F32)
        l2g_bc = SB(es, "l2g_bc", [128, D], F32)
        l2b_bc = SB(es, "l2b_bc", [128, D], F32)
        x1_F = [SB(es, "x1_F%d" % i, [128, D], F32) for i in range(2)]
        z_F = SB(es, "z_F", [128, D], F32)
        zn_F = SB(es, "zn_F", [128, D], F32)
        o_F = [SB(es, "o_F%d" % i, [128, D], F32) for i in range(2)]
        st_F = SB(es, "st_F", [128, 2, 6], F32)
        mv_F = SB(es, "mv_F", [128, 2], F32)
        rstd_F = SB(es, "rstd_F", [128, 1], F32)
        nb_F = SB(es, "nb_F", [128, 1], F32)
        load_bc(g2_bc, mod_d[0:1, 5120:6144], "g2_bc")
        load_bc(l2g_bc, ln2_g, "l2g_bc")
        load_bc(l2b_bc, ln2_b, "l2b_bc")
        for tt in range(NTO):
            s2 = tt % 2
            t0 = tt * 128
            P.dma(lambda e, s2=s2, t0=t0: e.dma_start(out=x1_F[s2][:], in_=x1_d[t0:t0 + 128, :]), key=("x1_F", s2), r=["x1_d"], w=[("x1_F", s2)])
            P.dve(lambda e, tt=tt: e.tensor_tensor(out=z_F[:], in0=yacc[:, tt, :], in1=g2_bc[:], op=ALU.mult), r=[("yacc", tt), "g2_bc"], w=["z_F"])
            P.dve(lambda e, s2=s2: e.scalar_tensor_tensor(out=z_F[:], in0=x1_F[s2][:], scalar=ALPHA, in1=z_F[:], op0=ALU.mult, op1=ALU.add), r=["z_F", ("x1_F", s2)], w=["z_F"])
            ln_tile(z_F[:], "z_F", st_F, mv_F, rstd_F, nb_F, zn_F[:], "zn_F", "F")
            P.dve(lambda e: e.tensor_tensor(out=zn_F[:], in0=zn_F[:], in1=l2g_bc[:], op=ALU.mult), r=["zn_F", "l2g_bc"], w=["zn_F"])
            P.dve(lambda e, s2=s2: e.tensor_tensor(out=o_F[s2][:], in0=zn_F[:], in1=l2b_bc[:], op=ALU.add), r=["zn_F", "l2b_bc"], w=[("o_F", s2)])
            P.dma(lambda e, s2=s2, t0=t0: e.dma_start(out=out[t0:t0 + 128, :], in_=o_F[s2][:]), key=("outd", s2), r=[("o_F", s2)], w=["out"])
        P.barrier()
    esE.close()
    return finish(nc, P, ges, out, dbg_out)


def finish(nc, P, ges, out, dbg_out):
    P.barrier()
    P.emit()
    ges.close()
    nc._dbg_out = dbg_out
    nc._stats = P.stats
    return nc


def rope_tables():
    rows = NLAT // 64
    row = np.repeat(np.arange(rows, dtype=np.float32), 64)
    col = np.tile(np.arange(64, dtype=np.float32), rows)
    inv = (np.float32(10000.0) ** (-np.arange(0, 64, 2, dtype=np.float32) / np.float32(64))).astype(np.float32)
    ang = np.stack([row[:, None] * inv, col[:, None] * inv], axis=1).astype(np.float32)
    tab = np.concatenate([np.cos(ang).reshape(NLAT, 64), np.sin(ang).reshape(NLAT, 64)], axis=1).astype(np.float32)
    return tab


def make_in_maps(inp):
    f32 = np.float32
    g = lambda k: np.asarray(inp[k], dtype=f32)
    x, c, ctx, c_ctx = g("x"), g("c"), g("ctx"), g("c_ctx")
    tab = rope_tables()
    tab_ctx = np.concatenate([np.ones((NCTX, 64), f32), np.zeros((NCTX, 64), f32)], axis=1)
    rope_full = np.concatenate([tab_ctx, tab], axis=0)
    tok = np.arange(128)
    mask8 = (tok[:, None] % 8 == np.arange(8)[None, :]).astype(f32)
    sel16 = (tok[:, None] // 8 == np.arange(16)[None, :]).astype(f32)
    mask8c = (tok[:, None] // 16 == np.arange(8)[None, :]).astype(f32)

    def pairlay(a):
        sh = a.shape
        a = a.reshape((2, 16, 2, 64) + sh[3:])
        perm = (2, 3, 0, 1) + tuple(range(4, a.ndim))
        a = a.transpose(perm)
        return np.ascontiguousarray(a.reshape((128, 2, 16) + sh[3:]))

    a_re, a_im = g("s5_a_re")[0], g("s5_a_im")[0]
    s5_a = np.stack([pairlay(a_re), pairlay(a_im)], axis=1)
    ldt = g("s5_log_dt")[0]
    s5_ldt = np.ascontiguousarray(np.broadcast_to(ldt.reshape(1, 2, 16, 2).transpose(0, 3, 1, 2), (64, 2, 2, 16)).transpose(1, 0, 2, 3).reshape(128, 2, 16))
    s5_b = np.stack([pairlay(g("s5_b_re")[0]), pairlay(g("s5_b_im")[0])], axis=1)
    cre = g("s5_c_re")[0].transpose(0, 1, 3, 2)
    cim = g("s5_c_im")[0].transpose(0, 1, 3, 2)
    s5_c = np.stack([pairlay(cre), pairlay(cim)], axis=1)
    dvec = g("s5_d")[0]
    s5_dcol = np.ascontiguousarray(np.broadcast_to(dvec.reshape(32, 16).T[None], (8, 16, 32)).reshape(128, 32))
    eZ = np.stack([63.0 - np.arange(64), np.arange(64)], axis=0).astype(f32)
    qs = np.arange(72)
    eRf = (qs - 7).astype(f32)
    eRb = (8 * (qs // 8 - 1) + 8 - (qs % 8)).astype(f32)
    eR = np.stack([eRf, eRb], axis=0)
    cst_eZ = np.ascontiguousarray(np.broadcast_to(eZ[None], (128, 2, 64)))
    cst_eR = np.ascontiguousarray(np.broadcast_to(eR[None], (128, 2, 72)))
    sidx = tok // 16
    cst_mf = (sidx[None, :] >= sidx[:, None]).astype(f32)
    cst_mb = (sidx[:, None] >= sidx[None, :]).astype(f32)
    selg = np.zeros((128, 8, 128), f32)
    for g8 in range(8):
        for co in range(16):
            selg[np.arange(8) * 16 + co, g8, g8 * 16 + co] = 1.0
    sele = np.zeros((32, 32, 128), f32)
    for e in range(32):
        sele[e, e, :] = 1.0
    w_rt = np.concatenate([g("w_router_group")[0], g("w_router_expert")[0]], axis=1)
    b_rt = np.concatenate([g("b_router_group")[0], g("b_router_expert")[0]], axis=0)[None]
    common = dict(
        w_mod=g("w_mod")[0], b_mod=g("b_mod"), w_in=g("w_in")[0], rope_f=rope_full,
        q_gain=g("q_gain"), k_gain=g("k_gain"), cst_mask8=mask8, cst_mask8c=mask8c, cst_sel16=sel16,
        s5_a=s5_a, s5_ldt=s5_ldt, s5_b=s5_b, s5_c=s5_c, s5_dcol=s5_dcol, cst_eZ=cst_eZ, cst_eR=cst_eR,
        cst_mf=cst_mf, cst_mb=cst_mb, cst_selg=selg,
        w_glu_a=g("w_glu_a")[0], w_glu_b=g("w_glu_b")[0], w_attn_o=g("w_attn_o")[0], w_out=g("w_out")[0],
        ln1_g=g("ln1_g"), ln1_b=g("ln1_b"), ln2_g=g("ln2_g"), ln2_b=g("ln2_b"),
        w_rt=w_rt, b_rt=b_rt, w_eg=g("w_exp_gate")[0], w_eu=g("w_exp_up")[0], w_ed=g("w_exp_down")[0],
        cst_sele=sele,
    )
    maps = []
    for core in range(8):
        b, r = core // 4, core % 4
        m = dict(common)
        m["xf"] = np.ascontiguousarray(np.concatenate([ctx[b], x[b]], axis=0))
        m["xo"] = np.ascontiguousarray(x[b, r * NOWN:(r + 1) * NOWN])
        cc = np.stack([c[b], c_ctx], axis=0)
        m["ccT"] = np.ascontiguousarray(cc.reshape(2, 8, 128).transpose(2, 1, 0))
        m["rope_o"] = np.ascontiguousarray(tab[r * NOWN:(r + 1) * NOWN])
        cm = np.zeros((128, 4), f32)
        cm[:, r] = 1.0
        m["cmask"] = cm
        maps.append(m)
    return maps


_NC_CACHE = {}


def kernel(**inputs):
    maps = make_in_maps(inputs)
    if "nc" not in _NC_CACHE:
        _NC_CACHE["nc"] = build()
    nc = _NC_CACHE["nc"]
    res = run_bass_kernel_spmd(nc, maps, core_ids=list(range(8)))
    outp = np.zeros((2, NLAT, D), np.float32)
    for core in range(8):
        b, r = core // 4, core % 4
        outp[b, r * NOWN:(r + 1) * NOWN] = res.results[core]["out"]
    return outp
```

```python
import os
import math
import numpy as np
from contextlib import ExitStack
import concourse.bass as bass
import concourse.mybir as mybir
from concourse.bass_utils import run_bass_kernel_spmd

F32 = mybir.dt.float32
BF16 = mybir.dt.bfloat16
ACT = mybir.ActivationFunctionType
ALU = mybir.AluOpType
AX = mybir.AxisListType

D = 1024
NLAT = 8192
NCTX = 256
NFULL = NLAT + NCTX
NOWN = 2048
NTF = NFULL // 128
NTO = NOWN // 128
NJ = NFULL // 64
NJF = NFULL // 8
EPS = 1e-6
ALPHA = 2.0 ** 0.25
STOP = int(os.environ.get("K_STOP", "99"))
DEBUG = os.environ.get("K_DEBUG", "") != ""


class Prog:
    ENGS = ["pe", "act", "dve", "pool", "sp"]

    def __init__(self, nc):
        self.nc = nc
        self.ops = []

    def add(self, eng, fn, r=(), w=(), dma=None, ndma=1):
        self.ops.append(dict(eng=eng, fn=fn, r=tuple(r), w=tuple(w), dma=dma, ndma=ndma, barrier=False))

    def pe(self, fn, r=(), w=()):
        self.add("pe", fn, r, w)

    def act(self, fn, r=(), w=()):
        self.add("act", fn, r, w)

    def dve(self, fn, r=(), w=()):
        self.add("dve", fn, r, w)

    def pool(self, fn, r=(), w=()):
        self.add("pool", fn, r, w)

    def dma(self, fn, key, r=(), w=(), eng="sp", n=1):
        self.add(eng, fn, r, w, dma=key, ndma=n)

    def capture(self):
        self._saved = self.ops
        self.ops = []

    def end_capture(self):
        lst = self.ops
        self.ops = self._saved
        return lst

    def barrier(self):
        for e in self.ENGS:
            self.ops.append(dict(eng=e, fn=None, r=(), w=(), dma=None, ndma=0, barrier=True))

    def emit(self):
        nc = self.nc
        ops = self.ops
        n = len(ops)
        last_w, readers = {}, {}
        deps = [None] * n
        last_eng, last_dma = {}, {}
        for i, op in enumerate(ops):
            d = set()
            if op["barrier"]:
                for e, j in last_eng.items():
                    if e != op["eng"]:
                        d.add(j)
                for k, j in last_dma.items():
                    d.add(j)
            for b in op["r"]:
                if b in last_w:
                    d.add(last_w[b])
            for b in op["w"]:
                if b in last_w:
                    d.add(last_w[b])
                for j in readers.get(b, ()):
                    d.add(j)
            for b in op["r"]:
                readers.setdefault(b, []).append(i)
            for b in op["w"]:
                readers[b] = []
                last_w[b] = i
            d.discard(i)
            deps[i] = d
            if op["dma"] is not None:
                last_dma[op["dma"]] = i
            elif not op["barrier"]:
                last_eng[op["eng"]] = i
        signal = [False] * n
        for i, op in enumerate(ops):
            for j in deps[i]:
                pj = ops[j]
                if pj["dma"] is not None:
                    continue
                if pj["eng"] == "pe" and op["eng"] == "pe" and op["dma"] is None:
                    continue
                signal[j] = True
        tick = [0] * n
        cnt = {e: 0 for e in self.ENGS}
        dcnt = {}
        for i, op in enumerate(ops):
            if op["dma"] is not None:
                dcnt[op["dma"]] = dcnt.get(op["dma"], 0) + op["ndma"]
                tick[i] = dcnt[op["dma"]] * 16
            elif signal[i]:
                cnt[op["eng"]] += 1
                tick[i] = cnt[op["eng"]]
        es = ExitStack()
        esem = {e: es.enter_context(nc.semaphore("s_" + e)) for e in self.ENGS}
        dsem = {}
        for k in dcnt:
            dsem[k] = es.enter_context(nc.semaphore("d_%d" % len(dsem)))
        waits = [None] * n
        seen = {e: {} for e in self.ENGS}
        for i, op in enumerate(ops):
            wl = {}
            for j in deps[i]:
                pj = ops[j]
                if pj["dma"] is not None:
                    key = ("d", pj["dma"])
                    sem = dsem[pj["dma"]]
                else:
                    if pj["eng"] == "pe" and op["eng"] == "pe" and op["dma"] is None:
                        continue
                    key = ("e", pj["eng"])
                    sem = esem[pj["eng"]]
                v = tick[j]
                if seen[op["eng"]].get(key, 0) >= v:
                    continue
                if key not in wl or wl[key][1] < v:
                    wl[key] = (sem, v)
            for key, (sem, v) in wl.items():
                seen[op["eng"]][key] = v
            waits[i] = list(wl.values())
        self.stats = dict(n=n, sig=dict(cnt), dkeys=len(dcnt), nwaits=sum(len(w) for w in waits))
        block = es.enter_context(nc.Block())

        def run(engname, eng):
            for i, op in enumerate(ops):
                if op["eng"] != engname:
                    continue
                for sem, v in waits[i]:
                    eng.wait_ge(sem, v)
                if op["fn"] is None:
                    continue
                res = op["fn"](eng)
                if op["dma"] is not None:
                    if not isinstance(res, (list, tuple)):
                        res = [res]
                    assert len(res) == op["ndma"], (len(res), op["ndma"])
                    for ins in res:
                        ins.then_inc(dsem[op["dma"]], 16)
                elif signal[i]:
                    if isinstance(res, (list, tuple)):
                        res = res[-1]
                    res.then_inc(esem[engname], 1)

        @block.tensor
        def _(e):
            run("pe", e)

        @block.scalar
        def _(e):
            run("act", e)

        @block.vector
        def _(e):
            run("dve", e)

        @block.gpsimd
        def _(e):
            run("pool", e)

        @block.sync
        def _(e):
            run("sp", e)

        es.close()


def AP_(t, offset, dims):
    return bass.AP(t, offset, [list(d) for d in dims])


def pstride(t):
    return t[:].ap[0][0]


def build(dbg_names=()):
    nc = bass.Bass("TRN2", target_bir_lowering=False)
    dram_in = lambda name, shape, dt=F32: nc.dram_tensor(name, list(shape), dt, kind="ExternalInput").ap()
    xf = dram_in("xf", [NFULL, D])
    xo = dram_in("xo", [NOWN, D])
    ccT = dram_in("ccT", [128, 8, 2])
    w_mod = dram_in("w_mod", [D, 6 * D])
    b_mod = dram_in("b_mod", [1, 6 * D])
    w_in = dram_in("w_in", [D, 4096])
    rope_f = dram_in("rope_f", [NFULL, 128])
    rope_o = dram_in("rope_o", [NOWN, 128])
    q_gain = dram_in("q_gain", [1, 128])
    k_gain = dram_in("k_gain", [1, 128])
    cst_mask8 = dram_in("cst_mask8", [128, 8])
    cst_mask8c = dram_in("cst_mask8c", [128, 8])
    cst_sel16 = dram_in("cst_sel16", [128, 16])
    s5_a = dram_in("s5_a", [128, 2, 2, 16])
    s5_ldt = dram_in("s5_ldt", [128, 2, 16])
    s5_b = dram_in("s5_b", [128, 2, 2, 16, 16])
    s5_c = dram_in("s5_c", [128, 2, 2, 16, 16])
    s5_dcol = dram_in("s5_dcol", [128, 32])
    cst_eZ = dram_in("cst_eZ", [128, 2, 64])
    cst_eR = dram_in("cst_eR", [128, 2, 72])
    cst_mf = dram_in("cst_mf", [128, 128])
    cst_mb = dram_in("cst_mb", [128, 128])
    cst_selg = dram_in("cst_selg", [128, 8, 128])
    cmask = dram_in("cmask", [128, 4])
    w_glu_a = dram_in("w_glu_a", [512, D])
    w_glu_b = dram_in("w_glu_b", [512, D])
    w_attn_o = dram_in("w_attn_o", [D, D])
    w_out = dram_in("w_out", [D, D])
    ln1_g = dram_in("ln1_g", [1, D])
    ln1_b = dram_in("ln1_b", [1, D])
    ln2_g = dram_in("ln2_g", [1, D])
    ln2_b = dram_in("ln2_b", [1, D])
    w_rt = dram_in("w_rt", [D, 36])
    b_rt = dram_in("b_rt", [1, 36])
    w_eg = dram_in("w_eg", [32, D, 512])
    w_eu = dram_in("w_eu", [32, D, 512])
    w_ed = dram_in("w_ed", [32, 512, D])
    cst_sele = dram_in("cst_sele", [32, 32, 128])
    out = nc.dram_tensor("out", [NOWN, D], F32, kind="ExternalOutput").ap()
    scr = lambda name, shape, dt: nc.dram_tensor(name, list(shape), dt, kind="Internal").ap()
    mod_d = scr("mod_d", [2, 6 * D], F32)
    kT_d = scr("kT_d", [2, 128, NFULL], BF16)
    v_d = scr("v_d", [NFULL, 256], BF16)
    x1_d = scr("x1_d", [NOWN, D], F32)
    dbg_out = {}

    P = Prog(nc)
    ges = ExitStack()

    free_list = [[16640, 229376]]
    peak = [0]

    def _alloc(nbytes):
        nbytes = (nbytes + 63) // 64 * 64
        for iv in free_list:
            if iv[1] - iv[0] >= nbytes:
                off = iv[0]
                iv[0] += nbytes
                peak[0] = max(peak[0], off + nbytes)
                return off, nbytes
        raise RuntimeError("SBUF manual allocator out of space for %d bytes; free=%s" % (nbytes, free_list))

    def _free(off, nbytes):
        free_list.append([off, off + nbytes])
        free_list.sort()
        merged = []
        for iv in free_list:
            if iv[1] == iv[0]:
                continue
            if merged and merged[-1][1] == iv[0]:
                merged[-1][1] = iv[1]
            else:
                merged.append(iv)
        free_list[:] = merged

    def SB(es, name, shape, dt):
        esz = 4 if dt == F32 else 2
        nb = esz
        for s_ in shape[1:]:
            nb *= s_
        off, nbytes = _alloc(nb)
        t = nc.alloc_sbuf_tensor_at(name, list(shape), dt, offset=off)
        es.callback(_free, off, nbytes)
        return t

    def PS(es, name, shape, dt):
        return es.enter_context(nc.psum_tensor(name, list(shape), dt))

    def dump(name, t_ap, shape, dt=F32, r=()):
        if name not in dbg_names:
            return
        o = nc.dram_tensor("dbg_" + name, list(shape), dt, kind="ExternalOutput").ap()
        dbg_out[name] = o
        P.dma(lambda e: e.dma_start(out=o, in_=t_ap), key="dbg_" + name, r=r, w=["dbg_" + name])

    ident_f = SB(ges, "ident_f", [128, 128], F32)
    ident_b = SB(ges, "ident_b", [128, 128], BF16)
    ones_b = SB(ges, "ones_b", [1, 128], BF16)
    eps_t = SB(ges, "eps_t", [128, 1], F32)
    modT = SB(ges, "modT", [128, 48, 2], F32)
    op1p = SB(ges, "op1p", [128, 8, 2], F32)
    sh1T = SB(ges, "sh1T", [128, 8, 2], F32)
    gk_bc = SB(ges, "gk_bc", [128, 128], F32)
    gq_bc = SB(ges, "gq_bc", [128, 128], F32)
    negC = SB(ges, "negC", [128, 1], F32)
    mask8 = SB(ges, "mask8", [128, 8], BF16)
    mask8f = SB(ges, "mask8f", [128, 8], F32)
    sel16 = SB(ges, "sel16", [128, 16], BF16)

    P.pool(lambda e: e.memset(ident_f[:], 1.0), w=["ident_f"])
    P.pool(lambda e: e.affine_select(out=ident_f[:], in_=ident_f[:], pattern=[[-1, 128]], compare_op=ALU.is_equal,
                                     fill=0.0, base=0, channel_multiplier=1), r=["ident_f"], w=["ident_f"])
    P.dve(lambda e: e.tensor_copy(out=ident_b[:], in_=ident_f[:]), r=["ident_f"], w=["ident_b"])
    P.dve(lambda e: e.memset(ones_b[:], 1.0), w=["ones_b"])
    P.dve(lambda e: e.memset(eps_t[:], EPS), w=["eps_t"])
    P.dma(lambda e: e.dma_start(out=gk_bc[:], in_=k_gain.partition_broadcast(128)), key="gk_bc", w=["gk_bc"])
    P.dma(lambda e: e.dma_start(out=gq_bc[:], in_=q_gain.partition_broadcast(128)), key="gq_bc", w=["gq_bc"])

    with ExitStack() as es:
        scT = SB(es, "scT", [128, 8, 2], F32)
        ccs = SB(es, "ccs", [128, 8, 2], F32)
        wm = [SB(es, "wm%d" % i, [128, 8, 512], F32) for i in range(2)]
        bm = SB(es, "bm", [2, 6 * D], F32)
        mod_sb = SB(es, "mod_sb", [2, 6 * D], F32)
        m8f = SB(es, "m8f", [128, 8], F32)
        s16f = SB(es, "s16f", [128, 16], F32)
        tmpg = SB(es, "tmpg", [128, 2], F32)
        pM = [PS(es, "pM%d" % i, [2, 512], F32) for i in range(2)]
        pMT = PS(es, "pMT", [128, 48, 2], F32)
        P.dma(lambda e: e.dma_start(out=ccs[:], in_=ccT), key="ccs", w=["ccs"])
        P.dma(lambda e: e.dma_start(out=bm[:], in_=b_mod.partition_broadcast(2)), key="bm", w=["bm"])
        P.dma(lambda e: e.dma_start(out=m8f[:], in_=cst_mask8), key="m8f", w=["m8f"])
        P.dma(lambda e: e.dma_start(out=s16f[:], in_=cst_sel16), key="s16f", w=["s16f"])
        P.dve(lambda e: e.tensor_copy(out=mask8[:], in_=m8f[:]), r=["m8f"], w=["mask8"])
        P.dve(lambda e: e.tensor_copy(out=mask8f[:], in_=m8f[:]), r=["m8f"], w=["mask8f"])
        P.dve(lambda e: e.tensor_copy(out=sel16[:], in_=s16f[:]), r=["s16f"], w=["sel16"])
        P.act(lambda e: e.activation(out=scT[:], in_=ccs[:], func=ACT.Silu), r=["ccs"], w=["scT"])
        P.dve(lambda e: e.tensor_reduce(out=tmpg[:, 0:1], in_=gq_bc[:], axis=AX.X, op=ALU.max, apply_absolute_value=True),
              r=["gq_bc"], w=["tmpg0"])
        P.dve(lambda e: e.tensor_reduce(out=tmpg[:, 1:2], in_=gk_bc[:], axis=AX.X, op=ALU.max, apply_absolute_value=True),
              r=["gk_bc"], w=["tmpg1"])
        P.dve(lambda e: e.scalar_tensor_tensor(out=negC[:], in0=tmpg[:, 0:1], scalar=-math.sqrt(128.0), in1=tmpg[:, 1:2],
                                               op0=ALU.mult, op1=ALU.mult), r=["tmpg0", "tmpg1"], w=["negC"])
        for nb in range(12):
            s = nb % 2
            P.dma(lambda e, nb=nb, s=s: e.dma_start(out=wm[s][:], in_=w_mod[:, nb * 512:(nb + 1) * 512].rearrange("(kc p) n -> p kc n", p=128)),
                  key=("wm", s), w=[("wm", s)])
            for kc in range(8):
                P.pe(lambda e, kc=kc, s=s: e.matmul(out=pM[s][:], lhsT=scT[:, kc, :], rhs=wm[s][:, kc, :], start=(kc == 0), stop=(kc == 7)),
                     r=["scT", ("wm", s)], w=[("pM", s)])
            P.dve(lambda e, nb=nb, s=s: e.tensor_tensor(out=mod_sb[:, nb * 512:(nb + 1) * 512], in0=pM[s][:], in1=bm[:, nb * 512:(nb + 1) * 512], op=ALU.add),
                  r=[("pM", s), "bm"], w=["mod_sb"])
        P.dma(lambda e: e.dma_start(out=mod_d, in_=mod_sb[:]), key="mod_d", r=["mod_sb"], w=["mod_d"])
        for j in range(48):
            P.pe(lambda e, j=j: e.transpose(out=pMT[:, j, :], in_=mod_sb[:, j * 128:(j + 1) * 128], identity=ident_f[0:2, 0:2]),
                 r=["mod_sb", "ident_f"], w=["pMT"])
        P.dve(lambda e: e.tensor_copy(out=modT[:], in_=pMT[:]), r=["pMT"], w=["modT"])
        P.dve(lambda e: e.tensor_copy(out=sh1T[:], in_=modT[:, 0:8, :]), r=["modT"], w=["sh1T"])
        P.dve(lambda e: e.tensor_scalar(out=op1p[:], in0=modT[:, 8:16, :], scalar1=1.0, scalar2=None, op0=ALU.add), r=["modT"], w=["op1p"])
        dump("mod", mod_sb[:], [2, 6 * D], r=["mod_sb"])
        P.barrier()
    if STOP <= 0:
        return finish(nc, P, ges, out, dbg_out)

    def fv(ap, off, dims):
        return bass.AP(ap.tensor, ap.offset + off, [list(ap.ap[0])] + [list(d) for d in dims])

    bias_sb = SB(ges, "bias_sb", [1, 5120], BF16)

    def prep_wblock(stage, pB, s, dst, col0, r, bcol, tag):
        P.dma(lambda e: e.dma_start(out=stage[s][:], in_=w_in[:, col0:col0 + 512].rearrange("(kc p) n -> p kc n", p=128)),
              key=("stg", s), w=[("stg", s)])
        for kc in range(8):
            P.pool(lambda e, kc=kc: e.tensor_scalar(out=dst[:, kc, :], in0=stage[s][:, kc, :], scalar1=op1p[:, kc, r:r + 1], scalar2=None, op0=ALU.mult),
                   r=[("stg", s), "op1p"], w=[tag])
        for kc in range(8):
            P.pe(lambda e, kc=kc: e.matmul(out=pB[:], lhsT=sh1T[:, kc, r:r + 1], rhs=stage[s][:, kc, :], start=(kc == 0), stop=(kc == 7)),
                 r=[("stg", s), "sh1T"], w=["pB"])
        P.act(lambda e: e.activation(out=bias_sb[:, bcol:bcol + 512], in_=pB[:], func=ACT.Copy), r=["pB"], w=["bias_sb"])

    def ln_tile(xt_ap, xkey, st, mv, rstd, nb, hb_ap, hkey, sfx):
        for i in range(2):
            P.dve(lambda e, i=i: e.bn_stats(out=st[:, i, :], in_=xt_ap[:, i * 512:(i + 1) * 512]), r=[xkey], w=[("st", sfx, i)])
        P.dve(lambda e: e.bn_aggr(out=mv[:], in_=st[:].rearrange("p a b -> p (a b)")), r=[("st", sfx, 0), ("st", sfx, 1)], w=[("mv", sfx)])
        P.act(lambda e: e.activation(out=rstd[:], in_=mv[:, 1:2], func=ACT.Sqrt, bias=eps_t[:], scale=1.0), r=[("mv", sfx), "eps_t"], w=[("rstd", sfx)])
        P.dve(lambda e: e.reciprocal(out=rstd[:], in_=rstd[:]), r=[("rstd", sfx)], w=[("rstd", sfx)])
        P.dve(lambda e: e.scalar_tensor_tensor(out=nb[:], in0=mv[:, 0:1], scalar=-1.0, in1=rstd[:], op0=ALU.mult, op1=ALU.mult),
              r=[("mv", sfx), ("rstd", sfx)], w=[("nb", sfx)])
        P.act(lambda e: e.activation(out=hb_ap, in_=xt_ap, func=ACT.Identity, bias=nb[:], scale=rstd[:]), r=[xkey, ("nb", sfx), ("rstd", sfx)], w=[hkey])

    def rms_rope(eng_add, src, nh, gain_bc, rt, dst, tmp, keys_r, key_w, sfx, eng2=None, tmp2=None):
        sq, ss, rk, ta, tb = tmp
        eng2 = eng2 or eng_add
        tc, td = tmp2 if tmp2 is not None else (ta, tb)
        kc_, kd_ = (("tc", sfx), ("td", sfx)) if tmp2 is not None else (("ta", sfx), ("tb", sfx))
        n = nh * 128
        P.dve(lambda e: e.tensor_tensor(out=sq[:, :n], in0=src[:, :n], in1=src[:, :n], op=ALU.mult), r=keys_r, w=[("sq", sfx)])
        P.dve(lambda e: e.tensor_reduce(out=ss[:, :nh], in_=sq[:, :n].rearrange("p (h d) -> p h d", d=128), axis=AX.X, op=ALU.add), r=[("sq", sfx)], w=[("ss", sfx)])
        P.act(lambda e: e.activation(out=rk[:, :nh], in_=ss[:, :nh], func=ACT.Sqrt, bias=eps_t[:], scale=1.0 / 128.0), r=[("ss", sfx), "eps_t"], w=[("rk", sfx)])
        P.dve(lambda e: e.reciprocal(out=rk[:, :nh], in_=rk[:, :nh]), r=[("rk", sfx)], w=[("rk", sfx)])
        P.dve(lambda e: e.tensor_tensor(out=src[:, :n].rearrange("p (h d) -> p h d", d=128), in0=src[:, :n].rearrange("p (h d) -> p h d", d=128),
                                        in1=fv(rk[:], 0, [[1, nh], [0, 128]]), op=ALU.mult), r=keys_r + [("rk", sfx)], w=keys_r)
        eng_add(lambda e: e.tensor_tensor(out=src[:, :n].rearrange("p (h d) -> p h d", d=128), in0=src[:, :n].rearrange("p (h d) -> p h d", d=128),
                                          in1=fv(gain_bc[:], 0, [[0, nh], [1, 128]]), op=ALU.mult), r=keys_r, w=keys_r)
        x1 = fv(src[:], 0, [[128, nh], [64, 2], [1, 32]])
        x2 = fv(src[:], 32, [[128, nh], [64, 2], [1, 32]])
        cs = fv(rt[:], 0, [[0, nh], [32, 2], [1, 32]])
        sn = fv(rt[:], 64, [[0, nh], [32, 2], [1, 32]])
        o1 = fv(dst[:], 0, [[128, nh], [64, 2], [1, 32]])
        o2 = fv(dst[:], 32, [[128, nh], [64, 2], [1, 32]])
        tav = fv(ta[:], 0, [[64, nh], [32, 2], [1, 32]])
        tbv = fv(tb[:], 0, [[64, nh], [32, 2], [1, 32]])
        tcv = fv(tc[:], 0, [[64, nh], [32, 2], [1, 32]])
        tdv = fv(td[:], 0, [[64, nh], [32, 2], [1, 32]])
        eng_add(lambda e: e.tensor_tensor(out=tav, in0=x1, in1=cs, op=ALU.mult), r=keys_r + [("rt", sfx)], w=[("ta", sfx)])
        eng2(lambda e: e.tensor_tensor(out=tbv, in0=x2, in1=sn, op=ALU.mult), r=keys_r + [("rt", sfx)], w=[("tb", sfx)])
        eng2(lambda e: e.tensor_tensor(out=o1, in0=tav, in1=tbv, op=ALU.subtract), r=[("ta", sfx), ("tb", sfx)], w=[key_w])
        eng_add(lambda e: e.tensor_tensor(out=tcv, in0=x1, in1=sn, op=ALU.mult), r=keys_r + [("rt", sfx)], w=[kc_])
        eng2(lambda e: e.tensor_tensor(out=tdv, in0=x2, in1=cs, op=ALU.mult), r=keys_r + [("rt", sfx)], w=[kd_])
        eng2(lambda e: e.tensor_tensor(out=o2, in0=tcv, in1=tdv, op=ALU.add), r=[kc_, kd_], w=[key_w])

    esA = ExitStack()
    u8 = SB(esA, "u8", [128, 32, NJF], BF16)
    with ExitStack() as es:
        stage = [SB(es, "stage%d" % i, [128, 8, 512], F32) for i in range(2)]
        Wkv = [SB(es, "Wkv%d" % r, [128, 8, 512], BF16) for r in range(2)]
        Wu = [SB(es, "Wu%d" % r, [128, 8, 512], BF16) for r in range(2)]
        xt = [SB(es, "xt%d" % i, [128, D], F32) for i in range(3)]
        rt = [SB(es, "rt%d" % i, [128, 128], F32) for i in range(2)]
        hb = [SB(es, "hb%d" % i, [128, D], BF16) for i in range(2)]
        hT = [SB(es, "hT%d" % i, [128, 8, 128], BF16) for i in range(2)]
        st = SB(es, "st", [128, 2, 6], F32)
        mv = SB(es, "mv", [128, 2], F32)
        rstd = SB(es, "rstd", [128, 1], F32)
        nbt = SB(es, "nbt", [128, 1], F32)
        k_sb = SB(es, "k_sb", [128, 256], F32)
        v_bf = [SB(es, "v_bf%d" % i, [128, 256], BF16) for i in range(2)]
        kr = SB(es, "kr", [128, 256], BF16)
        kT_sb = [SB(es, "kT_sb%d" % i, [128, 2, 128], BF16) for i in range(2)]
        tmp = (SB(es, "sq", [128, 256], F32), SB(es, "ss", [128, 2], F32), SB(es, "rk", [128, 2], F32),
               SB(es, "ta", [128, 128], F32), SB(es, "tb", [128, 128], F32))
        tmp2_A = (SB(es, "tcA", [128, 128], F32), SB(es, "tdA", [128, 128], F32))
        u_bf = SB(es, "u_bf", [128, 512], BF16)
        Um = [SB(es, "Um%d" % i, [128, 32, 128], BF16) for i in range(2)]
        pB = PS(es, "pB", [1, 512], F32)
        pT = [PS(es, "pT%d" % i, [128, 8, 128], BF16) for i in range(2)]
        pKV = PS(es, "pKV", [128, 512], F32)
        pU = PS(es, "pU", [128, 512], F32)
        pKT = PS(es, "pKT", [128, 2, 128], BF16)
        pU8 = PS(es, "pU8", [128, 32, 16], F32)
        prep_wblock(stage, pB, 0, Wkv[1], 1536, 1, 4096, ("Wkv", 1))
        prep_wblock(stage, pB, 1, Wu[1], 0, 1, 4608, ("Wu", 1))
        prep_wblock(stage, pB, 0, Wkv[0], 1536, 0, 1536, ("Wkv", 0))
        prep_wblock(stage, pB, 1, Wu[0], 0, 0, 0, ("Wu", 0))
        krA = [kr, SB(es, "kr2", [128, 256], BF16)]
        NTA = int(os.environ.get('K_NT', NTF))
        PIPE = int(os.environ.get('K_PIPEA', '3'))
        NSA = int(os.environ.get('K_NSA', '4'))
        stg = [[None] * NTA for _ in range(3)]
        stg1a = [None] * NTA
        for tt in range(NTA):
            r = 1 if tt < 2 else 0
            s3, s2 = tt % 3, tt % 2
            t0 = tt * 128
            bkv, bu = (4096, 4608) if r == 1 else (1536, 0)
            P.capture()
            P.dma(lambda e, s3=s3, t0=t0: e.dma_start(out=xt[s3][:], in_=xf[t0:t0 + 128, :]), key=("xt", s3), w=[("xt", s3)])
            P.dma(lambda e, s2=s2, t0=t0: e.dma_start(out=rt[s2][:], in_=rope_f[t0:t0 + 128, :]), key=("rtd", s2), w=[("rtA", s2)])
            ln_tile(xt[s3][:], ("xt", s3), st, mv, rstd, nbt, hb[s2][:], ("hb", s2), "A")
            stg[0][tt] = P.end_capture()
            P.capture()
            for kc in range(8):
                P.pe(lambda e, kc=kc, s2=s2: e.transpose(out=pT[s2][:, kc, :], in_=hb[s2][:, kc * 128:(kc + 1) * 128], identity=ident_b[:]),
                     r=[("hb", s2), "ident_b"], w=[("pT", s2)])
            P.dve(lambda e, s2=s2: e.tensor_copy(out=hT[s2][:], in_=pT[s2][:]), r=[("pT", s2)], w=[("hT", s2)])
            stg1a[tt] = P.end_capture()
            P.capture()
            for kc in range(8):
                P.pe(lambda e, kc=kc, s2=s2, r=r: e.matmul(out=pKV[:], lhsT=hT[s2][:, kc, :], rhs=Wkv[r][:, kc, :], start=(kc == 0), stop=False),
                     r=[("hT", s2), ("Wkv", r)], w=["pKV"])
            P.pe(lambda e, bkv=bkv: e.matmul(out=pKV[:], lhsT=ones_b[0:1, :], rhs=bias_sb[0:1, bkv:bkv + 512], start=False, stop=True),
                 r=["ones_b", "bias_sb"], w=["pKV"])
            for kc in range(8):
                P.pe(lambda e, kc=kc, s2=s2, r=r: e.matmul(out=pU[:], lhsT=hT[s2][:, kc, :], rhs=Wu[r][:, kc, :], start=(kc == 0), stop=False),
                     r=[("hT", s2), ("Wu", r)], w=["pU"])
            P.pe(lambda e, bu=bu: e.matmul(out=pU[:], lhsT=ones_b[0:1, :], rhs=bias_sb[0:1, bu:bu + 512], start=False, stop=True),
                 r=["ones_b", "bias_sb"], w=["pU"])
            P.act(lambda e: e.activation(out=k_sb[:], in_=pKV[:, 0:256], func=ACT.Copy), r=["pKV"], w=["k_sb"])
            P.act(lambda e, s2=s2: e.activation(out=v_bf[s2][:], in_=pKV[:, 256:512], func=ACT.Copy), r=["pKV"], w=[("v_bf", s2)])
            P.dma(lambda e, s2=s2, t0=t0: e.dma_start(out=v_d[t0:t0 + 128, :], in_=v_bf[s2][:]), key=("vd", s2), r=[("v_bf", s2)], w=["v_d"])
            P.act(lambda e: e.activation(out=u_bf[:], in_=pU[:], func=ACT.Copy), r=["pU"], w=["u_bf"])
            for s_ in range(NSA):
                P.act(lambda e, s2=s2, s_=s_: e.activation(out=Um[s2][:, :, s_ * 16:(s_ + 1) * 16], in_=u_bf[:].rearrange("p (g c) -> p g c", c=16),
                                                         func=ACT.Copy, scale=mask8f[:, s_:s_ + 1]), r=["u_bf", "mask8f"], w=[("Um", s2, s_)])
            P.dve(lambda e, s2=s2: e.tensor_tensor(out=Um[s2][:, :, NSA * 16:128].rearrange("p g (s c) -> p g s c", c=16), in0=fv(u_bf[:], 0, [[16, 32], [0, 8 - NSA], [1, 16]]),
                                                   in1=fv(mask8[:], NSA, [[0, 32], [1, 8 - NSA], [0, 16]]), op=ALU.mult), r=["u_bf", "mask8"], w=[("Um", s2, "v")])
            rms_rope(P.pool, k_sb, 2, gk_bc, rt[s2], krA[s2], tmp, ["k_sb", ("rtA", s2), "gk_bc"], ("kr", s2), "A", eng2=P.dve, tmp2=tmp2_A)
            stg[1][tt] = P.end_capture()
            P.capture()
            for h in range(2):
                P.pe(lambda e, h=h, s2=s2: e.transpose(out=pKT[:, h, :], in_=krA[s2][:, h * 128:(h + 1) * 128], identity=ident_b[:]), r=[("kr", s2), "ident_b"], w=["pKT"])
            P.act(lambda e, s2=s2: e.activation(out=kT_sb[s2][:], in_=pKT[:], func=ACT.Copy), r=["pKT"], w=[("kT_sb", s2)])
            P.dma(lambda e, s2=s2, t0=t0: e.dma_start(out=kT_d[:, :, t0:t0 + 128].rearrange("h p t -> p h t"), in_=kT_sb[s2][:]),
                  key=("kTd", s2), r=[("kT_sb", s2)], w=["kT_d"])
            for g in range(32):
                P.pe(lambda e, g=g, s2=s2: e.matmul(out=pU8[:, g, :], lhsT=Um[s2][:, g, :], rhs=sel16[:], start=True, stop=True),
                     r=[("Um", s2, x_) for x_ in list(range(NSA)) + ["v"]] + ["sel16"], w=["pU8"])
            P.act(lambda e, tt=tt: e.activation(out=u8[:, :, tt * 16:(tt + 1) * 16], in_=pU8[:], func=ACT.Copy), r=["pU8"], w=["u8"])
            stg[2][tt] = P.end_capture()
        if PIPE == 0:
            for tt in range(NTA):
                P.ops.extend(stg[0][tt]); P.ops.extend(stg1a[tt]); P.ops.extend(stg[1][tt]); P.ops.extend(stg[2][tt])
        else:
            for step in range(NTA + 2):
                for lst, off in ((stg1a, 1), (stg[0], 0), (stg[2], 2), (stg[1], 1)):
                    j = step - off
                    if 0 <= j < NTA:
                        P.ops.extend(lst[j])
        dump("u8", u8[:], [128, 32, NJF], BF16, r=["u8"])
        if "kT" in dbg_names:
            o = nc.dram_tensor("dbg_kT", [2, 128, NFULL], BF16, kind="ExternalOutput").ap()
            dbg_out["kT"] = o
            P.dma(lambda e: e.dma_start(out=o, in_=kT_d), key="dbg_kT", r=["kT_d"], w=["dbg_kT"])
        if "v" in dbg_names:
            o2 = nc.dram_tensor("dbg_v", [NFULL, 256], BF16, kind="ExternalOutput").ap()
            dbg_out["v"] = o2
            P.dma(lambda e: e.dma_start(out=o2, in_=v_d), key="dbg_v", r=["v_d"], w=["dbg_v"])
        P.barrier()
    if STOP <= 1:
        esA.close()
        return finish(nc, P, ges, out, dbg_out)

    TWO_PI = 2.0 * math.pi
    MAGIC = 12582912.0
    esS = ExitStack()
    esH = ExitStack()
    esZ = ExitStack()
    S_re = SB(esH, "S_re", [128, 2, 16, NJ], F32)
    S_im = SB(esH, "S_im", [128, 2, 16, NJ], F32)
    erZ = SB(esZ, "erZ", [128, 2, 16, 64], F32)
    eiZ = SB(esZ, "eiZ", [128, 2, 16, 64], F32)
    erZ8 = SB(esS, "erZ8", [128, 2, 16, 8], F32)
    eiZ8 = SB(esS, "eiZ8", [128, 2, 16, 8], F32)
    erR = SB(esS, "erR", [128, 2, 16, 72], F32)
    eiR = SB(esS, "eiR", [128, 2, 16, 72], F32)
    bbre = SB(esS, "bbre", [128, 2, 16, 16], F32)
    bbim = SB(esS, "bbim", [128, 2, 16, 16], F32)
    ccre = SB(esS, "ccre", [128, 2, 16, 16], F32)
    ccim = SB(esS, "ccim", [128, 2, 16, 16], F32)
    A64r = SB(esS, "A64r", [128, 2, 16, 1], F32)
    A64i = SB(esS, "A64i", [128, 2, 16, 1], F32)
    ardt = SB(esS, "ardt", [128, 2, 16], F32)
    aidt = SB(esS, "aidt", [128, 2, 16], F32)
    ptmp = []
    piT = SB(esS, "piT", [128, 1], F32)

    def powtab(exps_ap, n, er_t, ei_t, tag):
        def v4(t):
            return t[:, :, :, 0:n]
        a_b = lambda t: fv(t[:], 0, [[16, 2], [1, 16], [0, n]])
        e_b = fv(exps_ap, 0, [[exps_ap.ap[1][0], 2], [0, 16], [1, n]])
        t0, t1, t2 = ptmp
        k = lambda i: ("ptmp", i)
        P.dve(lambda e: e.tensor_tensor(out=v4(t0), in0=a_b(ardt), in1=e_b, op=ALU.mult), r=["ardt", tag + "_e"], w=[k(0)])
        P.act(lambda e: e.activation(out=v4(t0), in_=v4(t0), func=ACT.Exp), r=[k(0)], w=[k(0)])
        P.dve(lambda e: e.tensor_tensor(out=v4(t1), in0=a_b(aidt), in1=e_b, op=ALU.mult), r=["aidt", tag + "_e"], w=[k(1)])
        P.dve(lambda e: e.tensor_scalar(out=v4(t2), in0=v4(t1), scalar1=MAGIC, scalar2=None, op0=ALU.add), r=[k(1)], w=[k(2)])
        P.dve(lambda e: e.tensor_scalar(out=v4(t2), in0=v4(t2), scalar1=MAGIC, scalar2=None, op0=ALU.subtract), r=[k(2)], w=[k(2)])
        P.dve(lambda e: e.tensor_tensor(out=v4(t2), in0=v4(t1), in1=v4(t2), op=ALU.subtract), r=[k(1), k(2)], w=[k(2)])
        P.act(lambda e: e.activation(out=v4(t2), in_=v4(t2), func=ACT.Sin, scale=TWO_PI), r=[k(2)], w=[k(2)])
        P.dve(lambda e: e.tensor_tensor(out=v4(ei_t), in0=v4(t0), in1=v4(t2), op=ALU.mult), r=[k(0), k(2)], w=[tag + "_ei"])
        P.dve(lambda e: e.tensor_scalar(out=v4(t1), in0=v4(t1), scalar1=0.25, scalar2=None, op0=ALU.add), r=[k(1)], w=[k(1)])
        P.dve(lambda e: e.tensor_scalar(out=v4(t2), in0=v4(t1), scalar1=MAGIC, scalar2=None, op0=ALU.add), r=[k(1)], w=[k(2)])
        P.dve(lambda e: e.tensor_scalar(out=v4(t2), in0=v4(t2), scalar1=MAGIC, scalar2=None, op0=ALU.subtract), r=[k(2)], w=[k(2)])
        P.dve(lambda e: e.tensor_tensor(out=v4(t2), in0=v4(t1), in1=v4(t2), op=ALU.subtract), r=[k(1), k(2)], w=[k(2)])
        P.act(lambda e: e.activation(out=v4(t2), in_=v4(t2), func=ACT.Sin, scale=TWO_PI), r=[k(2)], w=[k(2)])
        P.dve(lambda e: e.tensor_tensor(out=v4(er_t), in0=v4(t0), in1=v4(t2), op=ALU.mult), r=[k(0), k(2)], w=[tag + "_er"])

    with ExitStack() as es:
        ptmp.extend([SB(es, "ptmp%d" % i, [128, 2, 16, 72], F32) for i in range(3)])
        a_sb = SB(es, "a_sb", [128, 2, 2, 16], F32)
        ldt_sb = SB(es, "ldt_sb", [128, 2, 16], F32)
        b_sb = SB(es, "b_sb", [128, 2, 2, 16, 16], F32)
        c_sb = SB(es, "c_sb", [128, 2, 2, 16, 16], F32)
        eZ_sb = SB(es, "eZ_sb", [128, 2, 64], F32)
        eR_sb = SB(es, "eR_sb", [128, 2, 72], F32)
        e1_sb = SB(es, "e1_sb", [128, 2, 1], F32)
        e64_sb = SB(es, "e64_sb", [128, 2, 1], F32)
        abr = SB(es, "abr", [128, 2, 16, 1], F32)
        abi = SB(es, "abi", [128, 2, 16, 1], F32)
        dsc = [SB(es, "dsc%d" % i, [128, 2, 16], F32) for i in range(5)]
        bt = [SB(es, "bt%d" % i, [128, 2, 16, 16], F32) for i in range(2)]
        P.dma(lambda e: e.dma_start(out=a_sb[:], in_=s5_a), key="a_sb", w=["a_sb"])
        P.dma(lambda e: e.dma_start(out=ldt_sb[:], in_=s5_ldt), key="ldt_sb", w=["ldt_sb"])
        P.dma(lambda e: e.dma_start(out=b_sb[:], in_=s5_b), key="b_sb", w=["b_sb"])
        P.dma(lambda e: e.dma_start(out=c_sb[:], in_=s5_c), key="c_sb", w=["c_sb"])
        P.dma(lambda e: e.dma_start(out=eZ_sb[:], in_=cst_eZ), key="eZ_sb", w=["Z_e"])
        P.dma(lambda e: e.dma_start(out=eR_sb[:], in_=cst_eR), key="eR_sb", w=["R_e"])
        P.dve(lambda e: e.memset(e1_sb[:], 1.0), w=["ab_e"])
        P.dve(lambda e: e.memset(e64_sb[:], 64.0), w=["A64_e"])
        P.act(lambda e: e.activation(out=ldt_sb[:], in_=ldt_sb[:], func=ACT.Exp), r=["ldt_sb"], w=["ldt_sb"])
        P.dve(lambda e: e.tensor_tensor(out=ardt[:], in0=a_sb[:, 0], in1=ldt_sb[:], op=ALU.mult), r=["a_sb", "ldt_sb"], w=["ardt"])
        P.dve(lambda e: e.scalar_tensor_tensor(out=aidt[:], in0=a_sb[:, 1], scalar=1.0 / TWO_PI, in1=ldt_sb[:], op0=ALU.mult, op1=ALU.mult),
              r=["a_sb", "ldt_sb"], w=["aidt"])
        powtab(e1_sb[:], 1, abr, abi, "ab")
        powtab(e64_sb[:], 1, A64r, A64i, "A64")
        powtab(eZ_sb[:], 64, erZ, eiZ, "Z")
        powtab(eR_sb[:], 72, erR, eiR, "R")
        for (src_t, dst_t, kk) in ((erZ, erZ8, "Z_er"), (eiZ, eiZ8, "Z_ei")):
            P.dve(lambda e, src_t=src_t, dst_t=dst_t: e.tensor_copy(out=dst_t[:, 0], in_=src_t[:, 0, :, 56:64]), r=[kk], w=[kk + "8"])
            P.dve(lambda e, src_t=src_t, dst_t=dst_t: e.tensor_copy(out=dst_t[:, 1], in_=src_t[:, 1, :, 0:8]), r=[kk], w=[kk + "8"])
        are, aim = a_sb[:, 0], a_sb[:, 1]
        d0, d1, d2, d3, d4 = [t[:] for t in dsc]
        ab_r = abr[:].rearrange("p d q o -> p d (q o)")
        ab_i = abi[:].rearrange("p d q o -> p d (q o)")
        kd = lambda i: ("dsc", i)
        P.dve(lambda e: e.tensor_tensor(out=d0, in0=are, in1=are, op=ALU.mult), r=["a_sb"], w=[kd(0)])
        P.dve(lambda e: e.tensor_tensor(out=d1, in0=aim, in1=aim, op=ALU.mult), r=["a_sb"], w=[kd(1)])
        P.dve(lambda e: e.tensor_tensor(out=d0, in0=d0, in1=d1, op=ALU.add), r=[kd(0), kd(1)], w=[kd(0)])
        P.dve(lambda e: e.reciprocal(out=d0, in_=d0), r=[kd(0)], w=[kd(0)])
        P.dve(lambda e: e.tensor_scalar(out=d1, in0=ab_r, scalar1=-1.0, scalar2=None, op0=ALU.add), r=["ab_er"], w=[kd(1)])
        P.dve(lambda e: e.tensor_tensor(out=d2, in0=d1, in1=are, op=ALU.mult), r=[kd(1), "a_sb"], w=[kd(2)])
        P.dve(lambda e: e.tensor_tensor(out=d3, in0=ab_i, in1=aim, op=ALU.mult), r=["ab_ei", "a_sb"], w=[kd(3)])
        P.dve(lambda e: e.tensor_tensor(out=d2, in0=d2, in1=d3, op=ALU.add), r=[kd(2), kd(3)], w=[kd(2)])
        P.dve(lambda e: e.tensor_tensor(out=d2, in0=d2, in1=d0, op=ALU.mult), r=[kd(2), kd(0)], w=[kd(2)])
        P.dve(lambda e: e.tensor_tensor(out=d3, in0=ab_i, in1=are, op=ALU.mult), r=["ab_ei", "a_sb"], w=[kd(3)])
        P.dve(lambda e: e.tensor_tensor(out=d4, in0=d1, in1=aim, op=ALU.mult), r=[kd(1), "a_sb"], w=[kd(4)])
        P.dve(lambda e: e.tensor_tensor(out=d3, in0=d3, in1=d4, op=ALU.subtract), r=[kd(3), kd(4)], w=[kd(3)])
        P.dve(lambda e: e.tensor_tensor(out=d3, in0=d3, in1=d0, op=ALU.mult), r=[kd(3), kd(0)], w=[kd(3)])
        rr_b = fv(dsc[2][:], 0, [[16, 2], [1, 16], [0, 16]])
        ri_b = fv(dsc[3][:], 0, [[16, 2], [1, 16], [0, 16]])
        bre, bim = b_sb[:, 0], b_sb[:, 1]
        P.dve(lambda e: e.tensor_tensor(out=bt[0][:], in0=bre, in1=rr_b, op=ALU.mult), r=["b_sb", kd(2)], w=["bt0"])
        P.dve(lambda e: e.tensor_tensor(out=bt[1][:], in0=bim, in1=ri_b, op=ALU.mult), r=["b_sb", kd(3)], w=["bt1"])
        P.dve(lambda e: e.tensor_tensor(out=bbre[:], in0=bt[0][:], in1=bt[1][:], op=ALU.subtract), r=["bt0", "bt1"], w=["bbre"])
        P.dve(lambda e: e.tensor_tensor(out=bt[0][:], in0=bim, in1=rr_b, op=ALU.mult), r=["b_sb", kd(2)], w=["bt0"])
        P.dve(lambda e: e.tensor_tensor(out=bt[1][:], in0=bre, in1=ri_b, op=ALU.mult), r=["b_sb", kd(3)], w=["bt1"])
        P.dve(lambda e: e.tensor_tensor(out=bbim[:], in0=bt[0][:], in1=bt[1][:], op=ALU.add), r=["bt0", "bt1"], w=["bbim"])
        P.dve(lambda e: e.tensor_copy(out=ccre[:], in_=c_sb[:, 0]), r=["c_sb"], w=["ccre"])
        P.dve(lambda e: e.tensor_copy(out=ccim[:], in_=c_sb[:, 1]), r=["c_sb"], w=["ccim"])
        P.barrier()

    def ztab(eng_add, pair, tsl, nt, bufs, tag, erZ=erZ, eiZ=eiZ, tmkey=None):
        zre, zim, ztm = bufs
        tmkey = tmkey or (tag + "ztm")
        def ev(t, d):
            return fv(t[:, d, pair, tsl[d]:tsl[d] + nt], 0, [[1, nt], [0, 16]])
        def bv(t, d):
            return fv(t[:, d, pair, :], 0, [[0, nt], [1, 16]])
        for d in range(2):
            eng_add(lambda e, d=d: e.tensor_tensor(out=zre[:, d], in0=bv(bbre, d), in1=ev(erZ, d), op=ALU.mult), r=["Z_er", "bbre"], w=[(tag + "zre", d)])
            eng_add(lambda e, d=d: e.tensor_tensor(out=ztm[:, d], in0=bv(bbim, d), in1=ev(eiZ, d), op=ALU.mult), r=["Z_ei", "bbim"], w=[(tmkey, d)])
            eng_add(lambda e, d=d: e.tensor_tensor(out=zre[:, d], in0=zre[:, d], in1=ztm[:, d], op=ALU.subtract), r=[(tag + "zre", d), (tmkey, d)], w=[(tag + "zre", d)])
            eng_add(lambda e, d=d: e.tensor_tensor(out=zim[:, d], in0=bv(bbim, d), in1=ev(erZ, d), op=ALU.mult), r=["Z_er", "bbim"], w=[(tag + "zim", d)])
            eng_add(lambda e, d=d: e.tensor_tensor(out=ztm[:, d], in0=bv(bbre, d), in1=ev(eiZ, d), op=ALU.mult), r=["Z_ei", "bbre"], w=[(tmkey, d)])
            eng_add(lambda e, d=d: e.tensor_tensor(out=zim[:, d], in0=zim[:, d], in1=ztm[:, d], op=ALU.add), r=[(tag + "zim", d), (tmkey, d)], w=[(tag + "zim", d)])

    with ExitStack() as es:
        Wsum = [SB(es, "Wsum%d" % i, [128, 2, 8, 2, 128], BF16) for i in range(2)]
        zb = [[SB(es, "zb%d_%d" % (i, j), [128, 2, 64, 16], F32) for j in range(3)] for i in range(1)]
        pW = [PS(es, "pW%d" % i, [128, 4, 128], F32) for i in range(2)]
        pSr = PS(es, "pSr", [128, 2, NJ], F32)
        pSi = PS(es, "pSi", [128, 2, NJ], F32)
        ci = 0
        for pair in range(16):
            sl = int(os.environ.get('K_SL', pair % 2))
            ea = P.dve
            zt_ = "z0"
            ztab(ea, pair, (0, 0), 64, zb[0], zt_)
            zre, zim, _ = zb[0]
            for d in range(2):
                for reim in range(2):
                    zt = zre if reim == 0 else zim
                    for mq in range(2):
                        pw = pW[ci % 2]
                        for j in range(4):
                            m_ = mq * 4 + j
                            P.pe(lambda e, zt=zt, d=d, m_=m_, pw=pw, j=j: e.transpose(out=pw[:, j, :], in_=zt[:, d, m_ * 8:(m_ + 1) * 8, :].rearrange("p s c -> p (s c)"), identity=ident_f[:]),
                                 r=[(zt_ + "z%s" % ("re" if reim == 0 else "im"), d), "ident_f"], w=[("pW", ci % 2)])
                        dst = Wsum[sl][:, d, mq * 4:(mq + 1) * 4, reim, :]
                        P.act(lambda e, dst=dst, pw=pw: e.activation(out=dst, in_=pw[:], func=ACT.Copy), r=[("pW", ci % 2)], w=[("Wsum", sl)])
                        ci += 1
            for d in range(2):
                for gh in range(2):
                    g = 2 * pair + gh
                    for reim, pS_ in ((0, pSr), (1, pSi)):
                        for m_ in range(8):
                            P.pe(lambda e, d=d, gh=gh, g=g, reim=reim, pS_=pS_, m_=m_, sl=sl: e.matmul(
                                out=pS_[64 * gh:64 * gh + 64, d, :], lhsT=Wsum[sl][:, d, m_, reim, 64 * gh:64 * gh + 64],
                                rhs=fv(u8[:, g, :], m_, [[8, NJ]]), start=(m_ == 0), stop=(m_ == 7)),
                                r=[("Wsum", sl), "u8"], w=["pSr" if reim == 0 else "pSi"])
            P.act(lambda e, pair=pair: e.activation(out=S_re[:, :, pair, :], in_=pSr[:], func=ACT.Copy), r=["pSr"], w=["S_re"])
            P.act(lambda e, pair=pair: e.activation(out=S_im[:, :, pair, :], in_=pSi[:], func=ACT.Copy), r=["pSi"], w=["S_im"])
        P.barrier()
    esA.close()
    esZ.close()

    with ExitStack() as es:
        sct = [[SB(es, "sct%d_%d" % (d, i), [128, 16], F32) for i in range(4)] for d in range(2)]
        orders = [[(J, J - 1 if J > 0 else None) for J in range(NJ)],
                  [(3, None), (2, 3), (1, 2), (0, 1), (131, 0)] + [(J, J + 1) for J in range(130, 3, -1)]]
        for d in range(2):
            ea = P.dve if d == 0 else P.pool
            Ar = A64r[:, d, :, 0]
            Ai = A64i[:, d, :, 0]
            t = [x[:] for x in sct[d]]
            kt = lambda i: ("sct", d, i)
            kr_, ki_ = ("Hre", d), ("Him", d)
            for (J, Jp) in orders[d]:
                if Jp is None:
                    continue
                hr_p, hi_p = S_re[:, d, :, Jp], S_im[:, d, :, Jp]
                hr, hi = S_re[:, d, :, J], S_im[:, d, :, J]
                ea(lambda e, t=t, hr_p=hr_p, Ar=Ar: e.tensor_tensor(out=t[0], in0=Ar, in1=hr_p, op=ALU.mult), r=["A64_er", kr_, "S_re"], w=[kt(0)])
                ea(lambda e, t=t, hi_p=hi_p, Ai=Ai: e.tensor_tensor(out=t[1], in0=Ai, in1=hi_p, op=ALU.mult), r=["A64_ei", ki_, "S_im"], w=[kt(1)])
                ea(lambda e, t=t: e.tensor_tensor(out=t[0], in0=t[0], in1=t[1], op=ALU.subtract), r=[kt(0), kt(1)], w=[kt(0)])
                ea(lambda e, t=t, hi_p=hi_p, Ar=Ar: e.tensor_tensor(out=t[2], in0=Ar, in1=hi_p, op=ALU.mult), r=["A64_er", ki_, "S_im"], w=[kt(2)])
                ea(lambda e, t=t, hr_p=hr_p, Ai=Ai: e.tensor_tensor(out=t[3], in0=Ai, in1=hr_p, op=ALU.mult), r=["A64_ei", kr_, "S_re"], w=[kt(3)])
                ea(lambda e, t=t: e.tensor_tensor(out=t[2], in0=t[2], in1=t[3], op=ALU.add), r=[kt(2), kt(3)], w=[kt(2)])
                ea(lambda e, t=t, hr=hr: e.tensor_tensor(out=hr, in0=hr, in1=t[0], op=ALU.add), r=[kt(0), kr_], w=[kr_])
                ea(lambda e, t=t, hi=hi: e.tensor_tensor(out=hi, in0=hi, in1=t[2], op=ALU.add), r=[kt(2), ki_], w=[ki_])
        P.barrier()
    dump("H_re", S_re[:], [128, 2, 16, NJ])
    dump("H_im", S_im[:], [128, 2, 16, NJ])
    HO = [[SB(esS, "HO%d_%d" % (d, x), [128, 16, 32], BF16) for x in range(2)] for d in range(2)]
    with ExitStack() as es:
        cm = SB(es, "cm", [128, 4], F32)
        hacc = SB(es, "hacc", [128, 16, 32], F32)
        P.dma(lambda e: e.dma_start(out=cm[:], in_=cmask), key="cm", w=["cm"])
        for d in range(2):
            for x, Sx in enumerate((S_re, S_im)):
                for r_ in range(4):
                    lo = (3 + 32 * r_) if d == 0 else (5 + 32 * r_)
                    segs = [(lo, 0, 32)] if not (d == 1 and r_ == 3) else [(lo, 0, 31), (0, 31, 1)]
                    for (a0, o0, n_) in segs:
                        src_ = Sx[:, d, :, a0:a0 + n_]
                        dst_ = hacc[:, :, o0:o0 + n_]
                        if r_ == 0:
                            P.dve(lambda e, src_=src_, dst_=dst_: e.tensor_scalar(out=dst_, in0=src_, scalar1=cm[:, 0:1], scalar2=None, op0=ALU.mult),
                                  r=["cm"], w=["hacc"])
                        else:
                            P.dve(lambda e, src_=src_, dst_=dst_, r_=r_: e.scalar_tensor_tensor(out=dst_, in0=src_, scalar=cm[:, r_:r_ + 1], in1=dst_, op0=ALU.mult, op1=ALU.add),
                                  r=["cm", "hacc"], w=["hacc"])
                P.dve(lambda e, d=d, x=x: e.tensor_copy(out=HO[d][x][:], in_=hacc[:]), r=["hacc"], w=[("HO", d, x)])
        P.barrier()
    esH.close()
    if STOP <= 2:
        esS.close()
        return finish(nc, P, ges, out, dbg_out)

    esP = ExitStack()
    hTo = SB(esP, "hTo", [128, 8, NOWN], BF16)
    qT = SB(esP, "qT", [128, 8, NOWN], BF16)
    esU = ExitStack()
    u8o = SB(esU, "u8o", [128, 32, 256], BF16)

    def load_bc(dst, src_row, key, add_one=False):
        P.dma(lambda e: e.dma_start(out=dst[:], in_=src_row.partition_broadcast(128)), key=key, w=[key])
        if add_one:
            P.dve(lambda e: e.tensor_scalar(out=dst[:], in0=dst[:], scalar1=1.0, scalar2=None, op0=ALU.add), r=[key], w=[key])

    with ExitStack() as es:
        Wq = SB(es, "Wq", [128, 8, 1024], BF16)
        Wuo = SB(es, "Wuo", [128, 8, 512], BF16)
        sc1p_bc = SB(es, "sc1p_bc", [128, D], F32)
        sh1_bc = SB(es, "sh1_bc", [128, D], F32)
        xt_B = [SB(es, "xtB%d" % i, [128, D], F32) for i in range(2)]
        rt_B = [SB(es, "rtB%d" % i, [128, 128], F32) for i in range(2)]
        hn = SB(es, "hn", [128, D], F32)
        hb_B = [SB(es, "hbB%d" % i, [128, D], BF16) for i in range(2)]
        st_B = SB(es, "stB", [128, 2, 6], F32)
        mv_B = SB(es, "mvB", [128, 2], F32)
        rstd_B = SB(es, "rstdB", [128, 1], F32)
        nbt_B = SB(es, "nbtB", [128, 1], F32)
        q_sbs = [SB(es, "q_sb%d" % i, [128, 1024], F32) for i in range(2)]
        tmp2_B = (SB(es, "tcB", [128, 512], F32), SB(es, "tdB", [128, 512], F32))
        qr = SB(es, "qr", [128, 1024], BF16)
        tmp_B = (SB(es, "sqB", [128, 1024], F32), SB(es, "ssB", [128, 8], F32), SB(es, "rkB", [128, 8], F32),
               SB(es, "taB", [128, 512], F32), SB(es, "tbB", [128, 512], F32))
        u_bf_B = SB(es, "u_bfB", [128, 512], BF16)
        Um_B = [SB(es, "UmB0", [128, 32, 128], BF16)] * 2
        pT_B = [PS(es, "pTB%d" % i, [128, 8, 128], BF16) for i in range(2)]
        pQ = [PS(es, "pQ%d" % i, [128, 512], F32) for i in range(2)]
        pQT = PS(es, "pQT", [128, 8, 128], BF16)
        pU_B = PS(es, "pUB", [128, 512], F32)
        pU8_B = PS(es, "pU8B", [128, 32, 16], F32)
        P.dma(lambda e: e.dma_start(out=Wq[:], in_=w_in[:, 512:1536].rearrange("(kc p) n -> p kc n", p=128)), key="Wq", w=["Wq"], eng="pool")
        P.dma(lambda e: e.dma_start(out=Wuo[:], in_=w_in[:, 0:512].rearrange("(kc p) n -> p kc n", p=128)), key="Wuo", w=["Wuo"], eng="pool")
        load_bc(sc1p_bc, mod_d[0:1, 1024:2048], "sc1p_bc", True)
        load_bc(sh1_bc, mod_d[0:1, 0:1024], "sh1_bc")
        sB0, sB1a, sB1b, sB2 = [None] * NTO, [None] * NTO, [None] * NTO, [None] * NTO
        for tt in range(NTO):
            s2 = tt % 2
            t0 = tt * 128
            P.capture()
            P.dma(lambda e, s2=s2, t0=t0: e.dma_start(out=xt_B[s2][:], in_=xo[t0:t0 + 128, :]), key=("xtB", s2), w=[("xtB", s2)])
            P.dma(lambda e, s2=s2, t0=t0: e.dma_start(out=rt_B[s2][:], in_=rope_o[t0:t0 + 128, :]), key=("rtB", s2), w=[("rtB", s2)])
            ln_tile(xt_B[s2][:], ("xtB", s2), st_B, mv_B, rstd_B, nbt_B, hn[:], "hn", "B")
            P.dve(lambda e: e.tensor_tensor(out=hn[:], in0=hn[:], in1=sc1p_bc[:], op=ALU.mult), r=["hn", "sc1p_bc"], w=["hn"])
            P.dve(lambda e, s2=s2: e.tensor_tensor(out=hb_B[s2][:], in0=hn[:], in1=sh1_bc[:], op=ALU.add), r=["hn", "sh1_bc"], w=[("hbB", s2)])
            sB0[tt] = P.end_capture()
            P.capture()
            for kc in range(8):
                P.pe(lambda e, kc=kc, s2=s2: e.transpose(out=pT_B[s2][:, kc, :], in_=hb_B[s2][:, kc * 128:(kc + 1) * 128], identity=ident_b[:]),
                     r=[("hbB", s2), "ident_b"], w=[("pTB", s2)])
            P.act(lambda e, s2=s2, t0=t0, tt=tt: e.activation(out=hTo[:, :, t0:t0 + 128], in_=pT_B[s2][:], func=ACT.Copy), r=[("pTB", s2)], w=[("hTo", tt)])
            sB1a[tt] = P.end_capture()
            P.capture()
            for half in range(2):
                for kc in range(8):
                    P.pe(lambda e, kc=kc, half=half, t0=t0: e.matmul(out=pQ[half][:], lhsT=hTo[:, kc, t0:t0 + 128], rhs=Wq[:, kc, half * 512:(half + 1) * 512],
                                                                     start=(kc == 0), stop=(kc == 7)), r=[("hTo", tt), "Wq"], w=[("pQ", half)])
                P.act(lambda e, half=half, s2=s2: e.activation(out=q_sbs[s2][:, half * 512:(half + 1) * 512], in_=pQ[half][:], func=ACT.Copy), r=[("pQ", half)], w=[("q_sb", s2)])
            for kc in range(8):
                P.pe(lambda e, kc=kc, t0=t0: e.matmul(out=pU_B[:], lhsT=hTo[:, kc, t0:t0 + 128], rhs=Wuo[:, kc, :], start=(kc == 0), stop=(kc == 7)),
                     r=[("hTo", tt), "Wuo"], w=["pUB"])
            P.act(lambda e: e.activation(out=u_bf_B[:], in_=pU_B[:], func=ACT.Copy), r=["pUB"], w=["u_bfB"])
            P.dve(lambda e, s2=s2: e.tensor_tensor(out=Um_B[s2][:].rearrange("p g (s c) -> p g s c", c=16), in0=fv(u_bf_B[:], 0, [[16, 32], [0, 8], [1, 16]]),
                                                   in1=fv(mask8[:], 0, [[0, 32], [1, 8], [0, 16]]), op=ALU.mult), r=["u_bfB", "mask8"], w=["UmB"])
            rms_rope(P.pool, q_sbs[s2], 8, gq_bc, rt_B[s2], qr, tmp_B, [("q_sb", s2), ("rtB", s2), "gq_bc"], "qr", "B", eng2=P.dve, tmp2=tmp2_B)
            sB1b[tt] = P.end_capture()
            P.capture()
            for h in range(8):
                P.pe(lambda e, h=h: e.transpose(out=pQT[:, h, :], in_=qr[:, h * 128:(h + 1) * 128], identity=ident_b[:]), r=["qr", "ident_b"], w=["pQT"])
            P.act(lambda e, t0=t0: e.activation(out=qT[:, :, t0:t0 + 128], in_=pQT[:], func=ACT.Copy), r=["pQT"], w=[("qT", tt)])
            for g in range(32):
                P.pe(lambda e, g=g, s2=s2: e.matmul(out=pU8_B[:, g, :], lhsT=Um_B[s2][:, g, :], rhs=sel16[:], start=True, stop=True),
                     r=["UmB", "sel16"], w=["pU8B"])
            P.act(lambda e, tt=tt: e.activation(out=u8o[:, :, tt * 16:(tt + 1) * 16], in_=pU8_B[:], func=ACT.Copy), r=["pU8B"], w=[("u8o", tt)])
            sB2[tt] = P.end_capture()
        if int(os.environ.get('K_PIPEB', '1')) == 0:
            for tt in range(NTO):
                for lst in (sB0, sB1a, sB1b, sB2):
                    P.ops.extend(lst[tt])
        else:
            for step in range(NTO + 2):
                for lst, off in ((sB1a, 1), (sB0, 0), (sB2, 2), (sB1b, 1)):
                    j = step - off
                    if 0 <= j < NTO:
                        P.ops.extend(lst[j])
        P.barrier()
    if "qT" in dbg_names:
        dump("qT", qT[:], [128, 8, NOWN], BF16)
    if STOP <= 3:
        esU.close(); esS.close(); esP.close()
        return finish(nc, P, ges, out, dbg_out)

    gT = SB(esP, "gT", [128, 4, NOWN], BF16)
    with ExitStack() as es:
        mf_sb = SB(es, "mf_sb", [128, 128], F32)
        mb_sb = SB(es, "mb_sb", [128, 128], F32)
        dcol_sb = SB(es, "dcol_sb", [128, 32], F32)
        selg_b = SB(es, "selg_b", [128, 8, 128], BF16)
        m8c_b = SB(es, "m8c_b", [128, 8], BF16)
        z8 = [SB(es, "z8_%d" % j, [128, 2, 8, 16], F32) for j in range(3)]
        Rre_p = SB(es, "Rre_p", [128, 2, 72, 16], F32)
        Rim_p = SB(es, "Rim_p", [128, 2, 72, 16], F32)
        Rt0 = SB(es, "Rt0", [128, 1, 72, 16], F32)
        Rt1 = SB(es, "Rt1", [128, 1, 72, 16], F32)
        Rre_b = SB(es, "Rre_b", [128, 2, 1152], BF16)
        Rim_b = SB(es, "Rim_b", [128, 2, 1152], BF16)
        Tw = [SB(es, "Tw0", [128, 2, 15, 128], BF16)] * 2
        tt0 = SB(es, "tt0", [128, 128], F32)
        tt1 = SB(es, "tt1", [128, 128], F32)
        Ye = [SB(es, "Ye0", [128, 2048], BF16)] * 2
        ysb = SB(es, "ysb", [128, 1024], F32)
        yx2 = SB(es, "yx2", [128, 1024], F32)
        pTb = [PS(es, "pTb%d" % i, [128, 128], F32) for i in range(2)]
        pY8 = [PS(es, "pY8_%d" % i, [128, 8, 32], F32) for i in range(2)]
        pYT = PS(es, "pYT", [128, 2048], F32)
        P.dma(lambda e: e.dma_start(out=mf_sb[:], in_=cst_mf), key="mf_sb", w=["mf_sb"])
        P.dma(lambda e: e.dma_start(out=mb_sb[:], in_=cst_mb), key="mb_sb", w=["mb_sb"])
        P.dma(lambda e: e.dma_start(out=dcol_sb[:], in_=s5_dcol), key="dcol_sb", w=["dcol_sb"])
        P.dma(lambda e: e.dma_start(out=selg_b[:], in_=cst_selg), key="selg_b", w=["selg_b"], eng="pool")
        P.dma(lambda e: e.dma_start(out=m8c_b[:], in_=cst_mask8c), key="m8c_b", w=["m8c_b"], eng="pool")
        ecnt = 0
        for pair in range(16):
            sl = 0
            ztab(P.dve, pair, (0, 0), 8, z8, "z8", erZ=erZ8, eiZ=eiZ8)
            for d in range(2):
                eb = lambda t, d=d, pair=pair: fv(t[:, d, pair, :], 0, [[1, 72], [0, 16]])
                cb = lambda t, d=d, pair=pair: fv(t[:, d, pair, :], 0, [[0, 72], [1, 16]])
                P.pool(lambda e, d=d, eb=eb, cb=cb: e.tensor_tensor(out=Rre_p[:, d], in0=cb(ccre), in1=eb(erR), op=ALU.mult), r=["R_er", "ccre"], w=["Rre_p"])
                P.pool(lambda e, d=d, eb=eb, cb=cb: e.tensor_tensor(out=Rt0[:, 0], in0=cb(ccim), in1=eb(eiR), op=ALU.mult), r=["R_ei", "ccim"], w=["Rt0"])
                P.pool(lambda e, d=d: e.tensor_tensor(out=Rre_p[:, d], in0=Rre_p[:, d], in1=Rt0[:, 0], op=ALU.subtract), r=["Rre_p", "Rt0"], w=["Rre_p"])
                P.dve(lambda e, d=d, eb=eb, cb=cb: e.tensor_tensor(out=Rim_p[:, d], in0=cb(ccim), in1=eb(erR), op=ALU.mult), r=["R_er", "ccim"], w=["Rim_p"])
                P.dve(lambda e, d=d, eb=eb, cb=cb: e.tensor_tensor(out=Rt1[:, 0], in0=cb(ccre), in1=eb(eiR), op=ALU.mult), r=["R_ei", "ccre"], w=["Rt1"])
                P.dve(lambda e, d=d: e.scalar_tensor_tensor(out=Rim_p[:, d], in0=Rt1[:, 0], scalar=-1.0, in1=Rim_p[:, d], op0=ALU.mult, op1=ALU.subtract),
                      r=["Rim_p", "Rt1"], w=["Rim_p"])
            P.act(lambda e: e.activation(out=Rre_b[:], in_=Rre_p[:].rearrange("p d i c -> p d (i c)"), func=ACT.Copy), r=["Rre_p"], w=["Rre_b"])
            P.act(lambda e: e.activation(out=Rim_b[:], in_=Rim_p[:].rearrange("p d i c -> p d (i c)"), func=ACT.Copy), r=["Rim_p"], w=["Rim_b"])
            for gh in range(2):
                g = 2 * pair + gh
                rows = slice(64 * gh, 64 * gh + 64)
                blocks = [(0, 0), (1, 0)] + [(0, k) for k in range(1, 8)] + [(1, k) for k in range(1, 8)]
                for (d, k) in blocks:
                    pt = pTb[ecnt % 2]
                    kk = ("pTb", ecnt % 2)
                    P.pe(lambda e, pt=pt, d=d, k=k, rows=rows: e.matmul(out=pt[:], lhsT=z8[0][rows, d].rearrange("p s c -> p (s c)"),
                                                                        rhs=Rre_p[rows, d, 8 * k:8 * k + 8, :].rearrange("p s c -> p (s c)"), start=True, stop=False),
                         r=[("z8zre", d), "Rre_p"], w=[kk])
                    P.pe(lambda e, pt=pt, d=d, k=k, rows=rows: e.matmul(out=pt[:], lhsT=z8[1][rows, d].rearrange("p s c -> p (s c)"),
                                                                        rhs=Rim_p[rows, d, 8 * k:8 * k + 8, :].rearrange("p s c -> p (s c)"), start=False, stop=True),
                         r=[("z8zim", d), "Rim_p"], w=[kk])
                    if k == 0 and d == 0:
                        P.dve(lambda e, pt=pt: e.tensor_tensor(out=tt0[:], in0=pt[:], in1=mf_sb[:], op=ALU.mult), r=[kk, "mf_sb"], w=["tt0"])
                    elif k == 0 and d == 1:
                        P.dve(lambda e, pt=pt: e.tensor_tensor(out=tt1[:], in0=pt[:], in1=mb_sb[:], op=ALU.mult), r=[kk, "mb_sb"], w=["tt1"])
                        P.dve(lambda e: e.tensor_tensor(out=tt0[:], in0=tt0[:], in1=tt1[:], op=ALU.add), r=["tt0", "tt1"], w=["tt0"])
                        P.dve(lambda e, g=g, gh=gh, sl=sl: e.scalar_tensor_tensor(out=Tw[sl][:, gh, 7, :], in0=ident_f[:], scalar=dcol_sb[:, g:g + 1], in1=tt0[:],
                                                                                 op0=ALU.mult, op1=ALU.add), r=["tt0", "dcol_sb", "ident_f"], w=[("Tw", sl)])
                    else:
                        idx = 7 + k if d == 0 else 7 - k
                        if ecnt % 2 == 0:
                            P.act(lambda e, pt=pt, gh=gh, sl=sl, idx=idx: e.activation(out=Tw[sl][:, gh, idx, :], in_=pt[:], func=ACT.Copy), r=[kk], w=[("Tw", sl)])
                        else:
                            P.dve(lambda e, pt=pt, gh=gh, sl=sl, idx=idx: e.tensor_copy(out=Tw[sl][:, gh, idx, :], in_=pt[:]), r=[kk], w=[("Tw", sl)])
                    ecnt += 1
            for gh in range(2):
                g = 2 * pair + gh
                rows = slice(64 * gh, 64 * gh + 64)
                py = pY8[g % 2]
                ky = ("pY8", g % 2)
                for m in range(8):
                    for m_ in range(8):
                        P.pe(lambda e, py=py, m=m, m_=m_, gh=gh, g=g, sl=sl: e.matmul(out=py[:, m, :], lhsT=Tw[sl][:, gh, 7 + m - m_, :],
                                                                                     rhs=fv(u8o[:, g, :], m_, [[8, 32]]), start=(m_ == 0), stop=False),
                             r=[("Tw", sl), "u8o"], w=[ky])
                    fo = 8 * (m + 1) * 16
                    bo = 8 * (8 - m) * 16
                    P.pe(lambda e, py=py, m=m, rows=rows, fo=fo, pair=pair: e.matmul(out=py[:, m, :], lhsT=Rre_b[rows, 0, fo:fo + 128], rhs=HO[0][0][rows, pair, :], start=False, stop=False),
                         r=["Rre_b", ("HO", 0, 0)], w=[ky])
                    P.pe(lambda e, py=py, m=m, rows=rows, fo=fo, pair=pair: e.matmul(out=py[:, m, :], lhsT=Rim_b[rows, 0, fo:fo + 128], rhs=HO[0][1][rows, pair, :], start=False, stop=False),
                         r=["Rim_b", ("HO", 0, 1)], w=[ky])
                    P.pe(lambda e, py=py, m=m, rows=rows, bo=bo, pair=pair: e.matmul(out=py[:, m, :], lhsT=Rre_b[rows, 1, bo:bo + 128], rhs=HO[1][0][rows, pair, :], start=False, stop=False),
                         r=["Rre_b", ("HO", 1, 0)], w=[ky])
                    P.pe(lambda e, py=py, m=m, rows=rows, bo=bo, pair=pair: e.matmul(out=py[:, m, :], lhsT=Rim_b[rows, 1, bo:bo + 128], rhs=HO[1][1][rows, pair, :], start=False, stop=True),
                         r=["Rim_b", ("HO", 1, 1)], w=[ky])
                ye = Ye[g % 2]
                P.dve(lambda e, py=py, ye=ye: e.tensor_tensor(out=ye[:].rearrange("p (j m s) -> p j m s", m=8, s=8), in0=fv(py[:], 0, [[1, 32], [32, 8], [0, 8]]),
                                                              in1=fv(m8c_b[:], 0, [[0, 32], [0, 8], [1, 8]]), op=ALU.mult), r=[ky, "m8c_b"], w=["Ye"])
                for c4 in range(4):
                    P.pe(lambda e, ye=ye, g=g, c4=c4: e.matmul(out=pYT[:, c4 * 512:(c4 + 1) * 512], lhsT=selg_b[:, g % 8, :], rhs=ye[:, c4 * 512:(c4 + 1) * 512],
                                                              start=(g % 8 == 0), stop=(g % 8 == 7)), r=["Ye", "selg_b"], w=["pYT"])
            if pair % 4 == 3:
                tile_ = pair // 4
                for hf in range(2):
                    cs = slice(hf * 1024, (hf + 1) * 1024)
                    P.act(lambda e, cs=cs: e.activation(out=ysb[:], in_=pYT[:, cs], func=ACT.Copy), r=["pYT"], w=["ysb"])
                    P.dve(lambda e: e.tensor_tensor(out=yx2[:], in0=ysb[:], in1=ysb[:], op=ALU.mult), r=["ysb"], w=["yx2"])
                    P.dve(lambda e: e.tensor_scalar(out=yx2[:], in0=yx2[:], scalar1=0.044715, scalar2=1.0, op0=ALU.mult, op1=ALU.add), r=["yx2"], w=["yx2"])
                    P.dve(lambda e: e.tensor_tensor(out=yx2[:], in0=yx2[:], in1=ysb[:], op=ALU.mult), r=["yx2", "ysb"], w=["yx2"])
                    P.act(lambda e: e.activation(out=yx2[:], in_=yx2[:], func=ACT.Sigmoid, scale=1.5957691216057308), r=["yx2"], w=["yx2"])
                    P.dve(lambda e, tile_=tile_, cs=cs: e.tensor_tensor(out=gT[:, tile_, cs], in0=ysb[:], in1=yx2[:], op=ALU.mult), r=["ysb", "yx2"], w=["gT"])
        P.barrier()
    esU.close()
    esS.close()
    if "gT" in dbg_names:
        dump("gT", gT[:], [128, 4, NOWN], BF16)
    if STOP <= 4:
        esP.close()
        return finish(nc, P, ges, out, dbg_out)

    oT = qT
    NKC = NFULL // 128
    SCALE = 128.0 ** -0.5
    with ExitStack() as es:
        kT_all = SB(es, "kT_all", [128, 2, NFULL], BF16)
        V_aug = SB(es, "V_aug", [128, NKC, 2, 132], BF16)
        pTt = [SB(es, "pTt%d" % i, [128, 512], BF16) for i in range(3)]
        rden = SB(es, "rden", [128, 4], F32)
        on_b = SB(es, "on_b", [128, 4, 128], BF16)
        pS = [PS(es, "pS%d" % i, [128, 512], F32) for i in range(2)]
        pO = [PS(es, "pO%d" % i, [128, 512], F32) for i in range(4)]
        pOT = PS(es, "pOT", [128, 4, 128], BF16)
        P.dve(lambda e: e.memset(V_aug[:], 1.0), w=["V_aug"])
        for h2 in range(2):
            P.dma(lambda e, h2=h2: e.dma_start(out=kT_all[:, h2, :], in_=kT_d[h2]), key=("kT_all", h2), r=["kT_d"], w=["kT_all"])
            P.dma(lambda e, h2=h2: e.dma_start(out=V_aug[:, :, h2, 0:128], in_=v_d[:, h2 * 128:(h2 + 1) * 128].rearrange("(c p) d -> p c d", p=128)),
                  key=("V_aug", h2), r=["v_d", "V_aug"], w=["V_aug"])
        iters = [(h, qc, kc) for h in range(8) for qc in range(4) for kc in range(NKC)]

        def emit_S(i):
            h, qc, kc = iters[i]
            kvh = h // 4
            s2 = i % 2
            qs = slice(qc * 512, (qc + 1) * 512)
            P.pe(lambda e, s2=s2, kvh=kvh, kc=kc, h=h, qs=qs: e.matmul(out=pS[s2][:], lhsT=kT_all[:, kvh, kc * 128:(kc + 1) * 128], rhs=qT[:, h, qs], start=True, stop=True),
                 r=["kT_all", ("qTc", h, qc)], w=[("pS", s2)])

        emit_S(0)
        for i, (h, qc, kc) in enumerate(iters):
            kvh = h // 4
            s2, s3 = i % 2, i % 3
            qs = slice(qc * 512, (qc + 1) * 512)
            if i + 1 < len(iters):
                emit_S(i + 1)
            P.act(lambda e, s2=s2, s3=s3: e.activation(out=pTt[s3][:], in_=pS[s2][:], func=ACT.Exp, bias=negC[:], scale=SCALE),
                  r=[("pS", s2), "negC"], w=[("pTt", s3)])
            for qi in range(4):
                P.pe(lambda e, s3=s3, qi=qi, kc=kc, kvh=kvh: e.matmul(out=pO[qi][:, 0:129], lhsT=pTt[s3][:, qi * 128:(qi + 1) * 128], rhs=V_aug[:, kc, kvh, 0:129],
                                                                     start=(kc == 0), stop=(kc == NKC - 1)), r=[("pTt", s3), "V_aug"], w=[("pO", qi)])
            if kc == NKC - 1:
                for qi in range(4):
                    P.dve(lambda e, qi=qi: e.reciprocal(out=rden[:, qi:qi + 1], in_=pO[qi][:, 128:129]), r=[("pO", qi)], w=[("rden", qi)])
                    P.dve(lambda e, qi=qi: e.tensor_scalar(out=on_b[:, qi, :], in0=pO[qi][:, 0:128], scalar1=rden[:, qi:qi + 1], scalar2=None, op0=ALU.mult),
                          r=[("pO", qi), ("rden", qi)], w=[("on_b", qi)])
                    P.pe(lambda e, qi=qi: e.transpose(out=pOT[:, qi, :], in_=on_b[:, qi, :], identity=ident_b[:]), r=[("on_b", qi), "ident_b"], w=["pOT"])
                P.dve(lambda e, h=h, qs=qs: e.tensor_copy(out=oT[:, h, qs], in_=pOT[:].rearrange("p a b -> p (a b)")), r=["pOT"], w=[("qTc", h, qc)])
        P.barrier()
    if "oT" in dbg_names:
        dump("oT", oT[:], [128, 8, NOWN], BF16)
    if STOP <= 5:
        esP.close()
        return finish(nc, P, ges, out, dbg_out)

    esM = ExitStack()
    mT_all = SB(esM, "mT_all", [128, 8, NOWN], BF16)
    with ExitStack() as es:
        Wg = SB(es, "Wg", [128, 8, 2048], BF16)
        Wa = SB(es, "Wa", [128, 4, 1024], BF16)
        Wb = SB(es, "Wb", [128, 4, 1024], BF16)
        Wo = SB(es, "Wo", [128, 8, 1024], BF16)
        sg1 = SB(es, "sg1", [128, 512], F32)
        sg2 = SB(es, "sg2", [128, 512], F32)
        sgb = SB(es, "sgb", [128, 512], F32)
        mt1 = SB(es, "mt1", [128, 512], F32)
        mt2 = SB(es, "mt2", [128, 512], F32)
        pG1 = PS(es, "pG1", [128, 512], F32)
        pG2 = PS(es, "pG2", [128, 512], F32)
        pA_D = PS(es, "pA_D", [128, 512], F32)
        pB_D = PS(es, "pB_D", [128, 512], F32)
        pC_D = PS(es, "pC_D", [128, 512], F32)
        for c2 in range(2):
            P.dma(lambda e, c2=c2: e.dma_start(out=Wg[:, :, c2 * 1024:(c2 + 1) * 1024], in_=w_in[:, 2048 + c2 * 1024:2048 + (c2 + 1) * 1024].rearrange("(kc p) n -> p kc n", p=128)),
                  key=("Wg", c2), w=["Wg"], eng="pool")
        P.dma(lambda e: e.dma_start(out=Wa[:], in_=w_glu_a.rearrange("(kc p) n -> p kc n", p=128)), key="Wa", w=["Wa"], eng="pool")
        P.dma(lambda e: e.dma_start(out=Wb[:], in_=w_glu_b.rearrange("(kc p) n -> p kc n", p=128)), key="Wb", w=["Wb"], eng="pool")
        P.dma(lambda e: e.dma_start(out=Wo[:], in_=w_attn_o.rearrange("(kc p) n -> p kc n", p=128)), key="Wo", w=["Wo"], eng="pool")
        for st_ in range(4):
            ts = slice(st_ * 512, (st_ + 1) * 512)
            for dt_ in range(8):
                ds = slice(dt_ * 128, (dt_ + 1) * 128)
                ds2 = slice(1024 + dt_ * 128, 1024 + (dt_ + 1) * 128)
                for kc in range(8):
                    P.pe(lambda e, kc=kc, ds=ds, ts=ts: e.matmul(out=pG1[:], lhsT=Wg[:, kc, ds], rhs=hTo[:, kc, ts], start=(kc == 0), stop=(kc == 7)), r=["Wg", "hTo"], w=["pG1"])
                for kc in range(8):
                    P.pe(lambda e, kc=kc, ds2=ds2, ts=ts: e.matmul(out=pG2[:], lhsT=Wg[:, kc, ds2], rhs=hTo[:, kc, ts], start=(kc == 0), stop=(kc == 7)), r=["Wg", "hTo"], w=["pG2"])
                for c in range(4):
                    P.pe(lambda e, c=c, ds=ds, ts=ts: e.matmul(out=pA_D[:], lhsT=Wa[:, c, ds], rhs=gT[:, c, ts], start=(c == 0), stop=(c == 3)), r=["Wa", "gT"], w=["pA_D"])
                for c in range(4):
                    P.pe(lambda e, c=c, ds=ds, ts=ts: e.matmul(out=pB_D[:], lhsT=Wb[:, c, ds], rhs=gT[:, c, ts], start=(c == 0), stop=(c == 3)), r=["Wb", "gT"], w=["pB_D"])
                for hh in range(8):
                    P.pe(lambda e, hh=hh, ds=ds, ts=ts: e.matmul(out=pC_D[:], lhsT=Wo[:, hh, ds], rhs=oT[:, hh, ts], start=(hh == 0), stop=(hh == 7)), r=["Wo", "oT"], w=["pC_D"])
                P.act(lambda e: e.activation(out=sg1[:], in_=pG1[:], func=ACT.Sigmoid), r=["pG1"], w=["sg1"])
                P.act(lambda e: e.activation(out=sg2[:], in_=pG2[:], func=ACT.Sigmoid), r=["pG2"], w=["sg2"])
                P.act(lambda e: e.activation(out=sgb[:], in_=pB_D[:], func=ACT.Sigmoid), r=["pB_D"], w=["sgb"])
                P.dve(lambda e: e.tensor_tensor(out=mt1[:], in0=pA_D[:], in1=sgb[:], op=ALU.mult), r=["pA_D", "sgb"], w=["mt1"])
                P.dve(lambda e: e.tensor_tensor(out=mt1[:], in0=mt1[:], in1=sg1[:], op=ALU.mult), r=["mt1", "sg1"], w=["mt1"])
                P.dve(lambda e: e.tensor_tensor(out=mt2[:], in0=pC_D[:], in1=sg2[:], op=ALU.mult), r=["pC_D", "sg2"], w=["mt2"])
                P.dve(lambda e, dt_=dt_, ts=ts: e.tensor_tensor(out=mT_all[:, dt_, ts], in0=mt1[:], in1=mt2[:], op=ALU.add), r=["mt1", "mt2"], w=["mT_all"])
        P.barrier()
    esP.close()
    if "mT" in dbg_names:
        dump("mT", mT_all[:], [128, 8, NOWN], BF16)
    if STOP <= 6:
        esM.close()
        return finish(nc, P, ges, out, dbg_out)

    esE = ExitStack()
    h2T = SB(esE, "h2T", [128, 8, NOWN], BF16)
    combT = SB(esE, "combT", [32, NOWN], BF16)
    with ExitStack() as es:
        Wout = SB(es, "Wout", [128, 8, 1024], BF16)
        wrt_sb = SB(es, "wrt_sb", [128, 8, 36], F32)
        brt_bc = SB(es, "brt_bc", [128, 36], F32)
        g1_bc = SB(es, "g1_bc", [128, D], F32)
        l1g_bc = SB(es, "l1g_bc", [128, D], F32)
        l1b_bc = SB(es, "l1b_bc", [128, D], F32)
        sc2p_bc = SB(es, "sc2p_bc", [128, D], F32)
        sh2_bc = SB(es, "sh2_bc", [128, D], F32)
        xt_D = [SB(es, "xt_D%d" % i, [128, D], F32) for i in range(2)]
        zt_D = SB(es, "zt_D", [128, D], F32)
        zn_D = SB(es, "zn_D", [128, D], F32)
        x1_D = [SB(es, "x1_D%d" % i, [128, D], F32) for i in range(2)]
        h2_D = SB(es, "h2_D", [128, D], F32)
        h2b_D = SB(es, "h2b_D", [128, D], BF16)
        h2Tf = SB(es, "h2Tf", [128, 8, 128], F32)
        st_D = SB(es, "st_D", [128, 2, 6], F32)
        mv_D = SB(es, "mv_D", [128, 2], F32)
        rstd_D = SB(es, "rstd_D", [128, 1], F32)
        nb_D = SB(es, "nb_D", [128, 1], F32)
        st_D2 = SB(es, "st_D2", [128, 2, 6], F32)
        mv_D2 = SB(es, "mv_D2", [128, 2], F32)
        rstd_D2 = SB(es, "rstd_D2", [128, 1], F32)
        nb_D2 = SB(es, "nb_D2", [128, 1], F32)
        L_D = SB(es, "L_D", [128, 36], F32)
        rs = SB(es, "rs", [128, 16], F32)
        ohg = SB(es, "ohg", [128, 4], F32)
        gex = SB(es, "gex", [128, 4], F32)
        msk = SB(es, "msk", [128, 32], F32)
        ein = SB(es, "ein", [128, 8], F32)
        e2_ = SB(es, "e2_", [128, 8], F32)
        oh1 = SB(es, "oh1", [128, 8], F32)
        oh2 = SB(es, "oh2", [128, 8], F32)
        cg = SB(es, "cg", [128, 8], F32)
        comb = SB(es, "comb", [128, 32], F32)
        pMix = [PS(es, "pMix%d" % i, [128, 512], F32) for i in range(2)]
        pT_D = PS(es, "pT_D", [128, 8, 128], BF16)
        pTf = PS(es, "pTf", [128, 4, 128], F32)
        pR = PS(es, "pR", [128, 36], F32)
        pCT = PS(es, "pCT", [32, 128], F32)
        P.dma(lambda e: e.dma_start(out=Wout[:], in_=w_out.rearrange("(kc p) n -> p kc n", p=128)), key="Wout", w=["Wout"], eng="pool")
        P.dma(lambda e: e.dma_start(out=wrt_sb[:], in_=w_rt.rearrange("(kc p) n -> p kc n", p=128)), key="wrt_sb", w=["wrt_sb"])
        load_bc(brt_bc, b_rt, "brt_bc")
        load_bc(g1_bc, mod_d[0:1, 2048:3072], "g1_bc")
        load_bc(l1g_bc, ln1_g, "l1g_bc")
        load_bc(l1b_bc, ln1_b, "l1b_bc")
        load_bc(sc2p_bc, mod_d[0:1, 4096:5120], "sc2p_bc", True)
        load_bc(sh2_bc, mod_d[0:1, 3072:4096], "sh2_bc")
        sD0, sD1, sD2 = [None] * NTO, [None] * NTO, [None] * NTO
        for tt in range(NTO):
            s2 = tt % 2
            t0 = tt * 128
            tsl_ = slice(t0, t0 + 128)
            P.capture()
            P.dma(lambda e, s2=s2, t0=t0: e.dma_start(out=xt_D[s2][:], in_=xo[t0:t0 + 128, :]), key=("xt_D", s2), w=[("xt_D", s2)])
            for half in range(2):
                hs = slice(half * 512, (half + 1) * 512)
                for kc in range(8):
                    P.pe(lambda e, kc=kc, half=half, hs=hs, tsl_=tsl_: e.matmul(out=pMix[half][:], lhsT=mT_all[:, kc, tsl_], rhs=Wout[:, kc, hs], start=(kc == 0), stop=(kc == 7)),
                         r=["mT_all", "Wout"], w=[("pMix", half)])
                P.dve(lambda e, half=half, hs=hs: e.tensor_tensor(out=zt_D[:, hs], in0=pMix[half][:], in1=g1_bc[:, hs], op=ALU.mult), r=[("pMix", half), "g1_bc"], w=["zt_D"])
            P.dve(lambda e, s2=s2: e.scalar_tensor_tensor(out=zt_D[:], in0=xt_D[s2][:], scalar=ALPHA, in1=zt_D[:], op0=ALU.mult, op1=ALU.add), r=["zt_D", ("xt_D", s2)], w=["zt_D"])
            ln_tile(zt_D[:], "zt_D", st_D, mv_D, rstd_D, nb_D, zn_D[:], "zn_D", "D1")
            P.dve(lambda e: e.tensor_tensor(out=zn_D[:], in0=zn_D[:], in1=l1g_bc[:], op=ALU.mult), r=["zn_D", "l1g_bc"], w=["zn_D"])
            P.dve(lambda e, s2=s2: e.tensor_tensor(out=x1_D[s2][:], in0=zn_D[:], in1=l1b_bc[:], op=ALU.add), r=["zn_D", "l1b_bc"], w=[("x1_D", s2)])
            P.dma(lambda e, s2=s2, t0=t0: e.dma_start(out=x1_d[t0:t0 + 128, :], in_=x1_D[s2][:]), key=("x1d", s2), r=[("x1_D", s2)], w=["x1_d"])
            sD0[tt] = P.end_capture()
            P.capture()
            ln_tile(x1_D[s2][:], ("x1_D", s2), st_D2, mv_D2, rstd_D2, nb_D2, h2_D[:], "h2_D", "D2")
            P.dve(lambda e: e.tensor_tensor(out=h2_D[:], in0=h2_D[:], in1=sc2p_bc[:], op=ALU.mult), r=["h2_D", "sc2p_bc"], w=["h2_D"])
            P.dve(lambda e: e.tensor_tensor(out=h2_D[:], in0=h2_D[:], in1=sh2_bc[:], op=ALU.add), r=["h2_D", "sh2_bc"], w=["h2_D"])
            P.act(lambda e: e.activation(out=h2b_D[:], in_=h2_D[:], func=ACT.Copy), r=["h2_D"], w=["h2b_D"])
            for kc in range(8):
                P.pe(lambda e, kc=kc: e.transpose(out=pT_D[:, kc, :], in_=h2b_D[:, kc * 128:(kc + 1) * 128], identity=ident_b[:]), r=["h2b_D", "ident_b"], w=["pT_D"])
            P.act(lambda e, tsl_=tsl_: e.activation(out=h2T[:, :, tsl_], in_=pT_D[:], func=ACT.Copy), r=["pT_D"], w=[("h2T", tt)])
            for q4 in range(2):
                for j in range(4):
                    kc = q4 * 4 + j
                    P.pe(lambda e, kc=kc, j=j: e.transpose(out=pTf[:, j, :], in_=h2_D[:, kc * 128:(kc + 1) * 128], identity=ident_f[:]), r=["h2_D", "ident_f"], w=["pTf"])
                P.dve(lambda e, q4=q4: e.tensor_copy(out=h2Tf[:, q4 * 4:(q4 + 1) * 4, :], in_=pTf[:]), r=["pTf"], w=["h2Tf"])
            for kc in range(8):
                P.pe(lambda e, kc=kc: e.matmul(out=pR[:], lhsT=h2Tf[:, kc, :], rhs=wrt_sb[:, kc, :], start=(kc == 0), stop=(kc == 7)), r=["h2Tf", "wrt_sb"], w=["pR"])
            P.dve(lambda e: e.tensor_tensor(out=L_D[:], in0=pR[:], in1=brt_bc[:], op=ALU.add), r=["pR", "brt_bc"], w=["L_D"])
            sD1[tt] = P.end_capture()
            P.capture()
            R_ = lambda i: rs[:, i:i + 1]
            kR = lambda i: ("rs", i)
            P.dve(lambda e: e.tensor_reduce(out=R_(0), in_=L_D[:, 0:4], axis=AX.X, op=ALU.max), r=["L_D"], w=[kR(0)])
            P.dve(lambda e: e.tensor_scalar(out=ohg[:], in0=L_D[:, 0:4], scalar1=R_(0), scalar2=None, op0=ALU.is_equal), r=["L_D", kR(0)], w=["ohg"])
            P.dve(lambda e: e.tensor_scalar(out=R_(1), in0=R_(0), scalar1=-1.0, scalar2=None, op0=ALU.mult), r=[kR(0)], w=[kR(1)])
            P.act(lambda e: e.activation(out=gex[:], in_=L_D[:, 0:4], func=ACT.Exp, bias=R_(1), scale=1.0), r=["L_D", kR(1)], w=["gex"])
            P.dve(lambda e: e.tensor_reduce(out=R_(2), in_=gex[:], axis=AX.X, op=ALU.add), r=["gex"], w=[kR(2)])
            P.dve(lambda e: e.reciprocal(out=R_(2), in_=R_(2)), r=[kR(2)], w=[kR(2)])
            P.dve(lambda e: e.tensor_tensor(out=msk[:].rearrange("p (g x) -> p g x", x=8), in0=L_D[:, 4:36].rearrange("p (g x) -> p g x", x=8),
                                            in1=fv(ohg[:], 0, [[1, 4], [0, 8]]), op=ALU.mult), r=["L_D", "ohg"], w=["msk"])
            P.dve(lambda e: e.tensor_reduce(out=ein[:], in_=fv(msk[:], 0, [[1, 8], [8, 4]]), axis=AX.X, op=ALU.add), r=["msk"], w=["ein"])
            P.dve(lambda e: e.tensor_reduce(out=R_(3), in_=ein[:], axis=AX.X, op=ALU.max), r=["ein"], w=[kR(3)])
            P.dve(lambda e: e.tensor_scalar(out=oh1[:], in0=ein[:], scalar1=R_(3), scalar2=None, op0=ALU.is_equal), r=["ein", kR(3)], w=["oh1"])
            P.dve(lambda e: e.scalar_tensor_tensor(out=e2_[:], in0=oh1[:], scalar=-1e30, in1=ein[:], op0=ALU.mult, op1=ALU.add), r=["oh1", "ein"], w=["e2_"])
            P.dve(lambda e: e.tensor_reduce(out=R_(4), in_=e2_[:], axis=AX.X, op=ALU.max), r=["e2_"], w=[kR(4)])
            P.dve(lambda e: e.tensor_scalar(out=oh2[:], in0=e2_[:], scalar1=R_(4), scalar2=None, op0=ALU.is_equal), r=["e2_", kR(4)], w=["oh2"])
            P.dve(lambda e: e.tensor_tensor(out=R_(5), in0=R_(4), in1=R_(3), op=ALU.subtract), r=[kR(3), kR(4)], w=[kR(5)])
            P.act(lambda e: e.activation(out=R_(6), in_=R_(5), func=ACT.Exp), r=[kR(5)], w=[kR(6)])
            P.dve(lambda e: e.tensor_scalar(out=R_(7), in0=R_(6), scalar1=1.0, scalar2=None, op0=ALU.add), r=[kR(6)], w=[kR(7)])
            P.dve(lambda e: e.reciprocal(out=R_(7), in_=R_(7)), r=[kR(7)], w=[kR(7)])
            P.dve(lambda e: e.tensor_tensor(out=R_(8), in0=R_(6), in1=R_(7), op=ALU.mult), r=[kR(6), kR(7)], w=[kR(8)])
            P.dve(lambda e: e.tensor_tensor(out=R_(7), in0=R_(7), in1=R_(2), op=ALU.mult), r=[kR(7), kR(2)], w=[kR(7)])
            P.dve(lambda e: e.tensor_tensor(out=R_(8), in0=R_(8), in1=R_(2), op=ALU.mult), r=[kR(8), kR(2)], w=[kR(8)])
            P.dve(lambda e: e.tensor_scalar(out=cg[:], in0=oh1[:], scalar1=R_(7), scalar2=None, op0=ALU.mult), r=["oh1", kR(7)], w=["cg"])
            P.dve(lambda e: e.scalar_tensor_tensor(out=cg[:], in0=oh2[:], scalar=R_(8), in1=cg[:], op0=ALU.mult, op1=ALU.add), r=["oh2", kR(8), "cg"], w=["cg"])
            P.dve(lambda e: e.tensor_tensor(out=comb[:].rearrange("p (g x) -> p g x", x=8), in0=fv(cg[:], 0, [[0, 4], [1, 8]]), in1=fv(ohg[:], 0, [[1, 4], [0, 8]]), op=ALU.mult),
                  r=["cg", "ohg"], w=["comb"])
            P.pe(lambda e: e.transpose(out=pCT[:], in_=comb[:], identity=ident_f[:]), r=["comb", "ident_f"], w=["pCT"])
            P.dve(lambda e, tsl_=tsl_, tt=tt: e.tensor_copy(out=combT[:, tsl_], in_=pCT[:]), r=["pCT"], w=[("combT", tt)])
            sD2[tt] = P.end_capture()
        if int(os.environ.get('K_PIPED', '1')) == 0:
            for tt in range(NTO):
                for lst in (sD0, sD1, sD2):
                    P.ops.extend(lst[tt])
        else:
            for step in range(NTO + 2):
                for lst, off in ((sD0, 0), (sD2, 2), (sD1, 1)):
                    j = step - off
                    if 0 <= j < NTO:
                        P.ops.extend(lst[j])
        P.barrier()
    esM.close()
    if "x1" in dbg_names:
        o_ = nc.dram_tensor("dbg_x1", [NOWN, D], F32, kind="ExternalOutput").ap()
        P.dma(lambda e: e.dma_start(out=o_, in_=x1_d), key="dbg_x1", r=["x1_d"], w=["dbg_x1"])
    if "combT" in dbg_names:
        dump("combT", combT[:], [32, NOWN], BF16)
    if STOP <= 7:
        esE.close()
        return finish(nc, P, ges, out, dbg_out)

    yacc = SB(esE, "yacc", [128, NTO, D], F32)
    with ExitStack() as es:
        Weg = [SB(es, "Weg%d" % i, [128, 8, 512], BF16) for i in range(2)]
        Weu = [SB(es, "Weu%d" % i, [128, 8, 512], BF16) for i in range(2)]
        Wed = [SB(es, "Wed%d" % i, [128, 4, 1024], BF16) for i in range(2)]
        sele_b = SB(es, "sele_b", [32, 32, 128], BF16)
        bc_sb = SB(es, "bc_sb", [128, 512], F32)
        sa_E = [SB(es, "sa_E%d" % i, [128, 512], F32) for i in range(2)]
        actT = [SB(es, "actT%d" % i, [128, 4, 512], BF16) for i in range(2)]
        pBC = PS(es, "pBC", [128, 512], F32)
        pA_E = [PS(es, "pA_E%d" % i, [128, 512], F32) for i in range(2)]
        pB_E = [PS(es, "pB_E%d" % i, [128, 512], F32) for i in range(2)]
        pY_E = [PS(es, "pY_E%d" % i, [128, 512], F32) for i in range(2)]
        P.dma(lambda e: e.dma_start(out=sele_b[:], in_=cst_sele), key="sele_b", w=["sele_b"], eng="pool")
        NEXP = int(os.environ.get("K_NEXP", "32"))
        fci = 0
        yi = 0
        for ex in range(NEXP):
            se = ex % 2
            P.dma(lambda e, ex=ex, se=se: e.dma_start(out=Weg[se][:], in_=w_eg[ex].rearrange("(kc p) f -> p kc f", p=128)), key=("Weg", se), w=[("Weg", se)], eng="pool")
            P.dma(lambda e, ex=ex, se=se: e.dma_start(out=Weu[se][:], in_=w_eu[ex].rearrange("(kc p) f -> p kc f", p=128)), key=("Weu", se), w=[("Weu", se)], eng="pool")
            P.dma(lambda e, ex=ex, se=se: e.dma_start(out=Wed[se][:], in_=w_ed[ex].rearrange("(fc p) n -> p fc n", p=128)), key=("Wed", se), w=[("Wed", se)], eng="pool")
            for st_ in range(4):
                ts = slice(st_ * 512, (st_ + 1) * 512)
                sa_ = (ex * 4 + st_) % 2
                P.pe(lambda e, ex=ex, ts=ts: e.matmul(out=pBC[:], lhsT=sele_b[:, ex, :], rhs=combT[:, ts], start=True, stop=True), r=["sele_b", "combT"], w=["pBC"])
                P.act(lambda e: e.activation(out=bc_sb[:], in_=pBC[:], func=ACT.Copy), r=["pBC"], w=["bc_sb"])
                for fc in range(4):
                    fs = slice(fc * 128, (fc + 1) * 128)
                    sp_ = fci % 2
                    for kc in range(8):
                        P.pe(lambda e, kc=kc, fs=fs, ts=ts, se=se, sp_=sp_: e.matmul(out=pA_E[sp_][:], lhsT=Weg[se][:, kc, fs], rhs=h2T[:, kc, ts], start=(kc == 0), stop=(kc == 7)),
                             r=[("Weg", se), "h2T"], w=[("pA_E", sp_)])
                    for kc in range(8):
                        P.pe(lambda e, kc=kc, fs=fs, ts=ts, se=se, sp_=sp_: e.matmul(out=pB_E[sp_][:], lhsT=Weu[se][:, kc, fs], rhs=h2T[:, kc, ts], start=(kc == 0), stop=(kc == 7)),
                             r=[("Weu", se), "h2T"], w=[("pB_E", sp_)])
                    P.act(lambda e, sp_=sp_: e.activation(out=sa_E[sp_][:], in_=pA_E[sp_][:], func=ACT.Silu), r=[("pA_E", sp_)], w=[("sa_E", sp_)])
                    P.dve(lambda e, sp_=sp_: e.tensor_tensor(out=sa_E[sp_][:], in0=sa_E[sp_][:], in1=pB_E[sp_][:], op=ALU.mult), r=[("sa_E", sp_), ("pB_E", sp_)], w=[("sa_E", sp_)])
                    P.dve(lambda e, sp_=sp_, sa_=sa_, fc=fc: e.tensor_tensor(out=actT[sa_][:, fc, :], in0=sa_E[sp_][:], in1=bc_sb[:], op=ALU.mult),
                          r=[("sa_E", sp_), "bc_sb"], w=[("actT", sa_)])
                    fci += 1
                for j in range(4):
                    tile_ = st_ * 4 + j
                    js = slice(j * 128, (j + 1) * 128)
                    for half in range(2):
                        hs = slice(half * 512, (half + 1) * 512)
                        sy = yi % 2
                        for fc in range(4):
                            P.pe(lambda e, fc=fc, js=js, hs=hs, sa_=sa_, se=se, sy=sy: e.matmul(out=pY_E[sy][:], lhsT=actT[sa_][:, fc, js], rhs=Wed[se][:, fc, hs], start=(fc == 0), stop=(fc == 3)),
                                 r=[("actT", sa_), ("Wed", se)], w=[("pY_E", sy)])
                        if ex == 0:
                            P.dve(lambda e, tile_=tile_, hs=hs, sy=sy: e.tensor_copy(out=yacc[:, tile_, hs], in_=pY_E[sy][:]), r=[("pY_E", sy)], w=[("yacc", tile_)])
                        else:
                            P.dve(lambda e, tile_=tile_, hs=hs, sy=sy: e.tensor_tensor(out=yacc[:, tile_, hs], in0=yacc[:, tile_, hs], in1=pY_E[sy][:], op=ALU.add),
                                  r=[("pY_E", sy), ("yacc", tile_)], w=[("yacc", tile_)])
                        yi += 1
        P.barrier()

    with ExitStack() as es:
        g2_bc = SB(es, "g2_bc", [128, D], F32)
        l2g_bc = SB(es, "l2g_bc", [128, D], F32)
        l2b_bc = SB(es, "l2b_bc", [128, D], F32)
        x1_F = [SB(es, "x1_F%d" % i, [128, D], F32) for i in range(2)]
        z_Fs = [SB(es, "z_F%d" % i, [128, D], F32) for i in range(2)]
        zn_F = SB(es, "zn_F", [128, D], F32)
        o_F = [SB(es, "o_F%d" % i, [128, D], F32) for i in range(2)]
        st_F = SB(es, "st_F", [128, 2, 6], F32)
        mv_F = SB(es, "mv_F", [128, 2], F32)
        rstd_Fs = [SB(es, "rstd_F%d" % i, [128, 1], F32) for i in range(2)]
        nb_Fs = [SB(es, "nb_F%d" % i, [128, 1], F32) for i in range(2)]
        load_bc(g2_bc, mod_d[0:1, 5120:6144], "g2_bc")
        load_bc(l2g_bc, ln2_g, "l2g_bc")
        load_bc(l2b_bc, ln2_b, "l2b_bc")
        sF0, sF1 = [None] * NTO, [None] * NTO
        for tt in range(NTO):
            s2 = tt % 2
            t0 = tt * 128
            z_F, rstd_F, nb_F = z_Fs[s2], rstd_Fs[s2], nb_Fs[s2]
            kz, sfx = ("z_F", s2), ("F", s2)
            P.capture()
            P.dma(lambda e, s2=s2, t0=t0: e.dma_start(out=x1_F[s2][:], in_=x1_d[t0:t0 + 128, :]), key=("x1_F", s2), r=["x1_d"], w=[("x1_F", s2)])
            P.dve(lambda e, tt=tt, z_F=z_F: e.tensor_tensor(out=z_F[:], in0=yacc[:, tt, :], in1=g2_bc[:], op=ALU.mult), r=[("yacc", tt), "g2_bc"], w=[kz])
            P.dve(lambda e, s2=s2, z_F=z_F: e.scalar_tensor_tensor(out=z_F[:], in0=x1_F[s2][:], scalar=ALPHA, in1=z_F[:], op0=ALU.mult, op1=ALU.add), r=[kz, ("x1_F", s2)], w=[kz])
            for i in range(2):
                P.dve(lambda e, i=i, z_F=z_F: e.bn_stats(out=st_F[:, i, :], in_=z_F[:, i * 512:(i + 1) * 512]), r=[kz], w=[("st", "F", i)])
            P.dve(lambda e: e.bn_aggr(out=mv_F[:], in_=st_F[:].rearrange("p a b -> p (a b)")), r=[("st", "F", 0), ("st", "F", 1)], w=[("mv", "F")])
            P.act(lambda e, rstd_F=rstd_F: e.activation(out=rstd_F[:], in_=mv_F[:, 1:2], func=ACT.Sqrt, bias=eps_t[:], scale=1.0), r=[("mv", "F"), "eps_t"], w=[("rstd", sfx)])
            P.dve(lambda e, rstd_F=rstd_F: e.reciprocal(out=rstd_F[:], in_=rstd_F[:]), r=[("rstd", sfx)], w=[("rstd", sfx)])
            P.dve(lambda e, rstd_F=rstd_F, nb_F=nb_F: e.scalar_tensor_tensor(out=nb_F[:], in0=mv_F[:, 0:1], scalar=-1.0, in1=rstd_F[:], op0=ALU.mult, op1=ALU.mult),
                  r=[("mv", "F"), ("rstd", sfx)], w=[("nb", sfx)])
            sF0[tt] = P.end_capture()
            P.capture()
            P.act(lambda e, z_F=z_F, rstd_F=rstd_F, nb_F=nb_F: e.activation(out=zn_F[:], in_=z_F[:], func=ACT.Identity, bias=nb_F[:], scale=rstd_F[:]),
                  r=[kz, ("nb", sfx), ("rstd", sfx)], w=["zn_F"])
            P.dve(lambda e: e.tensor_tensor(out=zn_F[:], in0=zn_F[:], in1=l2g_bc[:], op=ALU.mult), r=["zn_F", "l2g_bc"], w=["zn_F"])
            P.dve(lambda e, s2=s2: e.tensor_tensor(out=o_F[s2][:], in0=zn_F[:], in1=l2b_bc[:], op=ALU.add), r=["zn_F", "l2b_bc"], w=[("o_F", s2)])
            P.dma(lambda e, s2=s2, t0=t0: e.dma_start(out=out[t0:t0 + 128, :], in_=o_F[s2][:]), key=("outd", s2), r=[("o_F", s2)], w=["out"])
            sF1[tt] = P.end_capture()
        for step in range(NTO + 1):
            for lst, off in ((sF0, 0), (sF1, 1)):
                j = step - off
                if 0 <= j < NTO:
                    P.ops.extend(lst[j])
        P.barrier()
    esE.close()
    return finish(nc, P, ges, out, dbg_out)


def finish(nc, P, ges, out, dbg_out):
    P.barrier()
    P.emit()
    ges.close()
    nc._dbg_out = dbg_out
    nc._stats = P.stats
    return nc


def rope_tables():
    rows = NLAT // 64
    row = np.repeat(np.arange(rows, dtype=np.float32), 64)
    col = np.tile(np.arange(64, dtype=np.float32), rows)
    inv = (np.float32(10000.0) ** (-np.arange(0, 64, 2, dtype=np.float32) / np.float32(64))).astype(np.float32)
    ang = np.stack([row[:, None] * inv, col[:, None] * inv], axis=1).astype(np.float32)
    tab = np.concatenate([np.cos(ang).reshape(NLAT, 64), np.sin(ang).reshape(NLAT, 64)], axis=1).astype(np.float32)
    return tab


def make_in_maps(inp):
    f32 = np.float32
    g = lambda k: np.asarray(inp[k], dtype=f32)
    x, c, ctx, c_ctx = g("x"), g("c"), g("ctx"), g("c_ctx")
    tab = rope_tables()
    tab_ctx = np.concatenate([np.ones((NCTX, 64), f32), np.zeros((NCTX, 64), f32)], axis=1)
    rope_full = np.concatenate([tab_ctx, tab], axis=0)
    tok = np.arange(128)
    mask8 = (tok[:, None] % 8 == np.arange(8)[None, :]).astype(f32)
    sel16 = (tok[:, None] // 8 == np.arange(16)[None, :]).astype(f32)
    mask8c = (tok[:, None] // 16 == np.arange(8)[None, :]).astype(f32)

    def pairlay(a):
        sh = a.shape
        a = a.reshape((2, 16, 2, 64) + sh[3:])
        perm = (2, 3, 0, 1) + tuple(range(4, a.ndim))
        a = a.transpose(perm)
        return np.ascontiguousarray(a.reshape((128, 2, 16) + sh[3:]))

    a_re, a_im = g("s5_a_re")[0], g("s5_a_im")[0]
    s5_a = np.stack([pairlay(a_re), pairlay(a_im)], axis=1)
    ldt = g("s5_log_dt")[0]
    s5_ldt = np.ascontiguousarray(np.broadcast_to(ldt.reshape(1, 2, 16, 2).transpose(0, 3, 1, 2), (64, 2, 2, 16)).transpose(1, 0, 2, 3).reshape(128, 2, 16))
    s5_b = np.stack([pairlay(g("s5_b_re")[0]), pairlay(g("s5_b_im")[0])], axis=1)
    cre = g("s5_c_re")[0].transpose(0, 1, 3, 2)
    cim = g("s5_c_im")[0].transpose(0, 1, 3, 2)
    s5_c = np.stack([pairlay(cre), pairlay(cim)], axis=1)
    dvec = g("s5_d")[0]
    s5_dcol = np.ascontiguousarray(np.broadcast_to(dvec.reshape(32, 16).T[None], (8, 16, 32)).reshape(128, 32))
    eZ = np.stack([63.0 - np.arange(64), np.arange(64)], axis=0).astype(f32)
    qs = np.arange(72)
    eRf = (qs - 7).astype(f32)
    eRb = (8 * (qs // 8 - 1) + 8 - (qs % 8)).astype(f32)
    eR = np.stack([eRf, eRb], axis=0)
    cst_eZ = np.ascontiguousarray(np.broadcast_to(eZ[None], (128, 2, 64)))
    cst_eR = np.ascontiguousarray(np.broadcast_to(eR[None], (128, 2, 72)))
    sidx = tok // 16
    cst_mf = (sidx[None, :] >= sidx[:, None]).astype(f32)
    cst_mb = (sidx[:, None] >= sidx[None, :]).astype(f32)
    selg = np.zeros((128, 8, 128), f32)
    for g8 in range(8):
        for co in range(16):
            selg[np.arange(8) * 16 + co, g8, g8 * 16 + co] = 1.0
    sele = np.zeros((32, 32, 128), f32)
    for e in range(32):
        sele[e, e, :] = 1.0
    w_rt = np.concatenate([g("w_router_group")[0], g("w_router_expert")[0]], axis=1)
    b_rt = np.concatenate([g("b_router_group")[0], g("b_router_expert")[0]], axis=0)[None]
    common = dict(
        w_mod=g("w_mod")[0], b_mod=g("b_mod"), w_in=g("w_in")[0], rope_f=rope_full,
        q_gain=g("q_gain"), k_gain=g("k_gain"), cst_mask8=mask8, cst_mask8c=mask8c, cst_sel16=sel16,
        s5_a=s5_a, s5_ldt=s5_ldt, s5_b=s5_b, s5_c=s5_c, s5_dcol=s5_dcol, cst_eZ=cst_eZ, cst_eR=cst_eR,
        cst_mf=cst_mf, cst_mb=cst_mb, cst_selg=selg,
        w_glu_a=g("w_glu_a")[0], w_glu_b=g("w_glu_b")[0], w_attn_o=g("w_attn_o")[0], w_out=g("w_out")[0],
        ln1_g=g("ln1_g"), ln1_b=g("ln1_b"), ln2_g=g("ln2_g"), ln2_b=g("ln2_b"),
        w_rt=w_rt, b_rt=b_rt, w_eg=g("w_exp_gate")[0], w_eu=g("w_exp_up")[0], w_ed=g("w_exp_down")[0],
        cst_sele=sele,
    )
    maps = []
    for core in range(8):
        b, r = core // 4, core % 4
        m = dict(common)
        m["xf"] = np.ascontiguousarray(np.concatenate([ctx[b], x[b]], axis=0))
        m["xo"] = np.ascontiguousarray(x[b, r * NOWN:(r + 1) * NOWN])
        cc = np.stack([c[b], c_ctx], axis=0)
        m["ccT"] = np.ascontiguousarray(cc.reshape(2, 8, 128).transpose(2, 1, 0))
        m["rope_o"] = np.ascontiguousarray(tab[r * NOWN:(r + 1) * NOWN])
        cm = np.zeros((128, 4), f32)
        cm[:, r] = 1.0
        m["cmask"] = cm
        maps.append(m)
    return maps


_NC_CACHE = {}


def kernel(**inputs):
    maps = make_in_maps(inputs)
    if "nc" not in _NC_CACHE:
        _NC_CACHE["nc"] = build()
    nc = _NC_CACHE["nc"]
    res = run_bass_kernel_spmd(nc, maps, core_ids=list(range(8)))
    outp = np.zeros((2, NLAT, D), np.float32)
    for core in range(8):
        b, r = core // 4, core % 4
        outp[b, r * NOWN:(r + 1) * NOWN] = res.results[core]["out"]
    return outp
```

```python
import os
import math
import numpy as np
from contextlib import ExitStack
import concourse.bass as bass
import concourse.mybir as mybir
from concourse.bass_utils import run_bass_kernel_spmd

F32 = mybir.dt.float32
BF16 = mybir.dt.bfloat16
ACT = mybir.ActivationFunctionType
ALU = mybir.AluOpType
AX = mybir.AxisListType

D = 1024
NLAT = 8192
NCTX = 256
NFULL = NLAT + NCTX
NOWN = 2048
NTF = NFULL // 128
NTO = NOWN // 128
NJ = NFULL // 64
NJF = NFULL // 8
EPS = 1e-6
ALPHA = 2.0 ** 0.25
STOP = int(os.environ.get("K_STOP", "99"))
LNM_A = os.environ.get("K_LNMA", "act")
LNM_D = os.environ.get("K_LNMD", "dve")
DEBUG = os.environ.get("K_DEBUG", "") != ""


class Prog:
    ENGS = ["pe", "act", "dve", "pool", "sp"]

    def __init__(self, nc):
        self.nc = nc
        self.ops = []

    def add(self, eng, fn, r=(), w=(), dma=None, ndma=1):
        self.ops.append(dict(eng=eng, fn=fn, r=tuple(r), w=tuple(w), dma=dma, ndma=ndma, barrier=False))

    def pe(self, fn, r=(), w=()):
        self.add("pe", fn, r, w)

    def act(self, fn, r=(), w=()):
        self.add("act", fn, r, w)

    def dve(self, fn, r=(), w=()):
        self.add("dve", fn, r, w)

    def pool(self, fn, r=(), w=()):
        self.add("pool", fn, r, w)

    def dma(self, fn, key, r=(), w=(), eng="sp", n=1):
        self.add(eng, fn, r, w, dma=key, ndma=n)

    def capture(self):
        self._saved = self.ops
        self.ops = []

    def end_capture(self):
        lst = self.ops
        self.ops = self._saved
        return lst

    def barrier(self):
        for e in self.ENGS:
            self.ops.append(dict(eng=e, fn=None, r=(), w=(), dma=None, ndma=0, barrier=True))

    def emit(self):
        nc = self.nc
        ops = self.ops
        n = len(ops)
        last_w, readers = {}, {}
        deps = [None] * n
        last_eng, last_dma = {}, {}
        for i, op in enumerate(ops):
            d = set()
            if op["barrier"]:
                for e, j in last_eng.items():
                    if e != op["eng"]:
                        d.add(j)
                for k, j in last_dma.items():
                    d.add(j)
            for b in op["r"]:
                if b in last_w:
                    d.add(last_w[b])
            for b in op["w"]:
                if b in last_w:
                    d.add(last_w[b])
                for j in readers.get(b, ()):
                    d.add(j)
            for b in op["r"]:
                readers.setdefault(b, []).append(i)
            for b in op["w"]:
                readers[b] = []
                last_w[b] = i
            d.discard(i)
            deps[i] = d
            if op["dma"] is not None:
                last_dma[op["dma"]] = i
            elif not op["barrier"]:
                last_eng[op["eng"]] = i
        signal = [False] * n
        for i, op in enumerate(ops):
            for j in deps[i]:
                pj = ops[j]
                if pj["dma"] is not None:
                    continue
                if pj["eng"] == "pe" and op["eng"] == "pe" and op["dma"] is None:
                    continue
                signal[j] = True
        tick = [0] * n
        cnt = {e: 0 for e in self.ENGS}
        dcnt = {}
        for i, op in enumerate(ops):
            if op["dma"] is not None:
                dcnt[op["dma"]] = dcnt.get(op["dma"], 0) + op["ndma"]
                tick[i] = dcnt[op["dma"]] * 16
            elif signal[i]:
                cnt[op["eng"]] += 1
                tick[i] = cnt[op["eng"]]
        es = ExitStack()
        esem = {e: es.enter_context(nc.semaphore("s_" + e)) for e in self.ENGS}
        dsem = {}
        for k in dcnt:
            dsem[k] = es.enter_context(nc.semaphore("d_%d" % len(dsem)))
        waits = [None] * n
        seen = {e: {} for e in self.ENGS}
        for i, op in enumerate(ops):
            wl = {}
            for j in deps[i]:
                pj = ops[j]
                if pj["dma"] is not None:
                    key = ("d", pj["dma"])
                    sem = dsem[pj["dma"]]
                else:
                    if pj["eng"] == "pe" and op["eng"] == "pe" and op["dma"] is None:
                        continue
                    key = ("e", pj["eng"])
                    sem = esem[pj["eng"]]
                v = tick[j]
                if seen[op["eng"]].get(key, 0) >= v:
                    continue
                if key not in wl or wl[key][1] < v:
                    wl[key] = (sem, v)
            for key, (sem, v) in wl.items():
                seen[op["eng"]][key] = v
            waits[i] = list(wl.values())
        self.stats = dict(n=n, sig=dict(cnt), dkeys=len(dcnt), nwaits=sum(len(w) for w in waits))
        block = es.enter_context(nc.Block())

        def run(engname, eng):
            for i, op in enumerate(ops):
                if op["eng"] != engname:
                    continue
                for sem, v in waits[i]:
                    eng.wait_ge(sem, v)
                if op["fn"] is None:
                    continue
                res = op["fn"](eng)
                if op["dma"] is not None:
                    if not isinstance(res, (list, tuple)):
                        res = [res]
                    assert len(res) == op["ndma"], (len(res), op["ndma"])
                    for ins in res:
                        ins.then_inc(dsem[op["dma"]], 16)
                elif signal[i]:
                    if isinstance(res, (list, tuple)):
                        res = res[-1]
                    res.then_inc(esem[engname], 1)

        @block.tensor
        def _(e):
            run("pe", e)

        @block.scalar
        def _(e):
            run("act", e)

        @block.vector
        def _(e):
            run("dve", e)

        @block.gpsimd
        def _(e):
            run("pool", e)

        @block.sync
        def _(e):
            run("sp", e)

        es.close()


def AP_(t, offset, dims):
    return bass.AP(t, offset, [list(d) for d in dims])


def pstride(t):
    return t[:].ap[0][0]


def build(dbg_names=()):
    nc = bass.Bass("TRN2", target_bir_lowering=False)
    dram_in = lambda name, shape, dt=F32: nc.dram_tensor(name, list(shape), dt, kind="ExternalInput").ap()
    xf = dram_in("xf", [NFULL, D])
    xo = dram_in("xo", [NOWN, D])
    ccT = dram_in("ccT", [128, 8, 2])
    w_mod = dram_in("w_mod", [D, 6 * D])
    b_mod = dram_in("b_mod", [1, 6 * D])
    w_in = dram_in("w_in", [D, 4096])
    rope_f = dram_in("rope_f", [NFULL, 128])
    rope_o = dram_in("rope_o", [NOWN, 128])
    q_gain = dram_in("q_gain", [1, 128])
    k_gain = dram_in("k_gain", [1, 128])
    cst_mask8 = dram_in("cst_mask8", [128, 8])
    cst_mask8c = dram_in("cst_mask8c", [128, 8])
    cst_sel16 = dram_in("cst_sel16", [128, 16])
    s5_a = dram_in("s5_a", [128, 2, 2, 16])
    s5_ldt = dram_in("s5_ldt", [128, 2, 16])
    s5_b = dram_in("s5_b", [128, 2, 2, 16, 16])
    s5_c = dram_in("s5_c", [128, 2, 2, 16, 16])
    s5_dcol = dram_in("s5_dcol", [128, 32])
    cst_eZ = dram_in("cst_eZ", [128, 2, 64])
    cst_eR = dram_in("cst_eR", [128, 2, 72])
    cst_mf = dram_in("cst_mf", [128, 128])
    cst_mb = dram_in("cst_mb", [128, 128])
    cst_selg = dram_in("cst_selg", [128, 8, 128])
    cmask = dram_in("cmask", [128, 4])
    w_glu_a = dram_in("w_glu_a", [512, D])
    w_glu_b = dram_in("w_glu_b", [512, D])
    w_attn_o = dram_in("w_attn_o", [D, D])
    w_out = dram_in("w_out", [D, D])
    ln1_g = dram_in("ln1_g", [1, D])
    ln1_b = dram_in("ln1_b", [1, D])
    ln2_g = dram_in("ln2_g", [1, D])
    ln2_b = dram_in("ln2_b", [1, D])
    w_rt = dram_in("w_rt", [D, 36])
    b_rt = dram_in("b_rt", [1, 36])
    w_eg = dram_in("w_eg", [32, D, 512])
    w_eu = dram_in("w_eu", [32, D, 512])
    w_ed = dram_in("w_ed", [32, 512, D])
    cst_sele = dram_in("cst_sele", [32, 32, 128])
    out = nc.dram_tensor("out", [NOWN, D], F32, kind="ExternalOutput").ap()
    scr = lambda name, shape, dt: nc.dram_tensor(name, list(shape), dt, kind="Internal").ap()
    mod_d = scr("mod_d", [2, 6 * D], F32)
    kT_d = scr("kT_d", [2, 128, NFULL], BF16)
    v_d = scr("v_d", [NFULL, 256], BF16)
    x1_d = scr("x1_d", [NOWN, D], F32)
    dbg_out = {}

    P = Prog(nc)
    ges = ExitStack()

    free_list = [[16640, 229376]]
    peak = [0]

    def _alloc(nbytes):
        nbytes = (nbytes + 63) // 64 * 64
        for iv in free_list:
            if iv[1] - iv[0] >= nbytes:
                off = iv[0]
                iv[0] += nbytes
                peak[0] = max(peak[0], off + nbytes)
                return off, nbytes
        raise RuntimeError("SBUF manual allocator out of space for %d bytes; free=%s" % (nbytes, free_list))

    def _free(off, nbytes):
        free_list.append([off, off + nbytes])
        free_list.sort()
        merged = []
        for iv in free_list:
            if iv[1] == iv[0]:
                continue
            if merged and merged[-1][1] == iv[0]:
                merged[-1][1] = iv[1]
            else:
                merged.append(iv)
        free_list[:] = merged

    def SB(es, name, shape, dt):
        esz = 4 if dt == F32 else 2
        nb = esz
        for s_ in shape[1:]:
            nb *= s_
        off, nbytes = _alloc(nb)
        t = nc.alloc_sbuf_tensor_at(name, list(shape), dt, offset=off)
        es.callback(_free, off, nbytes)
        return t

    def PS(es, name, shape, dt):
        return es.enter_context(nc.psum_tensor(name, list(shape), dt))

    def dump(name, t_ap, shape, dt=F32, r=()):
        if name not in dbg_names:
            return
        o = nc.dram_tensor("dbg_" + name, list(shape), dt, kind="ExternalOutput").ap()
        dbg_out[name] = o
        P.dma(lambda e: e.dma_start(out=o, in_=t_ap), key="dbg_" + name, r=r, w=["dbg_" + name])

    ident_f = SB(ges, "ident_f", [128, 128], F32)
    ident_b = SB(ges, "ident_b", [128, 128], BF16)
    ones_b = SB(ges, "ones_b", [1, 128], BF16)
    eps_t = SB(ges, "eps_t", [128, 1], F32)
    modT = SB(ges, "modT", [128, 48, 2], F32)
    op1p = SB(ges, "op1p", [128, 8, 2], F32)
    sh1T = SB(ges, "sh1T", [128, 8, 2], F32)
    gk_bc = SB(ges, "gk_bc", [128, 128], F32)
    gq_bc = SB(ges, "gq_bc", [128, 128], F32)
    negC = SB(ges, "negC", [128, 1], F32)
    mask8 = SB(ges, "mask8", [128, 8], BF16)
    mask8f = SB(ges, "mask8f", [128, 8], F32)
    sel16 = SB(ges, "sel16", [128, 16], BF16)

    P.pool(lambda e: e.memset(ident_f[:], 1.0), w=["ident_f"])
    P.pool(lambda e: e.affine_select(out=ident_f[:], in_=ident_f[:], pattern=[[-1, 128]], compare_op=ALU.is_equal,
                                     fill=0.0, base=0, channel_multiplier=1), r=["ident_f"], w=["ident_f"])
    P.dve(lambda e: e.tensor_copy(out=ident_b[:], in_=ident_f[:]), r=["ident_f"], w=["ident_b"])
    P.dve(lambda e: e.memset(ones_b[:], 1.0), w=["ones_b"])
    P.dve(lambda e: e.memset(eps_t[:], EPS), w=["eps_t"])
    P.dma(lambda e: e.dma_start(out=gk_bc[:], in_=k_gain.partition_broadcast(128)), key="gk_bc", w=["gk_bc"])
    P.dma(lambda e: e.dma_start(out=gq_bc[:], in_=q_gain.partition_broadcast(128)), key="gq_bc", w=["gq_bc"])

    with ExitStack() as es:
        scT = SB(es, "scT", [128, 8, 2], F32)
        ccs = SB(es, "ccs", [128, 8, 2], F32)
        wm = [SB(es, "wm%d" % i, [128, 8, 512], F32) for i in range(2)]
        bm = SB(es, "bm", [2, 6 * D], F32)
        mod_sb = SB(es, "mod_sb", [2, 6 * D], F32)
        m8f = SB(es, "m8f", [128, 8], F32)
        s16f = SB(es, "s16f", [128, 16], F32)
        tmpg = SB(es, "tmpg", [128, 2], F32)
        pM = [PS(es, "pM%d" % i, [2, 512], F32) for i in range(2)]
        pMT = PS(es, "pMT", [128, 48, 2], F32)
        P.dma(lambda e: e.dma_start(out=ccs[:], in_=ccT), key="ccs", w=["ccs"])
        P.dma(lambda e: e.dma_start(out=bm[:], in_=b_mod.partition_broadcast(2)), key="bm", w=["bm"])
        P.dma(lambda e: e.dma_start(out=m8f[:], in_=cst_mask8), key="m8f", w=["m8f"])
        P.dma(lambda e: e.dma_start(out=s16f[:], in_=cst_sel16), key="s16f", w=["s16f"])
        P.dve(lambda e: e.tensor_copy(out=mask8[:], in_=m8f[:]), r=["m8f"], w=["mask8"])
        P.dve(lambda e: e.tensor_copy(out=mask8f[:], in_=m8f[:]), r=["m8f"], w=["mask8f"])
        P.dve(lambda e: e.tensor_copy(out=sel16[:], in_=s16f[:]), r=["s16f"], w=["sel16"])
        P.act(lambda e: e.activation(out=scT[:], in_=ccs[:], func=ACT.Silu), r=["ccs"], w=["scT"])
        P.dve(lambda e: e.tensor_reduce(out=tmpg[:, 0:1], in_=gq_bc[:], axis=AX.X, op=ALU.max, apply_absolute_value=True),
              r=["gq_bc"], w=["tmpg0"])
        P.dve(lambda e: e.tensor_reduce(out=tmpg[:, 1:2], in_=gk_bc[:], axis=AX.X, op=ALU.max, apply_absolute_value=True),
              r=["gk_bc"], w=["tmpg1"])
        P.dve(lambda e: e.scalar_tensor_tensor(out=negC[:], in0=tmpg[:, 0:1], scalar=-math.sqrt(128.0), in1=tmpg[:, 1:2],
                                               op0=ALU.mult, op1=ALU.mult), r=["tmpg0", "tmpg1"], w=["negC"])
        for nb in range(12):
            s = nb % 2
            P.dma(lambda e, nb=nb, s=s: e.dma_start(out=wm[s][:], in_=w_mod[:, nb * 512:(nb + 1) * 512].rearrange("(kc p) n -> p kc n", p=128)),
                  key=("wm", s), w=[("wm", s)])
            for kc in range(8):
                P.pe(lambda e, kc=kc, s=s: e.matmul(out=pM[s][:], lhsT=scT[:, kc, :], rhs=wm[s][:, kc, :], start=(kc == 0), stop=(kc == 7)),
                     r=["scT", ("wm", s)], w=[("pM", s)])
            P.dve(lambda e, nb=nb, s=s: e.tensor_tensor(out=mod_sb[:, nb * 512:(nb + 1) * 512], in0=pM[s][:], in1=bm[:, nb * 512:(nb + 1) * 512], op=ALU.add),
                  r=[("pM", s), "bm"], w=["mod_sb"])
        P.dma(lambda e: e.dma_start(out=mod_d, in_=mod_sb[:]), key="mod_d", r=["mod_sb"], w=["mod_d"])
        for j in range(48):
            P.pe(lambda e, j=j: e.transpose(out=pMT[:, j, :], in_=mod_sb[:, j * 128:(j + 1) * 128], identity=ident_f[0:2, 0:2]),
                 r=["mod_sb", "ident_f"], w=["pMT"])
        P.dve(lambda e: e.tensor_copy(out=modT[:], in_=pMT[:]), r=["pMT"], w=["modT"])
        P.dve(lambda e: e.tensor_copy(out=sh1T[:], in_=modT[:, 0:8, :]), r=["modT"], w=["sh1T"])
        P.dve(lambda e: e.tensor_scalar(out=op1p[:], in0=modT[:, 8:16, :], scalar1=1.0, scalar2=None, op0=ALU.add), r=["modT"], w=["op1p"])
        dump("mod", mod_sb[:], [2, 6 * D], r=["mod_sb"])
        P.barrier()
    if STOP <= 0:
        return finish(nc, P, ges, out, dbg_out)

    def fv(ap, off, dims):
        return bass.AP(ap.tensor, ap.offset + off, [list(ap.ap[0])] + [list(d) for d in dims])

    bias_sb = SB(ges, "bias_sb", [1, 5120], BF16)

    def prep_wblock(stage, pB, s, dst, col0, r, bcol, tag):
        P.dma(lambda e: e.dma_start(out=stage[s][:], in_=w_in[:, col0:col0 + 512].rearrange("(kc p) n -> p kc n", p=128)),
              key=("stg", s), w=[("stg", s)])
        for kc in range(8):
            P.pool(lambda e, kc=kc: e.tensor_scalar(out=dst[:, kc, :], in0=stage[s][:, kc, :], scalar1=op1p[:, kc, r:r + 1], scalar2=None, op0=ALU.mult),
                   r=[("stg", s), "op1p"], w=[tag])
        for kc in range(8):
            P.pe(lambda e, kc=kc: e.matmul(out=pB[:], lhsT=sh1T[:, kc, r:r + 1], rhs=stage[s][:, kc, :], start=(kc == 0), stop=(kc == 7)),
                 r=[("stg", s), "sh1T"], w=["pB"])
        P.act(lambda e: e.activation(out=bias_sb[:, bcol:bcol + 512], in_=pB[:], func=ACT.Copy), r=["pB"], w=["bias_sb"])

    def ln_tile(xt_ap, xkey, st, mv, rstd, nb, hb_ap, hkey, sfx, mode="act"):
        for i in range(2):
            P.dve(lambda e, i=i: e.bn_stats(out=st[:, i, :], in_=xt_ap[:, i * 512:(i + 1) * 512]), r=[xkey], w=[("st", sfx, i)])
        P.dve(lambda e: e.bn_aggr(out=mv[:], in_=st[:].rearrange("p a b -> p (a b)")), r=[("st", sfx, 0), ("st", sfx, 1)], w=[("mv", sfx)])
        P.act(lambda e: e.activation(out=rstd[:], in_=mv[:, 1:2], func=ACT.Sqrt, bias=eps_t[:], scale=1.0), r=[("mv", sfx), "eps_t"], w=[("rstd", sfx)])
        P.dve(lambda e: e.reciprocal(out=rstd[:], in_=rstd[:]), r=[("rstd", sfx)], w=[("rstd", sfx)])
        P.dve(lambda e: e.scalar_tensor_tensor(out=nb[:], in0=mv[:, 0:1], scalar=-1.0, in1=rstd[:], op0=ALU.mult, op1=ALU.mult),
              r=[("mv", sfx), ("rstd", sfx)], w=[("nb", sfx)])
        if mode == "dve":
            P.dve(lambda e: e.tensor_scalar(out=hb_ap, in0=xt_ap, scalar1=rstd[:], scalar2=nb[:], op0=ALU.mult, op1=ALU.add),
                  r=[xkey, ("nb", sfx), ("rstd", sfx)], w=[hkey])
            return
        P.act(lambda e: e.activation(out=hb_ap, in_=xt_ap, func=ACT.Identity, bias=nb[:], scale=rstd[:]), r=[xkey, ("nb", sfx), ("rstd", sfx)], w=[hkey])

    def rms_rope(eng_add, src, nh, gain_bc, rt, dst, tmp, keys_r, key_w, sfx, eng2=None, tmp2=None):
        sq, ss, rk, ta, tb = tmp
        eng2 = eng2 or eng_add
        tc, td = tmp2 if tmp2 is not None else (ta, tb)
        kc_, kd_ = (("tc", sfx), ("td", sfx)) if tmp2 is not None else (("ta", sfx), ("tb", sfx))
        n = nh * 128
        P.dve(lambda e: e.tensor_tensor(out=sq[:, :n], in0=src[:, :n], in1=src[:, :n], op=ALU.mult), r=keys_r, w=[("sq", sfx)])
        P.dve(lambda e: e.tensor_reduce(out=ss[:, :nh], in_=sq[:, :n].rearrange("p (h d) -> p h d", d=128), axis=AX.X, op=ALU.add), r=[("sq", sfx)], w=[("ss", sfx)])
        P.act(lambda e: e.activation(out=rk[:, :nh], in_=ss[:, :nh], func=ACT.Sqrt, bias=eps_t[:], scale=1.0 / 128.0), r=[("ss", sfx), "eps_t"], w=[("rk", sfx)])
        P.dve(lambda e: e.reciprocal(out=rk[:, :nh], in_=rk[:, :nh]), r=[("rk", sfx)], w=[("rk", sfx)])
        P.dve(lambda e: e.tensor_tensor(out=src[:, :n].rearrange("p (h d) -> p h d", d=128), in0=src[:, :n].rearrange("p (h d) -> p h d", d=128),
                                        in1=fv(rk[:], 0, [[1, nh], [0, 128]]), op=ALU.mult), r=keys_r + [("rk", sfx)], w=keys_r)
        eng_add(lambda e: e.tensor_tensor(out=src[:, :n].rearrange("p (h d) -> p h d", d=128), in0=src[:, :n].rearrange("p (h d) -> p h d", d=128),
                                          in1=fv(gain_bc[:], 0, [[0, nh], [1, 128]]), op=ALU.mult), r=keys_r, w=keys_r)
        x1 = fv(src[:], 0, [[128, nh], [64, 2], [1, 32]])
        x2 = fv(src[:], 32, [[128, nh], [64, 2], [1, 32]])
        cs = fv(rt[:], 0, [[0, nh], [32, 2], [1, 32]])
        sn = fv(rt[:], 64, [[0, nh], [32, 2], [1, 32]])
        o1 = fv(dst[:], 0, [[128, nh], [64, 2], [1, 32]])
        o2 = fv(dst[:], 32, [[128, nh], [64, 2], [1, 32]])
        tav = fv(ta[:], 0, [[64, nh], [32, 2], [1, 32]])
        tbv = fv(tb[:], 0, [[64, nh], [32, 2], [1, 32]])
        tcv = fv(tc[:], 0, [[64, nh], [32, 2], [1, 32]])
        tdv = fv(td[:], 0, [[64, nh], [32, 2], [1, 32]])
        eng_add(lambda e: e.tensor_tensor(out=tav, in0=x1, in1=cs, op=ALU.mult), r=keys_r + [("rt", sfx)], w=[("ta", sfx)])
        eng2(lambda e: e.tensor_tensor(out=tbv, in0=x2, in1=sn, op=ALU.mult), r=keys_r + [("rt", sfx)], w=[("tb", sfx)])
        eng2(lambda e: e.tensor_tensor(out=o1, in0=tav, in1=tbv, op=ALU.subtract), r=[("ta", sfx), ("tb", sfx)], w=[key_w])
        eng_add(lambda e: e.tensor_tensor(out=tcv, in0=x1, in1=sn, op=ALU.mult), r=keys_r + [("rt", sfx)], w=[kc_])
        eng2(lambda e: e.tensor_tensor(out=tdv, in0=x2, in1=cs, op=ALU.mult), r=keys_r + [("rt", sfx)], w=[kd_])
        eng2(lambda e: e.tensor_tensor(out=o2, in0=tcv, in1=tdv, op=ALU.add), r=[kc_, kd_], w=[key_w])

    esA = ExitStack()
    u8 = SB(esA, "u8", [128, 32, NJF], BF16)
    with ExitStack() as es:
        stage = [SB(es, "stage%d" % i, [128, 8, 512], F32) for i in range(2)]
        Wkv = [SB(es, "Wkv%d" % r, [128, 8, 512], BF16) for r in range(2)]
        Wu = [SB(es, "Wu%d" % r, [128, 8, 512], BF16) for r in range(2)]
        xt = [SB(es, "xt%d" % i, [128, D], F32) for i in range(3)]
        rt = [SB(es, "rt%d" % i, [128, 128], F32) for i in range(2)]
        hb = [SB(es, "hb%d" % i, [128, D], BF16) for i in range(2)]
        hT = [SB(es, "hT%d" % i, [128, 8, 128], BF16) for i in range(2)]
        st = SB(es, "st", [128, 2, 6], F32)
        mv = SB(es, "mv", [128, 2], F32)
        rstd = SB(es, "rstd", [128, 1], F32)
        nbt = SB(es, "nbt", [128, 1], F32)
        k_sb = SB(es, "k_sb", [128, 256], F32)
        v_bf = [SB(es, "v_bf%d" % i, [128, 256], BF16) for i in range(2)]
        kr = SB(es, "kr", [128, 256], BF16)
        kT_sb = [SB(es, "kT_sb%d" % i, [128, 2, 128], BF16) for i in range(2)]
        tmp = (SB(es, "sq", [128, 256], F32), SB(es, "ss", [128, 2], F32), SB(es, "rk", [128, 2], F32),
               SB(es, "ta", [128, 128], F32), SB(es, "tb", [128, 128], F32))
        tmp2_A = (SB(es, "tcA", [128, 128], F32), SB(es, "tdA", [128, 128], F32))
        u_bf = SB(es, "u_bf", [128, 512], BF16)
        Um = [SB(es, "Um%d" % i, [128, 32, 128], BF16) for i in range(2)]
        pB = PS(es, "pB", [1, 512], F32)
        pT = [PS(es, "pT%d" % i, [128, 8, 128], BF16) for i in range(2)]
        pKV = PS(es, "pKV", [128, 512], F32)
        pU = PS(es, "pU", [128, 512], F32)
        pKT = PS(es, "pKT", [128, 2, 128], BF16)
        pU8 = PS(es, "pU8", [128, 32, 16], F32)
        prep_wblock(stage, pB, 0, Wkv[1], 1536, 1, 4096, ("Wkv", 1))
        prep_wblock(stage, pB, 1, Wu[1], 0, 1, 4608, ("Wu", 1))
        prep_wblock(stage, pB, 0, Wkv[0], 1536, 0, 1536, ("Wkv", 0))
        prep_wblock(stage, pB, 1, Wu[0], 0, 0, 0, ("Wu", 0))
        krA = [kr, SB(es, "kr2", [128, 256], BF16)]
        NTA = int(os.environ.get('K_NT', NTF))
        PIPE = int(os.environ.get('K_PIPEA', '3'))
        NSA = int(os.environ.get('K_NSA', '4'))
        stg = [[None] * NTA for _ in range(3)]
        stg1a = [None] * NTA
        for tt in range(NTA):
            r = 1 if tt < 2 else 0
            s3, s2 = tt % 3, tt % 2
            t0 = tt * 128
            bkv, bu = (4096, 4608) if r == 1 else (1536, 0)
            P.capture()
            P.dma(lambda e, s3=s3, t0=t0: e.dma_start(out=xt[s3][:], in_=xf[t0:t0 + 128, :]), key=("xt", s3), w=[("xt", s3)])
            P.dma(lambda e, s2=s2, t0=t0: e.dma_start(out=rt[s2][:], in_=rope_f[t0:t0 + 128, :]), key=("rtd", s2), w=[("rtA", s2)])
            ln_tile(xt[s3][:], ("xt", s3), st, mv, rstd, nbt, hb[s2][:], ("hb", s2), "A", mode=LNM_A)
            stg[0][tt] = P.end_capture()
            P.capture()
            for kc in range(8):
                P.pe(lambda e, kc=kc, s2=s2: e.transpose(out=pT[s2][:, kc, :], in_=hb[s2][:, kc * 128:(kc + 1) * 128], identity=ident_b[:]),
                     r=[("hb", s2), "ident_b"], w=[("pT", s2)])
            P.dve(lambda e, s2=s2: e.tensor_copy(out=hT[s2][:], in_=pT[s2][:]), r=[("pT", s2)], w=[("hT", s2)])
            stg1a[tt] = P.end_capture()
            P.capture()
            for kc in range(8):
                P.pe(lambda e, kc=kc, s2=s2, r=r: e.matmul(out=pKV[:], lhsT=hT[s2][:, kc, :], rhs=Wkv[r][:, kc, :], start=(kc == 0), stop=False),
                     r=[("hT", s2), ("Wkv", r)], w=["pKV"])
            P.pe(lambda e, bkv=bkv: e.matmul(out=pKV[:], lhsT=ones_b[0:1, :], rhs=bias_sb[0:1, bkv:bkv + 512], start=False, stop=True),
                 r=["ones_b", "bias_sb"], w=["pKV"])
            for kc in range(8):
                P.pe(lambda e, kc=kc, s2=s2, r=r: e.matmul(out=pU[:], lhsT=hT[s2][:, kc, :], rhs=Wu[r][:, kc, :], start=(kc == 0), stop=False),
                     r=[("hT", s2), ("Wu", r)], w=["pU"])
            P.pe(lambda e, bu=bu: e.matmul(out=pU[:], lhsT=ones_b[0:1, :], rhs=bias_sb[0:1, bu:bu + 512], start=False, stop=True),
                 r=["ones_b", "bias_sb"], w=["pU"])
            P.act(lambda e: e.activation(out=k_sb[:], in_=pKV[:, 0:256], func=ACT.Copy), r=["pKV"], w=["k_sb"])
            P.act(lambda e, s2=s2: e.activation(out=v_bf[s2][:], in_=pKV[:, 256:512], func=ACT.Copy), r=["pKV"], w=[("v_bf", s2)])
            P.dma(lambda e, s2=s2, t0=t0: e.dma_start(out=v_d[t0:t0 + 128, :], in_=v_bf[s2][:]), key=("vd", s2), r=[("v_bf", s2)], w=["v_d"])
            P.act(lambda e: e.activation(out=u_bf[:], in_=pU[:], func=ACT.Copy), r=["pU"], w=["u_bf"])
            for s_ in range(NSA):
                P.act(lambda e, s2=s2, s_=s_: e.activation(out=Um[s2][:, :, s_ * 16:(s_ + 1) * 16], in_=u_bf[:].rearrange("p (g c) -> p g c", c=16),
                                                         func=ACT.Copy, scale=mask8f[:, s_:s_ + 1]), r=["u_bf", "mask8f"], w=[("Um", s2, s_)])
            P.dve(lambda e, s2=s2: e.tensor_tensor(out=Um[s2][:, :, NSA * 16:128].rearrange("p g (s c) -> p g s c", c=16), in0=fv(u_bf[:], 0, [[16, 32], [0, 8 - NSA], [1, 16]]),
                                                   in1=fv(mask8[:], NSA, [[0, 32], [1, 8 - NSA], [0, 16]]), op=ALU.mult), r=["u_bf", "mask8"], w=[("Um", s2, "v")])
            rms_rope(P.pool, k_sb, 2, gk_bc, rt[s2], krA[s2], tmp, ["k_sb", ("rtA", s2), "gk_bc"], ("kr", s2), "A", eng2=P.dve, tmp2=tmp2_A)
            stg[1][tt] = P.end_capture()
            P.capture()
            for h in range(2):
                P.pe(lambda e, h=h, s2=s2: e.transpose(out=pKT[:, h, :], in_=krA[s2][:, h * 128:(h + 1) * 128], identity=ident_b[:]), r=[("kr", s2), "ident_b"], w=["pKT"])
            P.act(lambda e, s2=s2: e.activation(out=kT_sb[s2][:], in_=pKT[:], func=ACT.Copy), r=["pKT"], w=[("kT_sb", s2)])
            P.dma(lambda e, s2=s2, t0=t0: e.dma_start(out=kT_d[:, :, t0:t0 + 128].rearrange("h p t -> p h t"), in_=kT_sb[s2][:]),
                  key=("kTd", s2), r=[("kT_sb", s2)], w=["kT_d"])
            for g in range(32):
                P.pe(lambda e, g=g, s2=s2: e.matmul(out=pU8[:, g, :], lhsT=Um[s2][:, g, :], rhs=sel16[:], start=True, stop=True),
                     r=[("Um", s2, x_) for x_ in list(range(NSA)) + ["v"]] + ["sel16"], w=["pU8"])
            P.act(lambda e, tt=tt: e.activation(out=u8[:, :, tt * 16:(tt + 1) * 16], in_=pU8[:], func=ACT.Copy), r=["pU8"], w=["u8"])
            stg[2][tt] = P.end_capture()
        if PIPE == 0:
            for tt in range(NTA):
                P.ops.extend(stg[0][tt]); P.ops.extend(stg1a[tt]); P.ops.extend(stg[1][tt]); P.ops.extend(stg[2][tt])
        else:
            for step in range(NTA + 2):
                for lst, off in ((stg1a, 1), (stg[0], 0), (stg[2], 2), (stg[1], 1)):
                    j = step - off
                    if 0 <= j < NTA:
                        P.ops.extend(lst[j])
        dump("u8", u8[:], [128, 32, NJF], BF16, r=["u8"])
        if "kT" in dbg_names:
            o = nc.dram_tensor("dbg_kT", [2, 128, NFULL], BF16, kind="ExternalOutput").ap()
            dbg_out["kT"] = o
            P.dma(lambda e: e.dma_start(out=o, in_=kT_d), key="dbg_kT", r=["kT_d"], w=["dbg_kT"])
        if "v" in dbg_names:
            o2 = nc.dram_tensor("dbg_v", [NFULL, 256], BF16, kind="ExternalOutput").ap()
            dbg_out["v"] = o2
            P.dma(lambda e: e.dma_start(out=o2, in_=v_d), key="dbg_v", r=["v_d"], w=["dbg_v"])
        P.barrier()
    if STOP <= 1:
        esA.close()
        return finish(nc, P, ges, out, dbg_out)

    TWO_PI = 2.0 * math.pi
    MAGIC = 12582912.0
    esS = ExitStack()
    esH = ExitStack()
    esZ = ExitStack()
    S_re = SB(esH, "S_re", [128, 2, 16, NJ], F32)
    S_im = SB(esH, "S_im", [128, 2, 16, NJ], F32)
    erZ = SB(esZ, "erZ", [128, 2, 16, 64], F32)
    eiZ = SB(esZ, "eiZ", [128, 2, 16, 64], F32)
    erZ8 = SB(esS, "erZ8", [128, 2, 16, 8], F32)
    eiZ8 = SB(esS, "eiZ8", [128, 2, 16, 8], F32)
    erR = SB(esS, "erR", [128, 2, 16, 72], F32)
    eiR = SB(esS, "eiR", [128, 2, 16, 72], F32)
    bbre = SB(esS, "bbre", [128, 2, 16, 16], F32)
    bbim = SB(esS, "bbim", [128, 2, 16, 16], F32)
    ccre = SB(esS, "ccre", [128, 2, 16, 16], F32)
    ccim = SB(esS, "ccim", [128, 2, 16, 16], F32)
    A64r = SB(esS, "A64r", [128, 2, 16, 1], F32)
    A64i = SB(esS, "A64i", [128, 2, 16, 1], F32)
    ardt = SB(esS, "ardt", [128, 2, 16], F32)
    aidt = SB(esS, "aidt", [128, 2, 16], F32)
    ptmp = []
    piT = SB(esS, "piT", [128, 1], F32)

    def powtab(exps_ap, n, er_t, ei_t, tag):
        def v4(t):
            return t[:, :, :, 0:n]
        a_b = lambda t: fv(t[:], 0, [[16, 2], [1, 16], [0, n]])
        e_b = fv(exps_ap, 0, [[exps_ap.ap[1][0], 2], [0, 16], [1, n]])
        t0, t1, t2 = ptmp
        k = lambda i: ("ptmp", i)
        P.dve(lambda e: e.tensor_tensor(out=v4(t0), in0=a_b(ardt), in1=e_b, op=ALU.mult), r=["ardt", tag + "_e"], w=[k(0)])
        P.act(lambda e: e.activation(out=v4(t0), in_=v4(t0), func=ACT.Exp), r=[k(0)], w=[k(0)])
        P.dve(lambda e: e.tensor_tensor(out=v4(t1), in0=a_b(aidt), in1=e_b, op=ALU.mult), r=["aidt", tag + "_e"], w=[k(1)])
        P.dve(lambda e: e.tensor_scalar(out=v4(t2), in0=v4(t1), scalar1=MAGIC, scalar2=None, op0=ALU.add), r=[k(1)], w=[k(2)])
        P.dve(lambda e: e.tensor_scalar(out=v4(t2), in0=v4(t2), scalar1=MAGIC, scalar2=None, op0=ALU.subtract), r=[k(2)], w=[k(2)])
        P.dve(lambda e: e.tensor_tensor(out=v4(t2), in0=v4(t1), in1=v4(t2), op=ALU.subtract), r=[k(1), k(2)], w=[k(2)])
        P.act(lambda e: e.activation(out=v4(t2), in_=v4(t2), func=ACT.Sin, scale=TWO_PI), r=[k(2)], w=[k(2)])
        P.dve(lambda e: e.tensor_tensor(out=v4(ei_t), in0=v4(t0), in1=v4(t2), op=ALU.mult), r=[k(0), k(2)], w=[tag + "_ei"])
        P.dve(lambda e: e.tensor_scalar(out=v4(t1), in0=v4(t1), scalar1=0.25, scalar2=None, op0=ALU.add), r=[k(1)], w=[k(1)])
        P.dve(lambda e: e.tensor_scalar(out=v4(t2), in0=v4(t1), scalar1=MAGIC, scalar2=None, op0=ALU.add), r=[k(1)], w=[k(2)])
        P.dve(lambda e: e.tensor_scalar(out=v4(t2), in0=v4(t2), scalar1=MAGIC, scalar2=None, op0=ALU.subtract), r=[k(2)], w=[k(2)])
        P.dve(lambda e: e.tensor_tensor(out=v4(t2), in0=v4(t1), in1=v4(t2), op=ALU.subtract), r=[k(1), k(2)], w=[k(2)])
        P.act(lambda e: e.activation(out=v4(t2), in_=v4(t2), func=ACT.Sin, scale=TWO_PI), r=[k(2)], w=[k(2)])
        P.dve(lambda e: e.tensor_tensor(out=v4(er_t), in0=v4(t0), in1=v4(t2), op=ALU.mult), r=[k(0), k(2)], w=[tag + "_er"])

    with ExitStack() as es:
        ptmp.extend([SB(es, "ptmp%d" % i, [128, 2, 16, 72], F32) for i in range(3)])
        a_sb = SB(es, "a_sb", [128, 2, 2, 16], F32)
        ldt_sb = SB(es, "ldt_sb", [128, 2, 16], F32)
        b_sb = SB(es, "b_sb", [128, 2, 2, 16, 16], F32)
        c_sb = SB(es, "c_sb", [128, 2, 2, 16, 16], F32)
        eZ_sb = SB(es, "eZ_sb", [128, 2, 64], F32)
        eR_sb = SB(es, "eR_sb", [128, 2, 72], F32)
        e1_sb = SB(es, "e1_sb", [128, 2, 1], F32)
        e64_sb = SB(es, "e64_sb", [128, 2, 1], F32)
        abr = SB(es, "abr", [128, 2, 16, 1], F32)
        abi = SB(es, "abi", [128, 2, 16, 1], F32)
        dsc = [SB(es, "dsc%d" % i, [128, 2, 16], F32) for i in range(5)]
        bt = [SB(es, "bt%d" % i, [128, 2, 16, 16], F32) for i in range(2)]
        P.dma(lambda e: e.dma_start(out=a_sb[:], in_=s5_a), key="a_sb", w=["a_sb"])
        P.dma(lambda e: e.dma_start(out=ldt_sb[:], in_=s5_ldt), key="ldt_sb", w=["ldt_sb"])
        P.dma(lambda e: e.dma_start(out=b_sb[:], in_=s5_b), key="b_sb", w=["b_sb"])
        P.dma(lambda e: e.dma_start(out=c_sb[:], in_=s5_c), key="c_sb", w=["c_sb"])
        P.dma(lambda e: e.dma_start(out=eZ_sb[:], in_=cst_eZ), key="eZ_sb", w=["Z_e"])
        P.dma(lambda e: e.dma_start(out=eR_sb[:], in_=cst_eR), key="eR_sb", w=["R_e"])
        P.dve(lambda e: e.memset(e1_sb[:], 1.0), w=["ab_e"])
        P.dve(lambda e: e.memset(e64_sb[:], 64.0), w=["A64_e"])
        P.act(lambda e: e.activation(out=ldt_sb[:], in_=ldt_sb[:], func=ACT.Exp), r=["ldt_sb"], w=["ldt_sb"])
        P.dve(lambda e: e.tensor_tensor(out=ardt[:], in0=a_sb[:, 0], in1=ldt_sb[:], op=ALU.mult), r=["a_sb", "ldt_sb"], w=["ardt"])
        P.dve(lambda e: e.scalar_tensor_tensor(out=aidt[:], in0=a_sb[:, 1], scalar=1.0 / TWO_PI, in1=ldt_sb[:], op0=ALU.mult, op1=ALU.mult),
              r=["a_sb", "ldt_sb"], w=["aidt"])
        powtab(e1_sb[:], 1, abr, abi, "ab")
        powtab(e64_sb[:], 1, A64r, A64i, "A64")
        powtab(eZ_sb[:], 64, erZ, eiZ, "Z")
        powtab(eR_sb[:], 72, erR, eiR, "R")
        for (src_t, dst_t, kk) in ((erZ, erZ8, "Z_er"), (eiZ, eiZ8, "Z_ei")):
            P.dve(lambda e, src_t=src_t, dst_t=dst_t: e.tensor_copy(out=dst_t[:, 0], in_=src_t[:, 0, :, 56:64]), r=[kk], w=[kk + "8"])
            P.dve(lambda e, src_t=src_t, dst_t=dst_t: e.tensor_copy(out=dst_t[:, 1], in_=src_t[:, 1, :, 0:8]), r=[kk], w=[kk + "8"])
        are, aim = a_sb[:, 0], a_sb[:, 1]
        d0, d1, d2, d3, d4 = [t[:] for t in dsc]
        ab_r = abr[:].rearrange("p d q o -> p d (q o)")
        ab_i = abi[:].rearrange("p d q o -> p d (q o)")
        kd = lambda i: ("dsc", i)
        P.dve(lambda e: e.tensor_tensor(out=d0, in0=are, in1=are, op=ALU.mult), r=["a_sb"], w=[kd(0)])
        P.dve(lambda e: e.tensor_tensor(out=d1, in0=aim, in1=aim, op=ALU.mult), r=["a_sb"], w=[kd(1)])
        P.dve(lambda e: e.tensor_tensor(out=d0, in0=d0, in1=d1, op=ALU.add), r=[kd(0), kd(1)], w=[kd(0)])
        P.dve(lambda e: e.reciprocal(out=d0, in_=d0), r=[kd(0)], w=[kd(0)])
        P.dve(lambda e: e.tensor_scalar(out=d1, in0=ab_r, scalar1=-1.0, scalar2=None, op0=ALU.add), r=["ab_er"], w=[kd(1)])
        P.dve(lambda e: e.tensor_tensor(out=d2, in0=d1, in1=are, op=ALU.mult), r=[kd(1), "a_sb"], w=[kd(2)])
        P.dve(lambda e: e.tensor_tensor(out=d3, in0=ab_i, in1=aim, op=ALU.mult), r=["ab_ei", "a_sb"], w=[kd(3)])
        P.dve(lambda e: e.tensor_tensor(out=d2, in0=d2, in1=d3, op=ALU.add), r=[kd(2), kd(3)], w=[kd(2)])
        P.dve(lambda e: e.tensor_tensor(out=d2, in0=d2, in1=d0, op=ALU.mult), r=[kd(2), kd(0)], w=[kd(2)])
        P.dve(lambda e: e.tensor_tensor(out=d3, in0=ab_i, in1=are, op=ALU.mult), r=["ab_ei", "a_sb"], w=[kd(3)])
        P.dve(lambda e: e.tensor_tensor(out=d4, in0=d1, in1=aim, op=ALU.mult), r=[kd(1), "a_sb"], w=[kd(4)])
        P.dve(lambda e: e.tensor_tensor(out=d3, in0=d3, in1=d4, op=ALU.subtract), r=[kd(3), kd(4)], w=[kd(3)])
        P.dve(lambda e: e.tensor_tensor(out=d3, in0=d3, in1=d0, op=ALU.mult), r=[kd(3), kd(0)], w=[kd(3)])
        rr_b = fv(dsc[2][:], 0, [[16, 2], [1, 16], [0, 16]])
        ri_b = fv(dsc[3][:], 0, [[16, 2], [1, 16], [0, 16]])
        bre, bim = b_sb[:, 0], b_sb[:, 1]
        P.dve(lambda e: e.tensor_tensor(out=bt[0][:], in0=bre, in1=rr_b, op=ALU.mult), r=["b_sb", kd(2)], w=["bt0"])
        P.dve(lambda e: e.tensor_tensor(out=bt[1][:], in0=bim, in1=ri_b, op=ALU.mult), r=["b_sb", kd(3)], w=["bt1"])
        P.dve(lambda e: e.tensor_tensor(out=bbre[:], in0=bt[0][:], in1=bt[1][:], op=ALU.subtract), r=["bt0", "bt1"], w=["bbre"])
        P.dve(lambda e: e.tensor_tensor(out=bt[0][:], in0=bim, in1=rr_b, op=ALU.mult), r=["b_sb", kd(2)], w=["bt0"])
        P.dve(lambda e: e.tensor_tensor(out=bt[1][:], in0=bre, in1=ri_b, op=ALU.mult), r=["b_sb", kd(3)], w=["bt1"])
        P.dve(lambda e: e.tensor_tensor(out=bbim[:], in0=bt[0][:], in1=bt[1][:], op=ALU.add), r=["bt0", "bt1"], w=["bbim"])
        P.dve(lambda e: e.tensor_copy(out=ccre[:], in_=c_sb[:, 0]), r=["c_sb"], w=["ccre"])
        P.dve(lambda e: e.tensor_copy(out=ccim[:], in_=c_sb[:, 1]), r=["c_sb"], w=["ccim"])
        P.barrier()

    def ztab(eng_add, pair, tsl, nt, bufs, tag, erZ=erZ, eiZ=eiZ, tmkey=None):
        zre, zim, ztm = bufs
        tmkey = tmkey or (tag + "ztm")
        def ev(t, d):
            return fv(t[:, d, pair, tsl[d]:tsl[d] + nt], 0, [[1, nt], [0, 16]])
        def bv(t, d):
            return fv(t[:, d, pair, :], 0, [[0, nt], [1, 16]])
        for d in range(2):
            eng_add(lambda e, d=d: e.tensor_tensor(out=zre[:, d], in0=bv(bbre, d), in1=ev(erZ, d), op=ALU.mult), r=["Z_er", "bbre"], w=[(tag + "zre", d)])
            eng_add(lambda e, d=d: e.tensor_tensor(out=ztm[:, d], in0=bv(bbim, d), in1=ev(eiZ, d), op=ALU.mult), r=["Z_ei", "bbim"], w=[(tmkey, d)])
            eng_add(lambda e, d=d: e.tensor_tensor(out=zre[:, d], in0=zre[:, d], in1=ztm[:, d], op=ALU.subtract), r=[(tag + "zre", d), (tmkey, d)], w=[(tag + "zre", d)])
            eng_add(lambda e, d=d: e.tensor_tensor(out=zim[:, d], in0=bv(bbim, d), in1=ev(erZ, d), op=ALU.mult), r=["Z_er", "bbim"], w=[(tag + "zim", d)])
            eng_add(lambda e, d=d: e.tensor_tensor(out=ztm[:, d], in0=bv(bbre, d), in1=ev(eiZ, d), op=ALU.mult), r=["Z_ei", "bbre"], w=[(tmkey, d)])
            eng_add(lambda e, d=d: e.tensor_tensor(out=zim[:, d], in0=zim[:, d], in1=ztm[:, d], op=ALU.add), r=[(tag + "zim", d), (tmkey, d)], w=[(tag + "zim", d)])

    with ExitStack() as es:
        Wsum = [SB(es, "Wsum%d" % i, [128, 2, 8, 2, 128], BF16) for i in range(2)]
        zb = [[SB(es, "zb%d_%d" % (i, j), [128, 2, 64, 16], F32) for j in range(3)] for i in range(1)]
        pW = [PS(es, "pW%d" % i, [128, 4, 128], F32) for i in range(2)]
        pSr = PS(es, "pSr", [128, 2, NJ], F32)
        pSi = PS(es, "pSi", [128, 2, NJ], F32)
        ci = 0
        for pair in range(16):
            sl = int(os.environ.get('K_SL', pair % 2))
            ea = P.dve
            zt_ = "z0"
            ztab(ea, pair, (0, 0), 64, zb[0], zt_)
            zre, zim, _ = zb[0]
            for d in range(2):
                for reim in range(2):
                    zt = zre if reim == 0 else zim
                    for mq in range(2):
                        pw = pW[ci % 2]
                        for j in range(4):
                            m_ = mq * 4 + j
                            P.pe(lambda e, zt=zt, d=d, m_=m_, pw=pw, j=j: e.transpose(out=pw[:, j, :], in_=zt[:, d, m_ * 8:(m_ + 1) * 8, :].rearrange("p s c -> p (s c)"), identity=ident_f[:]),
                                 r=[(zt_ + "z%s" % ("re" if reim == 0 else "im"), d), "ident_f"], w=[("pW", ci % 2)])
                        dst = Wsum[sl][:, d, mq * 4:(mq + 1) * 4, reim, :]
                        P.act(lambda e, dst=dst, pw=pw: e.activation(out=dst, in_=pw[:], func=ACT.Copy), r=[("pW", ci % 2)], w=[("Wsum", sl)])
                        ci += 1
            for d in range(2):
                for gh in range(2):
                    g = 2 * pair + gh
                    for reim, pS_ in ((0, pSr), (1, pSi)):
                        for m_ in range(8):
                            P.pe(lambda e, d=d, gh=gh, g=g, reim=reim, pS_=pS_, m_=m_, sl=sl: e.matmul(
                                out=pS_[64 * gh:64 * gh + 64, d, :], lhsT=Wsum[sl][:, d, m_, reim, 64 * gh:64 * gh + 64],
                                rhs=fv(u8[:, g, :], m_, [[8, NJ]]), start=(m_ == 0), stop=(m_ == 7)),
                                r=[("Wsum", sl), "u8"], w=["pSr" if reim == 0 else "pSi"])
            P.act(lambda e, pair=pair: e.activation(out=S_re[:, :, pair, :], in_=pSr[:], func=ACT.Copy), r=["pSr"], w=["S_re"])
            P.act(lambda e, pair=pair: e.activation(out=S_im[:, :, pair, :], in_=pSi[:], func=ACT.Copy), r=["pSi"], w=["S_im"])
        P.barrier()
    esA.close()
    esZ.close()

    with ExitStack() as es:
        sct = [[SB(es, "sct%d_%d" % (d, i), [128, 16], F32) for i in range(4)] for d in range(2)]
        orders = [[(J, J - 1 if J > 0 else None) for J in range(NJ)],
                  [(3, None), (2, 3), (1, 2), (0, 1), (131, 0)] + [(J, J + 1) for J in range(130, 3, -1)]]
        for d in range(2):
            ea = P.dve if d == 0 else P.pool
            Ar = A64r[:, d, :, 0]
            Ai = A64i[:, d, :, 0]
            t = [x[:] for x in sct[d]]
            kt = lambda i: ("sct", d, i)
            kr_, ki_ = ("Hre", d), ("Him", d)
            for (J, Jp) in orders[d]:
                if Jp is None:
                    continue
                hr_p, hi_p = S_re[:, d, :, Jp], S_im[:, d, :, Jp]
                hr, hi = S_re[:, d, :, J], S_im[:, d, :, J]
                ea(lambda e, t=t, hr_p=hr_p, Ar=Ar: e.tensor_tensor(out=t[0], in0=Ar, in1=hr_p, op=ALU.mult), r=["A64_er", kr_, "S_re"], w=[kt(0)])
                ea(lambda e, t=t, hi_p=hi_p, Ai=Ai: e.tensor_tensor(out=t[1], in0=Ai, in1=hi_p, op=ALU.mult), r=["A64_ei", ki_, "S_im"], w=[kt(1)])
                ea(lambda e, t=t: e.tensor_tensor(out=t[0], in0=t[0], in1=t[1], op=ALU.subtract), r=[kt(0), kt(1)], w=[kt(0)])
                ea(lambda e, t=t, hi_p=hi_p, Ar=Ar: e.tensor_tensor(out=t[2], in0=Ar, in1=hi_p, op=ALU.mult), r=["A64_er", ki_, "S_im"], w=[kt(2)])
                ea(lambda e, t=t, hr_p=hr_p, Ai=Ai: e.tensor_tensor(out=t[3], in0=Ai, in1=hr_p, op=ALU.mult), r=["A64_ei", kr_, "S_re"], w=[kt(3)])
                ea(lambda e, t=t: e.tensor_tensor(out=t[2], in0=t[2], in1=t[3], op=ALU.add), r=[kt(2), kt(3)], w=[kt(2)])
                ea(lambda e, t=t, hr=hr: e.tensor_tensor(out=hr, in0=hr, in1=t[0], op=ALU.add), r=[kt(0), kr_], w=[kr_])
                ea(lambda e, t=t, hi=hi: e.tensor_tensor(out=hi, in0=hi, in1=t[2], op=ALU.add), r=[kt(2), ki_], w=[ki_])
        P.barrier()
    dump("H_re", S_re[:], [128, 2, 16, NJ])
    dump("H_im", S_im[:], [128, 2, 16, NJ])
    HO = [[SB(esS, "HO%d_%d" % (d, x), [128, 16, 32], BF16) for x in range(2)] for d in range(2)]
    with ExitStack() as es:
        cm = SB(es, "cm", [128, 4], F32)
        hacc = SB(es, "hacc", [128, 16, 32], F32)
        P.dma(lambda e: e.dma_start(out=cm[:], in_=cmask), key="cm", w=["cm"])
        for d in range(2):
            for x, Sx in enumerate((S_re, S_im)):
                for r_ in range(4):
                    lo = (3 + 32 * r_) if d == 0 else (5 + 32 * r_)
                    segs = [(lo, 0, 32)] if not (d == 1 and r_ == 3) else [(lo, 0, 31), (0, 31, 1)]
                    for (a0, o0, n_) in segs:
                        src_ = Sx[:, d, :, a0:a0 + n_]
                        dst_ = hacc[:, :, o0:o0 + n_]
                        if r_ == 0:
                            P.dve(lambda e, src_=src_, dst_=dst_: e.tensor_scalar(out=dst_, in0=src_, scalar1=cm[:, 0:1], scalar2=None, op0=ALU.mult),
                                  r=["cm"], w=["hacc"])
                        else:
                            P.dve(lambda e, src_=src_, dst_=dst_, r_=r_: e.scalar_tensor_tensor(out=dst_, in0=src_, scalar=cm[:, r_:r_ + 1], in1=dst_, op0=ALU.mult, op1=ALU.add),
                                  r=["cm", "hacc"], w=["hacc"])
                P.dve(lambda e, d=d, x=x: e.tensor_copy(out=HO[d][x][:], in_=hacc[:]), r=["hacc"], w=[("HO", d, x)])
        P.barrier()
    esH.close()
    if STOP <= 2:
        esS.close()
        return finish(nc, P, ges, out, dbg_out)

    esP = ExitStack()
    hTo = SB(esP, "hTo", [128, 8, NOWN], BF16)
    qT = SB(esP, "qT", [128, 8, NOWN], BF16)
    esU = ExitStack()
    u8o = SB(esU, "u8o", [128, 32, 256], BF16)

    def load_bc(dst, src_row, key, add_one=False):
        P.dma(lambda e: e.dma_start(out=dst[:], in_=src_row.partition_broadcast(128)), key=key, w=[key])
        if add_one:
            P.dve(lambda e: e.tensor_scalar(out=dst[:], in0=dst[:], scalar1=1.0, scalar2=None, op0=ALU.add), r=[key], w=[key])

    with ExitStack() as es:
        Wq = SB(es, "Wq", [128, 8, 1024], BF16)
        Wuo = SB(es, "Wuo", [128, 8, 512], BF16)
        sc1p_bc = SB(es, "sc1p_bc", [128, D], F32)
        sh1_bc = SB(es, "sh1_bc", [128, D], F32)
        xt_B = [SB(es, "xtB%d" % i, [128, D], F32) for i in range(2)]
        rt_B = [SB(es, "rtB%d" % i, [128, 128], F32) for i in range(2)]
        hn = SB(es, "hn", [128, D], F32)
        hb_B = [SB(es, "hbB%d" % i, [128, D], BF16) for i in range(2)]
        st_B = SB(es, "stB", [128, 2, 6], F32)
        mv_B = SB(es, "mvB", [128, 2], F32)
        rstd_B = SB(es, "rstdB", [128, 1], F32)
        nbt_B = SB(es, "nbtB", [128, 1], F32)
        q_sbs = [SB(es, "q_sb%d" % i, [128, 1024], F32) for i in range(2)]
        tmp2_B = (SB(es, "tcB", [128, 512], F32), SB(es, "tdB", [128, 512], F32))
        qr = SB(es, "qr", [128, 1024], BF16)
        tmp_B = (SB(es, "sqB", [128, 1024], F32), SB(es, "ssB", [128, 8], F32), SB(es, "rkB", [128, 8], F32),
               SB(es, "taB", [128, 512], F32), SB(es, "tbB", [128, 512], F32))
        u_bf_B = SB(es, "u_bfB", [128, 512], BF16)
        Um_B = [SB(es, "UmB0", [128, 32, 128], BF16)] * 2
        pT_B = [PS(es, "pTB%d" % i, [128, 8, 128], BF16) for i in range(2)]
        pQ = [PS(es, "pQ%d" % i, [128, 512], F32) for i in range(2)]
        pQT = PS(es, "pQT", [128, 8, 128], BF16)
        pU_B = PS(es, "pUB", [128, 512], F32)
        pU8_B = PS(es, "pU8B", [128, 32, 16], F32)
        P.dma(lambda e: e.dma_start(out=Wq[:], in_=w_in[:, 512:1536].rearrange("(kc p) n -> p kc n", p=128)), key="Wq", w=["Wq"], eng="pool")
        P.dma(lambda e: e.dma_start(out=Wuo[:], in_=w_in[:, 0:512].rearrange("(kc p) n -> p kc n", p=128)), key="Wuo", w=["Wuo"], eng="pool")
        load_bc(sc1p_bc, mod_d[0:1, 1024:2048], "sc1p_bc", True)
        load_bc(sh1_bc, mod_d[0:1, 0:1024], "sh1_bc")
        sB0, sB1a, sB1b, sB2 = [None] * NTO, [None] * NTO, [None] * NTO, [None] * NTO
        for tt in range(NTO):
            s2 = tt % 2
            t0 = tt * 128
            P.capture()
            P.dma(lambda e, s2=s2, t0=t0: e.dma_start(out=xt_B[s2][:], in_=xo[t0:t0 + 128, :]), key=("xtB", s2), w=[("xtB", s2)])
            P.dma(lambda e, s2=s2, t0=t0: e.dma_start(out=rt_B[s2][:], in_=rope_o[t0:t0 + 128, :]), key=("rtB", s2), w=[("rtB", s2)])
            ln_tile(xt_B[s2][:], ("xtB", s2), st_B, mv_B, rstd_B, nbt_B, hn[:], "hn", "B", mode=LNM_A)
            P.dve(lambda e: e.tensor_tensor(out=hn[:], in0=hn[:], in1=sc1p_bc[:], op=ALU.mult), r=["hn", "sc1p_bc"], w=["hn"])
            P.dve(lambda e, s2=s2: e.tensor_tensor(out=hb_B[s2][:], in0=hn[:], in1=sh1_bc[:], op=ALU.add), r=["hn", "sh1_bc"], w=[("hbB", s2)])
            sB0[tt] = P.end_capture()
            P.capture()
            for kc in range(8):
                P.pe(lambda e, kc=kc, s2=s2: e.transpose(out=pT_B[s2][:, kc, :], in_=hb_B[s2][:, kc * 128:(kc + 1) * 128], identity=ident_b[:]),
                     r=[("hbB", s2), "ident_b"], w=[("pTB", s2)])
            P.act(lambda e, s2=s2, t0=t0, tt=tt: e.activation(out=hTo[:, :, t0:t0 + 128], in_=pT_B[s2][:], func=ACT.Copy), r=[("pTB", s2)], w=[("hTo", tt)])
            sB1a[tt] = P.end_capture()
            P.capture()
            for half in range(2):
                for kc in range(8):
                    P.pe(lambda e, kc=kc, half=half, t0=t0: e.matmul(out=pQ[half][:], lhsT=hTo[:, kc, t0:t0 + 128], rhs=Wq[:, kc, half * 512:(half + 1) * 512],
                                                                     start=(kc == 0), stop=(kc == 7)), r=[("hTo", tt), "Wq"], w=[("pQ", half)])
                P.act(lambda e, half=half, s2=s2: e.activation(out=q_sbs[s2][:, half * 512:(half + 1) * 512], in_=pQ[half][:], func=ACT.Copy), r=[("pQ", half)], w=[("q_sb", s2)])
            for kc in range(8):
                P.pe(lambda e, kc=kc, t0=t0: e.matmul(out=pU_B[:], lhsT=hTo[:, kc, t0:t0 + 128], rhs=Wuo[:, kc, :], start=(kc == 0), stop=(kc == 7)),
                     r=[("hTo", tt), "Wuo"], w=["pUB"])
            P.act(lambda e: e.activation(out=u_bf_B[:], in_=pU_B[:], func=ACT.Copy), r=["pUB"], w=["u_bfB"])
            P.dve(lambda e, s2=s2: e.tensor_tensor(out=Um_B[s2][:].rearrange("p g (s c) -> p g s c", c=16), in0=fv(u_bf_B[:], 0, [[16, 32], [0, 8], [1, 16]]),
                                                   in1=fv(mask8[:], 0, [[0, 32], [1, 8], [0, 16]]), op=ALU.mult), r=["u_bfB", "mask8"], w=["UmB"])
            rms_rope(P.pool, q_sbs[s2], 8, gq_bc, rt_B[s2], qr, tmp_B, [("q_sb", s2), ("rtB", s2), "gq_bc"], "qr", "B", eng2=P.dve, tmp2=tmp2_B)
            sB1b[tt] = P.end_capture()
            P.capture()
            for h in range(8):
                P.pe(lambda e, h=h: e.transpose(out=pQT[:, h, :], in_=qr[:, h * 128:(h + 1) * 128], identity=ident_b[:]), r=["qr", "ident_b"], w=["pQT"])
            P.act(lambda e, t0=t0: e.activation(out=qT[:, :, t0:t0 + 128], in_=pQT[:], func=ACT.Copy), r=["pQT"], w=[("qT", tt)])
            for g in range(32):
                P.pe(lambda e, g=g, s2=s2: e.matmul(out=pU8_B[:, g, :], lhsT=Um_B[s2][:, g, :], rhs=sel16[:], start=True, stop=True),
                     r=["UmB", "sel16"], w=["pU8B"])
            P.act(lambda e, tt=tt: e.activation(out=u8o[:, :, tt * 16:(tt + 1) * 16], in_=pU8_B[:], func=ACT.Copy), r=["pU8B"], w=[("u8o", tt)])
            sB2[tt] = P.end_capture()
        if int(os.environ.get('K_PIPEB', '1')) == 0:
            for tt in range(NTO):
                for lst in (sB0, sB1a, sB1b, sB2):
                    P.ops.extend(lst[tt])
        else:
            for step in range(NTO + 2):
                for lst, off in ((sB1a, 1), (sB0, 0), (sB2, 2), (sB1b, 1)):
                    j = step - off
                    if 0 <= j < NTO:
                        P.ops.extend(lst[j])
        P.barrier()
    if "qT" in dbg_names:
        dump("qT", qT[:], [128, 8, NOWN], BF16)
    if STOP <= 3:
        esU.close(); esS.close(); esP.close()
        return finish(nc, P, ges, out, dbg_out)

    gT = SB(esP, "gT", [128, 4, NOWN], BF16)
    with ExitStack() as es:
        mf_sb = SB(es, "mf_sb", [128, 128], F32)
        mb_sb = SB(es, "mb_sb", [128, 128], F32)
        dcol_sb = SB(es, "dcol_sb", [128, 32], F32)
        selg_b = SB(es, "selg_b", [128, 8, 128], BF16)
        m8c_b = SB(es, "m8c_b", [128, 8], BF16)
        z8 = [SB(es, "z8_%d" % j, [128, 2, 8, 16], F32) for j in range(3)]
        Rre_p = SB(es, "Rre_p", [128, 2, 72, 16], F32)
        Rim_p = SB(es, "Rim_p", [128, 2, 72, 16], F32)
        Rt0 = SB(es, "Rt0", [128, 1, 72, 16], F32)
        Rt1 = SB(es, "Rt1", [128, 1, 72, 16], F32)
        Rre_b = SB(es, "Rre_b", [128, 2, 1152], BF16)
        Rim_b = SB(es, "Rim_b", [128, 2, 1152], BF16)
        Tw = [SB(es, "Tw0", [128, 2, 15, 128], BF16)] * 2
        tt0 = SB(es, "tt0", [128, 128], F32)
        tt1 = SB(es, "tt1", [128, 128], F32)
        Ye = [SB(es, "Ye0", [128, 2048], BF16)] * 2
        ysb = SB(es, "ysb", [128, 1024], F32)
        yx2 = SB(es, "yx2", [128, 1024], F32)
        pTb = [PS(es, "pTb%d" % i, [128, 128], F32) for i in range(2)]
        pY8 = [PS(es, "pY8_%d" % i, [128, 8, 32], F32) for i in range(2)]
        pYT = PS(es, "pYT", [128, 2048], F32)
        P.dma(lambda e: e.dma_start(out=mf_sb[:], in_=cst_mf), key="mf_sb", w=["mf_sb"])
        P.dma(lambda e: e.dma_start(out=mb_sb[:], in_=cst_mb), key="mb_sb", w=["mb_sb"])
        P.dma(lambda e: e.dma_start(out=dcol_sb[:], in_=s5_dcol), key="dcol_sb", w=["dcol_sb"])
        P.dma(lambda e: e.dma_start(out=selg_b[:], in_=cst_selg), key="selg_b", w=["selg_b"], eng="pool")
        P.dma(lambda e: e.dma_start(out=m8c_b[:], in_=cst_mask8c), key="m8c_b", w=["m8c_b"], eng="pool")
        ecnt = 0
        for pair in range(16):
            sl = 0
            ztab(P.dve, pair, (0, 0), 8, z8, "z8", erZ=erZ8, eiZ=eiZ8)
            for d in range(2):
                eb = lambda t, d=d, pair=pair: fv(t[:, d, pair, :], 0, [[1, 72], [0, 16]])
                cb = lambda t, d=d, pair=pair: fv(t[:, d, pair, :], 0, [[0, 72], [1, 16]])
                P.pool(lambda e, d=d, eb=eb, cb=cb: e.tensor_tensor(out=Rre_p[:, d], in0=cb(ccre), in1=eb(erR), op=ALU.mult), r=["R_er", "ccre"], w=["Rre_p"])
                P.pool(lambda e, d=d, eb=eb, cb=cb: e.tensor_tensor(out=Rt0[:, 0], in0=cb(ccim), in1=eb(eiR), op=ALU.mult), r=["R_ei", "ccim"], w=["Rt0"])
                P.pool(lambda e, d=d: e.tensor_tensor(out=Rre_p[:, d], in0=Rre_p[:, d], in1=Rt0[:, 0], op=ALU.subtract), r=["Rre_p", "Rt0"], w=["Rre_p"])
                P.dve(lambda e, d=d, eb=eb, cb=cb: e.tensor_tensor(out=Rim_p[:, d], in0=cb(ccim), in1=eb(erR), op=ALU.mult), r=["R_er", "ccim"], w=["Rim_p"])
                P.dve(lambda e, d=d, eb=eb, cb=cb: e.tensor_tensor(out=Rt1[:, 0], in0=cb(ccre), in1=eb(eiR), op=ALU.mult), r=["R_ei", "ccre"], w=["Rt1"])
                P.dve(lambda e, d=d: e.scalar_tensor_tensor(out=Rim_p[:, d], in0=Rt1[:, 0], scalar=-1.0, in1=Rim_p[:, d], op0=ALU.mult, op1=ALU.subtract),
                      r=["Rim_p", "Rt1"], w=["Rim_p"])
            P.act(lambda e: e.activation(out=Rre_b[:], in_=Rre_p[:].rearrange("p d i c -> p d (i c)"), func=ACT.Copy), r=["Rre_p"], w=["Rre_b"])
            P.act(lambda e: e.activation(out=Rim_b[:], in_=Rim_p[:].rearrange("p d i c -> p d (i c)"), func=ACT.Copy), r=["Rim_p"], w=["Rim_b"])
            for gh in range(2):
                g = 2 * pair + gh
                rows = slice(64 * gh, 64 * gh + 64)
                blocks = [(0, 0), (1, 0)] + [(0, k) for k in range(1, 8)] + [(1, k) for k in range(1, 8)]
                for (d, k) in blocks:
                    pt = pTb[ecnt % 2]
                    kk = ("pTb", ecnt % 2)
                    P.pe(lambda e, pt=pt, d=d, k=k, rows=rows: e.matmul(out=pt[:], lhsT=z8[0][rows, d].rearrange("p s c -> p (s c)"),
                                                                        rhs=Rre_p[rows, d, 8 * k:8 * k + 8, :].rearrange("p s c -> p (s c)"), start=True, stop=False),
                         r=[("z8zre", d), "Rre_p"], w=[kk])
                    P.pe(lambda e, pt=pt, d=d, k=k, rows=rows: e.matmul(out=pt[:], lhsT=z8[1][rows, d].rearrange("p s c -> p (s c)"),
                                                                        rhs=Rim_p[rows, d, 8 * k:8 * k + 8, :].rearrange("p s c -> p (s c)"), start=False, stop=True),
                         r=[("z8zim", d), "Rim_p"], w=[kk])
                    if k == 0 and d == 0:
                        P.dve(lambda e, pt=pt: e.tensor_tensor(out=tt0[:], in0=pt[:], in1=mf_sb[:], op=ALU.mult), r=[kk, "mf_sb"], w=["tt0"])
                    elif k == 0 and d == 1:
                        P.dve(lambda e, pt=pt: e.tensor_tensor(out=tt1[:], in0=pt[:], in1=mb_sb[:], op=ALU.mult), r=[kk, "mb_sb"], w=["tt1"])
                        P.dve(lambda e: e.tensor_tensor(out=tt0[:], in0=tt0[:], in1=tt1[:], op=ALU.add), r=["tt0", "tt1"], w=["tt0"])
                        P.dve(lambda e, g=g, gh=gh, sl=sl: e.scalar_tensor_tensor(out=Tw[sl][:, gh, 7, :], in0=ident_f[:], scalar=dcol_sb[:, g:g + 1], in1=tt0[:],
                                                                                 op0=ALU.mult, op1=ALU.add), r=["tt0", "dcol_sb", "ident_f"], w=[("Tw", sl)])
                    else:
                        idx = 7 + k if d == 0 else 7 - k
                        if ecnt % 2 == 0:
                            P.act(lambda e, pt=pt, gh=gh, sl=sl, idx=idx: e.activation(out=Tw[sl][:, gh, idx, :], in_=pt[:], func=ACT.Copy), r=[kk], w=[("Tw", sl)])
                        else:
                            P.dve(lambda e, pt=pt, gh=gh, sl=sl, idx=idx: e.tensor_copy(out=Tw[sl][:, gh, idx, :], in_=pt[:]), r=[kk], w=[("Tw", sl)])
                    ecnt += 1
            for gh in range(2):
                g = 2 * pair + gh
                rows = slice(64 * gh, 64 * gh + 64)
                py = pY8[g % 2]
                ky = ("pY8", g % 2)
                for m in range(8):
                    for m_ in range(8):
                        P.pe(lambda e, py=py, m=m, m_=m_, gh=gh, g=g, sl=sl: e.matmul(out=py[:, m, :], lhsT=Tw[sl][:, gh, 7 + m - m_, :],
                                                                                     rhs=fv(u8o[:, g, :], m_, [[8, 32]]), start=(m_ == 0), stop=False),
                             r=[("Tw", sl), "u8o"], w=[ky])
                    fo = 8 * (m + 1) * 16
                    bo = 8 * (8 - m) * 16
                    P.pe(lambda e, py=py, m=m, rows=rows, fo=fo, pair=pair: e.matmul(out=py[:, m, :], lhsT=Rre_b[rows, 0, fo:fo + 128], rhs=HO[0][0][rows, pair, :], start=False, stop=False),
                         r=["Rre_b", ("HO", 0, 0)], w=[ky])
                    P.pe(lambda e, py=py, m=m, rows=rows, fo=fo, pair=pair: e.matmul(out=py[:, m, :], lhsT=Rim_b[rows, 0, fo:fo + 128], rhs=HO[0][1][rows, pair, :], start=False, stop=False),
                         r=["Rim_b", ("HO", 0, 1)], w=[ky])
                    P.pe(lambda e, py=py, m=m, rows=rows, bo=bo, pair=pair: e.matmul(out=py[:, m, :], lhsT=Rre_b[rows, 1, bo:bo + 128], rhs=HO[1][0][rows, pair, :], start=False, stop=False),
                         r=["Rre_b", ("HO", 1, 0)], w=[ky])
                    P.pe(lambda e, py=py, m=m, rows=rows, bo=bo, pair=pair: e.matmul(out=py[:, m, :], lhsT=Rim_b[rows, 1, bo:bo + 128], rhs=HO[1][1][rows, pair, :], start=False, stop=True),
                         r=["Rim_b", ("HO", 1, 1)], w=[ky])
                ye = Ye[g % 2]
                P.dve(lambda e, py=py, ye=ye: e.tensor_tensor(out=ye[:].rearrange("p (j m s) -> p j m s", m=8, s=8), in0=fv(py[:], 0, [[1, 32], [32, 8], [0, 8]]),
                                                              in1=fv(m8c_b[:], 0, [[0, 32], [0, 8], [1, 8]]), op=ALU.mult), r=[ky, "m8c_b"], w=["Ye"])
                for c4 in range(4):
                    P.pe(lambda e, ye=ye, g=g, c4=c4: e.matmul(out=pYT[:, c4 * 512:(c4 + 1) * 512], lhsT=selg_b[:, g % 8, :], rhs=ye[:, c4 * 512:(c4 + 1) * 512],
                                                              start=(g % 8 == 0), stop=(g % 8 == 7)), r=["Ye", "selg_b"], w=["pYT"])
            if pair % 4 == 3:
                tile_ = pair // 4
                for hf in range(2):
                    cs = slice(hf * 1024, (hf + 1) * 1024)
                    P.act(lambda e, cs=cs: e.activation(out=ysb[:], in_=pYT[:, cs], func=ACT.Copy), r=["pYT"], w=["ysb"])
                    P.dve(lambda e: e.tensor_tensor(out=yx2[:], in0=ysb[:], in1=ysb[:], op=ALU.mult), r=["ysb"], w=["yx2"])
                    P.dve(lambda e: e.tensor_scalar(out=yx2[:], in0=yx2[:], scalar1=0.044715, scalar2=1.0, op0=ALU.mult, op1=ALU.add), r=["yx2"], w=["yx2"])
                    P.dve(lambda e: e.tensor_tensor(out=yx2[:], in0=yx2[:], in1=ysb[:], op=ALU.mult), r=["yx2", "ysb"], w=["yx2"])
                    P.act(lambda e: e.activation(out=yx2[:], in_=yx2[:], func=ACT.Sigmoid, scale=1.5957691216057308), r=["yx2"], w=["yx2"])
                    P.dve(lambda e, tile_=tile_, cs=cs: e.tensor_tensor(out=gT[:, tile_, cs], in0=ysb[:], in1=yx2[:], op=ALU.mult), r=["ysb", "yx2"], w=["gT"])
        P.barrier()
    esU.close()
    esS.close()
    if "gT" in dbg_names:
        dump("gT", gT[:], [128, 4, NOWN], BF16)
    if STOP <= 4:
        esP.close()
        return finish(nc, P, ges, out, dbg_out)

    oT = qT
    NKC = NFULL // 128
    SCALE = 128.0 ** -0.5
    with ExitStack() as es:
        kT_all = SB(es, "kT_all", [128, 2, NFULL], BF16)
        V_aug = SB(es, "V_aug", [128, NKC, 2, 132], BF16)
        pTt = [SB(es, "pTt%d" % i, [128, 512], BF16) for i in range(3)]
        rden = SB(es, "rden", [128, 4], F32)
        on_b = SB(es, "on_b", [128, 4, 128], BF16)
        pS = [PS(es, "pS%d" % i, [128, 512], F32) for i in range(2)]
        pO = [PS(es, "pO%d" % i, [128, 512], F32) for i in range(4)]
        pOT = PS(es, "pOT", [128, 4, 128], BF16)
        P.dve(lambda e: e.memset(V_aug[:], 1.0), w=["V_aug"])
        for h2 in range(2):
            P.dma(lambda e, h2=h2: e.dma_start(out=kT_all[:, h2, :], in_=kT_d[h2]), key=("kT_all", h2), r=["kT_d"], w=["kT_all"])
            P.dma(lambda e, h2=h2: e.dma_start(out=V_aug[:, :, h2, 0:128], in_=v_d[:, h2 * 128:(h2 + 1) * 128].rearrange("(c p) d -> p c d", p=128)),
                  key=("V_aug", h2), r=["v_d", "V_aug"], w=["V_aug"])
        iters = [(h, qc, kc) for h in range(8) for qc in range(4) for kc in range(NKC)]

        def emit_S(i):
            h, qc, kc = iters[i]
            kvh = h // 4
            s2 = i % 2
            qs = slice(qc * 512, (qc + 1) * 512)
            P.pe(lambda e, s2=s2, kvh=kvh, kc=kc, h=h, qs=qs: e.matmul(out=pS[s2][:], lhsT=kT_all[:, kvh, kc * 128:(kc + 1) * 128], rhs=qT[:, h, qs], start=True, stop=True),
                 r=["kT_all", ("qTc", h, qc)], w=[("pS", s2)])

        emit_S(0)
        for i, (h, qc, kc) in enumerate(iters):
            kvh = h // 4
            s2, s3 = i % 2, i % 3
            qs = slice(qc * 512, (qc + 1) * 512)
            if i + 1 < len(iters):
                emit_S(i + 1)
            P.act(lambda e, s2=s2, s3=s3: e.activation(out=pTt[s3][:], in_=pS[s2][:], func=ACT.Exp, bias=negC[:], scale=SCALE),
                  r=[("pS", s2), "negC"], w=[("pTt", s3)])
            for qi in range(4):
                P.pe(lambda e, s3=s3, qi=qi, kc=kc, kvh=kvh: e.matmul(out=pO[qi][:, 0:129], lhsT=pTt[s3][:, qi * 128:(qi + 1) * 128], rhs=V_aug[:, kc, kvh, 0:129],
                                                                     start=(kc == 0), stop=(kc == NKC - 1)), r=[("pTt", s3), "V_aug"], w=[("pO", qi)])
            if kc == NKC - 1:
                for qi in range(4):
                    P.dve(lambda e, qi=qi: e.reciprocal(out=rden[:, qi:qi + 1], in_=pO[qi][:, 128:129]), r=[("pO", qi)], w=[("rden", qi)])
                    P.dve(lambda e, qi=qi: e.tensor_scalar(out=on_b[:, qi, :], in0=pO[qi][:, 0:128], scalar1=rden[:, qi:qi + 1], scalar2=None, op0=ALU.mult),
                          r=[("pO", qi), ("rden", qi)], w=[("on_b", qi)])
                    P.pe(lambda e, qi=qi: e.transpose(out=pOT[:, qi, :], in_=on_b[:, qi, :], identity=ident_b[:]), r=[("on_b", qi), "ident_b"], w=["pOT"])
                P.dve(lambda e, h=h, qs=qs: e.tensor_copy(out=oT[:, h, qs], in_=pOT[:].rearrange("p a b -> p (a b)")), r=["pOT"], w=[("qTc", h, qc)])
        P.barrier()
    if "oT" in dbg_names:
        dump("oT", oT[:], [128, 8, NOWN], BF16)
    if STOP <= 5:
        esP.close()
        return finish(nc, P, ges, out, dbg_out)

    esM = ExitStack()
    mT_all = SB(esM, "mT_all", [128, 8, NOWN], BF16)
    with ExitStack() as es:
        Wg = SB(es, "Wg", [128, 8, 2048], BF16)
        Wa = SB(es, "Wa", [128, 4, 1024], BF16)
        Wb = SB(es, "Wb", [128, 4, 1024], BF16)
        Wo = SB(es, "Wo", [128, 8, 1024], BF16)
        sg1 = SB(es, "sg1", [128, 512], F32)
        sg2 = SB(es, "sg2", [128, 512], F32)
        sgb = SB(es, "sgb", [128, 512], F32)
        mt1 = SB(es, "mt1", [128, 512], F32)
        mt2 = SB(es, "mt2", [128, 512], F32)
        pG1 = PS(es, "pG1", [128, 512], F32)
        pG2 = PS(es, "pG2", [128, 512], F32)
        pA_D = PS(es, "pA_D", [128, 512], F32)
        pB_D = PS(es, "pB_D", [128, 512], F32)
        pC_D = PS(es, "pC_D", [128, 512], F32)
        for c2 in range(2):
            P.dma(lambda e, c2=c2: e.dma_start(out=Wg[:, :, c2 * 1024:(c2 + 1) * 1024], in_=w_in[:, 2048 + c2 * 1024:2048 + (c2 + 1) * 1024].rearrange("(kc p) n -> p kc n", p=128)),
                  key=("Wg", c2), w=["Wg"], eng="pool")
        P.dma(lambda e: e.dma_start(out=Wa[:], in_=w_glu_a.rearrange("(kc p) n -> p kc n", p=128)), key="Wa", w=["Wa"], eng="pool")
        P.dma(lambda e: e.dma_start(out=Wb[:], in_=w_glu_b.rearrange("(kc p) n -> p kc n", p=128)), key="Wb", w=["Wb"], eng="pool")
        P.dma(lambda e: e.dma_start(out=Wo[:], in_=w_attn_o.rearrange("(kc p) n -> p kc n", p=128)), key="Wo", w=["Wo"], eng="pool")
        for st_ in range(4):
            ts = slice(st_ * 512, (st_ + 1) * 512)
            for dt_ in range(8):
                ds = slice(dt_ * 128, (dt_ + 1) * 128)
                ds2 = slice(1024 + dt_ * 128, 1024 + (dt_ + 1) * 128)
                for kc in range(8):
                    P.pe(lambda e, kc=kc, ds=ds, ts=ts: e.matmul(out=pG1[:], lhsT=Wg[:, kc, ds], rhs=hTo[:, kc, ts], start=(kc == 0), stop=(kc == 7)), r=["Wg", "hTo"], w=["pG1"])
                for kc in range(8):
                    P.pe(lambda e, kc=kc, ds2=ds2, ts=ts: e.matmul(out=pG2[:], lhsT=Wg[:, kc, ds2], rhs=hTo[:, kc, ts], start=(kc == 0), stop=(kc == 7)), r=["Wg", "hTo"], w=["pG2"])
                for c in range(4):
                    P.pe(lambda e, c=c, ds=ds, ts=ts: e.matmul(out=pA_D[:], lhsT=Wa[:, c, ds], rhs=gT[:, c, ts], start=(c == 0), stop=(c == 3)), r=["Wa", "gT"], w=["pA_D"])
                for c in range(4):
                    P.pe(lambda e, c=c, ds=ds, ts=ts: e.matmul(out=pB_D[:], lhsT=Wb[:, c, ds], rhs=gT[:, c, ts], start=(c == 0), stop=(c == 3)), r=["Wb", "gT"], w=["pB_D"])
                for hh in range(8):
                    P.pe(lambda e, hh=hh, ds=ds, ts=ts: e.matmul(out=pC_D[:], lhsT=Wo[:, hh, ds], rhs=oT[:, hh, ts], start=(hh == 0), stop=(hh == 7)), r=["Wo", "oT"], w=["pC_D"])
                P.act(lambda e: e.activation(out=sg1[:], in_=pG1[:], func=ACT.Sigmoid), r=["pG1"], w=["sg1"])
                P.act(lambda e: e.activation(out=sg2[:], in_=pG2[:], func=ACT.Sigmoid), r=["pG2"], w=["sg2"])
                P.act(lambda e: e.activation(out=sgb[:], in_=pB_D[:], func=ACT.Sigmoid), r=["pB_D"], w=["sgb"])
                P.dve(lambda e: e.tensor_tensor(out=mt1[:], in0=pA_D[:], in1=sgb[:], op=ALU.mult), r=["pA_D", "sgb"], w=["mt1"])
                P.dve(lambda e: e.tensor_tensor(out=mt1[:], in0=mt1[:], in1=sg1[:], op=ALU.mult), r=["mt1", "sg1"], w=["mt1"])
                P.dve(lambda e: e.tensor_tensor(out=mt2[:], in0=pC_D[:], in1=sg2[:], op=ALU.mult), r=["pC_D", "sg2"], w=["mt2"])
                P.dve(lambda e, dt_=dt_, ts=ts: e.tensor_tensor(out=mT_all[:, dt_, ts], in0=mt1[:], in1=mt2[:], op=ALU.add), r=["mt1", "mt2"], w=["mT_all"])
        P.barrier()
    esP.close()
    if "mT" in dbg_names:
        dump("mT", mT_all[:], [128, 8, NOWN], BF16)
    if STOP <= 6:
        esM.close()
        return finish(nc, P, ges, out, dbg_out)

    esE = ExitStack()
    h2T = SB(esE, "h2T", [128, 8, NOWN], BF16)
    combT = SB(esE, "combT", [32, NOWN], BF16)
    with ExitStack() as es:
        Wout = SB(es, "Wout", [128, 8, 1024], BF16)
        wrt_sb = SB(es, "wrt_sb", [128, 8, 36], F32)
        brt_bc = SB(es, "brt_bc", [128, 36], F32)
        g1_bc = SB(es, "g1_bc", [128, D], F32)
        l1g_bc = SB(es, "l1g_bc", [128, D], F32)
        l1b_bc = SB(es, "l1b_bc", [128, D], F32)
        sc2p_bc = SB(es, "sc2p_bc", [128, D], F32)
        sh2_bc = SB(es, "sh2_bc", [128, D], F32)
        xt_D = [SB(es, "xt_D%d" % i, [128, D], F32) for i in range(2)]
        zt_D = SB(es, "zt_D", [128, D], F32)
        zn_D = SB(es, "zn_D", [128, D], F32)
        x1_D = [SB(es, "x1_D%d" % i, [128, D], F32) for i in range(2)]
        h2_D = SB(es, "h2_D", [128, D], F32)
        h2b_D = SB(es, "h2b_D", [128, D], BF16)
        h2Tf = SB(es, "h2Tf", [128, 8, 128], F32)
        st_D = SB(es, "st_D", [128, 2, 6], F32)
        mv_D = SB(es, "mv_D", [128, 2], F32)
        rstd_D = SB(es, "rstd_D", [128, 1], F32)
        nb_D = SB(es, "nb_D", [128, 1], F32)
        st_D2 = SB(es, "st_D2", [128, 2, 6], F32)
        mv_D2 = SB(es, "mv_D2", [128, 2], F32)
        rstd_D2 = SB(es, "rstd_D2", [128, 1], F32)
        nb_D2 = SB(es, "nb_D2", [128, 1], F32)
        L_D = SB(es, "L_D", [128, 36], F32)
        rs = SB(es, "rs", [128, 16], F32)
        ohg = SB(es, "ohg", [128, 4], F32)
        gex = SB(es, "gex", [128, 4], F32)
        msk = SB(es, "msk", [128, 32], F32)
        ein = SB(es, "ein", [128, 8], F32)
        e2_ = SB(es, "e2_", [128, 8], F32)
        oh1 = SB(es, "oh1", [128, 8], F32)
        oh2 = SB(es, "oh2", [128, 8], F32)
        cg = SB(es, "cg", [128, 8], F32)
        comb = SB(es, "comb", [128, 32], F32)
        pMix = [PS(es, "pMix%d" % i, [128, 512], F32) for i in range(2)]
        pT_D = PS(es, "pT_D", [128, 8, 128], BF16)
        pTf = PS(es, "pTf", [128, 4, 128], F32)
        pR = PS(es, "pR", [128, 36], F32)
        pCT = PS(es, "pCT", [32, 128], F32)
        P.dma(lambda e: e.dma_start(out=Wout[:], in_=w_out.rearrange("(kc p) n -> p kc n", p=128)), key="Wout", w=["Wout"], eng="pool")
        P.dma(lambda e: e.dma_start(out=wrt_sb[:], in_=w_rt.rearrange("(kc p) n -> p kc n", p=128)), key="wrt_sb", w=["wrt_sb"])
        load_bc(brt_bc, b_rt, "brt_bc")
        load_bc(g1_bc, mod_d[0:1, 2048:3072], "g1_bc")
        load_bc(l1g_bc, ln1_g, "l1g_bc")
        load_bc(l1b_bc, ln1_b, "l1b_bc")
        load_bc(sc2p_bc, mod_d[0:1, 4096:5120], "sc2p_bc", True)
        load_bc(sh2_bc, mod_d[0:1, 3072:4096], "sh2_bc")
        sD0, sD1, sD2 = [None] * NTO, [None] * NTO, [None] * NTO
        for tt in range(NTO):
            s2 = tt % 2
            t0 = tt * 128
            tsl_ = slice(t0, t0 + 128)
            P.capture()
            P.dma(lambda e, s2=s2, t0=t0: e.dma_start(out=xt_D[s2][:], in_=xo[t0:t0 + 128, :]), key=("xt_D", s2), w=[("xt_D", s2)])
            for half in range(2):
                hs = slice(half * 512, (half + 1) * 512)
                for kc in range(8):
                    P.pe(lambda e, kc=kc, half=half, hs=hs, tsl_=tsl_: e.matmul(out=pMix[half][:], lhsT=mT_all[:, kc, tsl_], rhs=Wout[:, kc, hs], start=(kc == 0), stop=(kc == 7)),
                         r=["mT_all", "Wout"], w=[("pMix", half)])
                P.dve(lambda e, half=half, hs=hs: e.tensor_tensor(out=zt_D[:, hs], in0=pMix[half][:], in1=g1_bc[:, hs], op=ALU.mult), r=[("pMix", half), "g1_bc"], w=["zt_D"])
            P.dve(lambda e, s2=s2: e.scalar_tensor_tensor(out=zt_D[:], in0=xt_D[s2][:], scalar=ALPHA, in1=zt_D[:], op0=ALU.mult, op1=ALU.add), r=["zt_D", ("xt_D", s2)], w=["zt_D"])
            ln_tile(zt_D[:], "zt_D", st_D, mv_D, rstd_D, nb_D, zn_D[:], "zn_D", "D1", mode=LNM_D)
            P.dve(lambda e: e.tensor_tensor(out=zn_D[:], in0=zn_D[:], in1=l1g_bc[:], op=ALU.mult), r=["zn_D", "l1g_bc"], w=["zn_D"])
            P.dve(lambda e, s2=s2: e.tensor_tensor(out=x1_D[s2][:], in0=zn_D[:], in1=l1b_bc[:], op=ALU.add), r=["zn_D", "l1b_bc"], w=[("x1_D", s2)])
            P.dma(lambda e, s2=s2, t0=t0: e.dma_start(out=x1_d[t0:t0 + 128, :], in_=x1_D[s2][:]), key=("x1d", s2), r=[("x1_D", s2)], w=["x1_d"])
            sD0[tt] = P.end_capture()
            P.capture()
            ln_tile(x1_D[s2][:], ("x1_D", s2), st_D2, mv_D2, rstd_D2, nb_D2, h2_D[:], "h2_D", "D2", mode=LNM_D)
            P.dve(lambda e: e.tensor_tensor(out=h2_D[:], in0=h2_D[:], in1=sc2p_bc[:], op=ALU.mult), r=["h2_D", "sc2p_bc"], w=["h2_D"])
            P.dve(lambda e: e.tensor_tensor(out=h2_D[:], in0=h2_D[:], in1=sh2_bc[:], op=ALU.add), r=["h2_D", "sh2_bc"], w=["h2_D"])
            P.act(lambda e: e.activation(out=h2b_D[:], in_=h2_D[:], func=ACT.Copy), r=["h2_D"], w=["h2b_D"])
            for kc in range(8):
                P.pe(lambda e, kc=kc: e.transpose(out=pT_D[:, kc, :], in_=h2b_D[:, kc * 128:(kc + 1) * 128], identity=ident_b[:]), r=["h2b_D", "ident_b"], w=["pT_D"])
            P.act(lambda e, tsl_=tsl_: e.activation(out=h2T[:, :, tsl_], in_=pT_D[:], func=ACT.Copy), r=["pT_D"], w=[("h2T", tt)])
            for q4 in range(2):
                for j in range(4):
                    kc = q4 * 4 + j
                    P.pe(lambda e, kc=kc, j=j: e.transpose(out=pTf[:, j, :], in_=h2_D[:, kc * 128:(kc + 1) * 128], identity=ident_f[:]), r=["h2_D", "ident_f"], w=["pTf"])
                P.dve(lambda e, q4=q4: e.tensor_copy(out=h2Tf[:, q4 * 4:(q4 + 1) * 4, :], in_=pTf[:]), r=["pTf"], w=["h2Tf"])
            for kc in range(8):
                P.pe(lambda e, kc=kc: e.matmul(out=pR[:], lhsT=h2Tf[:, kc, :], rhs=wrt_sb[:, kc, :], start=(kc == 0), stop=(kc == 7)), r=["h2Tf", "wrt_sb"], w=["pR"])
            P.dve(lambda e: e.tensor_tensor(out=L_D[:], in0=pR[:], in1=brt_bc[:], op=ALU.add), r=["pR", "brt_bc"], w=["L_D"])
            sD1[tt] = P.end_capture()
            P.capture()
            R_ = lambda i: rs[:, i:i + 1]
            kR = lambda i: ("rs", i)
            P.dve(lambda e: e.tensor_reduce(out=R_(0), in_=L_D[:, 0:4], axis=AX.X, op=ALU.max), r=["L_D"], w=[kR(0)])
            P.dve(lambda e: e.tensor_scalar(out=ohg[:], in0=L_D[:, 0:4], scalar1=R_(0), scalar2=None, op0=ALU.is_equal), r=["L_D", kR(0)], w=["ohg"])
            P.dve(lambda e: e.tensor_scalar(out=R_(1), in0=R_(0), scalar1=-1.0, scalar2=None, op0=ALU.mult), r=[kR(0)], w=[kR(1)])
            P.act(lambda e: e.activation(out=gex[:], in_=L_D[:, 0:4], func=ACT.Exp, bias=R_(1), scale=1.0), r=["L_D", kR(1)], w=["gex"])
            P.dve(lambda e: e.tensor_reduce(out=R_(2), in_=gex[:], axis=AX.X, op=ALU.add), r=["gex"], w=[kR(2)])
            P.dve(lambda e: e.reciprocal(out=R_(2), in_=R_(2)), r=[kR(2)], w=[kR(2)])
            P.dve(lambda e: e.tensor_tensor(out=msk[:].rearrange("p (g x) -> p g x", x=8), in0=L_D[:, 4:36].rearrange("p (g x) -> p g x", x=8),
                                            in1=fv(ohg[:], 0, [[1, 4], [0, 8]]), op=ALU.mult), r=["L_D", "ohg"], w=["msk"])
            P.dve(lambda e: e.tensor_reduce(out=ein[:], in_=fv(msk[:], 0, [[1, 8], [8, 4]]), axis=AX.X, op=ALU.add), r=["msk"], w=["ein"])
            P.dve(lambda e: e.tensor_reduce(out=R_(3), in_=ein[:], axis=AX.X, op=ALU.max), r=["ein"], w=[kR(3)])
            P.dve(lambda e: e.tensor_scalar(out=oh1[:], in0=ein[:], scalar1=R_(3), scalar2=None, op0=ALU.is_equal), r=["ein", kR(3)], w=["oh1"])
            P.dve(lambda e: e.scalar_tensor_tensor(out=e2_[:], in0=oh1[:], scalar=-1e30, in1=ein[:], op0=ALU.mult, op1=ALU.add), r=["oh1", "ein"], w=["e2_"])
            P.dve(lambda e: e.tensor_reduce(out=R_(4), in_=e2_[:], axis=AX.X, op=ALU.max), r=["e2_"], w=[kR(4)])
            P.dve(lambda e: e.tensor_scalar(out=oh2[:], in0=e2_[:], scalar1=R_(4), scalar2=None, op0=ALU.is_equal), r=["e2_", kR(4)], w=["oh2"])
            P.dve(lambda e: e.tensor_tensor(out=R_(5), in0=R_(4), in1=R_(3), op=ALU.subtract), r=[kR(3), kR(4)], w=[kR(5)])
            P.act(lambda e: e.activation(out=R_(6), in_=R_(5), func=ACT.Exp), r=[kR(5)], w=[kR(6)])
            P.dve(lambda e: e.tensor_scalar(out=R_(7), in0=R_(6), scalar1=1.0, scalar2=None, op0=ALU.add), r=[kR(6)], w=[kR(7)])
            P.dve(lambda e: e.reciprocal(out=R_(7), in_=R_(7)), r=[kR(7)], w=[kR(7)])
            P.dve(lambda e: e.tensor_tensor(out=R_(8), in0=R_(6), in1=R_(7), op=ALU.mult), r=[kR(6), kR(7)], w=[kR(8)])
            P.dve(lambda e: e.tensor_tensor(out=R_(7), in0=R_(7), in1=R_(2), op=ALU.mult), r=[kR(7), kR(2)], w=[kR(7)])
            P.dve(lambda e: e.tensor_tensor(out=R_(8), in0=R_(8), in1=R_(2), op=ALU.mult), r=[kR(8), kR(2)], w=[kR(8)])
            P.dve(lambda e: e.tensor_scalar(out=cg[:], in0=oh1[:], scalar1=R_(7), scalar2=None, op0=ALU.mult), r=["oh1", kR(7)], w=["cg"])
            P.dve(lambda e: e.scalar_tensor_tensor(out=cg[:], in0=oh2[:], scalar=R_(8), in1=cg[:], op0=ALU.mult, op1=ALU.add), r=["oh2", kR(8), "cg"], w=["cg"])
            P.dve(lambda e: e.tensor_tensor(out=comb[:].rearrange("p (g x) -> p g x", x=8), in0=fv(cg[:], 0, [[0, 4], [1, 8]]), in1=fv(ohg[:], 0, [[1, 4], [0, 8]]), op=ALU.mult),
                  r=["cg", "ohg"], w=["comb"])
            P.pe(lambda e: e.transpose(out=pCT[:], in_=comb[:], identity=ident_f[:]), r=["comb", "ident_f"], w=["pCT"])
            P.dve(lambda e, tsl_=tsl_, tt=tt: e.tensor_copy(out=combT[:, tsl_], in_=pCT[:]), r=["pCT"], w=[("combT", tt)])
            sD2[tt] = P.end_capture()
        if int(os.environ.get('K_PIPED', '1')) == 0:
            for tt in range(NTO):
                for lst in (sD0, sD1, sD2):
                    P.ops.extend(lst[tt])
        else:
            for step in range(NTO + 2):
                for lst, off in ((sD0, 0), (sD2, 2), (sD1, 1)):
                    j = step - off
                    if 0 <= j < NTO:
                        P.ops.extend(lst[j])
        P.barrier()
    esM.close()
    if "x1" in dbg_names:
        o_ = nc.dram_tensor("dbg_x1", [NOWN, D], F32, kind="ExternalOutput").ap()
        P.dma(lambda e: e.dma_start(out=o_, in_=x1_d), key="dbg_x1", r=["x1_d"], w=["dbg_x1"])
    if "combT" in dbg_names:
        dump("combT", combT[:], [32, NOWN], BF16)
    if STOP <= 7:
        esE.close()
        return finish(nc, P, ges, out, dbg_out)

    yacc = SB(esE, "yacc", [128, NTO, D], F32)
    with ExitStack() as es:
        Weg = [SB(es, "Weg%d" % i, [128, 8, 512], BF16) for i in range(2)]
        Weu = [SB(es, "Weu%d" % i, [128, 8, 512], BF16) for i in range(2)]
        Wed = [SB(es, "Wed%d" % i, [128, 4, 1024], BF16) for i in range(2)]
        sele_b = SB(es, "sele_b", [32, 32, 128], BF16)
        bc_sb = SB(es, "bc_sb", [128, 512], F32)
        sa_E = [SB(es, "sa_E%d" % i, [128, 512], F32) for i in range(2)]
        actT = [SB(es, "actT%d" % i, [128, 4, 512], BF16) for i in range(2)]
        pBC = PS(es, "pBC", [128, 512], F32)
        pA_E = [PS(es, "pA_E%d" % i, [128, 512], F32) for i in range(2)]
        pB_E = [PS(es, "pB_E%d" % i, [128, 512], F32) for i in range(2)]
        pY_E = [PS(es, "pY_E%d" % i, [128, 512], F32) for i in range(2)]
        P.dma(lambda e: e.dma_start(out=sele_b[:], in_=cst_sele), key="sele_b", w=["sele_b"], eng="pool")
        NEXP = int(os.environ.get("K_NEXP", "32"))
        fci = 0
        yi = 0
        mG0, mG1, mD, mW = [], [], [], []
        for ex in range(NEXP):
            se = ex % 2
            P.capture()
            P.dma(lambda e, ex=ex, se=se: e.dma_start(out=Weg[se][:], in_=w_eg[ex].rearrange("(kc p) f -> p kc f", p=128)), key=("Weg", se), w=[("Weg", se)], eng="pool")
            P.dma(lambda e, ex=ex, se=se: e.dma_start(out=Weu[se][:], in_=w_eu[ex].rearrange("(kc p) f -> p kc f", p=128)), key=("Weu", se), w=[("Weu", se)], eng="pool")
            P.dma(lambda e, ex=ex, se=se: e.dma_start(out=Wed[se][:], in_=w_ed[ex].rearrange("(fc p) n -> p fc n", p=128)), key=("Wed", se), w=[("Wed", se)], eng="pool")
            mW.append(P.end_capture())
            for st_ in range(4):
                ts = slice(st_ * 512, (st_ + 1) * 512)
                sa_ = (ex * 4 + st_) % 2
                P.capture()
                P.pe(lambda e, ex=ex, ts=ts: e.matmul(out=pBC[:], lhsT=sele_b[:, ex, :], rhs=combT[:, ts], start=True, stop=True), r=["sele_b", "combT"], w=["pBC"])
                P.act(lambda e: e.activation(out=bc_sb[:], in_=pBC[:], func=ACT.Copy), r=["pBC"], w=["bc_sb"])
                for fc in range(4):
                    fs = slice(fc * 128, (fc + 1) * 128)
                    sp_ = fci % 2
                    for kc in range(8):
                        P.pe(lambda e, kc=kc, fs=fs, ts=ts, se=se, sp_=sp_: e.matmul(out=pA_E[sp_][:], lhsT=Weg[se][:, kc, fs], rhs=h2T[:, kc, ts], start=(kc == 0), stop=(kc == 7)),
                             r=[("Weg", se), "h2T"], w=[("pA_E", sp_)])
                    for kc in range(8):
                        P.pe(lambda e, kc=kc, fs=fs, ts=ts, se=se, sp_=sp_: e.matmul(out=pB_E[sp_][:], lhsT=Weu[se][:, kc, fs], rhs=h2T[:, kc, ts], start=(kc == 0), stop=(kc == 7)),
                             r=[("Weu", se), "h2T"], w=[("pB_E", sp_)])
                    P.act(lambda e, sp_=sp_: e.activation(out=sa_E[sp_][:], in_=pA_E[sp_][:], func=ACT.Silu), r=[("pA_E", sp_)], w=[("sa_E", sp_)])
                    P.dve(lambda e, sp_=sp_: e.tensor_tensor(out=sa_E[sp_][:], in0=sa_E[sp_][:], in1=pB_E[sp_][:], op=ALU.mult), r=[("sa_E", sp_), ("pB_E", sp_)], w=[("sa_E", sp_)])
                    P.dve(lambda e, sp_=sp_, sa_=sa_, fc=fc: e.tensor_tensor(out=actT[sa_][:, fc, :], in0=sa_E[sp_][:], in1=bc_sb[:], op=ALU.mult),
                          r=[("sa_E", sp_), "bc_sb"], w=[("actT", sa_)])
                    fci += 1
                    if fc == 0:
                        mG0.append(P.end_capture())
                        P.capture()
                mG1.append(P.end_capture())
                P.capture()
                for j in range(4):
                    tile_ = st_ * 4 + j
                    js = slice(j * 128, (j + 1) * 128)
                    for half in range(2):
                        hs = slice(half * 512, (half + 1) * 512)
                        sy = yi % 2
                        for fc in range(4):
                            P.pe(lambda e, fc=fc, js=js, hs=hs, sa_=sa_, se=se, sy=sy: e.matmul(out=pY_E[sy][:], lhsT=actT[sa_][:, fc, js], rhs=Wed[se][:, fc, hs], start=(fc == 0), stop=(fc == 3)),
                                 r=[("actT", sa_), ("Wed", se)], w=[("pY_E", sy)])
                        if ex == 0:
                            P.dve(lambda e, tile_=tile_, hs=hs, sy=sy: e.tensor_copy(out=yacc[:, tile_, hs], in_=pY_E[sy][:]), r=[("pY_E", sy)], w=[("yacc", tile_)])
                        else:
                            P.dve(lambda e, tile_=tile_, hs=hs, sy=sy: e.tensor_tensor(out=yacc[:, tile_, hs], in0=yacc[:, tile_, hs], in1=pY_E[sy][:], op=ALU.add),
                                  r=[("pY_E", sy), ("yacc", tile_)], w=[("yacc", tile_)])
                        yi += 1
                mD.append(P.end_capture())
        nst = len(mG0)
        for i in range(nst):
            if i % 4 == 0:
                P.ops.extend(mW[i // 4])
            P.ops.extend(mG0[i])
            if i > 0:
                P.ops.extend(mD[i - 1])
            P.ops.extend(mG1[i])
        P.ops.extend(mD[nst - 1])
        P.barrier()

    with ExitStack() as es:
        g2_bc = SB(es, "g2_bc", [128, D], F32)
        l2g_bc = SB(es, "l2g_bc", [128, D], F32)
        l2b_bc = SB(es, "l2b_bc", [128, D], F32)
        x1_F = [SB(es, "x1_F%d" % i, [128, D], F32) for i in range(2)]
        z_Fs = [SB(es, "z_F%d" % i, [128, D], F32) for i in range(2)]
        zn_F = SB(es, "zn_F", [128, D], F32)
        o_F = [SB(es, "o_F%d" % i, [128, D], F32) for i in range(2)]
        st_F = SB(es, "st_F", [128, 2, 6], F32)
        mv_F = SB(es, "mv_F", [128, 2], F32)
        rstd_Fs = [SB(es, "rstd_F%d" % i, [128, 1], F32) for i in range(2)]
        nb_Fs = [SB(es, "nb_F%d" % i, [128, 1], F32) for i in range(2)]
        load_bc(g2_bc, mod_d[0:1, 5120:6144], "g2_bc")
        load_bc(l2g_bc, ln2_g, "l2g_bc")
        load_bc(l2b_bc, ln2_b, "l2b_bc")
        sF0, sF1 = [None] * NTO, [None] * NTO
        for tt in range(NTO):
            s2 = tt % 2
            t0 = tt * 128
            z_F, rstd_F, nb_F = z_Fs[s2], rstd_Fs[s2], nb_Fs[s2]
            kz, sfx = ("z_F", s2), ("F", s2)
            P.capture()
            P.dma(lambda e, s2=s2, t0=t0: e.dma_start(out=x1_F[s2][:], in_=x1_d[t0:t0 + 128, :]), key=("x1_F", s2), r=["x1_d"], w=[("x1_F", s2)])
            P.dve(lambda e, tt=tt, z_F=z_F: e.tensor_tensor(out=z_F[:], in0=yacc[:, tt, :], in1=g2_bc[:], op=ALU.mult), r=[("yacc", tt), "g2_bc"], w=[kz])
            P.dve(lambda e, s2=s2, z_F=z_F: e.scalar_tensor_tensor(out=z_F[:], in0=x1_F[s2][:], scalar=ALPHA, in1=z_F[:], op0=ALU.mult, op1=ALU.add), r=[kz, ("x1_F", s2)], w=[kz])
            for i in range(2):
                P.dve(lambda e, i=i, z_F=z_F: e.bn_stats(out=st_F[:, i, :], in_=z_F[:, i * 512:(i + 1) * 512]), r=[kz], w=[("st", "F", i)])
            P.dve(lambda e: e.bn_aggr(out=mv_F[:], in_=st_F[:].rearrange("p a b -> p (a b)")), r=[("st", "F", 0), ("st", "F", 1)], w=[("mv", "F")])
            if LNM_A == "act":
                P.act(lambda e, rstd_F=rstd_F: e.activation(out=rstd_F[:], in_=mv_F[:, 1:2], func=ACT.Sqrt, bias=eps_t[:], scale=1.0), r=[("mv", "F"), "eps_t"], w=[("rstd", sfx)])
                P.dve(lambda e, rstd_F=rstd_F: e.reciprocal(out=rstd_F[:], in_=rstd_F[:]), r=[("rstd", sfx)], w=[("rstd", sfx)])
            else:
                P.dve(lambda e, rstd_F=rstd_F: e.tensor_scalar(out=rstd_F[:], in0=mv_F[:, 1:2], scalar1=EPS, scalar2=None, op0=ALU.add), r=[("mv", "F")], w=[("rstd", sfx)])
                P.dve(lambda e, rstd_F=rstd_F: e.tensor_scalar(out=rstd_F[:], in0=rstd_F[:], scalar1=-0.5, scalar2=None, op0=ALU.pow), r=[("rstd", sfx)], w=[("rstd", sfx)])
            P.dve(lambda e, rstd_F=rstd_F, nb_F=nb_F: e.scalar_tensor_tensor(out=nb_F[:], in0=mv_F[:, 0:1], scalar=-1.0, in1=rstd_F[:], op0=ALU.mult, op1=ALU.mult),
                  r=[("mv", "F"), ("rstd", sfx)], w=[("nb", sfx)])
            sF0[tt] = P.end_capture()
            P.capture()
            P.act(lambda e, z_F=z_F, rstd_F=rstd_F, nb_F=nb_F: e.activation(out=zn_F[:], in_=z_F[:], func=ACT.Identity, bias=nb_F[:], scale=rstd_F[:]),
                  r=[kz, ("nb", sfx), ("rstd", sfx)], w=["zn_F"])
            P.dve(lambda e: e.tensor_tensor(out=zn_F[:], in0=zn_F[:], in1=l2g_bc[:], op=ALU.mult), r=["zn_F", "l2g_bc"], w=["zn_F"])
            P.dve(lambda e, s2=s2: e.tensor_tensor(out=o_F[s2][:], in0=zn_F[:], in1=l2b_bc[:], op=ALU.add), r=["zn_F", "l2b_bc"], w=[("o_F", s2)])
            P.dma(lambda e, s2=s2, t0=t0: e.dma_start(out=out[t0:t0 + 128, :], in_=o_F[s2][:]), key=("outd", s2), r=[("o_F", s2)], w=["out"])
            sF1[tt] = P.end_capture()
        for step in range(NTO + 1):
            for lst, off in ((sF0, 0), (sF1, 1)):
                j = step - off
                if 0 <= j < NTO:
                    P.ops.extend(lst[j])
        P.barrier()
    esE.close()
    return finish(nc, P, ges, out, dbg_out)


def finish(nc, P, ges, out, dbg_out):
    P.barrier()
    P.emit()
    ges.close()
    nc._dbg_out = dbg_out
    nc._stats = P.stats
    return nc


def rope_tables():
    rows = NLAT // 64
    row = np.repeat(np.arange(rows, dtype=np.float32), 64)
    col = np.tile(np.arange(64, dtype=np.float32), rows)
    inv = (np.float32(10000.0) ** (-np.arange(0, 64, 2, dtype=np.float32) / np.float32(64))).astype(np.float32)
    ang = np.stack([row[:, None] * inv, col[:, None] * inv], axis=1).astype(np.float32)
    tab = np.concatenate([np.cos(ang).reshape(NLAT, 64), np.sin(ang).reshape(NLAT, 64)], axis=1).astype(np.float32)
    return tab


def make_in_maps(inp):
    f32 = np.float32
    g = lambda k: np.asarray(inp[k], dtype=f32)
    x, c, ctx, c_ctx = g("x"), g("c"), g("ctx"), g("c_ctx")
    tab = rope_tables()
    tab_ctx = np.concatenate([np.ones((NCTX, 64), f32), np.zeros((NCTX, 64), f32)], axis=1)
    rope_full = np.concatenate([tab_ctx, tab], axis=0)
    tok = np.arange(128)
    mask8 = (tok[:, None] % 8 == np.arange(8)[None, :]).astype(f32)
    sel16 = (tok[:, None] // 8 == np.arange(16)[None, :]).astype(f32)
    mask8c = (tok[:, None] // 16 == np.arange(8)[None, :]).astype(f32)

    def pairlay(a):
        sh = a.shape
        a = a.reshape((2, 16, 2, 64) + sh[3:])
        perm = (2, 3, 0, 1) + tuple(range(4, a.ndim))
        a = a.transpose(perm)
        return np.ascontiguousarray(a.reshape((128, 2, 16) + sh[3:]))

    a_re, a_im = g("s5_a_re")[0], g("s5_a_im")[0]
    s5_a = np.stack([pairlay(a_re), pairlay(a_im)], axis=1)
    ldt = g("s5_log_dt")[0]
    s5_ldt = np.ascontiguousarray(np.broadcast_to(ldt.reshape(1, 2, 16, 2).transpose(0, 3, 1, 2), (64, 2, 2, 16)).transpose(1, 0, 2, 3).reshape(128, 2, 16))
    s5_b = np.stack([pairlay(g("s5_b_re")[0]), pairlay(g("s5_b_im")[0])], axis=1)
    cre = g("s5_c_re")[0].transpose(0, 1, 3, 2)
    cim = g("s5_c_im")[0].transpose(0, 1, 3, 2)
    s5_c = np.stack([pairlay(cre), pairlay(cim)], axis=1)
    dvec = g("s5_d")[0]
    s5_dcol = np.ascontiguousarray(np.broadcast_to(dvec.reshape(32, 16).T[None], (8, 16, 32)).reshape(128, 32))
    eZ = np.stack([63.0 - np.arange(64), np.arange(64)], axis=0).astype(f32)
    qs = np.arange(72)
    eRf = (qs - 7).astype(f32)
    eRb = (8 * (qs // 8 - 1) + 8 - (qs % 8)).astype(f32)
    eR = np.stack([eRf, eRb], axis=0)
    cst_eZ = np.ascontiguousarray(np.broadcast_to(eZ[None], (128, 2, 64)))
    cst_eR = np.ascontiguousarray(np.broadcast_to(eR[None], (128, 2, 72)))
    sidx = tok // 16
    cst_mf = (sidx[None, :] >= sidx[:, None]).astype(f32)
    cst_mb = (sidx[:, None] >= sidx[None, :]).astype(f32)
    selg = np.zeros((128, 8, 128), f32)
    for g8 in range(8):
        for co in range(16):
            selg[np.arange(8) * 16 + co, g8, g8 * 16 + co] = 1.0
    sele = np.zeros((32, 32, 128), f32)
    for e in range(32):
        sele[e, e, :] = 1.0
    w_rt = np.concatenate([g("w_router_group")[0], g("w_router_expert")[0]], axis=1)
    b_rt = np.concatenate([g("b_router_group")[0], g("b_router_expert")[0]], axis=0)[None]
    common = dict(
        w_mod=g("w_mod")[0], b_mod=g("b_mod"), w_in=g("w_in")[0], rope_f=rope_full,
        q_gain=g("q_gain"), k_gain=g("k_gain"), cst_mask8=mask8, cst_mask8c=mask8c, cst_sel16=sel16,
        s5_a=s5_a, s5_ldt=s5_ldt, s5_b=s5_b, s5_c=s5_c, s5_dcol=s5_dcol, cst_eZ=cst_eZ, cst_eR=cst_eR,
        cst_mf=cst_mf, cst_mb=cst_mb, cst_selg=selg,
        w_glu_a=g("w_glu_a")[0], w_glu_b=g("w_glu_b")[0], w_attn_o=g("w_attn_o")[0], w_out=g("w_out")[0],
        ln1_g=g("ln1_g"), ln1_b=g("ln1_b"), ln2_g=g("ln2_g"), ln2_b=g("ln2_b"),
        w_rt=w_rt, b_rt=b_rt, w_eg=g("w_exp_gate")[0], w_eu=g("w_exp_up")[0], w_ed=g("w_exp_down")[0],
        cst_sele=sele,
    )
    maps = []
    for core in range(8):
        b, r = core // 4, core % 4
        m = dict(common)
        m["xf"] = np.ascontiguousarray(np.concatenate([ctx[b], x[b]], axis=0))
        m["xo"] = np.ascontiguousarray(x[b, r * NOWN:(r + 1) * NOWN])
        cc = np.stack([c[b], c_ctx], axis=0)
        m["ccT"] = np.ascontiguousarray(cc.reshape(2, 8, 128).transpose(2, 1, 0))
        m["rope_o"] = np.ascontiguousarray(tab[r * NOWN:(r + 1) * NOWN])
        cm = np.zeros((128, 4), f32)
        cm[:, r] = 1.0
        m["cmask"] = cm
        maps.append(m)
    return maps


_NC_CACHE = {}


def kernel(**inputs):
    maps = make_in_maps(inputs)
    if "nc" not in _NC_CACHE:
        _NC_CACHE["nc"] = build()
    nc = _NC_CACHE["nc"]
    res = run_bass_kernel_spmd(nc, maps, core_ids=list(range(8)))
    outp = np.zeros((2, NLAT, D), np.float32)
    for core in range(8):
        b, r = core // 4, core % 4
        outp[b, r * NOWN:(r + 1) * NOWN] = res.results[core]["out"]
    return outp
```

```python
import os
import math
import numpy as np
from contextlib import ExitStack
import concourse.bass as bass
import concourse.mybir as mybir
from concourse.bass_utils import run_bass_kernel_spmd

F32 = mybir.dt.float32
BF16 = mybir.dt.bfloat16
ACT = mybir.ActivationFunctionType
ALU = mybir.AluOpType
AX = mybir.AxisListType

D = 1024
NLAT = 8192
NCTX = 256
NFULL = NLAT + NCTX
NOWN = 2048
NTF = NFULL // 128
NTO = NOWN // 128
NJ = NFULL // 64
NJF = NFULL // 8
EPS = 1e-6
ALPHA = 2.0 ** 0.25
STOP = int(os.environ.get("K_STOP", "99"))
LNM_A = os.environ.get("K_LNMA", "act")
LNM_D = os.environ.get("K_LNMD", "dve")
DEBUG = os.environ.get("K_DEBUG", "") != ""


class Prog:
    ENGS = ["pe", "act", "dve", "pool", "sp"]

    def __init__(self, nc):
        self.nc = nc
        self.ops = []

    def add(self, eng, fn, r=(), w=(), dma=None, ndma=1):
        self.ops.append(dict(eng=eng, fn=fn, r=tuple(r), w=tuple(w), dma=dma, ndma=ndma, barrier=False))

    def pe(self, fn, r=(), w=()):
        self.add("pe", fn, r, w)

    def act(self, fn, r=(), w=()):
        self.add("act", fn, r, w)

    def dve(self, fn, r=(), w=()):
        self.add("dve", fn, r, w)

    def pool(self, fn, r=(), w=()):
        self.add("pool", fn, r, w)

    def dma(self, fn, key, r=(), w=(), eng="sp", n=1):
        self.add(eng, fn, r, w, dma=key, ndma=n)

    def capture(self):
        self._saved = self.ops
        self.ops = []

    def end_capture(self):
        lst = self.ops
        self.ops = self._saved
        return lst

    def barrier(self):
        for e in self.ENGS:
            self.ops.append(dict(eng=e, fn=None, r=(), w=(), dma=None, ndma=0, barrier=True))

    def emit(self):
        nc = self.nc
        ops = self.ops
        n = len(ops)
        last_w, readers = {}, {}
        deps = [None] * n
        last_eng, last_dma = {}, {}
        for i, op in enumerate(ops):
            d = set()
            if op["barrier"]:
                for e, j in last_eng.items():
                    if e != op["eng"]:
                        d.add(j)
                for k, j in last_dma.items():
                    d.add(j)
            for b in op["r"]:
                if b in last_w:
                    d.add(last_w[b])
            for b in op["w"]:
                if b in last_w:
                    d.add(last_w[b])
                for j in readers.get(b, ()):
                    d.add(j)
            for b in op["r"]:
                readers.setdefault(b, []).append(i)
            for b in op["w"]:
                readers[b] = []
                last_w[b] = i
            d.discard(i)
            deps[i] = d
            if op["dma"] is not None:
                last_dma[op["dma"]] = i
            elif not op["barrier"]:
                last_eng[op["eng"]] = i
        signal = [False] * n
        for i, op in enumerate(ops):
            for j in deps[i]:
                pj = ops[j]
                if pj["dma"] is not None:
                    continue
                if pj["eng"] == "pe" and op["eng"] == "pe" and op["dma"] is None:
                    continue
                signal[j] = True
        tick = [0] * n
        cnt = {e: 0 for e in self.ENGS}
        dcnt = {}
        for i, op in enumerate(ops):
            if op["dma"] is not None:
                dcnt[op["dma"]] = dcnt.get(op["dma"], 0) + op["ndma"]
                tick[i] = dcnt[op["dma"]] * 16
            elif signal[i]:
                cnt[op["eng"]] += 1
                tick[i] = cnt[op["eng"]]
        es = ExitStack()
        esem = {e: es.enter_context(nc.semaphore("s_" + e)) for e in self.ENGS}
        dsem = {}
        for k in dcnt:
            dsem[k] = es.enter_context(nc.semaphore("d_%d" % len(dsem)))
        waits = [None] * n
        seen = {e: {} for e in self.ENGS}
        for i, op in enumerate(ops):
            wl = {}
            for j in deps[i]:
                pj = ops[j]
                if pj["dma"] is not None:
                    key = ("d", pj["dma"])
                    sem = dsem[pj["dma"]]
                else:
                    if pj["eng"] == "pe" and op["eng"] == "pe" and op["dma"] is None:
                        continue
                    key = ("e", pj["eng"])
                    sem = esem[pj["eng"]]
                v = tick[j]
                if seen[op["eng"]].get(key, 0) >= v:
                    continue
                if key not in wl or wl[key][1] < v:
                    wl[key] = (sem, v)
            for key, (sem, v) in wl.items():
                seen[op["eng"]][key] = v
            waits[i] = list(wl.values())
        self.stats = dict(n=n, sig=dict(cnt), dkeys=len(dcnt), nwaits=sum(len(w) for w in waits))
        block = es.enter_context(nc.Block())

        def run(engname, eng):
            for i, op in enumerate(ops):
                if op["eng"] != engname:
                    continue
                for sem, v in waits[i]:
                    eng.wait_ge(sem, v)
                if op["fn"] is None:
                    continue
                res = op["fn"](eng)
                if op["dma"] is not None:
                    if not isinstance(res, (list, tuple)):
                        res = [res]
                    assert len(res) == op["ndma"], (len(res), op["ndma"])
                    for ins in res:
                        ins.then_inc(dsem[op["dma"]], 16)
                elif signal[i]:
                    if isinstance(res, (list, tuple)):
                        res = res[-1]
                    res.then_inc(esem[engname], 1)

        @block.tensor
        def _(e):
            run("pe", e)

        @block.scalar
        def _(e):
            run("act", e)

        @block.vector
        def _(e):
            run("dve", e)

        @block.gpsimd
        def _(e):
            run("pool", e)

        @block.sync
        def _(e):
            run("sp", e)

        es.close()


def AP_(t, offset, dims):
    return bass.AP(t, offset, [list(d) for d in dims])


def pstride(t):
    return t[:].ap[0][0]


def build(dbg_names=()):
    nc = bass.Bass("TRN2", target_bir_lowering=False)
    dram_in = lambda name, shape, dt=F32: nc.dram_tensor(name, list(shape), dt, kind="ExternalInput").ap()
    xf = dram_in("xf", [NFULL, D])
    xo = dram_in("xo", [NOWN, D])
    ccT = dram_in("ccT", [128, 8, 2])
    w_mod = dram_in("w_mod", [D, 6 * D])
    b_mod = dram_in("b_mod", [1, 6 * D])
    w_in = dram_in("w_in", [D, 4096])
    rope_f = dram_in("rope_f", [NFULL, 128])
    rope_o = dram_in("rope_o", [NOWN, 128])
    q_gain = dram_in("q_gain", [1, 128])
    k_gain = dram_in("k_gain", [1, 128])
    cst_mask8 = dram_in("cst_mask8", [128, 8])
    cst_mask8c = dram_in("cst_mask8c", [128, 8])
    cst_sel16 = dram_in("cst_sel16", [128, 16])
    s5_a = dram_in("s5_a", [128, 2, 2, 16])
    s5_ldt = dram_in("s5_ldt", [128, 2, 16])
    s5_b = dram_in("s5_b", [128, 2, 2, 16, 16])
    s5_c = dram_in("s5_c", [128, 2, 2, 16, 16])
    s5_dcol = dram_in("s5_dcol", [128, 32])
    cst_eZ = dram_in("cst_eZ", [128, 2, 64])
    cst_eR = dram_in("cst_eR", [128, 2, 72])
    cst_mf = dram_in("cst_mf", [128, 128])
    cst_mb = dram_in("cst_mb", [128, 128])
    cst_selg = dram_in("cst_selg", [128, 8, 128])
    cmask = dram_in("cmask", [128, 4])
    w_glu_a = dram_in("w_glu_a", [512, D])
    w_glu_b = dram_in("w_glu_b", [512, D])
    w_attn_o = dram_in("w_attn_o", [D, D])
    w_out = dram_in("w_out", [D, D])
    ln1_g = dram_in("ln1_g", [1, D])
    ln1_b = dram_in("ln1_b", [1, D])
    ln2_g = dram_in("ln2_g", [1, D])
    ln2_b = dram_in("ln2_b", [1, D])
    w_rt = dram_in("w_rt", [D, 36])
    b_rt = dram_in("b_rt", [1, 36])
    w_eg = dram_in("w_eg", [32, D, 512])
    w_eu = dram_in("w_eu", [32, D, 512])
    w_ed = dram_in("w_ed", [32, 512, D])
    cst_sele = dram_in("cst_sele", [32, 32, 128])
    out = nc.dram_tensor("out", [NOWN, D], F32, kind="ExternalOutput").ap()
    scr = lambda name, shape, dt: nc.dram_tensor(name, list(shape), dt, kind="Internal").ap()
    mod_d = scr("mod_d", [2, 6 * D], F32)
    kT_d = scr("kT_d", [2, 128, NFULL], BF16)
    v_d = scr("v_d", [NFULL, 256], BF16)
    x1_d = scr("x1_d", [NOWN, D], F32)
    dbg_out = {}

    P = Prog(nc)
    ges = ExitStack()

    free_list = [[16640, 229376]]
    peak = [0]

    def _alloc(nbytes):
        nbytes = (nbytes + 63) // 64 * 64
        for iv in free_list:
            if iv[1] - iv[0] >= nbytes:
                off = iv[0]
                iv[0] += nbytes
                peak[0] = max(peak[0], off + nbytes)
                return off, nbytes
        raise RuntimeError("SBUF manual allocator out of space for %d bytes; free=%s" % (nbytes, free_list))

    def _free(off, nbytes):
        free_list.append([off, off + nbytes])
        free_list.sort()
        merged = []
        for iv in free_list:
            if iv[1] == iv[0]:
                continue
            if merged and merged[-1][1] == iv[0]:
                merged[-1][1] = iv[1]
            else:
                merged.append(iv)
        free_list[:] = merged

    def SB(es, name, shape, dt):
        esz = 4 if dt == F32 else 2
        nb = esz
        for s_ in shape[1:]:
            nb *= s_
        off, nbytes = _alloc(nb)
        t = nc.alloc_sbuf_tensor_at(name, list(shape), dt, offset=off)
        es.callback(_free, off, nbytes)
        return t

    def PS(es, name, shape, dt):
        return es.enter_context(nc.psum_tensor(name, list(shape), dt))

    def dump(name, t_ap, shape, dt=F32, r=()):
        if name not in dbg_names:
            return
        o = nc.dram_tensor("dbg_" + name, list(shape), dt, kind="ExternalOutput").ap()
        dbg_out[name] = o
        P.dma(lambda e: e.dma_start(out=o, in_=t_ap), key="dbg_" + name, r=r, w=["dbg_" + name])

    ident_f = SB(ges, "ident_f", [128, 128], F32)
    ident_b = SB(ges, "ident_b", [128, 128], BF16)
    ones_b = SB(ges, "ones_b", [1, 128], BF16)
    eps_t = SB(ges, "eps_t", [128, 1], F32)
    modT = SB(ges, "modT", [128, 48, 2], F32)
    op1p = SB(ges, "op1p", [128, 8, 2], F32)
    sh1T = SB(ges, "sh1T", [128, 8, 2], F32)
    gk_bc = SB(ges, "gk_bc", [128, 128], F32)
    gq_bc = SB(ges, "gq_bc", [128, 128], F32)
    negC = SB(ges, "negC", [128, 1], F32)
    mask8 = SB(ges, "mask8", [128, 8], BF16)
    mask8f = SB(ges, "mask8f", [128, 8], F32)
    sel16 = SB(ges, "sel16", [128, 16], BF16)

    P.pool(lambda e: e.memset(ident_f[:], 1.0), w=["ident_f"])
    P.pool(lambda e: e.affine_select(out=ident_f[:], in_=ident_f[:], pattern=[[-1, 128]], compare_op=ALU.is_equal,
                                     fill=0.0, base=0, channel_multiplier=1), r=["ident_f"], w=["ident_f"])
    P.dve(lambda e: e.tensor_copy(out=ident_b[:], in_=ident_f[:]), r=["ident_f"], w=["ident_b"])
    P.dve(lambda e: e.memset(ones_b[:], 1.0), w=["ones_b"])
    P.dve(lambda e: e.memset(eps_t[:], EPS), w=["eps_t"])
    P.dma(lambda e: e.dma_start(out=gk_bc[:], in_=k_gain.partition_broadcast(128)), key="gk_bc", w=["gk_bc"])
    P.dma(lambda e: e.dma_start(out=gq_bc[:], in_=q_gain.partition_broadcast(128)), key="gq_bc", w=["gq_bc"])

    with ExitStack() as es:
        scT = SB(es, "scT", [128, 8, 2], F32)
        ccs = SB(es, "ccs", [128, 8, 2], F32)
        wm = [SB(es, "wm%d" % i, [128, 8, 512], F32) for i in range(4)]
        bm = SB(es, "bm", [2, 6 * D], F32)
        mod_sb = SB(es, "mod_sb", [2, 6 * D], F32)
        m8f = SB(es, "m8f", [128, 8], F32)
        s16f = SB(es, "s16f", [128, 16], F32)
        tmpg = SB(es, "tmpg", [128, 2], F32)
        pM = [PS(es, "pM%d" % i, [2, 512], F32) for i in range(2)]
        pMT = PS(es, "pMT", [128, 48, 2], F32)
        P.dma(lambda e: e.dma_start(out=ccs[:], in_=ccT), key="ccs", w=["ccs"])
        P.dma(lambda e: e.dma_start(out=bm[:], in_=b_mod.partition_broadcast(2)), key="bm", w=["bm"])
        P.dma(lambda e: e.dma_start(out=m8f[:], in_=cst_mask8), key="m8f", w=["m8f"])
        P.dma(lambda e: e.dma_start(out=s16f[:], in_=cst_sel16), key="s16f", w=["s16f"])
        P.dve(lambda e: e.tensor_copy(out=mask8[:], in_=m8f[:]), r=["m8f"], w=["mask8"])
        P.dve(lambda e: e.tensor_copy(out=mask8f[:], in_=m8f[:]), r=["m8f"], w=["mask8f"])
        P.dve(lambda e: e.tensor_copy(out=sel16[:], in_=s16f[:]), r=["s16f"], w=["sel16"])
        P.act(lambda e: e.activation(out=scT[:], in_=ccs[:], func=ACT.Silu), r=["ccs"], w=["scT"])
        P.dve(lambda e: e.tensor_reduce(out=tmpg[:, 0:1], in_=gq_bc[:], axis=AX.X, op=ALU.max, apply_absolute_value=True),
              r=["gq_bc"], w=["tmpg0"])
        P.dve(lambda e: e.tensor_reduce(out=tmpg[:, 1:2], in_=gk_bc[:], axis=AX.X, op=ALU.max, apply_absolute_value=True),
              r=["gk_bc"], w=["tmpg1"])
        P.dve(lambda e: e.scalar_tensor_tensor(out=negC[:], in0=tmpg[:, 0:1], scalar=-math.sqrt(128.0), in1=tmpg[:, 1:2],
                                               op0=ALU.mult, op1=ALU.mult), r=["tmpg0", "tmpg1"], w=["negC"])
        for nb in range(12):
            s = nb % 2
            s4 = nb % 4
            P.dma(lambda e, nb=nb, s4=s4: e.dma_start(out=wm[s4][:], in_=w_mod[:, nb * 512:(nb + 1) * 512].rearrange("(kc p) n -> p kc n", p=128)),
                  key=("wm", s4), w=[("wm", s4)], eng=("sp" if nb % 2 == 0 else "pool"))
            for kc in range(8):
                P.pe(lambda e, kc=kc, s=s, s4=s4: e.matmul(out=pM[s][:], lhsT=scT[:, kc, :], rhs=wm[s4][:, kc, :], start=(kc == 0), stop=(kc == 7)),
                     r=["scT", ("wm", s4)], w=[("pM", s)])
            P.dve(lambda e, nb=nb, s=s: e.tensor_tensor(out=mod_sb[:, nb * 512:(nb + 1) * 512], in0=pM[s][:], in1=bm[:, nb * 512:(nb + 1) * 512], op=ALU.add),
                  r=[("pM", s), "bm"], w=["mod_sb"])
        P.dma(lambda e: e.dma_start(out=mod_d, in_=mod_sb[:]), key="mod_d", r=["mod_sb"], w=["mod_d"])
        for j in range(48):
            P.pe(lambda e, j=j: e.transpose(out=pMT[:, j, :], in_=mod_sb[:, j * 128:(j + 1) * 128], identity=ident_f[0:2, 0:2]),
                 r=["mod_sb", "ident_f"], w=["pMT"])
        P.dve(lambda e: e.tensor_copy(out=modT[:], in_=pMT[:]), r=["pMT"], w=["modT"])
        P.dve(lambda e: e.tensor_copy(out=sh1T[:], in_=modT[:, 0:8, :]), r=["modT"], w=["sh1T"])
        P.dve(lambda e: e.tensor_scalar(out=op1p[:], in0=modT[:, 8:16, :], scalar1=1.0, scalar2=None, op0=ALU.add), r=["modT"], w=["op1p"])
        dump("mod", mod_sb[:], [2, 6 * D], r=["mod_sb"])
        P.barrier()
    if STOP <= 0:
        return finish(nc, P, ges, out, dbg_out)

    def fv(ap, off, dims):
        return bass.AP(ap.tensor, ap.offset + off, [list(ap.ap[0])] + [list(d) for d in dims])

    bias_sb = SB(ges, "bias_sb", [1, 5120], BF16)

    def prep_wblock(stage, pB, s, dst, col0, r, bcol, tag):
        P.dma(lambda e: e.dma_start(out=stage[s][:], in_=w_in[:, col0:col0 + 512].rearrange("(kc p) n -> p kc n", p=128)),
              key=("stg", s), w=[("stg", s)])
        for kc in range(8):
            P.dve(lambda e, kc=kc: e.tensor_scalar(out=dst[:, kc, :], in0=stage[s][:, kc, :], scalar1=op1p[:, kc, r:r + 1], scalar2=None, op0=ALU.mult),
                  r=[("stg", s), "op1p"], w=[tag])
        for kc in range(8):
            P.pe(lambda e, kc=kc: e.matmul(out=pB[:], lhsT=sh1T[:, kc, r:r + 1], rhs=stage[s][:, kc, :], start=(kc == 0), stop=(kc == 7)),
                 r=[("stg", s), "sh1T"], w=["pB"])
        P.act(lambda e: e.activation(out=bias_sb[:, bcol:bcol + 512], in_=pB[:], func=ACT.Copy), r=["pB"], w=["bias_sb"])

    def ln_tile(xt_ap, xkey, st, mv, rstd, nb, hb_ap, hkey, sfx, mode="act"):
        for i in range(2):
            P.dve(lambda e, i=i: e.bn_stats(out=st[:, i, :], in_=xt_ap[:, i * 512:(i + 1) * 512]), r=[xkey], w=[("st", sfx, i)])
        P.dve(lambda e: e.bn_aggr(out=mv[:], in_=st[:].rearrange("p a b -> p (a b)")), r=[("st", sfx, 0), ("st", sfx, 1)], w=[("mv", sfx)])
        P.act(lambda e: e.activation(out=rstd[:], in_=mv[:, 1:2], func=ACT.Sqrt, bias=eps_t[:], scale=1.0), r=[("mv", sfx), "eps_t"], w=[("rstd", sfx)])
        P.dve(lambda e: e.reciprocal(out=rstd[:], in_=rstd[:]), r=[("rstd", sfx)], w=[("rstd", sfx)])
        P.dve(lambda e: e.scalar_tensor_tensor(out=nb[:], in0=mv[:, 0:1], scalar=-1.0, in1=rstd[:], op0=ALU.mult, op1=ALU.mult),
              r=[("mv", sfx), ("rstd", sfx)], w=[("nb", sfx)])
        if mode == "dve":
            P.dve(lambda e: e.tensor_scalar(out=hb_ap, in0=xt_ap, scalar1=rstd[:], scalar2=nb[:], op0=ALU.mult, op1=ALU.add),
                  r=[xkey, ("nb", sfx), ("rstd", sfx)], w=[hkey])
            return
        P.act(lambda e: e.activation(out=hb_ap, in_=xt_ap, func=ACT.Identity, bias=nb[:], scale=rstd[:]), r=[xkey, ("nb", sfx), ("rstd", sfx)], w=[hkey])

    def rms_rope(eng_add, src, nh, gain_bc, rt, dst, tmp, keys_r, key_w, sfx, eng2=None, tmp2=None):
        sq, ss, rk, ta, tb = tmp
        eng2 = eng2 or eng_add
        tc, td = tmp2 if tmp2 is not None else (ta, tb)
        kc_, kd_ = (("tc", sfx), ("td", sfx)) if tmp2 is not None else (("ta", sfx), ("tb", sfx))
        n = nh * 128
        P.dve(lambda e: e.tensor_tensor(out=sq[:, :n], in0=src[:, :n], in1=src[:, :n], op=ALU.mult), r=keys_r, w=[("sq", sfx)])
        P.dve(lambda e: e.tensor_reduce(out=ss[:, :nh], in_=sq[:, :n].rearrange("p (h d) -> p h d", d=128), axis=AX.X, op=ALU.add), r=[("sq", sfx)], w=[("ss", sfx)])
        P.act(lambda e: e.activation(out=rk[:, :nh], in_=ss[:, :nh], func=ACT.Sqrt, bias=eps_t[:], scale=1.0 / 128.0), r=[("ss", sfx), "eps_t"], w=[("rk", sfx)])
        P.dve(lambda e: e.reciprocal(out=rk[:, :nh], in_=rk[:, :nh]), r=[("rk", sfx)], w=[("rk", sfx)])
        P.dve(lambda e: e.tensor_tensor(out=src[:, :n].rearrange("p (h d) -> p h d", d=128), in0=src[:, :n].rearrange("p (h d) -> p h d", d=128),
                                        in1=fv(rk[:], 0, [[1, nh], [0, 128]]), op=ALU.mult), r=keys_r + [("rk", sfx)], w=keys_r)
        eng_add(lambda e: e.tensor_tensor(out=src[:, :n].rearrange("p (h d) -> p h d", d=128), in0=src[:, :n].rearrange("p (h d) -> p h d", d=128),
                                          in1=fv(gain_bc[:], 0, [[0, nh], [1, 128]]), op=ALU.mult), r=keys_r, w=keys_r)
        x1 = fv(src[:], 0, [[128, nh], [64, 2], [1, 32]])
        x2 = fv(src[:], 32, [[128, nh], [64, 2], [1, 32]])
        cs = fv(rt[:], 0, [[0, nh], [32, 2], [1, 32]])
        sn = fv(rt[:], 64, [[0, nh], [32, 2], [1, 32]])
        o1 = fv(dst[:], 0, [[128, nh], [64, 2], [1, 32]])
        o2 = fv(dst[:], 32, [[128, nh], [64, 2], [1, 32]])
        tav = fv(ta[:], 0, [[64, nh], [32, 2], [1, 32]])
        tbv = fv(tb[:], 0, [[64, nh], [32, 2], [1, 32]])
        tcv = fv(tc[:], 0, [[64, nh], [32, 2], [1, 32]])
        tdv = fv(td[:], 0, [[64, nh], [32, 2], [1, 32]])
        eng_add(lambda e: e.tensor_tensor(out=tav, in0=x1, in1=cs, op=ALU.mult), r=keys_r + [("rt", sfx)], w=[("ta", sfx)])
        eng2(lambda e: e.tensor_tensor(out=tbv, in0=x2, in1=sn, op=ALU.mult), r=keys_r + [("rt", sfx)], w=[("tb", sfx)])
        eng2(lambda e: e.tensor_tensor(out=o1, in0=tav, in1=tbv, op=ALU.subtract), r=[("ta", sfx), ("tb", sfx)], w=[key_w])
        eng_add(lambda e: e.tensor_tensor(out=tcv, in0=x1, in1=sn, op=ALU.mult), r=keys_r + [("rt", sfx)], w=[kc_])
        eng2(lambda e: e.tensor_tensor(out=tdv, in0=x2, in1=cs, op=ALU.mult), r=keys_r + [("rt", sfx)], w=[kd_])
        eng2(lambda e: e.tensor_tensor(out=o2, in0=tcv, in1=tdv, op=ALU.add), r=[kc_, kd_], w=[key_w])

    esA = ExitStack()
    u8 = SB(esA, "u8", [128, 32, NJF], BF16)
    with ExitStack() as es:
        stage = [SB(es, "stage%d" % i, [128, 8, 512], F32) for i in range(2)]
        Wkv = [SB(es, "Wkv%d" % r, [128, 8, 512], BF16) for r in range(2)]
        Wu = [SB(es, "Wu%d" % r, [128, 8, 512], BF16) for r in range(2)]
        xt = [SB(es, "xt%d" % i, [128, D], F32) for i in range(3)]
        rt = [SB(es, "rt%d" % i, [128, 128], F32) for i in range(2)]
        hb = [SB(es, "hb%d" % i, [128, D], BF16) for i in range(2)]
        hT = [SB(es, "hT%d" % i, [128, 8, 128], BF16) for i in range(2)]
        st = SB(es, "st", [128, 2, 6], F32)
        mv = SB(es, "mv", [128, 2], F32)
        rstd = SB(es, "rstd", [128, 1], F32)
        nbt = SB(es, "nbt", [128, 1], F32)
        k_sb = SB(es, "k_sb", [128, 256], F32)
        v_bf = [SB(es, "v_bf%d" % i, [128, 256], BF16) for i in range(2)]
        kr = SB(es, "kr", [128, 256], BF16)
        kT_sb = [SB(es, "kT_sb%d" % i, [128, 2, 128], BF16) for i in range(2)]
        tmp = (SB(es, "sq", [128, 256], F32), SB(es, "ss", [128, 2], F32), SB(es, "rk", [128, 2], F32),
               SB(es, "ta", [128, 128], F32), SB(es, "tb", [128, 128], F32))
        tmp2_A = (SB(es, "tcA", [128, 128], F32), SB(es, "tdA", [128, 128], F32))
        u_bf = SB(es, "u_bf", [128, 512], BF16)
        Um = [SB(es, "Um%d" % i, [128, 32, 128], BF16) for i in range(2)]
        pB = PS(es, "pB", [1, 512], F32)
        pT = [PS(es, "pT%d" % i, [128, 8, 128], BF16) for i in range(2)]
        pKV = PS(es, "pKV", [128, 512], F32)
        pU = PS(es, "pU", [128, 512], F32)
        pKT = PS(es, "pKT", [128, 2, 128], BF16)
        pU8 = PS(es, "pU8", [128, 32, 16], F32)
        prep_wblock(stage, pB, 0, Wkv[1], 1536, 1, 4096, ("Wkv", 1))
        prep_wblock(stage, pB, 1, Wu[1], 0, 1, 4608, ("Wu", 1))
        prep_wblock(stage, pB, 0, Wkv[0], 1536, 0, 1536, ("Wkv", 0))
        prep_wblock(stage, pB, 1, Wu[0], 0, 0, 0, ("Wu", 0))
        krA = [kr, SB(es, "kr2", [128, 256], BF16)]
        NTA = int(os.environ.get('K_NT', NTF))
        PIPE = int(os.environ.get('K_PIPEA', '3'))
        NSA = int(os.environ.get('K_NSA', '4'))
        stg = [[None] * NTA for _ in range(3)]
        stg1a = [None] * NTA
        for tt in range(NTA):
            r = 1 if tt < 2 else 0
            s3, s2 = tt % 3, tt % 2
            t0 = tt * 128
            bkv, bu = (4096, 4608) if r == 1 else (1536, 0)
            P.capture()
            P.dma(lambda e, s3=s3, t0=t0: e.dma_start(out=xt[s3][:], in_=xf[t0:t0 + 128, :]), key=("xt", s3), w=[("xt", s3)])
            P.dma(lambda e, s2=s2, t0=t0: e.dma_start(out=rt[s2][:], in_=rope_f[t0:t0 + 128, :]), key=("rtd", s2), w=[("rtA", s2)])
            ln_tile(xt[s3][:], ("xt", s3), st, mv, rstd, nbt, hb[s2][:], ("hb", s2), "A", mode=LNM_A)
            stg[0][tt] = P.end_capture()
            P.capture()
            for kc in range(8):
                P.pe(lambda e, kc=kc, s2=s2: e.transpose(out=pT[s2][:, kc, :], in_=hb[s2][:, kc * 128:(kc + 1) * 128], identity=ident_b[:]),
                     r=[("hb", s2), "ident_b"], w=[("pT", s2)])
            P.dve(lambda e, s2=s2: e.tensor_copy(out=hT[s2][:], in_=pT[s2][:]), r=[("pT", s2)], w=[("hT", s2)])
            stg1a[tt] = P.end_capture()
            P.capture()
            for kc in range(8):
                P.pe(lambda e, kc=kc, s2=s2, r=r: e.matmul(out=pKV[:], lhsT=hT[s2][:, kc, :], rhs=Wkv[r][:, kc, :], start=(kc == 0), stop=False),
                     r=[("hT", s2), ("Wkv", r)], w=["pKV"])
            P.pe(lambda e, bkv=bkv: e.matmul(out=pKV[:], lhsT=ones_b[0:1, :], rhs=bias_sb[0:1, bkv:bkv + 512], start=False, stop=True),
                 r=["ones_b", "bias_sb"], w=["pKV"])
            for kc in range(8):
                P.pe(lambda e, kc=kc, s2=s2, r=r: e.matmul(out=pU[:], lhsT=hT[s2][:, kc, :], rhs=Wu[r][:, kc, :], start=(kc == 0), stop=False),
                     r=[("hT", s2), ("Wu", r)], w=["pU"])
            P.pe(lambda e, bu=bu: e.matmul(out=pU[:], lhsT=ones_b[0:1, :], rhs=bias_sb[0:1, bu:bu + 512], start=False, stop=True),
                 r=["ones_b", "bias_sb"], w=["pU"])
            P.act(lambda e: e.activation(out=k_sb[:], in_=pKV[:, 0:256], func=ACT.Copy), r=["pKV"], w=["k_sb"])
            P.act(lambda e, s2=s2: e.activation(out=v_bf[s2][:], in_=pKV[:, 256:512], func=ACT.Copy), r=["pKV"], w=[("v_bf", s2)])
            P.dma(lambda e, s2=s2, t0=t0: e.dma_start(out=v_d[t0:t0 + 128, :], in_=v_bf[s2][:]), key=("vd", s2), r=[("v_bf", s2)], w=["v_d"])
            P.act(lambda e: e.activation(out=u_bf[:], in_=pU[:], func=ACT.Copy), r=["pU"], w=["u_bf"])
            for s_ in range(NSA):
                P.act(lambda e, s2=s2, s_=s_: e.activation(out=Um[s2][:, :, s_ * 16:(s_ + 1) * 16], in_=u_bf[:].rearrange("p (g c) -> p g c", c=16),
                                                         func=ACT.Copy, scale=mask8f[:, s_:s_ + 1]), r=["u_bf", "mask8f"], w=[("Um", s2, s_)])
            P.dve(lambda e, s2=s2: e.tensor_tensor(out=Um[s2][:, :, NSA * 16:128].rearrange("p g (s c) -> p g s c", c=16), in0=fv(u_bf[:], 0, [[16, 32], [0, 8 - NSA], [1, 16]]),
                                                   in1=fv(mask8[:], NSA, [[0, 32], [1, 8 - NSA], [0, 16]]), op=ALU.mult), r=["u_bf", "mask8"], w=[("Um", s2, "v")])
            rms_rope(P.pool, k_sb, 2, gk_bc, rt[s2], krA[s2], tmp, ["k_sb", ("rtA", s2), "gk_bc"], ("kr", s2), "A", eng2=P.dve, tmp2=tmp2_A)
            stg[1][tt] = P.end_capture()
            P.capture()
            for h in range(2):
                P.pe(lambda e, h=h, s2=s2: e.transpose(out=pKT[:, h, :], in_=krA[s2][:, h * 128:(h + 1) * 128], identity=ident_b[:]), r=[("kr", s2), "ident_b"], w=["pKT"])
            P.act(lambda e, s2=s2: e.activation(out=kT_sb[s2][:], in_=pKT[:], func=ACT.Copy), r=["pKT"], w=[("kT_sb", s2)])
            P.dma(lambda e, s2=s2, t0=t0: e.dma_start(out=kT_d[:, :, t0:t0 + 128].rearrange("h p t -> p h t"), in_=kT_sb[s2][:]),
                  key=("kTd", s2), r=[("kT_sb", s2)], w=["kT_d"])
            for g in range(32):
                P.pe(lambda e, g=g, s2=s2: e.matmul(out=pU8[:, g, :], lhsT=Um[s2][:, g, :], rhs=sel16[:], start=True, stop=True),
                     r=[("Um", s2, x_) for x_ in list(range(NSA)) + ["v"]] + ["sel16"], w=["pU8"])
            P.act(lambda e, tt=tt: e.activation(out=u8[:, :, tt * 16:(tt + 1) * 16], in_=pU8[:], func=ACT.Copy), r=["pU8"], w=["u8"])
            stg[2][tt] = P.end_capture()
        if PIPE == 0:
            for tt in range(NTA):
                P.ops.extend(stg[0][tt]); P.ops.extend(stg1a[tt]); P.ops.extend(stg[1][tt]); P.ops.extend(stg[2][tt])
        else:
            for step in range(NTA + 2):
                for lst, off in ((stg1a, 1), (stg[0], 0), (stg[2], 2), (stg[1], 1)):
                    j = step - off
                    if 0 <= j < NTA:
                        P.ops.extend(lst[j])
        dump("u8", u8[:], [128, 32, NJF], BF16, r=["u8"])
        if "kT" in dbg_names:
            o = nc.dram_tensor("dbg_kT", [2, 128, NFULL], BF16, kind="ExternalOutput").ap()
            dbg_out["kT"] = o
            P.dma(lambda e: e.dma_start(out=o, in_=kT_d), key="dbg_kT", r=["kT_d"], w=["dbg_kT"])
        if "v" in dbg_names:
            o2 = nc.dram_tensor("dbg_v", [NFULL, 256], BF16, kind="ExternalOutput").ap()
            dbg_out["v"] = o2
            P.dma(lambda e: e.dma_start(out=o2, in_=v_d), key="dbg_v", r=["v_d"], w=["dbg_v"])
        P.barrier()
    if STOP <= 1:
        esA.close()
        return finish(nc, P, ges, out, dbg_out)

    TWO_PI = 2.0 * math.pi
    MAGIC = 12582912.0
    esS = ExitStack()
    esH = ExitStack()
    esZ = ExitStack()
    S_re = SB(esH, "S_re", [128, 2, 16, NJ], F32)
    S_im = SB(esH, "S_im", [128, 2, 16, NJ], F32)
    erZ = SB(esZ, "erZ", [128, 2, 16, 64], F32)
    eiZ = SB(esZ, "eiZ", [128, 2, 16, 64], F32)
    erZ8 = SB(esS, "erZ8", [128, 2, 16, 8], F32)
    eiZ8 = SB(esS, "eiZ8", [128, 2, 16, 8], F32)
    erR = SB(esS, "erR", [128, 2, 16, 72], F32)
    eiR = SB(esS, "eiR", [128, 2, 16, 72], F32)
    bbre = SB(esS, "bbre", [128, 2, 16, 16], F32)
    bbim = SB(esS, "bbim", [128, 2, 16, 16], F32)
    ccre = SB(esS, "ccre", [128, 2, 16, 16], F32)
    ccim = SB(esS, "ccim", [128, 2, 16, 16], F32)
    A64r = SB(esS, "A64r", [128, 2, 16, 1], F32)
    A64i = SB(esS, "A64i", [128, 2, 16, 1], F32)
    ardt = SB(esS, "ardt", [128, 2, 16], F32)
    aidt = SB(esS, "aidt", [128, 2, 16], F32)
    ptmp = []
    piT = SB(esS, "piT", [128, 1], F32)

    def powtab(exps_ap, n, er_t, ei_t, tag):
        def v4(t):
            return t[:, :, :, 0:n]
        a_b = lambda t: fv(t[:], 0, [[16, 2], [1, 16], [0, n]])
        e_b = fv(exps_ap, 0, [[exps_ap.ap[1][0], 2], [0, 16], [1, n]])
        t0, t1, t2 = ptmp
        k = lambda i: ("ptmp", i)
        P.dve(lambda e: e.tensor_tensor(out=v4(t0), in0=a_b(ardt), in1=e_b, op=ALU.mult), r=["ardt", tag + "_e"], w=[k(0)])
        P.act(lambda e: e.activation(out=v4(t0), in_=v4(t0), func=ACT.Exp), r=[k(0)], w=[k(0)])
        P.dve(lambda e: e.tensor_tensor(out=v4(t1), in0=a_b(aidt), in1=e_b, op=ALU.mult), r=["aidt", tag + "_e"], w=[k(1)])
        P.dve(lambda e: e.tensor_scalar(out=v4(t2), in0=v4(t1), scalar1=MAGIC, scalar2=None, op0=ALU.add), r=[k(1)], w=[k(2)])
        P.dve(lambda e: e.tensor_scalar(out=v4(t2), in0=v4(t2), scalar1=MAGIC, scalar2=None, op0=ALU.subtract), r=[k(2)], w=[k(2)])
        P.dve(lambda e: e.tensor_tensor(out=v4(t2), in0=v4(t1), in1=v4(t2), op=ALU.subtract), r=[k(1), k(2)], w=[k(2)])
        P.act(lambda e: e.activation(out=v4(t2), in_=v4(t2), func=ACT.Sin, scale=TWO_PI), r=[k(2)], w=[k(2)])
        P.dve(lambda e: e.tensor_tensor(out=v4(ei_t), in0=v4(t0), in1=v4(t2), op=ALU.mult), r=[k(0), k(2)], w=[tag + "_ei"])
        P.dve(lambda e: e.tensor_scalar(out=v4(t1), in0=v4(t1), scalar1=0.25, scalar2=None, op0=ALU.add), r=[k(1)], w=[k(1)])
        P.dve(lambda e: e.tensor_scalar(out=v4(t2), in0=v4(t1), scalar1=MAGIC, scalar2=None, op0=ALU.add), r=[k(1)], w=[k(2)])
        P.dve(lambda e: e.tensor_scalar(out=v4(t2), in0=v4(t2), scalar1=MAGIC, scalar2=None, op0=ALU.subtract), r=[k(2)], w=[k(2)])
        P.dve(lambda e: e.tensor_tensor(out=v4(t2), in0=v4(t1), in1=v4(t2), op=ALU.subtract), r=[k(1), k(2)], w=[k(2)])
        P.act(lambda e: e.activation(out=v4(t2), in_=v4(t2), func=ACT.Sin, scale=TWO_PI), r=[k(2)], w=[k(2)])
        P.dve(lambda e: e.tensor_tensor(out=v4(er_t), in0=v4(t0), in1=v4(t2), op=ALU.mult), r=[k(0), k(2)], w=[tag + "_er"])

    with ExitStack() as es:
        ptmp.extend([SB(es, "ptmp%d" % i, [128, 2, 16, 72], F32) for i in range(3)])
        a_sb = SB(es, "a_sb", [128, 2, 2, 16], F32)
        ldt_sb = SB(es, "ldt_sb", [128, 2, 16], F32)
        b_sb = SB(es, "b_sb", [128, 2, 2, 16, 16], F32)
        c_sb = SB(es, "c_sb", [128, 2, 2, 16, 16], F32)
        eZ_sb = SB(es, "eZ_sb", [128, 2, 64], F32)
        eR_sb = SB(es, "eR_sb", [128, 2, 72], F32)
        e1_sb = SB(es, "e1_sb", [128, 2, 1], F32)
        e64_sb = SB(es, "e64_sb", [128, 2, 1], F32)
        abr = SB(es, "abr", [128, 2, 16, 1], F32)
        abi = SB(es, "abi", [128, 2, 16, 1], F32)
        dsc = [SB(es, "dsc%d" % i, [128, 2, 16], F32) for i in range(5)]
        bt = [SB(es, "bt%d" % i, [128, 2, 16, 16], F32) for i in range(2)]
        P.dma(lambda e: e.dma_start(out=a_sb[:], in_=s5_a), key="a_sb", w=["a_sb"])
        P.dma(lambda e: e.dma_start(out=ldt_sb[:], in_=s5_ldt), key="ldt_sb", w=["ldt_sb"])
        P.dma(lambda e: e.dma_start(out=b_sb[:], in_=s5_b), key="b_sb", w=["b_sb"])
        P.dma(lambda e: e.dma_start(out=c_sb[:], in_=s5_c), key="c_sb", w=["c_sb"])
        P.dma(lambda e: e.dma_start(out=eZ_sb[:], in_=cst_eZ), key="eZ_sb", w=["Z_e"])
        P.dma(lambda e: e.dma_start(out=eR_sb[:], in_=cst_eR), key="eR_sb", w=["R_e"])
        P.dve(lambda e: e.memset(e1_sb[:], 1.0), w=["ab_e"])
        P.dve(lambda e: e.memset(e64_sb[:], 64.0), w=["A64_e"])
        P.act(lambda e: e.activation(out=ldt_sb[:], in_=ldt_sb[:], func=ACT.Exp), r=["ldt_sb"], w=["ldt_sb"])
        P.dve(lambda e: e.tensor_tensor(out=ardt[:], in0=a_sb[:, 0], in1=ldt_sb[:], op=ALU.mult), r=["a_sb", "ldt_sb"], w=["ardt"])
        P.dve(lambda e: e.scalar_tensor_tensor(out=aidt[:], in0=a_sb[:, 1], scalar=1.0 / TWO_PI, in1=ldt_sb[:], op0=ALU.mult, op1=ALU.mult),
              r=["a_sb", "ldt_sb"], w=["aidt"])
        powtab(e1_sb[:], 1, abr, abi, "ab")
        powtab(e64_sb[:], 1, A64r, A64i, "A64")
        powtab(eZ_sb[:], 64, erZ, eiZ, "Z")
        powtab(eR_sb[:], 72, erR, eiR, "R")
        for (src_t, dst_t, kk) in ((erZ, erZ8, "Z_er"), (eiZ, eiZ8, "Z_ei")):
            P.dve(lambda e, src_t=src_t, dst_t=dst_t: e.tensor_copy(out=dst_t[:, 0], in_=src_t[:, 0, :, 56:64]), r=[kk], w=[kk + "8"])
            P.dve(lambda e, src_t=src_t, dst_t=dst_t: e.tensor_copy(out=dst_t[:, 1], in_=src_t[:, 1, :, 0:8]), r=[kk], w=[kk + "8"])
        are, aim = a_sb[:, 0], a_sb[:, 1]
        d0, d1, d2, d3, d4 = [t[:] for t in dsc]
        ab_r = abr[:].rearrange("p d q o -> p d (q o)")
        ab_i = abi[:].rearrange("p d q o -> p d (q o)")
        kd = lambda i: ("dsc", i)
        P.dve(lambda e: e.tensor_tensor(out=d0, in0=are, in1=are, op=ALU.mult), r=["a_sb"], w=[kd(0)])
        P.dve(lambda e: e.tensor_tensor(out=d1, in0=aim, in1=aim, op=ALU.mult), r=["a_sb"], w=[kd(1)])
        P.dve(lambda e: e.tensor_tensor(out=d0, in0=d0, in1=d1, op=ALU.add), r=[kd(0), kd(1)], w=[kd(0)])
        P.dve(lambda e: e.reciprocal(out=d0, in_=d0), r=[kd(0)], w=[kd(0)])
        P.dve(lambda e: e.tensor_scalar(out=d1, in0=ab_r, scalar1=-1.0, scalar2=None, op0=ALU.add), r=["ab_er"], w=[kd(1)])
        P.dve(lambda e: e.tensor_tensor(out=d2, in0=d1, in1=are, op=ALU.mult), r=[kd(1), "a_sb"], w=[kd(2)])
        P.dve(lambda e: e.tensor_tensor(out=d3, in0=ab_i, in1=aim, op=ALU.mult), r=["ab_ei", "a_sb"], w=[kd(3)])
        P.dve(lambda e: e.tensor_tensor(out=d2, in0=d2, in1=d3, op=ALU.add), r=[kd(2), kd(3)], w=[kd(2)])
        P.dve(lambda e: e.tensor_tensor(out=d2, in0=d2, in1=d0, op=ALU.mult), r=[kd(2), kd(0)], w=[kd(2)])
        P.dve(lambda e: e.tensor_tensor(out=d3, in0=ab_i, in1=are, op=ALU.mult), r=["ab_ei", "a_sb"], w=[kd(3)])
        P.dve(lambda e: e.tensor_tensor(out=d4, in0=d1, in1=aim, op=ALU.mult), r=[kd(1), "a_sb"], w=[kd(4)])
        P.dve(lambda e: e.tensor_tensor(out=d3, in0=d3, in1=d4, op=ALU.subtract), r=[kd(3), kd(4)], w=[kd(3)])
        P.dve(lambda e: e.tensor_tensor(out=d3, in0=d3, in1=d0, op=ALU.mult), r=[kd(3), kd(0)], w=[kd(3)])
        rr_b = fv(dsc[2][:], 0, [[16, 2], [1, 16], [0, 16]])
        ri_b = fv(dsc[3][:], 0, [[16, 2], [1, 16], [0, 16]])
        bre, bim = b_sb[:, 0], b_sb[:, 1]
        P.dve(lambda e: e.tensor_tensor(out=bt[0][:], in0=bre, in1=rr_b, op=ALU.mult), r=["b_sb", kd(2)], w=["bt0"])
        P.dve(lambda e: e.tensor_tensor(out=bt[1][:], in0=bim, in1=ri_b, op=ALU.mult), r=["b_sb", kd(3)], w=["bt1"])
        P.dve(lambda e: e.tensor_tensor(out=bbre[:], in0=bt[0][:], in1=bt[1][:], op=ALU.subtract), r=["bt0", "bt1"], w=["bbre"])
        P.dve(lambda e: e.tensor_tensor(out=bt[0][:], in0=bim, in1=rr_b, op=ALU.mult), r=["b_sb", kd(2)], w=["bt0"])
        P.dve(lambda e: e.tensor_tensor(out=bt[1][:], in0=bre, in1=ri_b, op=ALU.mult), r=["b_sb", kd(3)], w=["bt1"])
        P.dve(lambda e: e.tensor_tensor(out=bbim[:], in0=bt[0][:], in1=bt[1][:], op=ALU.add), r=["bt0", "bt1"], w=["bbim"])
        P.dve(lambda e: e.tensor_copy(out=ccre[:], in_=c_sb[:, 0]), r=["c_sb"], w=["ccre"])
        P.dve(lambda e: e.tensor_copy(out=ccim[:], in_=c_sb[:, 1]), r=["c_sb"], w=["ccim"])
        P.barrier()

    def ztab(eng_add, pair, tsl, nt, bufs, tag, erZ=erZ, eiZ=eiZ, tmkey=None):
        zre, zim, ztm = bufs
        tmkey = tmkey or (tag + "ztm")
        def ev(t, d):
            return fv(t[:, d, pair, tsl[d]:tsl[d] + nt], 0, [[1, nt], [0, 16]])
        def bv(t, d):
            return fv(t[:, d, pair, :], 0, [[0, nt], [1, 16]])
        for d in range(2):
            eng_add(lambda e, d=d: e.tensor_tensor(out=zre[:, d], in0=bv(bbre, d), in1=ev(erZ, d), op=ALU.mult), r=["Z_er", "bbre"], w=[(tag + "zre", d)])
            eng_add(lambda e, d=d: e.tensor_tensor(out=ztm[:, d], in0=bv(bbim, d), in1=ev(eiZ, d), op=ALU.mult), r=["Z_ei", "bbim"], w=[(tmkey, d)])
            eng_add(lambda e, d=d: e.tensor_tensor(out=zre[:, d], in0=zre[:, d], in1=ztm[:, d], op=ALU.subtract), r=[(tag + "zre", d), (tmkey, d)], w=[(tag + "zre", d)])
            eng_add(lambda e, d=d: e.tensor_tensor(out=zim[:, d], in0=bv(bbim, d), in1=ev(erZ, d), op=ALU.mult), r=["Z_er", "bbim"], w=[(tag + "zim", d)])
            eng_add(lambda e, d=d: e.tensor_tensor(out=ztm[:, d], in0=bv(bbre, d), in1=ev(eiZ, d), op=ALU.mult), r=["Z_ei", "bbre"], w=[(tmkey, d)])
            eng_add(lambda e, d=d: e.tensor_tensor(out=zim[:, d], in0=zim[:, d], in1=ztm[:, d], op=ALU.add), r=[(tag + "zim", d), (tmkey, d)], w=[(tag + "zim", d)])

    with ExitStack() as es:
        Wsum = [SB(es, "Wsum%d" % i, [128, 2, 8, 2, 128], BF16) for i in range(2)]
        zb = [[SB(es, "zb%d_%d" % (i, j), [128, 2, 64, 16], F32) for j in range(3)] for i in range(1)]
        pW = [PS(es, "pW%d" % i, [128, 4, 128], F32) for i in range(2)]
        pSr = PS(es, "pSr", [128, 2, NJ], F32)
        pSi = PS(es, "pSi", [128, 2, NJ], F32)
        ci = 0
        for pair in range(16):
            sl = int(os.environ.get('K_SL', pair % 2))
            ea = P.dve
            zt_ = "z0"
            ztab(ea, pair, (0, 0), 64, zb[0], zt_)
            zre, zim, _ = zb[0]
            for d in range(2):
                for reim in range(2):
                    zt = zre if reim == 0 else zim
                    for mq in range(2):
                        pw = pW[ci % 2]
                        for j in range(4):
                            m_ = mq * 4 + j
                            P.pe(lambda e, zt=zt, d=d, m_=m_, pw=pw, j=j: e.transpose(out=pw[:, j, :], in_=zt[:, d, m_ * 8:(m_ + 1) * 8, :].rearrange("p s c -> p (s c)"), identity=ident_f[:]),
                                 r=[(zt_ + "z%s" % ("re" if reim == 0 else "im"), d), "ident_f"], w=[("pW", ci % 2)])
                        dst = Wsum[sl][:, d, mq * 4:(mq + 1) * 4, reim, :]
                        P.act(lambda e, dst=dst, pw=pw: e.activation(out=dst, in_=pw[:], func=ACT.Copy), r=[("pW", ci % 2)], w=[("Wsum", sl)])
                        ci += 1
            for d in range(2):
                for gh in range(2):
                    g = 2 * pair + gh
                    for reim, pS_ in ((0, pSr), (1, pSi)):
                        for m_ in range(8):
                            P.pe(lambda e, d=d, gh=gh, g=g, reim=reim, pS_=pS_, m_=m_, sl=sl: e.matmul(
                                out=pS_[64 * gh:64 * gh + 64, d, :], lhsT=Wsum[sl][:, d, m_, reim, 64 * gh:64 * gh + 64],
                                rhs=fv(u8[:, g, :], m_, [[8, NJ]]), start=(m_ == 0), stop=(m_ == 7)),
                                r=[("Wsum", sl), "u8"], w=["pSr" if reim == 0 else "pSi"])
            P.act(lambda e, pair=pair: e.activation(out=S_re[:, :, pair, :], in_=pSr[:], func=ACT.Copy), r=["pSr"], w=["S_re"])
            P.act(lambda e, pair=pair: e.activation(out=S_im[:, :, pair, :], in_=pSi[:], func=ACT.Copy), r=["pSi"], w=["S_im"])
        P.barrier()
    esA.close()
    esZ.close()

    with ExitStack() as es:
        sct = [[SB(es, "sct%d_%d" % (d, i), [128, 16], F32) for i in range(4)] for d in range(2)]
        orders = [[(J, J - 1 if J > 0 else None) for J in range(NJ)],
                  [(3, None), (2, 3), (1, 2), (0, 1), (131, 0)] + [(J, J + 1) for J in range(130, 3, -1)]]
        for d in range(2):
            ea = P.dve if d == 0 else P.pool
            Ar = A64r[:, d, :, 0]
            Ai = A64i[:, d, :, 0]
            t = [x[:] for x in sct[d]]
            kt = lambda i: ("sct", d, i)
            kr_, ki_ = ("Hre", d), ("Him", d)
            for (J, Jp) in orders[d]:
                if Jp is None:
                    continue
                hr_p, hi_p = S_re[:, d, :, Jp], S_im[:, d, :, Jp]
                hr, hi = S_re[:, d, :, J], S_im[:, d, :, J]
                ea(lambda e, t=t, hr_p=hr_p, Ar=Ar: e.tensor_tensor(out=t[0], in0=Ar, in1=hr_p, op=ALU.mult), r=["A64_er", kr_, "S_re"], w=[kt(0)])
                ea(lambda e, t=t, hi_p=hi_p, Ai=Ai: e.tensor_tensor(out=t[1], in0=Ai, in1=hi_p, op=ALU.mult), r=["A64_ei", ki_, "S_im"], w=[kt(1)])
                ea(lambda e, t=t: e.tensor_tensor(out=t[0], in0=t[0], in1=t[1], op=ALU.subtract), r=[kt(0), kt(1)], w=[kt(0)])
                ea(lambda e, t=t, hi_p=hi_p, Ar=Ar: e.tensor_tensor(out=t[2], in0=Ar, in1=hi_p, op=ALU.mult), r=["A64_er", ki_, "S_im"], w=[kt(2)])
                ea(lambda e, t=t, hr_p=hr_p, Ai=Ai: e.tensor_tensor(out=t[3], in0=Ai, in1=hr_p, op=ALU.mult), r=["A64_ei", kr_, "S_re"], w=[kt(3)])
                ea(lambda e, t=t: e.tensor_tensor(out=t[2], in0=t[2], in1=t[3], op=ALU.add), r=[kt(2), kt(3)], w=[kt(2)])
                ea(lambda e, t=t, hr=hr: e.tensor_tensor(out=hr, in0=hr, in1=t[0], op=ALU.add), r=[kt(0), kr_], w=[kr_])
                ea(lambda e, t=t, hi=hi: e.tensor_tensor(out=hi, in0=hi, in1=t[2], op=ALU.add), r=[kt(2), ki_], w=[ki_])
        P.barrier()
    dump("H_re", S_re[:], [128, 2, 16, NJ])
    dump("H_im", S_im[:], [128, 2, 16, NJ])
    HO = [[SB(esS, "HO%d_%d" % (d, x), [128, 16, 32], BF16) for x in range(2)] for d in range(2)]
    with ExitStack() as es:
        cm = SB(es, "cm", [128, 4], F32)
        hacc = SB(es, "hacc", [128, 16, 32], F32)
        P.dma(lambda e: e.dma_start(out=cm[:], in_=cmask), key="cm", w=["cm"])
        for d in range(2):
            for x, Sx in enumerate((S_re, S_im)):
                for r_ in range(4):
                    lo = (3 + 32 * r_) if d == 0 else (5 + 32 * r_)
                    segs = [(lo, 0, 32)] if not (d == 1 and r_ == 3) else [(lo, 0, 31), (0, 31, 1)]
                    for (a0, o0, n_) in segs:
                        src_ = Sx[:, d, :, a0:a0 + n_]
                        dst_ = hacc[:, :, o0:o0 + n_]
                        if r_ == 0:
                            P.dve(lambda e, src_=src_, dst_=dst_: e.tensor_scalar(out=dst_, in0=src_, scalar1=cm[:, 0:1], scalar2=None, op0=ALU.mult),
                                  r=["cm"], w=["hacc"])
                        else:
                            P.dve(lambda e, src_=src_, dst_=dst_, r_=r_: e.scalar_tensor_tensor(out=dst_, in0=src_, scalar=cm[:, r_:r_ + 1], in1=dst_, op0=ALU.mult, op1=ALU.add),
                                  r=["cm", "hacc"], w=["hacc"])
                P.dve(lambda e, d=d, x=x: e.tensor_copy(out=HO[d][x][:], in_=hacc[:]), r=["hacc"], w=[("HO", d, x)])
        P.barrier()
    esH.close()
    if STOP <= 2:
        esS.close()
        return finish(nc, P, ges, out, dbg_out)

    esP = ExitStack()
    hTo = SB(esP, "hTo", [128, 8, NOWN], BF16)
    qT = SB(esP, "qT", [128, 8, NOWN], BF16)
    esU = ExitStack()
    u8o = SB(esU, "u8o", [128, 32, 256], BF16)

    def load_bc(dst, src_row, key, add_one=False):
        P.dma(lambda e: e.dma_start(out=dst[:], in_=src_row.partition_broadcast(128)), key=key, w=[key])
        if add_one:
            P.dve(lambda e: e.tensor_scalar(out=dst[:], in0=dst[:], scalar1=1.0, scalar2=None, op0=ALU.add), r=[key], w=[key])

    with ExitStack() as es:
        Wq = SB(es, "Wq", [128, 8, 1024], BF16)
        Wuo = SB(es, "Wuo", [128, 8, 512], BF16)
        sc1p_bc = SB(es, "sc1p_bc", [128, D], F32)
        sh1_bc = SB(es, "sh1_bc", [128, D], F32)
        xt_B = [SB(es, "xtB%d" % i, [128, D], F32) for i in range(2)]
        rt_B = [SB(es, "rtB%d" % i, [128, 128], F32) for i in range(2)]
        hn = SB(es, "hn", [128, D], F32)
        hb_B = [SB(es, "hbB%d" % i, [128, D], BF16) for i in range(2)]
        st_B = SB(es, "stB", [128, 2, 6], F32)
        mv_B = SB(es, "mvB", [128, 2], F32)
        rstd_B = SB(es, "rstdB", [128, 1], F32)
        nbt_B = SB(es, "nbtB", [128, 1], F32)
        q_sbs = [SB(es, "q_sb%d" % i, [128, 1024], F32) for i in range(2)]
        tmp2_B = (SB(es, "tcB", [128, 512], F32), SB(es, "tdB", [128, 512], F32))
        qr = SB(es, "qr", [128, 1024], BF16)
        tmp_B = (SB(es, "sqB", [128, 1024], F32), SB(es, "ssB", [128, 8], F32), SB(es, "rkB", [128, 8], F32),
               SB(es, "taB", [128, 512], F32), SB(es, "tbB", [128, 512], F32))
        u_bf_B = SB(es, "u_bfB", [128, 512], BF16)
        Um_B = [SB(es, "UmB0", [128, 32, 128], BF16)] * 2
        pT_B = [PS(es, "pTB%d" % i, [128, 8, 128], BF16) for i in range(2)]
        pQ = [PS(es, "pQ%d" % i, [128, 512], F32) for i in range(2)]
        pQT = PS(es, "pQT", [128, 8, 128], BF16)
        pU_B = PS(es, "pUB", [128, 512], F32)
        pU8_B = PS(es, "pU8B", [128, 32, 16], F32)
        P.dma(lambda e: e.dma_start(out=Wq[:], in_=w_in[:, 512:1536].rearrange("(kc p) n -> p kc n", p=128)), key="Wq", w=["Wq"], eng="pool")
        P.dma(lambda e: e.dma_start(out=Wuo[:], in_=w_in[:, 0:512].rearrange("(kc p) n -> p kc n", p=128)), key="Wuo", w=["Wuo"], eng="pool")
        load_bc(sc1p_bc, mod_d[0:1, 1024:2048], "sc1p_bc", True)
        load_bc(sh1_bc, mod_d[0:1, 0:1024], "sh1_bc")
        sB0, sB1a, sB1b, sB2 = [None] * NTO, [None] * NTO, [None] * NTO, [None] * NTO
        for tt in range(NTO):
            s2 = tt % 2
            t0 = tt * 128
            P.capture()
            P.dma(lambda e, s2=s2, t0=t0: e.dma_start(out=xt_B[s2][:], in_=xo[t0:t0 + 128, :]), key=("xtB", s2), w=[("xtB", s2)])
            P.dma(lambda e, s2=s2, t0=t0: e.dma_start(out=rt_B[s2][:], in_=rope_o[t0:t0 + 128, :]), key=("rtB", s2), w=[("rtB", s2)])
            ln_tile(xt_B[s2][:], ("xtB", s2), st_B, mv_B, rstd_B, nbt_B, hn[:], "hn", "B", mode=LNM_A)
            P.dve(lambda e: e.tensor_tensor(out=hn[:], in0=hn[:], in1=sc1p_bc[:], op=ALU.mult), r=["hn", "sc1p_bc"], w=["hn"])
            P.dve(lambda e, s2=s2: e.tensor_tensor(out=hb_B[s2][:], in0=hn[:], in1=sh1_bc[:], op=ALU.add), r=["hn", "sh1_bc"], w=[("hbB", s2)])
            sB0[tt] = P.end_capture()
            P.capture()
            for kc in range(8):
                P.pe(lambda e, kc=kc, s2=s2: e.transpose(out=pT_B[s2][:, kc, :], in_=hb_B[s2][:, kc * 128:(kc + 1) * 128], identity=ident_b[:]),
                     r=[("hbB", s2), "ident_b"], w=[("pTB", s2)])
            P.act(lambda e, s2=s2, t0=t0, tt=tt: e.activation(out=hTo[:, :, t0:t0 + 128], in_=pT_B[s2][:], func=ACT.Copy), r=[("pTB", s2)], w=[("hTo", tt)])
            sB1a[tt] = P.end_capture()
            P.capture()
            for half in range(2):
                for kc in range(8):
                    P.pe(lambda e, kc=kc, half=half, t0=t0: e.matmul(out=pQ[half][:], lhsT=hTo[:, kc, t0:t0 + 128], rhs=Wq[:, kc, half * 512:(half + 1) * 512],
                                                                     start=(kc == 0), stop=(kc == 7)), r=[("hTo", tt), "Wq"], w=[("pQ", half)])
                P.act(lambda e, half=half, s2=s2: e.activation(out=q_sbs[s2][:, half * 512:(half + 1) * 512], in_=pQ[half][:], func=ACT.Copy), r=[("pQ", half)], w=[("q_sb", s2)])
            for kc in range(8):
                P.pe(lambda e, kc=kc, t0=t0: e.matmul(out=pU_B[:], lhsT=hTo[:, kc, t0:t0 + 128], rhs=Wuo[:, kc, :], start=(kc == 0), stop=(kc == 7)),
                     r=[("hTo", tt), "Wuo"], w=["pUB"])
            P.act(lambda e: e.activation(out=u_bf_B[:], in_=pU_B[:], func=ACT.Copy), r=["pUB"], w=["u_bfB"])
            P.dve(lambda e, s2=s2: e.tensor_tensor(out=Um_B[s2][:].rearrange("p g (s c) -> p g s c", c=16), in0=fv(u_bf_B[:], 0, [[16, 32], [0, 8], [1, 16]]),
                                                   in1=fv(mask8[:], 0, [[0, 32], [1, 8], [0, 16]]), op=ALU.mult), r=["u_bfB", "mask8"], w=["UmB"])
            rms_rope(P.pool, q_sbs[s2], 8, gq_bc, rt_B[s2], qr, tmp_B, [("q_sb", s2), ("rtB", s2), "gq_bc"], "qr", "B", eng2=P.dve, tmp2=tmp2_B)
            sB1b[tt] = P.end_capture()
            P.capture()
            for h in range(8):
                P.pe(lambda e, h=h: e.transpose(out=pQT[:, h, :], in_=qr[:, h * 128:(h + 1) * 128], identity=ident_b[:]), r=["qr", "ident_b"], w=["pQT"])
            P.act(lambda e, t0=t0: e.activation(out=qT[:, :, t0:t0 + 128], in_=pQT[:], func=ACT.Copy), r=["pQT"], w=[("qT", tt)])
            for g in range(32):
                P.pe(lambda e, g=g, s2=s2: e.matmul(out=pU8_B[:, g, :], lhsT=Um_B[s2][:, g, :], rhs=sel16[:], start=True, stop=True),
                     r=["UmB", "sel16"], w=["pU8B"])
            P.act(lambda e, tt=tt: e.activation(out=u8o[:, :, tt * 16:(tt + 1) * 16], in_=pU8_B[:], func=ACT.Copy), r=["pU8B"], w=[("u8o", tt)])
            sB2[tt] = P.end_capture()
        if int(os.environ.get('K_PIPEB', '1')) == 0:
            for tt in range(NTO):
                for lst in (sB0, sB1a, sB1b, sB2):
                    P.ops.extend(lst[tt])
        else:
            for step in range(NTO + 2):
                for lst, off in ((sB1a, 1), (sB0, 0), (sB2, 2), (sB1b, 1)):
                    j = step - off
                    if 0 <= j < NTO:
                        P.ops.extend(lst[j])
        P.barrier()
    if "qT" in dbg_names:
        dump("qT", qT[:], [128, 8, NOWN], BF16)
    if STOP <= 3:
        esU.close(); esS.close(); esP.close()
        return finish(nc, P, ges, out, dbg_out)

    gT = SB(esP, "gT", [128, 4, NOWN], BF16)
    with ExitStack() as es:
        mf_sb = SB(es, "mf_sb", [128, 128], F32)
        mb_sb = SB(es, "mb_sb", [128, 128], F32)
        dcol_sb = SB(es, "dcol_sb", [128, 32], F32)
        selg_b = SB(es, "selg_b", [128, 8, 128], BF16)
        m8c_b = SB(es, "m8c_b", [128, 8], BF16)
        z8 = [SB(es, "z8_%d" % j, [128, 2, 8, 16], F32) for j in range(3)]
        Rre_p = SB(es, "Rre_p", [128, 2, 72, 16], F32)
        Rim_p = SB(es, "Rim_p", [128, 2, 72, 16], F32)
        Rt0 = SB(es, "Rt0", [128, 1, 72, 16], F32)
        Rt1 = SB(es, "Rt1", [128, 1, 72, 16], F32)
        Rre_b = SB(es, "Rre_b", [128, 2, 1152], BF16)
        Rim_b = SB(es, "Rim_b", [128, 2, 1152], BF16)
        Tw = [SB(es, "Tw0", [128, 2, 15, 128], BF16)] * 2
        tt0 = SB(es, "tt0", [128, 128], F32)
        tt1 = SB(es, "tt1", [128, 128], F32)
        Ye = [SB(es, "Ye0", [128, 2048], BF16)] * 2
        ysb = SB(es, "ysb", [128, 1024], F32)
        yx2 = SB(es, "yx2", [128, 1024], F32)
        pTb = [PS(es, "pTb%d" % i, [128, 128], F32) for i in range(2)]
        pY8 = [PS(es, "pY8_%d" % i, [128, 8, 32], F32) for i in range(2)]
        pYT = PS(es, "pYT", [128, 2048], F32)
        P.dma(lambda e: e.dma_start(out=mf_sb[:], in_=cst_mf), key="mf_sb", w=["mf_sb"])
        P.dma(lambda e: e.dma_start(out=mb_sb[:], in_=cst_mb), key="mb_sb", w=["mb_sb"])
        P.dma(lambda e: e.dma_start(out=dcol_sb[:], in_=s5_dcol), key="dcol_sb", w=["dcol_sb"])
        P.dma(lambda e: e.dma_start(out=selg_b[:], in_=cst_selg), key="selg_b", w=["selg_b"], eng="pool")
        P.dma(lambda e: e.dma_start(out=m8c_b[:], in_=cst_mask8c), key="m8c_b", w=["m8c_b"], eng="pool")
        ecnt = 0
        for pair in range(16):
            sl = 0
            ztab(P.dve, pair, (0, 0), 8, z8, "z8", erZ=erZ8, eiZ=eiZ8)
            for d in range(2):
                eb = lambda t, d=d, pair=pair: fv(t[:, d, pair, :], 0, [[1, 72], [0, 16]])
                cb = lambda t, d=d, pair=pair: fv(t[:, d, pair, :], 0, [[0, 72], [1, 16]])
                P.pool(lambda e, d=d, eb=eb, cb=cb: e.tensor_tensor(out=Rre_p[:, d], in0=cb(ccre), in1=eb(erR), op=ALU.mult), r=["R_er", "ccre"], w=["Rre_p"])
                P.pool(lambda e, d=d, eb=eb, cb=cb: e.tensor_tensor(out=Rt0[:, 0], in0=cb(ccim), in1=eb(eiR), op=ALU.mult), r=["R_ei", "ccim"], w=["Rt0"])
                P.pool(lambda e, d=d: e.tensor_tensor(out=Rre_p[:, d], in0=Rre_p[:, d], in1=Rt0[:, 0], op=ALU.subtract), r=["Rre_p", "Rt0"], w=["Rre_p"])
                P.dve(lambda e, d=d, eb=eb, cb=cb: e.tensor_tensor(out=Rim_p[:, d], in0=cb(ccim), in1=eb(erR), op=ALU.mult), r=["R_er", "ccim"], w=["Rim_p"])
                P.dve(lambda e, d=d, eb=eb, cb=cb: e.tensor_tensor(out=Rt1[:, 0], in0=cb(ccre), in1=eb(eiR), op=ALU.mult), r=["R_ei", "ccre"], w=["Rt1"])
                P.dve(lambda e, d=d: e.scalar_tensor_tensor(out=Rim_p[:, d], in0=Rt1[:, 0], scalar=-1.0, in1=Rim_p[:, d], op0=ALU.mult, op1=ALU.subtract),
                      r=["Rim_p", "Rt1"], w=["Rim_p"])
            P.act(lambda e: e.activation(out=Rre_b[:], in_=Rre_p[:].rearrange("p d i c -> p d (i c)"), func=ACT.Copy), r=["Rre_p"], w=["Rre_b"])
            P.act(lambda e: e.activation(out=Rim_b[:], in_=Rim_p[:].rearrange("p d i c -> p d (i c)"), func=ACT.Copy), r=["Rim_p"], w=["Rim_b"])
            for gh in range(2):
                g = 2 * pair + gh
                rows = slice(64 * gh, 64 * gh + 64)
                blocks = [(0, 0), (1, 0)] + [(0, k) for k in range(1, 8)] + [(1, k) for k in range(1, 8)]
                for (d, k) in blocks:
                    pt = pTb[ecnt % 2]
                    kk = ("pTb", ecnt % 2)
                    P.pe(lambda e, pt=pt, d=d, k=k, rows=rows: e.matmul(out=pt[:], lhsT=z8[0][rows, d].rearrange("p s c -> p (s c)"),
                                                                        rhs=Rre_p[rows, d, 8 * k:8 * k + 8, :].rearrange("p s c -> p (s c)"), start=True, stop=False),
                         r=[("z8zre", d), "Rre_p"], w=[kk])
                    P.pe(lambda e, pt=pt, d=d, k=k, rows=rows: e.matmul(out=pt[:], lhsT=z8[1][rows, d].rearrange("p s c -> p (s c)"),
                                                                        rhs=Rim_p[rows, d, 8 * k:8 * k + 8, :].rearrange("p s c -> p (s c)"), start=False, stop=True),
                         r=[("z8zim", d), "Rim_p"], w=[kk])
                    if k == 0 and d == 0:
                        P.dve(lambda e, pt=pt: e.tensor_tensor(out=tt0[:], in0=pt[:], in1=mf_sb[:], op=ALU.mult), r=[kk, "mf_sb"], w=["tt0"])
                    elif k == 0 and d == 1:
                        P.dve(lambda e, pt=pt: e.tensor_tensor(out=tt1[:], in0=pt[:], in1=mb_sb[:], op=ALU.mult), r=[kk, "mb_sb"], w=["tt1"])
                        P.dve(lambda e: e.tensor_tensor(out=tt0[:], in0=tt0[:], in1=tt1[:], op=ALU.add), r=["tt0", "tt1"], w=["tt0"])
                        P.dve(lambda e, g=g, gh=gh, sl=sl: e.scalar_tensor_tensor(out=Tw[sl][:, gh, 7, :], in0=ident_f[:], scalar=dcol_sb[:, g:g + 1], in1=tt0[:],
                                                                                 op0=ALU.mult, op1=ALU.add), r=["tt0", "dcol_sb", "ident_f"], w=[("Tw", sl)])
                    else:
                        idx = 7 + k if d == 0 else 7 - k
                        if ecnt % 2 == 0:
                            P.act(lambda e, pt=pt, gh=gh, sl=sl, idx=idx: e.activation(out=Tw[sl][:, gh, idx, :], in_=pt[:], func=ACT.Copy), r=[kk], w=[("Tw", sl)])
                        else:
                            P.dve(lambda e, pt=pt, gh=gh, sl=sl, idx=idx: e.tensor_copy(out=Tw[sl][:, gh, idx, :], in_=pt[:]), r=[kk], w=[("Tw", sl)])
                    ecnt += 1
            for gh in range(2):
                g = 2 * pair + gh
                rows = slice(64 * gh, 64 * gh + 64)
                py = pY8[g % 2]
                ky = ("pY8", g % 2)
                for m in range(8):
                    for m_ in range(8):
                        P.pe(lambda e, py=py, m=m, m_=m_, gh=gh, g=g, sl=sl: e.matmul(out=py[:, m, :], lhsT=Tw[sl][:, gh, 7 + m - m_, :],
                                                                                     rhs=fv(u8o[:, g, :], m_, [[8, 32]]), start=(m_ == 0), stop=False),
                             r=[("Tw", sl), "u8o"], w=[ky])
                    fo = 8 * (m + 1) * 16
                    bo = 8 * (8 - m) * 16
                    P.pe(lambda e, py=py, m=m, rows=rows, fo=fo, pair=pair: e.matmul(out=py[:, m, :], lhsT=Rre_b[rows, 0, fo:fo + 128], rhs=HO[0][0][rows, pair, :], start=False, stop=False),
                         r=["Rre_b", ("HO", 0, 0)], w=[ky])
                    P.pe(lambda e, py=py, m=m, rows=rows, fo=fo, pair=pair: e.matmul(out=py[:, m, :], lhsT=Rim_b[rows, 0, fo:fo + 128], rhs=HO[0][1][rows, pair, :], start=False, stop=False),
                         r=["Rim_b", ("HO", 0, 1)], w=[ky])
                    P.pe(lambda e, py=py, m=m, rows=rows, bo=bo, pair=pair: e.matmul(out=py[:, m, :], lhsT=Rre_b[rows, 1, bo:bo + 128], rhs=HO[1][0][rows, pair, :], start=False, stop=False),
                         r=["Rre_b", ("HO", 1, 0)], w=[ky])
                    P.pe(lambda e, py=py, m=m, rows=rows, bo=bo, pair=pair: e.matmul(out=py[:, m, :], lhsT=Rim_b[rows, 1, bo:bo + 128], rhs=HO[1][1][rows, pair, :], start=False, stop=True),
                         r=["Rim_b", ("HO", 1, 1)], w=[ky])
                ye = Ye[g % 2]
                P.dve(lambda e, py=py, ye=ye: e.tensor_tensor(out=ye[:].rearrange("p (j m s) -> p j m s", m=8, s=8), in0=fv(py[:], 0, [[1, 32], [32, 8], [0, 8]]),
                                                              in1=fv(m8c_b[:], 0, [[0, 32], [0, 8], [1, 8]]), op=ALU.mult), r=[ky, "m8c_b"], w=["Ye"])
                for c4 in range(4):
                    P.pe(lambda e, ye=ye, g=g, c4=c4: e.matmul(out=pYT[:, c4 * 512:(c4 + 1) * 512], lhsT=selg_b[:, g % 8, :], rhs=ye[:, c4 * 512:(c4 + 1) * 512],
                                                              start=(g % 8 == 0), stop=(g % 8 == 7)), r=["Ye", "selg_b"], w=["pYT"])
            if pair % 4 == 3:
                tile_ = pair // 4
                for hf in range(2):
                    cs = slice(hf * 1024, (hf + 1) * 1024)
                    P.act(lambda e, cs=cs: e.activation(out=ysb[:], in_=pYT[:, cs], func=ACT.Copy), r=["pYT"], w=["ysb"])
                    P.dve(lambda e: e.tensor_tensor(out=yx2[:], in0=ysb[:], in1=ysb[:], op=ALU.mult), r=["ysb"], w=["yx2"])
                    P.dve(lambda e: e.tensor_scalar(out=yx2[:], in0=yx2[:], scalar1=0.044715, scalar2=1.0, op0=ALU.mult, op1=ALU.add), r=["yx2"], w=["yx2"])
                    P.dve(lambda e: e.tensor_tensor(out=yx2[:], in0=yx2[:], in1=ysb[:], op=ALU.mult), r=["yx2", "ysb"], w=["yx2"])
                    P.act(lambda e: e.activation(out=yx2[:], in_=yx2[:], func=ACT.Sigmoid, scale=1.5957691216057308), r=["yx2"], w=["yx2"])
                    P.dve(lambda e, tile_=tile_, cs=cs: e.tensor_tensor(out=gT[:, tile_, cs], in0=ysb[:], in1=yx2[:], op=ALU.mult), r=["ysb", "yx2"], w=["gT"])
        P.barrier()
    esU.close()
    esS.close()
    if "gT" in dbg_names:
        dump("gT", gT[:], [128, 4, NOWN], BF16)
    if STOP <= 4:
        esP.close()
        return finish(nc, P, ges, out, dbg_out)

    oT = qT
    NKC = NFULL // 128
    SCALE = 128.0 ** -0.5
    with ExitStack() as es:
        kT_all = SB(es, "kT_all", [128, 2, NFULL], BF16)
        V_aug = SB(es, "V_aug", [128, NKC, 2, 132], BF16)
        pTt = [SB(es, "pTt%d" % i, [128, 512], BF16) for i in range(3)]
        rden = SB(es, "rden", [128, 4], F32)
        on_b = SB(es, "on_b", [128, 4, 128], BF16)
        pS = [PS(es, "pS%d" % i, [128, 512], F32) for i in range(2)]
        pO = [PS(es, "pO%d" % i, [128, 512], F32) for i in range(4)]
        pOT = PS(es, "pOT", [128, 4, 128], BF16)
        P.dve(lambda e: e.memset(V_aug[:], 1.0), w=["V_aug"])
        for h2 in range(2):
            P.dma(lambda e, h2=h2: e.dma_start(out=kT_all[:, h2, :], in_=kT_d[h2]), key=("kT_all", h2), r=["kT_d"], w=["kT_all"])
            P.dma(lambda e, h2=h2: e.dma_start(out=V_aug[:, :, h2, 0:128], in_=v_d[:, h2 * 128:(h2 + 1) * 128].rearrange("(c p) d -> p c d", p=128)),
                  key=("V_aug", h2), r=["v_d", "V_aug"], w=["V_aug"])
        iters = [(h, qc, kc) for h in range(8) for qc in range(4) for kc in range(NKC)]

        def emit_S(i):
            h, qc, kc = iters[i]
            kvh = h // 4
            s2 = i % 2
            qs = slice(qc * 512, (qc + 1) * 512)
            P.pe(lambda e, s2=s2, kvh=kvh, kc=kc, h=h, qs=qs: e.matmul(out=pS[s2][:], lhsT=kT_all[:, kvh, kc * 128:(kc + 1) * 128], rhs=qT[:, h, qs], start=True, stop=True),
                 r=["kT_all", ("qTc", h, qc)], w=[("pS", s2)])

        emit_S(0)
        for i, (h, qc, kc) in enumerate(iters):
            kvh = h // 4
            s2, s3 = i % 2, i % 3
            qs = slice(qc * 512, (qc + 1) * 512)
            if i + 1 < len(iters):
                emit_S(i + 1)
            P.act(lambda e, s2=s2, s3=s3: e.activation(out=pTt[s3][:], in_=pS[s2][:], func=ACT.Exp, bias=negC[:], scale=SCALE),
                  r=[("pS", s2), "negC"], w=[("pTt", s3)])
            for qi in range(4):
                P.pe(lambda e, s3=s3, qi=qi, kc=kc, kvh=kvh: e.matmul(out=pO[qi][:, 0:129], lhsT=pTt[s3][:, qi * 128:(qi + 1) * 128], rhs=V_aug[:, kc, kvh, 0:129],
                                                                     start=(kc == 0), stop=(kc == NKC - 1)), r=[("pTt", s3), "V_aug"], w=[("pO", qi)])
            if kc == NKC - 1:
                for qi in range(4):
                    P.dve(lambda e, qi=qi: e.reciprocal(out=rden[:, qi:qi + 1], in_=pO[qi][:, 128:129]), r=[("pO", qi)], w=[("rden", qi)])
                    P.dve(lambda e, qi=qi: e.tensor_scalar(out=on_b[:, qi, :], in0=pO[qi][:, 0:128], scalar1=rden[:, qi:qi + 1], scalar2=None, op0=ALU.mult),
                          r=[("pO", qi), ("rden", qi)], w=[("on_b", qi)])
                    P.pe(lambda e, qi=qi: e.transpose(out=pOT[:, qi, :], in_=on_b[:, qi, :], identity=ident_b[:]), r=[("on_b", qi), "ident_b"], w=["pOT"])
                P.dve(lambda e, h=h, qs=qs: e.tensor_copy(out=oT[:, h, qs], in_=pOT[:].rearrange("p a b -> p (a b)")), r=["pOT"], w=[("qTc", h, qc)])
        P.barrier()
    if "oT" in dbg_names:
        dump("oT", oT[:], [128, 8, NOWN], BF16)
    if STOP <= 5:
        esP.close()
        return finish(nc, P, ges, out, dbg_out)

    esM = ExitStack()
    mT_all = SB(esM, "mT_all", [128, 8, NOWN], BF16)
    with ExitStack() as es:
        Wg = SB(es, "Wg", [128, 8, 2048], BF16)
        Wa = SB(es, "Wa", [128, 4, 1024], BF16)
        Wb = SB(es, "Wb", [128, 4, 1024], BF16)
        Wo = SB(es, "Wo", [128, 8, 1024], BF16)
        sg1 = SB(es, "sg1", [128, 512], F32)
        sg2 = SB(es, "sg2", [128, 512], F32)
        sgb = SB(es, "sgb", [128, 512], F32)
        mt1 = SB(es, "mt1", [128, 512], F32)
        mt2 = SB(es, "mt2", [128, 512], F32)
        pG1 = PS(es, "pG1", [128, 512], F32)
        pG2 = PS(es, "pG2", [128, 512], F32)
        pA_D = PS(es, "pA_D", [128, 512], F32)
        pB_D = PS(es, "pB_D", [128, 512], F32)
        pC_D = PS(es, "pC_D", [128, 512], F32)
        for c2 in range(2):
            P.dma(lambda e, c2=c2: e.dma_start(out=Wg[:, :, c2 * 1024:(c2 + 1) * 1024], in_=w_in[:, 2048 + c2 * 1024:2048 + (c2 + 1) * 1024].rearrange("(kc p) n -> p kc n", p=128)),
                  key=("Wg", c2), w=["Wg"], eng="pool")
        P.dma(lambda e: e.dma_start(out=Wa[:], in_=w_glu_a.rearrange("(kc p) n -> p kc n", p=128)), key="Wa", w=["Wa"], eng="pool")
        P.dma(lambda e: e.dma_start(out=Wb[:], in_=w_glu_b.rearrange("(kc p) n -> p kc n", p=128)), key="Wb", w=["Wb"], eng="pool")
        P.dma(lambda e: e.dma_start(out=Wo[:], in_=w_attn_o.rearrange("(kc p) n -> p kc n", p=128)), key="Wo", w=["Wo"], eng="pool")
        for st_ in range(4):
            ts = slice(st_ * 512, (st_ + 1) * 512)
            for dt_ in range(8):
                ds = slice(dt_ * 128, (dt_ + 1) * 128)
                ds2 = slice(1024 + dt_ * 128, 1024 + (dt_ + 1) * 128)
                for kc in range(8):
                    P.pe(lambda e, kc=kc, ds=ds, ts=ts: e.matmul(out=pG1[:], lhsT=Wg[:, kc, ds], rhs=hTo[:, kc, ts], start=(kc == 0), stop=(kc == 7)), r=["Wg", "hTo"], w=["pG1"])
                for kc in range(8):
                    P.pe(lambda e, kc=kc, ds2=ds2, ts=ts: e.matmul(out=pG2[:], lhsT=Wg[:, kc, ds2], rhs=hTo[:, kc, ts], start=(kc == 0), stop=(kc == 7)), r=["Wg", "hTo"], w=["pG2"])
                for c in range(4):
                    P.pe(lambda e, c=c, ds=ds, ts=ts: e.matmul(out=pA_D[:], lhsT=Wa[:, c, ds], rhs=gT[:, c, ts], start=(c == 0), stop=(c == 3)), r=["Wa", "gT"], w=["pA_D"])
                for c in range(4):
                    P.pe(lambda e, c=c, ds=ds, ts=ts: e.matmul(out=pB_D[:], lhsT=Wb[:, c, ds], rhs=gT[:, c, ts], start=(c == 0), stop=(c == 3)), r=["Wb", "gT"], w=["pB_D"])
                for hh in range(8):
                    P.pe(lambda e, hh=hh, ds=ds, ts=ts: e.matmul(out=pC_D[:], lhsT=Wo[:, hh, ds], rhs=oT[:, hh, ts], start=(hh == 0), stop=(hh == 7)), r=["Wo", "oT"], w=["pC_D"])
                P.act(lambda e: e.activation(out=sg1[:], in_=pG1[:], func=ACT.Sigmoid), r=["pG1"], w=["sg1"])
                P.act(lambda e: e.activation(out=sg2[:], in_=pG2[:], func=ACT.Sigmoid), r=["pG2"], w=["sg2"])
                P.act(lambda e: e.activation(out=sgb[:], in_=pB_D[:], func=ACT.Sigmoid), r=["pB_D"], w=["sgb"])
                P.dve(lambda e: e.tensor_tensor(out=mt1[:], in0=pA_D[:], in1=sgb[:], op=ALU.mult), r=["pA_D", "sgb"], w=["mt1"])
                P.dve(lambda e: e.tensor_tensor(out=mt1[:], in0=mt1[:], in1=sg1[:], op=ALU.mult), r=["mt1", "sg1"], w=["mt1"])
                P.dve(lambda e: e.tensor_tensor(out=mt2[:], in0=pC_D[:], in1=sg2[:], op=ALU.mult), r=["pC_D", "sg2"], w=["mt2"])
                P.dve(lambda e, dt_=dt_, ts=ts: e.tensor_tensor(out=mT_all[:, dt_, ts], in0=mt1[:], in1=mt2[:], op=ALU.add), r=["mt1", "mt2"], w=["mT_all"])
        P.barrier()
    esP.close()
    if "mT" in dbg_names:
        dump("mT", mT_all[:], [128, 8, NOWN], BF16)
    if STOP <= 6:
        esM.close()
        return finish(nc, P, ges, out, dbg_out)

    esE = ExitStack()
    h2T = SB(esE, "h2T", [128, 8, NOWN], BF16)
    combT = SB(esE, "combT", [32, NOWN], BF16)
    with ExitStack() as es:
        Wout = SB(es, "Wout", [128, 8, 1024], BF16)
        wrt_sb = SB(es, "wrt_sb", [128, 8, 36], F32)
        brt_bc = SB(es, "brt_bc", [128, 36], F32)
        g1_bc = SB(es, "g1_bc", [128, D], F32)
        l1g_bc = SB(es, "l1g_bc", [128, D], F32)
        l1b_bc = SB(es, "l1b_bc", [128, D], F32)
        sc2p_bc = SB(es, "sc2p_bc", [128, D], F32)
        sh2_bc = SB(es, "sh2_bc", [128, D], F32)
        xt_D = [SB(es, "xt_D%d" % i, [128, D], F32) for i in range(2)]
        zt_D = SB(es, "zt_D", [128, D], F32)
        zn_D = SB(es, "zn_D", [128, D], F32)
        x1_D = [SB(es, "x1_D%d" % i, [128, D], F32) for i in range(2)]
        h2_D = SB(es, "h2_D", [128, D], F32)
        h2b_D = SB(es, "h2b_D", [128, D], BF16)
        h2Tf = SB(es, "h2Tf", [128, 8, 128], F32)
        st_D = SB(es, "st_D", [128, 2, 6], F32)
        mv_D = SB(es, "mv_D", [128, 2], F32)
        rstd_D = SB(es, "rstd_D", [128, 1], F32)
        nb_D = SB(es, "nb_D", [128, 1], F32)
        st_D2 = SB(es, "st_D2", [128, 2, 6], F32)
        mv_D2 = SB(es, "mv_D2", [128, 2], F32)
        rstd_D2 = SB(es, "rstd_D2", [128, 1], F32)
        nb_D2 = SB(es, "nb_D2", [128, 1], F32)
        L_D = SB(es, "L_D", [128, 36], F32)
        rs = SB(es, "rs", [128, 16], F32)
        ohg = SB(es, "ohg", [128, 4], F32)
        gex = SB(es, "gex", [128, 4], F32)
        msk = SB(es, "msk", [128, 32], F32)
        ein = SB(es, "ein", [128, 8], F32)
        e2_ = SB(es, "e2_", [128, 8], F32)
        oh1 = SB(es, "oh1", [128, 8], F32)
        oh2 = SB(es, "oh2", [128, 8], F32)
        cg = SB(es, "cg", [128, 8], F32)
        comb = SB(es, "comb", [128, 32], F32)
        pMix = [PS(es, "pMix%d" % i, [128, 512], F32) for i in range(2)]
        pT_D = PS(es, "pT_D", [128, 8, 128], BF16)
        pTf = PS(es, "pTf", [128, 4, 128], F32)
        pR = PS(es, "pR", [128, 36], F32)
        pCT = PS(es, "pCT", [32, 128], F32)
        P.dma(lambda e: e.dma_start(out=Wout[:], in_=w_out.rearrange("(kc p) n -> p kc n", p=128)), key="Wout", w=["Wout"], eng="pool")
        P.dma(lambda e: e.dma_start(out=wrt_sb[:], in_=w_rt.rearrange("(kc p) n -> p kc n", p=128)), key="wrt_sb", w=["wrt_sb"])
        load_bc(brt_bc, b_rt, "brt_bc")
        load_bc(g1_bc, mod_d[0:1, 2048:3072], "g1_bc")
        load_bc(l1g_bc, ln1_g, "l1g_bc")
        load_bc(l1b_bc, ln1_b, "l1b_bc")
        load_bc(sc2p_bc, mod_d[0:1, 4096:5120], "sc2p_bc", True)
        load_bc(sh2_bc, mod_d[0:1, 3072:4096], "sh2_bc")
        sD0, sD1, sD2 = [None] * NTO, [None] * NTO, [None] * NTO
        sD0a = [None] * NTO
        for tt in range(NTO):
            s2 = tt % 2
            t0 = tt * 128
            tsl_ = slice(t0, t0 + 128)
            P.capture()
            P.dma(lambda e, s2=s2, t0=t0: e.dma_start(out=xt_D[s2][:], in_=xo[t0:t0 + 128, :]), key=("xt_D", s2), w=[("xt_D", s2)])
            for half in range(2):
                hs = slice(half * 512, (half + 1) * 512)
                for kc in range(8):
                    P.pe(lambda e, kc=kc, half=half, hs=hs, tsl_=tsl_: e.matmul(out=pMix[half][:], lhsT=mT_all[:, kc, tsl_], rhs=Wout[:, kc, hs], start=(kc == 0), stop=(kc == 7)),
                         r=["mT_all", "Wout"], w=[("pMix", half)])
                P.dve(lambda e, half=half, hs=hs: e.tensor_tensor(out=zt_D[:, hs], in0=pMix[half][:], in1=g1_bc[:, hs], op=ALU.mult), r=[("pMix", half), "g1_bc"], w=["zt_D"])
            sD0a[tt] = P.end_capture()
            P.capture()
            P.dve(lambda e, s2=s2: e.scalar_tensor_tensor(out=zt_D[:], in0=xt_D[s2][:], scalar=ALPHA, in1=zt_D[:], op0=ALU.mult, op1=ALU.add), r=["zt_D", ("xt_D", s2)], w=["zt_D"])
            ln_tile(zt_D[:], "zt_D", st_D, mv_D, rstd_D, nb_D, zn_D[:], "zn_D", "D1", mode=LNM_D)
            P.dve(lambda e: e.tensor_tensor(out=zn_D[:], in0=zn_D[:], in1=l1g_bc[:], op=ALU.mult), r=["zn_D", "l1g_bc"], w=["zn_D"])
            P.dve(lambda e, s2=s2: e.tensor_tensor(out=x1_D[s2][:], in0=zn_D[:], in1=l1b_bc[:], op=ALU.add), r=["zn_D", "l1b_bc"], w=[("x1_D", s2)])
            P.dma(lambda e, s2=s2, t0=t0: e.dma_start(out=x1_d[t0:t0 + 128, :], in_=x1_D[s2][:]), key=("x1d", s2), r=[("x1_D", s2)], w=["x1_d"])
            sD0[tt] = P.end_capture()
            P.capture()
            ln_tile(x1_D[s2][:], ("x1_D", s2), st_D2, mv_D2, rstd_D2, nb_D2, h2_D[:], "h2_D", "D2", mode=LNM_D)
            P.dve(lambda e: e.tensor_tensor(out=h2_D[:], in0=h2_D[:], in1=sc2p_bc[:], op=ALU.mult), r=["h2_D", "sc2p_bc"], w=["h2_D"])
            P.dve(lambda e: e.tensor_tensor(out=h2_D[:], in0=h2_D[:], in1=sh2_bc[:], op=ALU.add), r=["h2_D", "sh2_bc"], w=["h2_D"])
            P.act(lambda e: e.activation(out=h2b_D[:], in_=h2_D[:], func=ACT.Copy), r=["h2_D"], w=["h2b_D"])
            for kc in range(8):
                P.pe(lambda e, kc=kc: e.transpose(out=pT_D[:, kc, :], in_=h2b_D[:, kc * 128:(kc + 1) * 128], identity=ident_b[:]), r=["h2b_D", "ident_b"], w=["pT_D"])
            P.act(lambda e, tsl_=tsl_: e.activation(out=h2T[:, :, tsl_], in_=pT_D[:], func=ACT.Copy), r=["pT_D"], w=[("h2T", tt)])
            for q4 in range(2):
                for j in range(4):
                    kc = q4 * 4 + j
                    P.pe(lambda e, kc=kc, j=j: e.transpose(out=pTf[:, j, :], in_=h2_D[:, kc * 128:(kc + 1) * 128], identity=ident_f[:]), r=["h2_D", "ident_f"], w=["pTf"])
                P.dve(lambda e, q4=q4: e.tensor_copy(out=h2Tf[:, q4 * 4:(q4 + 1) * 4, :], in_=pTf[:]), r=["pTf"], w=["h2Tf"])
            for kc in range(8):
                P.pe(lambda e, kc=kc: e.matmul(out=pR[:], lhsT=h2Tf[:, kc, :], rhs=wrt_sb[:, kc, :], start=(kc == 0), stop=(kc == 7)), r=["h2Tf", "wrt_sb"], w=["pR"])
            P.dve(lambda e: e.tensor_tensor(out=L_D[:], in0=pR[:], in1=brt_bc[:], op=ALU.add), r=["pR", "brt_bc"], w=["L_D"])
            sD1[tt] = P.end_capture()
            P.capture()
            R_ = lambda i: rs[:, i:i + 1]
            kR = lambda i: ("rs", i)
            P.dve(lambda e: e.tensor_reduce(out=R_(0), in_=L_D[:, 0:4], axis=AX.X, op=ALU.max), r=["L_D"], w=[kR(0)])
            P.dve(lambda e: e.tensor_scalar(out=ohg[:], in0=L_D[:, 0:4], scalar1=R_(0), scalar2=None, op0=ALU.is_equal), r=["L_D", kR(0)], w=["ohg"])
            P.dve(lambda e: e.tensor_scalar(out=R_(1), in0=R_(0), scalar1=-1.0, scalar2=None, op0=ALU.mult), r=[kR(0)], w=[kR(1)])
            P.act(lambda e: e.activation(out=gex[:], in_=L_D[:, 0:4], func=ACT.Exp, bias=R_(1), scale=1.0), r=["L_D", kR(1)], w=["gex"])
            P.dve(lambda e: e.tensor_reduce(out=R_(2), in_=gex[:], axis=AX.X, op=ALU.add), r=["gex"], w=[kR(2)])
            P.dve(lambda e: e.reciprocal(out=R_(2), in_=R_(2)), r=[kR(2)], w=[kR(2)])
            P.dve(lambda e: e.tensor_tensor(out=msk[:].rearrange("p (g x) -> p g x", x=8), in0=L_D[:, 4:36].rearrange("p (g x) -> p g x", x=8),
                                            in1=fv(ohg[:], 0, [[1, 4], [0, 8]]), op=ALU.mult), r=["L_D", "ohg"], w=["msk"])
            P.dve(lambda e: e.tensor_reduce(out=ein[:], in_=fv(msk[:], 0, [[1, 8], [8, 4]]), axis=AX.X, op=ALU.add), r=["msk"], w=["ein"])
            P.dve(lambda e: e.tensor_reduce(out=R_(3), in_=ein[:], axis=AX.X, op=ALU.max), r=["ein"], w=[kR(3)])
            P.dve(lambda e: e.tensor_scalar(out=oh1[:], in0=ein[:], scalar1=R_(3), scalar2=None, op0=ALU.is_equal), r=["ein", kR(3)], w=["oh1"])
            P.dve(lambda e: e.scalar_tensor_tensor(out=e2_[:], in0=oh1[:], scalar=-1e30, in1=ein[:], op0=ALU.mult, op1=ALU.add), r=["oh1", "ein"], w=["e2_"])
            P.dve(lambda e: e.tensor_reduce(out=R_(4), in_=e2_[:], axis=AX.X, op=ALU.max), r=["e2_"], w=[kR(4)])
            P.dve(lambda e: e.tensor_scalar(out=oh2[:], in0=e2_[:], scalar1=R_(4), scalar2=None, op0=ALU.is_equal), r=["e2_", kR(4)], w=["oh2"])
            P.dve(lambda e: e.tensor_tensor(out=R_(5), in0=R_(4), in1=R_(3), op=ALU.subtract), r=[kR(3), kR(4)], w=[kR(5)])
            P.act(lambda e: e.activation(out=R_(6), in_=R_(5), func=ACT.Exp), r=[kR(5)], w=[kR(6)])
            P.dve(lambda e: e.tensor_scalar(out=R_(7), in0=R_(6), scalar1=1.0, scalar2=None, op0=ALU.add), r=[kR(6)], w=[kR(7)])
            P.dve(lambda e: e.reciprocal(out=R_(7), in_=R_(7)), r=[kR(7)], w=[kR(7)])
            P.dve(lambda e: e.tensor_tensor(out=R_(8), in0=R_(6), in1=R_(7), op=ALU.mult), r=[kR(6), kR(7)], w=[kR(8)])
            P.dve(lambda e: e.tensor_tensor(out=R_(7), in0=R_(7), in1=R_(2), op=ALU.mult), r=[kR(7), kR(2)], w=[kR(7)])
            P.dve(lambda e: e.tensor_tensor(out=R_(8), in0=R_(8), in1=R_(2), op=ALU.mult), r=[kR(8), kR(2)], w=[kR(8)])
            P.dve(lambda e: e.tensor_scalar(out=cg[:], in0=oh1[:], scalar1=R_(7), scalar2=None, op0=ALU.mult), r=["oh1", kR(7)], w=["cg"])
            P.dve(lambda e: e.scalar_tensor_tensor(out=cg[:], in0=oh2[:], scalar=R_(8), in1=cg[:], op0=ALU.mult, op1=ALU.add), r=["oh2", kR(8), "cg"], w=["cg"])
            P.dve(lambda e: e.tensor_tensor(out=comb[:].rearrange("p (g x) -> p g x", x=8), in0=fv(cg[:], 0, [[0, 4], [1, 8]]), in1=fv(ohg[:], 0, [[1, 4], [0, 8]]), op=ALU.mult),
                  r=["cg", "ohg"], w=["comb"])
            P.pe(lambda e: e.transpose(out=pCT[:], in_=comb[:], identity=ident_f[:]), r=["comb", "ident_f"], w=["pCT"])
            P.dve(lambda e, tsl_=tsl_, tt=tt: e.tensor_copy(out=combT[:, tsl_], in_=pCT[:]), r=["pCT"], w=[("combT", tt)])
            sD2[tt] = P.end_capture()
        if int(os.environ.get('K_PIPED', '1')) == 0:
            for tt in range(NTO):
                for lst in (sD0a, sD0, sD1, sD2):
                    P.ops.extend(lst[tt])
        else:
            P.ops.extend(sD0a[0])
            for step in range(NTO + 2):
                for lst, off in ((sD0, 0), (sD0a, -1), (sD2, 2), (sD1, 1)):
                    j = step - off
                    if 0 <= j < NTO:
                        P.ops.extend(lst[j])
        P.barrier()
    esM.close()
    if "x1" in dbg_names:
        o_ = nc.dram_tensor("dbg_x1", [NOWN, D], F32, kind="ExternalOutput").ap()
        P.dma(lambda e: e.dma_start(out=o_, in_=x1_d), key="dbg_x1", r=["x1_d"], w=["dbg_x1"])
    if "combT" in dbg_names:
        dump("combT", combT[:], [32, NOWN], BF16)
    if STOP <= 7:
        esE.close()
        return finish(nc, P, ges, out, dbg_out)

    yacc = SB(esE, "yacc", [128, NTO, D], F32)
    with ExitStack() as es:
        Weg = [SB(es, "Weg%d" % i, [128, 8, 512], BF16) for i in range(2)]
        Weu = [SB(es, "Weu%d" % i, [128, 8, 512], BF16) for i in range(2)]
        Wed = [SB(es, "Wed%d" % i, [128, 4, 1024], BF16) for i in range(2)]
        sele_b = SB(es, "sele_b", [32, 32, 128], BF16)
        bc_sb = SB(es, "bc_sb", [128, 512], F32)
        sa_E = [SB(es, "sa_E%d" % i, [128, 512], F32) for i in range(2)]
        actT = [SB(es, "actT%d" % i, [128, 4, 512], BF16) for i in range(2)]
        pBC = PS(es, "pBC", [128, 512], F32)
        pA_E = [PS(es, "pA_E%d" % i, [128, 512], F32) for i in range(2)]
        pB_E = [PS(es, "pB_E%d" % i, [128, 512], F32) for i in range(2)]
        pY_E = [PS(es, "pY_E%d" % i, [128, 512], F32) for i in range(2)]
        P.dma(lambda e: e.dma_start(out=sele_b[:], in_=cst_sele), key="sele_b", w=["sele_b"], eng="pool")
        NEXP = int(os.environ.get("K_NEXP", "32"))
        fci = 0
        yi = 0
        mG0, mG1, mD, mW = [], [], [], []
        for ex in range(NEXP):
            se = ex % 2
            P.capture()
            P.dma(lambda e, ex=ex, se=se: e.dma_start(out=Weg[se][:], in_=w_eg[ex].rearrange("(kc p) f -> p kc f", p=128)), key=("Weg", se), w=[("Weg", se)], eng="pool")
            P.dma(lambda e, ex=ex, se=se: e.dma_start(out=Weu[se][:], in_=w_eu[ex].rearrange("(kc p) f -> p kc f", p=128)), key=("Weu", se), w=[("Weu", se)], eng="pool")
            P.dma(lambda e, ex=ex, se=se: e.dma_start(out=Wed[se][:], in_=w_ed[ex].rearrange("(fc p) n -> p fc n", p=128)), key=("Wed", se), w=[("Wed", se)], eng="pool")
            mW.append(P.end_capture())
            for st_ in range(4):
                ts = slice(st_ * 512, (st_ + 1) * 512)
                sa_ = (ex * 4 + st_) % 2
                P.capture()
                P.pe(lambda e, ex=ex, ts=ts: e.matmul(out=pBC[:], lhsT=sele_b[:, ex, :], rhs=combT[:, ts], start=True, stop=True), r=["sele_b", "combT"], w=["pBC"])
                P.act(lambda e: e.activation(out=bc_sb[:], in_=pBC[:], func=ACT.Copy), r=["pBC"], w=["bc_sb"])
                for fc in range(4):
                    fs = slice(fc * 128, (fc + 1) * 128)
                    sp_ = fci % 2
                    for kc in range(8):
                        P.pe(lambda e, kc=kc, fs=fs, ts=ts, se=se, sp_=sp_: e.matmul(out=pA_E[sp_][:], lhsT=Weg[se][:, kc, fs], rhs=h2T[:, kc, ts], start=(kc == 0), stop=(kc == 7)),
                             r=[("Weg", se), "h2T"], w=[("pA_E", sp_)])
                    for kc in range(8):
                        P.pe(lambda e, kc=kc, fs=fs, ts=ts, se=se, sp_=sp_: e.matmul(out=pB_E[sp_][:], lhsT=Weu[se][:, kc, fs], rhs=h2T[:, kc, ts], start=(kc == 0), stop=(kc == 7)),
                             r=[("Weu", se), "h2T"], w=[("pB_E", sp_)])
                    P.act(lambda e, sp_=sp_: e.activation(out=sa_E[sp_][:], in_=pA_E[sp_][:], func=ACT.Silu), r=[("pA_E", sp_)], w=[("sa_E", sp_)])
                    P.dve(lambda e, sp_=sp_: e.tensor_tensor(out=sa_E[sp_][:], in0=sa_E[sp_][:], in1=pB_E[sp_][:], op=ALU.mult), r=[("sa_E", sp_), ("pB_E", sp_)], w=[("sa_E", sp_)])
                    P.dve(lambda e, sp_=sp_, sa_=sa_, fc=fc: e.tensor_tensor(out=actT[sa_][:, fc, :], in0=sa_E[sp_][:], in1=bc_sb[:], op=ALU.mult),
                          r=[("sa_E", sp_), "bc_sb"], w=[("actT", sa_)])
                    fci += 1
                    if fc == 0:
                        mG0.append(P.end_capture())
                        P.capture()
                mG1.append(P.end_capture())
                P.capture()
                for j in range(4):
                    tile_ = st_ * 4 + j
                    js = slice(j * 128, (j + 1) * 128)
                    for half in range(2):
                        hs = slice(half * 512, (half + 1) * 512)
                        sy = yi % 2
                        for fc in range(4):
                            P.pe(lambda e, fc=fc, js=js, hs=hs, sa_=sa_, se=se, sy=sy: e.matmul(out=pY_E[sy][:], lhsT=actT[sa_][:, fc, js], rhs=Wed[se][:, fc, hs], start=(fc == 0), stop=(fc == 3)),
                                 r=[("actT", sa_), ("Wed", se)], w=[("pY_E", sy)])
                        if ex == 0:
                            P.dve(lambda e, tile_=tile_, hs=hs, sy=sy: e.tensor_copy(out=yacc[:, tile_, hs], in_=pY_E[sy][:]), r=[("pY_E", sy)], w=[("yacc", tile_)])
                        else:
                            P.dve(lambda e, tile_=tile_, hs=hs, sy=sy: e.tensor_tensor(out=yacc[:, tile_, hs], in0=yacc[:, tile_, hs], in1=pY_E[sy][:], op=ALU.add),
                                  r=[("pY_E", sy), ("yacc", tile_)], w=[("yacc", tile_)])
                        yi += 1
                mD.append(P.end_capture())
        nst = len(mG0)
        for i in range(nst):
            if i % 4 == 0:
                P.ops.extend(mW[i // 4])
            P.ops.extend(mG0[i])
            if i > 0:
                P.ops.extend(mD[i - 1])
            P.ops.extend(mG1[i])
        P.ops.extend(mD[nst - 1])
        P.barrier()

    with ExitStack() as es:
        g2_bc = SB(es, "g2_bc", [128, D], F32)
        l2g_bc = SB(es, "l2g_bc", [128, D], F32)
        l2b_bc = SB(es, "l2b_bc", [128, D], F32)
        x1_F = [SB(es, "x1_F%d" % i, [128, D], F32) for i in range(2)]
        z_Fs = [SB(es, "z_F%d" % i, [128, D], F32) for i in range(2)]
        zn_F = SB(es, "zn_F", [128, D], F32)
        o_F = [SB(es, "o_F%d" % i, [128, D], F32) for i in range(2)]
        st_F = SB(es, "st_F", [128, 2, 6], F32)
        mv_F = SB(es, "mv_F", [128, 2], F32)
        rstd_Fs = [SB(es, "rstd_F%d" % i, [128, 1], F32) for i in range(2)]
        nb_Fs = [SB(es, "nb_F%d" % i, [128, 1], F32) for i in range(2)]
        load_bc(g2_bc, mod_d[0:1, 5120:6144], "g2_bc")
        load_bc(l2g_bc, ln2_g, "l2g_bc")
        load_bc(l2b_bc, ln2_b, "l2b_bc")
        sF0, sF1 = [None] * NTO, [None] * NTO
        for tt in range(NTO):
            s2 = tt % 2
            t0 = tt * 128
            z_F, rstd_F, nb_F = z_Fs[s2], rstd_Fs[s2], nb_Fs[s2]
            kz, sfx = ("z_F", s2), ("F", s2)
            P.capture()
            P.dma(lambda e, s2=s2, t0=t0: e.dma_start(out=x1_F[s2][:], in_=x1_d[t0:t0 + 128, :]), key=("x1_F", s2), r=["x1_d"], w=[("x1_F", s2)])
            P.dve(lambda e, tt=tt, z_F=z_F: e.tensor_tensor(out=z_F[:], in0=yacc[:, tt, :], in1=g2_bc[:], op=ALU.mult), r=[("yacc", tt), "g2_bc"], w=[kz])
            P.dve(lambda e, s2=s2, z_F=z_F: e.scalar_tensor_tensor(out=z_F[:], in0=x1_F[s2][:], scalar=ALPHA, in1=z_F[:], op0=ALU.mult, op1=ALU.add), r=[kz, ("x1_F", s2)], w=[kz])
            for i in range(2):
                P.dve(lambda e, i=i, z_F=z_F: e.bn_stats(out=st_F[:, i, :], in_=z_F[:, i * 512:(i + 1) * 512]), r=[kz], w=[("st", "F", i)])
            P.dve(lambda e: e.bn_aggr(out=mv_F[:], in_=st_F[:].rearrange("p a b -> p (a b)")), r=[("st", "F", 0), ("st", "F", 1)], w=[("mv", "F")])
            if LNM_A == "act":
                P.act(lambda e, rstd_F=rstd_F: e.activation(out=rstd_F[:], in_=mv_F[:, 1:2], func=ACT.Sqrt, bias=eps_t[:], scale=1.0), r=[("mv", "F"), "eps_t"], w=[("rstd", sfx)])
                P.dve(lambda e, rstd_F=rstd_F: e.reciprocal(out=rstd_F[:], in_=rstd_F[:]), r=[("rstd", sfx)], w=[("rstd", sfx)])
            else:
                P.dve(lambda e, rstd_F=rstd_F: e.tensor_scalar(out=rstd_F[:], in0=mv_F[:, 1:2], scalar1=EPS, scalar2=None, op0=ALU.add), r=[("mv", "F")], w=[("rstd", sfx)])
                P.dve(lambda e, rstd_F=rstd_F: e.tensor_scalar(out=rstd_F[:], in0=rstd_F[:], scalar1=-0.5, scalar2=None, op0=ALU.pow), r=[("rstd", sfx)], w=[("rstd", sfx)])
            P.dve(lambda e, rstd_F=rstd_F, nb_F=nb_F: e.scalar_tensor_tensor(out=nb_F[:], in0=mv_F[:, 0:1], scalar=-1.0, in1=rstd_F[:], op0=ALU.mult, op1=ALU.mult),
                  r=[("mv", "F"), ("rstd", sfx)], w=[("nb", sfx)])
            sF0[tt] = P.end_capture()
            P.capture()
            P.act(lambda e, z_F=z_F, rstd_F=rstd_F, nb_F=nb_F: e.activation(out=zn_F[:], in_=z_F[:], func=ACT.Identity, bias=nb_F[:], scale=rstd_F[:]),
                  r=[kz, ("nb", sfx), ("rstd", sfx)], w=["zn_F"])
            P.dve(lambda e: e.tensor_tensor(out=zn_F[:], in0=zn_F[:], in1=l2g_bc[:], op=ALU.mult), r=["zn_F", "l2g_bc"], w=["zn_F"])
            P.dve(lambda e, s2=s2: e.tensor_tensor(out=o_F[s2][:], in0=zn_F[:], in1=l2b_bc[:], op=ALU.add), r=["zn_F", "l2b_bc"], w=[("o_F", s2)])
            P.dma(lambda e, s2=s2, t0=t0: e.dma_start(out=out[t0:t0 + 128, :], in_=o_F[s2][:]), key=("outd", s2), r=[("o_F", s2)], w=["out"])
            sF1[tt] = P.end_capture()
        for step in range(NTO + 1):
            for lst, off in ((sF0, 0), (sF1, 1)):
                j = step - off
                if 0 <= j < NTO:
                    P.ops.extend(lst[j])
        P.barrier()
    esE.close()
    return finish(nc, P, ges, out, dbg_out)


def finish(nc, P, ges, out, dbg_out):
    P.barrier()
    P.emit()
    ges.close()
    nc._dbg_out = dbg_out
    nc._stats = P.stats
    return nc


def rope_tables():
    rows = NLAT // 64
    row = np.repeat(np.arange(rows, dtype=np.float32), 64)
    col = np.tile(np.arange(64, dtype=np.float32), rows)
    inv = (np.float32(10000.0) ** (-np.arange(0, 64, 2, dtype=np.float32) / np.float32(64))).astype(np.float32)
    ang = np.stack([row[:, None] * inv, col[:, None] * inv], axis=1).astype(np.float32)
    tab = np.concatenate([np.cos(ang).reshape(NLAT, 64), np.sin(ang).reshape(NLAT, 64)], axis=1).astype(np.float32)
    return tab


def make_in_maps(inp):
    f32 = np.float32
    g = lambda k: np.asarray(inp[k], dtype=f32)
    x, c, ctx, c_ctx = g("x"), g("c"), g("ctx"), g("c_ctx")
    tab = rope_tables()
    tab_ctx = np.concatenate([np.ones((NCTX, 64), f32), np.zeros((NCTX, 64), f32)], axis=1)
    rope_full = np.concatenate([tab_ctx, tab], axis=0)
    tok = np.arange(128)
    mask8 = (tok[:, None] % 8 == np.arange(8)[None, :]).astype(f32)
    sel16 = (tok[:, None] // 8 == np.arange(16)[None, :]).astype(f32)
    mask8c = (tok[:, None] // 16 == np.arange(8)[None, :]).astype(f32)

    def pairlay(a):
        sh = a.shape
        a = a.reshape((2, 16, 2, 64) + sh[3:])
        perm = (2, 3, 0, 1) + tuple(range(4, a.ndim))
        a = a.transpose(perm)
        return np.ascontiguousarray(a.reshape((128, 2, 16) + sh[3:]))

    a_re, a_im = g("s5_a_re")[0], g("s5_a_im")[0]
    s5_a = np.stack([pairlay(a_re), pairlay(a_im)], axis=1)
    ldt = g("s5_log_dt")[0]
    s5_ldt = np.ascontiguousarray(np.broadcast_to(ldt.reshape(1, 2, 16, 2).transpose(0, 3, 1, 2), (64, 2, 2, 16)).transpose(1, 0, 2, 3).reshape(128, 2, 16))
    s5_b = np.stack([pairlay(g("s5_b_re")[0]), pairlay(g("s5_b_im")[0])], axis=1)
    cre = g("s5_c_re")[0].transpose(0, 1, 3, 2)
    cim = g("s5_c_im")[0].transpose(0, 1, 3, 2)
    s5_c = np.stack([pairlay(cre), pairlay(cim)], axis=1)
    dvec = g("s5_d")[0]
    s5_dcol = np.ascontiguousarray(np.broadcast_to(dvec.reshape(32, 16).T[None], (8, 16, 32)).reshape(128, 32))
    eZ = np.stack([63.0 - np.arange(64), np.arange(64)], axis=0).astype(f32)
    qs = np.arange(72)
    eRf = (qs - 7).astype(f32)
    eRb = (8 * (qs // 8 - 1) + 8 - (qs % 8)).astype(f32)
    eR = np.stack([eRf, eRb], axis=0)
    cst_eZ = np.ascontiguousarray(np.broadcast_to(eZ[None], (128, 2, 64)))
    cst_eR = np.ascontiguousarray(np.broadcast_to(eR[None], (128, 2, 72)))
    sidx = tok // 16
    cst_mf = (sidx[None, :] >= sidx[:, None]).astype(f32)
    cst_mb = (sidx[:, None] >= sidx[None, :]).astype(f32)
    selg = np.zeros((128, 8, 128), f32)
    for g8 in range(8):
        for co in range(16):
            selg[np.arange(8) * 16 + co, g8, g8 * 16 + co] = 1.0
    sele = np.zeros((32, 32, 128), f32)
    for e in range(32):
        sele[e, e, :] = 1.0
    w_rt = np.concatenate([g("w_router_group")[0], g("w_router_expert")[0]], axis=1)
    b_rt = np.concatenate([g("b_router_group")[0], g("b_router_expert")[0]], axis=0)[None]
    common = dict(
        w_mod=g("w_mod")[0], b_mod=g("b_mod"), w_in=g("w_in")[0], rope_f=rope_full,
        q_gain=g("q_gain"), k_gain=g("k_gain"), cst_mask8=mask8, cst_mask8c=mask8c, cst_sel16=sel16,
        s5_a=s5_a, s5_ldt=s5_ldt, s5_b=s5_b, s5_c=s5_c, s5_dcol=s5_dcol, cst_eZ=cst_eZ, cst_eR=cst_eR,
        cst_mf=cst_mf, cst_mb=cst_mb, cst_selg=selg,
        w_glu_a=g("w_glu_a")[0], w_glu_b=g("w_glu_b")[0], w_attn_o=g("w_attn_o")[0], w_out=g("w_out")[0],
        ln1_g=g("ln1_g"), ln1_b=g("ln1_b"), ln2_g=g("ln2_g"), ln2_b=g("ln2_b"),
        w_rt=w_rt, b_rt=b_rt, w_eg=g("w_exp_gate")[0], w_eu=g("w_exp_up")[0], w_ed=g("w_exp_down")[0],
        cst_sele=sele,
    )
    maps = []
    for core in range(8):
        b, r = core // 4, core % 4
        m = dict(common)
        m["xf"] = np.ascontiguousarray(np.concatenate([ctx[b], x[b]], axis=0))
        m["xo"] = np.ascontiguousarray(x[b, r * NOWN:(r + 1) * NOWN])
        cc = np.stack([c[b], c_ctx], axis=0)
        m["ccT"] = np.ascontiguousarray(cc.reshape(2, 8, 128).transpose(2, 1, 0))
        m["rope_o"] = np.ascontiguousarray(tab[r * NOWN:(r + 1) * NOWN])
        cm = np.zeros((128, 4), f32)
        cm[:, r] = 1.0
        m["cmask"] = cm
        maps.append(m)
    return maps


_NC_CACHE = {}


def kernel(**inputs):
    maps = make_in_maps(inputs)
    if "nc" not in _NC_CACHE:
        _NC_CACHE["nc"] = build()
    nc = _NC_CACHE["nc"]
    res = run_bass_kernel_spmd(nc, maps, core_ids=list(range(8)))
    outp = np.zeros((2, NLAT, D), np.float32)
    for core in range(8):
        b, r = core // 4, core % 4
        outp[b, r * NOWN:(r + 1) * NOWN] = res.results[core]["out"]
    return outp
```
